# Optimizing a Trainium2 kernel written in Bass

```python
import math
import jax, jax.numpy as jnp
from jax import lax
import numpy as np

D_MODEL = 1024
BATCH = 32
SEQ = 2048
DEPTH = 2

N_EVEN = (DEPTH + 1) // 2
N_ODD = DEPTH // 2
ATT_HEAD_DIM = 64
N_HEADS_FOX = 8
N_HEADS_DIL = 8
FOX_WIDTH = N_HEADS_FOX * ATT_HEAD_DIM
DIL_WIDTH = N_HEADS_DIL * ATT_HEAD_DIM
DIL_CONFIGS = ((128, 1), (512, 4), (2048, 16))
ROPE_THETA = 500000.0
ROPE_DIMS = ATT_HEAD_DIM // 4
Q_BLOCK = 128
N_HEADS_MLSTM = 8
MLSTM_HEAD_DIM = D_MODEL // N_HEADS_MLSTM
MLSTM_CHUNK = 64
CONV_WIDTH = 4
D_FF_DENSE = 2816
N_EXPERTS = 8
TOP_K = 2
D_FF_EXPERT = 3584
DEEPNORM_ALPHA = (2 * DEPTH) ** 0.25
DEEPNORM_BETA = (8 * DEPTH) ** -0.25
LN_EPS = 1e-5
EVEN_IN_WIDTH = 3 * FOX_WIDTH + N_HEADS_FOX + 3 * DIL_WIDTH
ODD_IN_WIDTH = 4 * D_MODEL + 2 * N_HEADS_MLSTM

kernel_name = 'hybrid_fox_dilated_mlstm_moe_deepnorm'


def _split(a, sizes):
    idx = np.cumsum(np.array(sizes))[:-1].tolist()
    return jnp.split(a, idx, axis=-1)


def layer_norm(x, g, b):
    xf = x.astype(jnp.float32)
    mu = jnp.mean(xf, axis=-1, keepdims=True)
    var = jnp.mean(jnp.square(xf - mu), axis=-1, keepdims=True)
    return ((xf - mu) * lax.rsqrt(var + LN_EPS) * g + b).astype(x.dtype)


def partial_rotary(x, pos):
    half = ROPE_DIMS // 2
    inv = ROPE_THETA ** (-jnp.arange(half, dtype=jnp.float32) / half)
    ang = pos.astype(jnp.float32)[:, None] * inv[None, :]
    cos = jnp.cos(ang)[None, :, None, :]
    sin = jnp.sin(ang)[None, :, None, :]
    xr = x[..., :ROPE_DIMS].astype(jnp.float32)
    x1, x2 = xr[..., :half], xr[..., half:]
    rot = jnp.concatenate([x1 * cos - x2 * sin, x2 * cos + x1 * sin], axis=-1).astype(x.dtype)
    return jnp.concatenate([rot, x[..., ROPE_DIMS:]], axis=-1)


def forgetting_attention(q, k, v, log_f):
    B, S, H, D = q.shape
    nb = S // Q_BLOCK
    scale = D ** -0.5
    c = jnp.cumsum(log_f, axis=1).transpose(0, 2, 1)
    qb = q.reshape(B, nb, Q_BLOCK, H, D).transpose(1, 0, 2, 3, 4)
    cb = c.reshape(B, H, nb, Q_BLOCK).transpose(2, 0, 1, 3)
    starts = jnp.arange(nb, dtype=jnp.int32) * Q_BLOCK
    kpos = jnp.arange(S, dtype=jnp.int32)

    def block(args):
        q_blk, c_blk, start = args
        s = jnp.einsum('bqhd,bkhd->bhqk', q_blk, k).astype(jnp.float32) * scale
        s = s + (c_blk[..., :, None] - c[..., None, :])
        qpos = start + jnp.arange(Q_BLOCK, dtype=jnp.int32)
        s = jnp.where(kpos[None, :] <= qpos[:, None], s, -jnp.inf)
        p = jax.nn.softmax(s, axis=-1).astype(v.dtype)
        return jnp.einsum('bhqk,bkhd->bqhd', p, v)

    o = lax.map(block, (qb, cb, starts))
    return o.transpose(1, 0, 2, 3, 4).reshape(B, S, H, D)


def dilated_branch(q, k, v, window, dilation):
    B, S, H, D = q.shape
    L = S // dilation
    W = window // dilation
    Qb = math.gcd(L, Q_BLOCK)
    nb = L // Qb
    scale = D ** -0.5

    def strided(t):
        return t.reshape(B, L, dilation, H, D).transpose(0, 2, 1, 3, 4)

    qs, ks, vs = strided(q), strided(k), strided(v)
    pad = ((0, 0), (0, 0), (W, 0), (0, 0), (0, 0))
    kp, vp = jnp.pad(ks, pad), jnp.pad(vs, pad)
    kidx = (jnp.arange(nb) * Qb)[:, None] + jnp.arange(Qb + W)[None, :]
    kb = kp[:, :, kidx]
    vb = vp[:, :, kidx]
    qb = qs.reshape(B, dilation, nb, Qb, H, D)
    s = jnp.einsum('brnqhd,brnkhd->brnhqk', qb, kb).astype(jnp.float32) * scale
    i = jnp.arange(Qb)[:, None]
    j = jnp.arange(Qb + W)[None, :]
    dist = i - j + W
    key_sub = (jnp.arange(nb) * Qb)[:, None, None] + j[None] - W
    valid = (dist >= 0)[None] & (dist <= W)[None] & (key_sub >= 0)
    s = jnp.where(valid[None, None, :, None], s, -jnp.inf)
    lse = jax.nn.logsumexp(s, axis=-1)
    p = jnp.exp(s - lse[..., None]).astype(v.dtype)
    o = jnp.einsum('brnhqk,brnkhd->brnqhd', p, vb)
    o = o.reshape(B, dilation, L, H, D).transpose(0, 2, 1, 3, 4).reshape(B, S, H, D)
    lse = lse.transpose(0, 1, 2, 4, 3).reshape(B, dilation, L, H).transpose(0, 2, 1, 3).reshape(B, S, H)
    return o, lse


def dilated_attention(q, k, v):
    outs, lses = [], []
    for window, dilation in DIL_CONFIGS:
        o, lse = dilated_branch(q, k, v, window, dilation)
        outs.append(o)
        lses.append(lse)
    wts = jax.nn.softmax(jnp.stack(lses, axis=0), axis=0).astype(v.dtype)
    return jnp.einsum('nbsh,nbshd->bshd', wts, jnp.stack(outs, axis=0))


def fox_dilated_mixer(x, w_in, b_forget, w_out, pos):
    B, S, _ = x.shape
    qa, ka, va, fa, qd, kd, vd = _split(x @ w_in, [FOX_WIDTH] * 3 + [N_HEADS_FOX] + [DIL_WIDTH] * 3)
    log_f = jax.nn.log_sigmoid((fa + b_forget).astype(jnp.float32))
    o_fox = forgetting_attention(qa.reshape(B, S, N_HEADS_FOX, ATT_HEAD_DIM),
                                 ka.reshape(B, S, N_HEADS_FOX, ATT_HEAD_DIM),
                                 va.reshape(B, S, N_HEADS_FOX, ATT_HEAD_DIM), log_f)
    qd = partial_rotary(qd.reshape(B, S, N_HEADS_DIL, ATT_HEAD_DIM), pos)
    kd = partial_rotary(kd.reshape(B, S, N_HEADS_DIL, ATT_HEAD_DIM), pos)
    o_dil = dilated_attention(qd, kd, vd.reshape(B, S, N_HEADS_DIL, ATT_HEAD_DIM))
    o = jnp.concatenate([o_fox.reshape(B, S, FOX_WIDTH), o_dil.reshape(B, S, DIL_WIDTH)], axis=-1)
    return o @ w_out


def causal_depthwise_conv(x, w):
    S = x.shape[1]
    K = w.shape[0]
    xp = jnp.pad(x, ((0, 0), (K - 1, 0), (0, 0)))
    y = xp[:, 0:S] * w[0]
    for i in range(1, K):
        y = y + xp[:, i:i + S] * w[i]
    return y


def mlstm_chunkwise(q, k, v, log_i, log_f):
    B, S, H, D = q.shape
    L = MLSTM_CHUNK
    nc = S // L
    f32 = jnp.float32

    def to_chunks(a):
        a = a.reshape((B, nc, L, H) + a.shape[3:])
        return jnp.moveaxis(a, (1, 3), (0, 2))

    xs = (to_chunks(q.astype(f32)), to_chunks(k.astype(f32)), to_chunks(v.astype(f32)),
          to_chunks(log_i), to_chunks(log_f))
    causal = jnp.tril(jnp.ones((L, L), dtype=bool))

    def body(carry, xc):
        C, n, m = carry
        qc, kc, vc, ic, fc = xc
        b = jnp.cumsum(fc, axis=-1)
        dlog = jnp.where(causal, b[..., :, None] - b[..., None, :] + ic[..., None, :], -jnp.inf)
        inter = b + m[..., None]
        m_t = jnp.maximum(inter, jnp.max(dlog, axis=-1))
        s = jnp.einsum('bhtd,bhsd->bhts', qc, kc) * jnp.exp(dlog - m_t[..., None])
        inter_w = jnp.exp(inter - m_t)
        num = jnp.einsum('bhts,bhsd->bhtd', s, vc) + inter_w[..., None] * jnp.einsum('bhtk,bhkv->bhtv', qc, C)
        den = jnp.sum(s, axis=-1) + inter_w * jnp.einsum('bhtk,bhk->bht', qc, n)
        h = num / jnp.maximum(jnp.abs(den), jnp.exp(-m_t))[..., None]
        b_last = b[..., -1]
        g = b_last[..., None] - b + ic
        m_new = jnp.maximum(b_last + m, jnp.max(g, axis=-1))
        w = jnp.exp(g - m_new[..., None])
        decay = jnp.exp(b_last + m - m_new)
        C = decay[..., None, None] * C + jnp.einsum('bhs,bhsk,bhsv->bhkv', w, kc, vc)
        n = decay[..., None] * n + jnp.einsum('bhs,bhsk->bhk', w, kc)
        return (C, n, m_new), h

    init = (jnp.zeros((B, H, D, D), f32), jnp.zeros((B, H, D), f32), jnp.full((B, H), -jnp.inf, f32))
    _, h = lax.scan(body, init, xs)
    return jnp.moveaxis(h, (0, 2), (1, 3)).reshape(B, S, H, D).astype(q.dtype)


def mlstm_mixer(x, w_in, b_igate, b_fgate, w_conv, norm_g, w_out):
    B, S, _ = x.shape
    qk, v, ig, fg, og = _split(x @ w_in, [2 * D_MODEL, D_MODEL, N_HEADS_MLSTM, N_HEADS_MLSTM, D_MODEL])
    qk = jax.nn.silu(causal_depthwise_conv(qk, w_conv))
    q, k = jnp.split(qk, 2, axis=-1)
    shp = (B, S, N_HEADS_MLSTM, MLSTM_HEAD_DIM)
    log_i = (ig + b_igate).astype(jnp.float32)
    log_f = jax.nn.log_sigmoid((fg + b_fgate).astype(jnp.float32))
    h = mlstm_chunkwise(q.reshape(shp), k.reshape(shp) * (MLSTM_HEAD_DIM ** -0.5), v.reshape(shp), log_i, log_f)
    hf = h.astype(jnp.float32)
    mu = jnp.mean(hf, axis=-1, keepdims=True)
    var = jnp.mean(jnp.square(hf - mu), axis=-1, keepdims=True)
    hn = ((hf - mu) * lax.rsqrt(var + LN_EPS)).reshape(B, S, D_MODEL) * norm_g
    h = hn.astype(x.dtype) * jax.nn.sigmoid(og)
    return h @ w_out


def swiglu(x, w_gate, w_up, w_down):
    return (jax.nn.silu(x @ w_gate) * (x @ w_up)) @ w_down


def moe_swiglu(x, w_router, w_gate, w_up, w_down):
    B, S, Dm = x.shape
    xt = x.reshape(B * S, Dm)
    logits = (xt @ w_router).astype(jnp.float32)
    top_v, top_i = lax.top_k(logits, TOP_K)
    wts = jax.nn.softmax(top_v, axis=-1)
    gates = jnp.einsum('tk,tke->te', wts, jax.nn.one_hot(top_i, N_EXPERTS, dtype=jnp.float32)).astype(x.dtype)
    y = jnp.zeros_like(xt)
    for e in range(N_EXPERTS):
        y = y + gates[:, e:e + 1] * swiglu(xt, w_gate[e], w_up[e], w_down[e])
    return y.reshape(B, S, Dm)


def setup_inputs(seed: int = 0) -> dict:
    key = jax.random.key(seed)
    ks = jax.random.split(key, 25)
    E, O = N_EVEN, N_ODD

    def nrm(k, shape, scale):
        return jax.random.normal(k, shape, jnp.float32) * scale

    def gain(k, shape):
        return 1.0 + 0.02 * jax.random.normal(k, shape, jnp.float32)

    return {
        'x': nrm(ks[0], (BATCH, SEQ, D_MODEL), 1.0),
        'w_in_e': nrm(ks[1], (E, D_MODEL, EVEN_IN_WIDTH), D_MODEL ** -0.5),
        'b_forget_e': jax.random.uniform(ks[2], (E, N_HEADS_FOX), dtype=jnp.float32, minval=1.0, maxval=4.0),
        'w_out_e': nrm(ks[3], (E, D_MODEL, D_MODEL), DEEPNORM_BETA * D_MODEL ** -0.5),
        'ln_mix_g_e': gain(ks[4], (E, D_MODEL)),
        'ln_mix_b_e': nrm(ks[5], (E, D_MODEL), 0.02),
        'ffn_w_gate_e': nrm(ks[6], (E, D_MODEL, D_FF_DENSE), D_MODEL ** -0.5),
        'ffn_w_up_e': nrm(ks[7], (E, D_MODEL, D_FF_DENSE), D_MODEL ** -0.5),
        'ffn_w_down_e': nrm(ks[8], (E, D_FF_DENSE, D_MODEL), DEEPNORM_BETA * D_FF_DENSE ** -0.5),
        'ln_ffn_g_e': gain(ks[9], (E, D_MODEL)),
        'ln_ffn_b_e': nrm(ks[10], (E, D_MODEL), 0.02),
        'w_in_o': nrm(ks[11], (O, D_MODEL, ODD_IN_WIDTH), D_MODEL ** -0.5),
        'b_igate_o': nrm(ks[12], (O, N_HEADS_MLSTM), 0.1),
        'b_fgate_o': jnp.linspace(3.0, 6.0, N_HEADS_MLSTM, dtype=jnp.float32)[None, :] + nrm(ks[13], (O, N_HEADS_MLSTM), 0.1),
        'w_conv_o': nrm(ks[14], (O, CONV_WIDTH, 2 * D_MODEL), CONV_WIDTH ** -0.5),
        'mlstm_norm_g_o': gain(ks[15], (O, D_MODEL)),
        'w_out_o': nrm(ks[16], (O, D_MODEL, D_MODEL), DEEPNORM_BETA * D_MODEL ** -0.5),
        'ln_mix_g_o': gain(ks[17], (O, D_MODEL)),
        'ln_mix_b_o': nrm(ks[18], (O, D_MODEL), 0.02),
        'w_router_o': nrm(ks[19], (O, D_MODEL, N_EXPERTS), D_MODEL ** -0.5),
        'moe_w_gate_o': nrm(ks[20], (O, N_EXPERTS, D_MODEL, D_FF_EXPERT), D_MODEL ** -0.5),
        'moe_w_up_o': nrm(ks[21], (O, N_EXPERTS, D_MODEL, D_FF_EXPERT), D_MODEL ** -0.5),
        'moe_w_down_o': nrm(ks[22], (O, N_EXPERTS, D_FF_EXPERT, D_MODEL), DEEPNORM_BETA * D_FF_EXPERT ** -0.5),
        'ln_ffn_g_o': gain(ks[23], (O, D_MODEL)),
        'ln_ffn_b_o': nrm(ks[24], (O, D_MODEL), 0.02),
    }


def reference(x, w_in_e, b_forget_e, w_out_e, ln_mix_g_e, ln_mix_b_e, ffn_w_gate_e, ffn_w_up_e,
              ffn_w_down_e, ln_ffn_g_e, ln_ffn_b_e, w_in_o, b_igate_o, b_fgate_o, w_conv_o,
              mlstm_norm_g_o, w_out_o, ln_mix_g_o, ln_mix_b_o, w_router_o, moe_w_gate_o,
              moe_w_up_o, moe_w_down_o, ln_ffn_g_o, ln_ffn_b_o):
    pos = jnp.arange(x.shape[1], dtype=jnp.int32)
    h = x
    for layer in range(DEPTH):
        i = layer // 2
        if layer % 2 == 0:
            mix = fox_dilated_mixer(h, w_in_e[i], b_forget_e[i], w_out_e[i], pos)
            h = layer_norm(DEEPNORM_ALPHA * h + mix, ln_mix_g_e[i], ln_mix_b_e[i])
            ffn = swiglu(h, ffn_w_gate_e[i], ffn_w_up_e[i], ffn_w_down_e[i])
            h = layer_norm(DEEPNORM_ALPHA * h + ffn, ln_ffn_g_e[i], ln_ffn_b_e[i])
        else:
            mix = mlstm_mixer(h, w_in_o[i], b_igate_o[i], b_fgate_o[i], w_conv_o[i], mlstm_norm_g_o[i], w_out_o[i])
            h = layer_norm(DEEPNORM_ALPHA * h + mix, ln_mix_g_o[i], ln_mix_b_o[i])
            ffn = moe_swiglu(h, w_router_o[i], moe_w_gate_o[i], moe_w_up_o[i], moe_w_down_o[i])
            h = layer_norm(DEEPNORM_ALPHA * h + ffn, ln_ffn_g_o[i], ln_ffn_b_o[i])
    return h
```

```python
import numpy as np
from contextlib import ExitStack
import concourse.bass as bass
import concourse.mybir as mybir
from concourse.bass_utils import run_bass_kernel_spmd

F32 = mybir.dt.float32
BF16 = mybir.dt.bfloat16
AF = mybir.ActivationFunctionType
ALU = mybir.AluOpType
AX = mybir.AxisListType

S = 2048
D = 1024
TB = 16
KC = 8
ALPHA = 4.0 ** 0.25
EPS = 1e-5
NEG = -30000.0
DFF = 2816
DFE = 3584
NE = 8
LNS = float(np.log(128.0 ** -0.5))
ENGS = ("pe", "act", "dve", "pool", "sp")


class Op:
    __slots__ = ("eng", "fn", "deps", "sig", "val", "dsem", "key")


class Buf:
    __slots__ = ("lastw", "readers", "dsem", "name")

    def __init__(self, name, dsem=None):
        self.lastw = None
        self.readers = {}
        self.dsem = dsem
        self.name = name


class Prog:
    def __init__(self, nc, es):
        self.nc = nc
        self.es = es
        self.ops = {e: [] for e in ENGS}
        self.esem = {e: es.enter_context(nc.semaphore("s_" + e)) for e in ENGS}
        self.dcnt = {}
        self.last = {e: None for e in ENGS}
        self.lastd = {}
        self.pend = {e: [] for e in ENGS}
        self.nsem = 0
        self.sem_pool = []
        self.pool_idx = None

    def newsem(self):
        if self.pool_idx is not None:
            if self.pool_idx >= len(self.sem_pool):
                self.nsem += 1
                self.sem_pool.append(self.es.enter_context(self.nc.semaphore("d%d" % self.nsem)))
            sm = self.sem_pool[self.pool_idx]
            self.pool_idx += 1
            return sm
        self.nsem += 1
        return self.es.enter_context(self.nc.semaphore("d%d" % self.nsem))

    def phase_begin(self):
        self.pool_idx = 0

    def buf(self, name, dma=False):
        return Buf(name, self.newsem() if dma else None)

    def op(self, eng, fn, r=(), w=(), dsem=None):
        o = Op()
        o.eng = eng
        o.fn = fn
        o.sig = False
        o.val = 0
        o.dsem = dsem
        o.key = ("d", id(dsem)) if dsem is not None else eng
        deps = []
        for b in r:
            if b.lastw is not None:
                deps.append((b.lastw, True))
        for b in w:
            if b.lastw is not None:
                deps.append((b.lastw, False))
            for rd in b.readers.values():
                deps.append((rd, False))
        for d in self.pend[eng]:
            deps.append((d, True))
        self.pend[eng] = []
        dd = []
        for d, raw in deps:
            if d is o:
                continue
            if d.key == o.key and (not raw or eng == "pe"):
                continue
            if d not in dd:
                dd.append(d)
        o.deps = dd
        if dsem is not None:
            c = self.dcnt.get(id(dsem), 0) + 16
            self.dcnt[id(dsem)] = c
            o.val = c
            self.lastd[id(dsem)] = o
        for d in dd:
            if d.dsem is None:
                d.sig = True
        for b in r:
            b.readers[o.key] = o
        for b in w:
            b.lastw = o
            b.readers = {}
        self.ops[eng].append(o)
        self.last[eng] = o
        return o

    def barrier(self):
        lasts = [self.last[e] for e in ENGS if self.last[e] is not None] + list(self.lastd.values())
        for e in ENGS:
            self.pend[e] = list(lasts)

    def emit(self, block):
        for e in ENGS:
            c = 0
            for o in self.ops[e]:
                if o.dsem is None and o.sig:
                    c += 1
                    o.val = c

        def runner(ename):
            def f(eh):
                seen = {}
                for o in self.ops[ename]:
                    for d in o.deps:
                        if seen.get(d.key, 0) >= d.val:
                            continue
                        sem = d.dsem if d.dsem is not None else self.esem[d.eng]
                        eh.wait_ge(sem, d.val)
                        seen[d.key] = d.val
                    inst = o.fn(eh)
                    if inst is None:
                        continue
                    if o.dsem is not None:
                        inst.then_inc(o.dsem, 16)
                    elif o.sig:
                        inst.then_inc(self.esem[ename], 1)
            return f

        block.tensor(runner("pe"))
        block.scalar(runner("act"))
        block.vector(runner("dve"))
        block.gpsimd(runner("pool"))
        block.sync(runner("sp"))


class Rot:
    def __init__(self, items):
        self.items = items
        self.i = 0

    def next(self):
        it = self.items[self.i % len(self.items)]
        self.i += 1
        return it


WNAMES = {
    "wqa": (D, 512), "wka": (D, 512), "wva": (D, 512), "wfa": (D, 1024),
    "wqd": (D, 512), "wkd": (D, 512), "wqds": (D, 512), "wkds": (D, 512), "wvd": (D, 512),
    "wo0": (D, D), "fg": (D, DFF), "fu": (D, DFF), "fd": (DFF, D),
    "wq1": (D, D), "wk1": (D, D), "wv1": (D, D), "wog": (D, D), "wig": (D, D), "wfg": (D, D),
    "wo1": (D, D), "wr": (D, 8), "mg": (NE * D, DFE), "mu": (NE * D, DFE), "md": (NE * DFE, D),
    "lnp": (4 * 2 * 128, D), "gb": (128, 24), "ng": (128, 8), "wc": (128, 64),
    "ident": (128, 128), "masks": (128, 3 * 128), "mask4": (128, 3 * 512), "rope": (128, 2 * S), "augc": (128, 6),
}


def build_nc(nseq, debug=False, phases=5):
    nc = bass.Bass("TRN2", target_bir_lowering=False)
    dr = {}
    dr["x"] = nc.dram_tensor("x", [nseq * S, D], F32, kind="ExternalInput").ap()
    for k, shp in WNAMES.items():
        dr[k] = nc.dram_tensor(k, list(shp), F32, kind="ExternalInput").ap()
    out = nc.dram_tensor("out", [nseq * S, D], F32, kind="ExternalOutput").ap()
    hsp = nc.dram_tensor("hsp", [S, D], F32, kind="Internal").ap()
    dbg = None
    if debug:
        dbg = nc.dram_tensor("dbg", [4 * S, D], F32, kind="ExternalOutput").ap()
        dbg2 = nc.dram_tensor("dbg2", [2 * 128, KC * S], F32, kind="ExternalOutput").ap()

    with ExitStack() as es:
        p = Prog(nc, es)

        uid = [0]

        def sb(st, name, shape, dt):
            uid[0] += 1
            return st.enter_context(nc.sbuf_tensor("%s_s%d" % (name, uid[0]), shape, dt))

        def ps(st, name, shape, dt=F32):
            uid[0] += 1
            return st.enter_context(nc.psum_tensor("%s_p%d" % (name, uid[0]), shape, dt))

        h = sb(es, "h", [128, TB, D], F32)
        hB = [p.buf("h%d" % i, dma=True) for i in range(TB)]
        hs = h[:, :, :].rearrange("p a b -> p (a b)")
        hT = sb(es, "hT", [128, KC, S], BF16)
        hTB = [p.buf("hT%d" % i) for i in range(TB)]
        oT = sb(es, "oT", [128, KC, S], BF16)
        oTB = [p.buf("oT%d" % i) for i in range(KC)]
        ident = sb(es, "ident", [128, 128], BF16)
        identB = p.buf("ident", dma=True)
        masks = sb(es, "masks", [128, 3, 128], BF16)
        mask4 = sb(es, "mask4", [128, 3, 512], BF16)
        augc = sb(es, "augc", [128, 6], F32)
        gb = sb(es, "gb", [128, 24], F32)
        ngb = sb(es, "ngb", [128, 24], F32)
        ng = sb(es, "ng", [128, 8], F32)
        wc = sb(es, "wc", [128, 16, 4], F32)
        ones_bf = sb(es, "ones_bf", [128, S], BF16)
        onesf = sb(es, "onesf", [128, 128], F32)
        constB = p.buf("const", dma=True)
        const2B = p.buf("const2")

        p.op("pool", lambda e: e.dma_start(out=ident[:], in_=dr["ident"][:, :]), w=[identB], dsem=identB.dsem)
        p.op("pool", lambda e: e.dma_start(out=masks[:], in_=dr["masks"].rearrange("p (a b) -> p a b", a=3)), w=[constB], dsem=constB.dsem)
        p.op("pool", lambda e: e.dma_start(out=mask4[:], in_=dr["mask4"].rearrange("p (a b) -> p a b", a=3)), w=[constB], dsem=constB.dsem)
        p.op("sp", lambda e: e.dma_start(out=augc[:], in_=dr["augc"][:, :]), w=[constB], dsem=constB.dsem)
        p.op("sp", lambda e: e.dma_start(out=gb[:], in_=dr["gb"][:, :]), w=[constB], dsem=constB.dsem)
        p.op("sp", lambda e: e.dma_start(out=ng[:], in_=dr["ng"][:, :]), w=[constB], dsem=constB.dsem)
        p.op("sp", lambda e: e.dma_start(out=wc[:], in_=dr["wc"].rearrange("p (a b) -> p a b", b=4)), w=[constB], dsem=constB.dsem)
        p.op("dve", lambda e: e.memset(ones_bf[:], 1.0), w=[const2B])
        p.op("dve", lambda e: e.memset(onesf[:], 1.0 / 128.0), w=[const2B])
        p.op("dve", lambda e: e.tensor_scalar(out=ngb[:], in0=gb[:], scalar1=-1.0, scalar2=None, op0=ALU.mult), r=[constB], w=[const2B])
        CB = [constB, const2B, identB]

        def wview(name, rows_off=0, nrows=D):
            return dr[name][rows_off:rows_off + nrows, :].rearrange("(k p) n -> p k n", p=128)

        def mm(e, o, l, r_, st=True, sp=True):
            return e.matmul(o, l, r_, start=st, stop=sp)

        def build_hT(tb, L):
            xb, xbB = L["xb"].next()
            pst, pstB = L["pst"].next()
            p.op("act", lambda e: e.activation(out=xb[:], in_=h[:, tb, :], func=AF.Copy), r=[hB[tb]], w=[xbB])

            def tr(e):
                inst = None
                for kc in range(KC):
                    inst = e.transpose(pst[:, kc * 128:(kc + 1) * 128], xb[:, kc * 128:(kc + 1) * 128], ident[:])
                return inst
            p.op("pe", tr, r=[xbB, identB], w=[pstB])
            p.op("dve", lambda e: e.tensor_copy(out=hT[:, :, tb * 128:(tb + 1) * 128],
                                                in_=pst[:].rearrange("p (k t) -> p k t", k=KC)), r=[pstB], w=[hTB[tb]])

        def ln_alloc(st, tag):
            L = {}
            L["xb"] = Rot([(sb(st, "xb%s%d" % (tag, i), [128, D], BF16), p.buf("xb")) for i in range(2)])
            L["pst"] = Rot([(ps(st, "pst%s%d" % (tag, i), [128, D], BF16), p.buf("pst")) for i in range(2)])
            L["st6"] = Rot([(sb(st, "st6%s%d" % (tag, i), [128, 12], F32), p.buf("st6")) for i in range(2)])
            L["mv"] = Rot([(sb(st, "mv%s%d" % (tag, i), [128, 4], F32), p.buf("mv")) for i in range(2)])
            L["lnp"] = sb(st, "lnp%s" % tag, [128, 2, D], F32)
            L["lnpB"] = p.buf("lnp", dma=True)
            return L

        def ln_load(L, li):
            src = dr["lnp"][li * 256:(li + 1) * 256, :].rearrange("(a p) n -> p a n", a=2)
            p.op("sp", lambda e: e.dma_start(out=L["lnp"][:], in_=src), w=[L["lnpB"]], dsem=L["lnpB"].dsem)

        def ln_block(tb, L, to_hT=True, out_row0=None, dbg_row0=None, spill=False):
            st6, st6B = L["st6"].next()
            mv, mvB = L["mv"].next()
            lnp = L["lnp"]
            p.op("dve", lambda e: e.bn_stats(out=st6[:, 0:6], in_=h[:, tb, 0:512]), r=[hB[tb]], w=[st6B])
            p.op("dve", lambda e: e.bn_stats(out=st6[:, 6:12], in_=h[:, tb, 512:1024]), r=[hB[tb]], w=[st6B])
            p.op("dve", lambda e: e.bn_aggr(out=mv[:, 0:2], in_=st6[:]), r=[st6B], w=[mvB])
            p.op("act", lambda e: e.activation(out=mv[:, 2:3], in_=mv[:, 1:2], func=AF.Ln, bias=L["eps"][:, 0:1]), r=[mvB, const2B], w=[mvB])
            p.op("act", lambda e: e.activation(out=mv[:, 3:4], in_=mv[:, 2:3], func=AF.Exp, scale=-0.5), r=[mvB], w=[mvB])
            p.op("dve", lambda e: e.tensor_scalar(out=h[:, tb, :], in0=h[:, tb, :], scalar1=mv[:, 0:1], scalar2=mv[:, 3:4],
                                                  op0=ALU.subtract, op1=ALU.mult), r=[hB[tb], mvB], w=[hB[tb]])
            p.op("pool", lambda e: e.tensor_tensor(out=h[:, tb, :], in0=h[:, tb, :], in1=lnp[:, 0, :], op=ALU.mult), r=[hB[tb], L["lnpB"]], w=[hB[tb]])
            p.op("pool", lambda e: e.tensor_tensor(out=h[:, tb, :], in0=h[:, tb, :], in1=lnp[:, 1, :], op=ALU.add), r=[hB[tb], L["lnpB"]], w=[hB[tb]])
            if dbg_row0 is not None:
                p.op("sp", lambda e: e.dma_start(out=dbg[dbg_row0 + tb * 128: dbg_row0 + (tb + 1) * 128, :], in_=h[:, tb, :]),
                     r=[hB[tb]], dsem=hB[tb].dsem)
            if spill:
                p.op("sp", lambda e: e.dma_start(out=hsp[tb * 128:(tb + 1) * 128, :], in_=h[:, tb, :]), r=[hB[tb]], dsem=hB[tb].dsem)
            if out_row0 is not None:
                return p.op("sp", lambda e: e.dma_start(out=out[out_row0 + tb * 128: out_row0 + (tb + 1) * 128, :], in_=h[:, tb, :]),
                            r=[hB[tb]], dsem=hB[tb].dsem)
            if to_hT:
                build_hT(tb, L)
            return None

        epsT = sb(es, "epsT", [128, 1], F32)
        onesf_one = sb(es, "oneT", [128, 1], F32)
        p.op("dve", lambda e: e.memset(epsT[:], EPS), w=[const2B])
        p.op("dve", lambda e: e.memset(onesf_one[:], 1.0), w=[const2B])

        def wload(dst_ap, src_ap, B):
            return p.op("pool", lambda e: e.dma_start(out=dst_ap, in_=src_ap), w=[B], dsem=B.dsem)

        def proj_fm(W, wname, col0, evac, ncols=128):
            wt, wtB = W["wA"].next()
            wload(wt[:, :, 0:ncols], wview(wname)[:, :, col0:col0 + ncols], wtB)
            for tc in range(4):
                pp, ppB = W["pp"].next()

                def f(e, pp=pp, wt=wt, tc=tc):
                    inst = None
                    for kc in range(KC):
                        inst = mm(e, pp[0:ncols, :], wt[:, kc, 0:ncols], hT[:, kc, tc * 512:(tc + 1) * 512], kc == 0, kc == KC - 1)
                    return inst
                p.op("pe", f, r=[wtB] + hTB[4 * tc:4 * tc + 4], w=[ppB])
                evac(tc, pp, ppB)

        def proj_tm(W, wname, col0, ncols, sets, evac):
            wt, wtB = W["wA"].next()
            wload(wt[:, :, 0:ncols], wview(wname)[:, :, col0:col0 + ncols], wtB)
            for si, tsl in enumerate(sets):
                pp, ppB = W["pp"].next()

                def f(e, pp=pp, wt=wt, tsl=tsl):
                    inst = None
                    for kc in range(KC):
                        inst = mm(e, pp[:, 0:ncols], hT[:, kc, tsl], wt[:, kc, 0:ncols], kc == 0, kc == KC - 1)
                    return inst
                p.op("pe", f, r=[wtB] + hTB, w=[ppB])
                evac(si, pp, ppB)

        def do_seq(s):
            row0 = s * S
            def _ph1():
                with ExitStack() as st:
                    p.phase_begin()
                    L = ln_alloc(st, "p0")
                    L["eps"] = epsT
                    for tb in range(TB):
                        p.op("sp", lambda e, tb=tb, row0=row0: e.dma_start(out=h[:, tb, :], in_=dr["x"][row0 + tb * 128: row0 + (tb + 1) * 128, :]),
                             w=[hB[tb]], dsem=hB[tb].dsem)
                    for tb in range(TB):
                        build_hT(tb, L)
                    p.barrier()
            _ph1()
            if phases < 1:
                return

            def _ph2():
                with ExitStack() as st:
                    p.phase_begin()
                    W = {}
                    W["wA"] = Rot([(sb(st, "wA%d" % i, [128, KC, 128], BF16), p.buf("wA", dma=True)) for i in range(3)])
                    W["pp"] = Rot([(ps(st, "pp%d" % i, [128, 512]), p.buf("pp")) for i in range(2)])
                    qT = sb(st, "qT", [128, S], BF16); qTB = p.buf("qT")
                    kT = sb(st, "kT", [128, S], BF16); kTB = p.buf("kT")
                    Vt = sb(st, "Vt", [128, 3, TB, 128], BF16); VB = [p.buf("V%d" % i) for i in range(3)]
                    t0 = hs[:, 0:S]; t0B = p.buf("t0")
                    t1 = hs[:, S:2 * S]; t1B = p.buf("t1")
                    hi = sb(st, "hi", [4, S], BF16); lo = sb(st, "lo", [4, S], BF16); hlB = p.buf("hl")
                    augQ = sb(st, "augQ", [4, S], BF16); augQB = p.buf("augQ")
                    augK = sb(st, "augK", [4, S], BF16); augKB = p.buf("augK")
                    tq = sb(st, "tq", [4, S], BF16); tqB = p.buf("tq")
                    pT = Rot([(sb(st, "pT%d" % i, [128, 512], BF16), p.buf("pT")) for i in range(2)])
                    rec = hs[0:64, 2 * S:3 * S]; recB = p.buf("rec")
                    sts = Rot([(ps(st, "st%d" % i, [128, 512]), p.buf("st")) for i in range(2)])
                    accn = Rot([(ps(st, "accn%d" % i, [64, 512]), p.buf("accn")) for i in range(2)])
                    accd = Rot([(ps(st, "accd%d" % i, [64, 512]), p.buf("accd")) for i in range(2)])
                    rope = hs[:, 3 * S:5 * S].rearrange("p (a b) -> p a b", a=2); ropeB = p.buf("rope", dma=True)
                    rt = Rot([(hs[:, 5 * S + i * 1024:5 * S + (i + 1) * 1024].rearrange("p (a b) -> p a b", a=2), p.buf("rt")) for i in range(2)])
                    an = hs[0:64, 6 * S:7 * S]; anB = p.buf("an")
                    ad = hs[0:64, 7 * S:8 * S]; adB = p.buf("ad")
                    p.op("sp", lambda e: e.dma_start(out=rope, in_=dr["rope"].rearrange("p (a b) -> p a b", a=2)), w=[ropeB], dsem=ropeB.dsem)

                    def make_aug(src_ap, srcB):
                        p.op("dve", lambda e: e.tensor_copy(out=hi[:], in_=src_ap), r=[srcB], w=[hlB])
                        p.op("dve", lambda e: e.tensor_tensor(out=lo[:], in0=src_ap, in1=hi[:], op=ALU.subtract), r=[srcB, hlB], w=[hlB])

                    def fin_aug(dst, dstB, c0):
                        p.op("dve", lambda e: e.tensor_scalar(out=tq[:], in0=hi[:], scalar1=augc[0:4, c0:c0 + 1], scalar2=augc[0:4, c0 + 2:c0 + 3],
                                                              op0=ALU.mult, op1=ALU.add), r=[hlB] + CB, w=[tqB])
                        p.op("dve", lambda e: e.scalar_tensor_tensor(out=dst[:], in0=lo[:], scalar=augc[0:4, c0 + 1:c0 + 2], in1=tq[:],
                                                                     op0=ALU.mult, op1=ALU.add), r=[hlB, tqB] + CB, w=[dstB])

                    for c in range(4):
                        def ev_q(tc, pp, ppB):
                            p.op("act", lambda e: e.activation(out=qT[:, tc * 512:(tc + 1) * 512], in_=pp[:], func=AF.Copy, scale=0.125), r=[ppB], w=[qTB])

                        def ev_k(tc, pp, ppB):
                            p.op("dve", lambda e: e.tensor_copy(out=kT[:, tc * 512:(tc + 1) * 512], in_=pp[:]), r=[ppB], w=[kTB])

                        def ev_v(si, pp, ppB):
                            p.op("act", lambda e: e.activation(out=Vt[:, 0, si, :], in_=pp[:, 0:128], func=AF.Copy), r=[ppB], w=[VB[0]])
                        proj_fm(W, "wqa", c * 128, ev_q)
                        proj_fm(W, "wka", c * 128, ev_k)
                        proj_tm(W, "wva", c * 128, 128, [slice(tb * 128, (tb + 1) * 128) for tb in range(TB)], ev_v)
                        for hh in range(2):
                            head = 2 * c + hh
                            r0 = 64 * hh

                            def ev_g(tc, pp, ppB, head=head):
                                p.op("act", lambda e: e.activation(out=t0[:, tc * 512:(tc + 1) * 512], in_=pp[:], func=AF.Exp,
                                                                   bias=ngb[:, head:head + 1], scale=-1.0), r=[ppB] + CB, w=[t0B])
                            proj_fm(W, "wfa", head * 128, ev_g)
                            p.op("act", lambda e: e.activation(out=t0[:], in_=t0[:], func=AF.Ln, bias=onesf_one[:, 0:1]), r=[t0B] + CB, w=[t0B])
                            p.op("dve", lambda e: e.tensor_tensor_scan(out=t1[:], data0=ones_bf[:], data1=t0[:], initial=0.0,
                                                                       op0=ALU.mult, op1=ALU.add), r=[t0B] + CB, w=[t1B])
                            make_aug(t1[0:4, :], t1B)
                            fin_aug(augQ, augQB, 0)
                            fin_aug(augK, augKB, 3)
                            for qc in range(4):
                                an_, anB_ = accn.next()
                                ad_, adB_ = accd.next()
                                nkb = 4 * qc + 4
                                for kb in range(nkb):
                                    j0 = max(0, kb - 4 * qc)
                                    c0 = j0 * 128
                                    stt, sttB = sts.next()
                                    pt, ptB = pT.next()
                                    diag = kb >= 4 * qc

                                    def fs(e, stt=stt, kb=kb, qc=qc, c0=c0, diag=diag, r0=r0):
                                        kblk = slice(kb * 128, (kb + 1) * 128)
                                        q0 = qc * 512
                                        inst = None
                                        if diag:
                                            qs = slice(q0 + c0, q0 + c0 + 128)
                                            mm(e, stt[:, c0:c0 + 128], kT[r0:r0 + 64, kblk], qT[r0:r0 + 64, qs], True, False)
                                            mm(e, stt[:, c0:c0 + 128], augK[0:4, kblk], augQ[0:4, qs], False, False)
                                            inst = mm(e, stt[:, c0:c0 + 128], ident[:], masks[:, 0, :], False, True)
                                            c1 = c0 + 128
                                        else:
                                            c1 = c0
                                        if c1 < 512:
                                            qs = slice(q0 + c1, q0 + 512)
                                            mm(e, stt[:, c1:512], kT[r0:r0 + 64, kblk], qT[r0:r0 + 64, qs], True, False)
                                            inst = mm(e, stt[:, c1:512], augK[0:4, kblk], augQ[0:4, qs], False, True)
                                        return inst
                                    p.op("pe", fs, r=[kTB, qTB, augKB, augQB] + CB, w=[sttB])
                                    p.op("act", lambda e, stt=stt, pt=pt, c0=c0: e.activation(out=pt[:, c0:512], in_=stt[:, c0:512], func=AF.Exp),
                                         r=[sttB], w=[ptB])

                                    def fpv(e, pt=pt, kb=kb, c0=c0, hh=hh, an_=an_, ad_=ad_, nkb=nkb):
                                        mm(e, an_[:, c0:512], Vt[:, 0, kb, hh * 64:(hh + 1) * 64], pt[:, c0:512], kb == 0, kb == nkb - 1)
                                        return mm(e, ad_[:, c0:512], ones_bf[:, 0:64], pt[:, c0:512], kb == 0, kb == nkb - 1)
                                    p.op("pe", fpv, r=[ptB, VB[0]] + CB, w=[anB_, adB_])
                                p.op("dve", lambda e, ad_=ad_, qc=qc: e.reciprocal(out=rec[:, qc * 512:(qc + 1) * 512], in_=ad_[:]), r=[adB_], w=[recB])
                                p.op("dve", lambda e, an_=an_, qc=qc, r0=r0, c=c: e.tensor_tensor(
                                    out=oT[r0:r0 + 64, c, qc * 512:(qc + 1) * 512], in0=an_[:], in1=rec[:, qc * 512:(qc + 1) * 512], op=ALU.mult),
                                    r=[anB_, recB], w=[oTB[c]])

                    def tokset(bi, si):
                        if bi == 0:
                            return slice(si * 128, (si + 1) * 128)
                        if bi == 1:
                            r_, n_ = si // 4, si % 4
                            return slice(512 * n_ + r_, 512 * (n_ + 1), 4)
                        return slice(si, S, 16)

                    def accview(t, bi, si):
                        if bi == 0:
                            return t[:, :].rearrange("p (n j) -> p n j", j=128)[:, si:si + 2, :]
                        if bi == 1:
                            r_, n_ = si // 4, si % 4
                            return t[:, :].rearrange("p (n j r) -> p n j r", n=4, j=128, r=4)[:, n_:n_ + 2, :, r_]
                        return t[:, :].rearrange("p (j r) -> p r j", r=16)[:, si:si + 2, :]

                    for c in range(4):
                        def mk_rope(dst, dstB, wn, wns, c=c):
                            store = {}

                            def ev_a(tc, pp, ppB):
                                rtt, rtB = rt.next()
                                store[tc] = (rtt, rtB)
                                p.op("dve", lambda e: e.tensor_tensor(out=rtt[:, 0, :], in0=pp[:], in1=rope[:, 0, tc * 512:(tc + 1) * 512], op=ALU.mult),
                                     r=[ppB, ropeB], w=[rtB])

                            def ev_b(tc, pp, ppB):
                                rtt, rtB = store[tc]
                                p.op("dve", lambda e: e.tensor_tensor(out=rtt[:, 1, :], in0=pp[:], in1=rope[:, 1, tc * 512:(tc + 1) * 512], op=ALU.mult),
                                     r=[ppB, ropeB], w=[rtB])
                                p.op("pool", lambda e: e.tensor_tensor(out=dst[:, tc * 512:(tc + 1) * 512], in0=rtt[:, 0, :], in1=rtt[:, 1, :], op=ALU.add),
                                     r=[rtB], w=[dstB])
                            wa, waB = W["wA"].next()
                            wb, wbB = W["wA"].next()
                            wload(wa[:], wview(wn)[:, :, c * 128:(c + 1) * 128], waB)
                            wload(wb[:], wview(wns)[:, :, c * 128:(c + 1) * 128], wbB)
                            for tc in range(4):
                                for (wt_, wtB_, ev) in ((wa, waB, ev_a), (wb, wbB, ev_b)):
                                    pp, ppB = W["pp"].next()

                                    def f(e, pp=pp, wt_=wt_, tc=tc):
                                        inst = None
                                        for kc in range(KC):
                                            inst = mm(e, pp[:], wt_[:, kc, :], hT[:, kc, tc * 512:(tc + 1) * 512], kc == 0, kc == KC - 1)
                                        return inst
                                    p.op("pe", f, r=[wtB_] + hTB[4 * tc:4 * tc + 4], w=[ppB])
                                    ev(tc, pp, ppB)
                        mk_rope(qT, qTB, "wqd", "wqds")
                        mk_rope(kT, kTB, "wkd", "wkds")
                        for bi in range(3):
                            def ev_v(si, pp, ppB, bi=bi):
                                p.op("act", lambda e: e.activation(out=Vt[:, bi, si, :], in_=pp[:, 0:128], func=AF.Copy), r=[ppB], w=[VB[bi]])
                            proj_tm(W, "wvd", c * 128, 128, [tokset(bi, si) for si in range(16)], ev_v)
                        for hh in range(2):
                            r0 = 64 * hh
                            for bi in range(3):
                                for si in range(0, 16, 2):
                                    blocks = []
                                    for sq in (si, si + 1):
                                        if bi == 0:
                                            prev = sq - 1 if sq >= 1 else None
                                        elif bi == 1:
                                            prev = sq - 1 if (sq % 4) >= 1 else None
                                        else:
                                            prev = None
                                        blocks.append((sq, prev if prev is not None else sq))
                                        blocks.append((sq, sq))
                                    if bi == 2:
                                        mi = 2
                                    elif (bi == 0 and si == 0) or (bi == 1 and si % 4 == 0):
                                        mi = 1
                                    else:
                                        mi = 0
                                    stt, sttB = sts.next()
                                    pt, ptB = pT.next()
                                    an_, anB_ = accn.next()
                                    ad_, adB_ = accd.next()

                                    def fs(e, stt=stt, blocks=blocks, mi=mi, bi=bi, r0=r0):
                                        inst = None
                                        for bk, (sq, sk) in enumerate(blocks):
                                            mm(e, stt[:, bk * 128:(bk + 1) * 128], kT[r0:r0 + 64, tokset(bi, sk)], qT[r0:r0 + 64, tokset(bi, sq)], True, False)
                                            inst = mm(e, stt[:, bk * 128:(bk + 1) * 128], ident[:], mask4[:, mi, bk * 128:(bk + 1) * 128], False, True)
                                        return inst
                                    p.op("pe", fs, r=[kTB, qTB] + CB, w=[sttB])
                                    p.op("act", lambda e, stt=stt, pt=pt: e.activation(out=pt[:], in_=stt[:], func=AF.Exp, scale=0.125), r=[sttB], w=[ptB])

                                    def fpv(e, pt=pt, blocks=blocks, bi=bi, hh=hh, an_=an_, ad_=ad_):
                                        inst = None
                                        for bk, (sq, sk) in enumerate(blocks):
                                            qi = bk // 2
                                            first = (bk % 2 == 0)
                                            mm(e, an_[:, qi * 128:(qi + 1) * 128], Vt[:, bi, sk, hh * 64:(hh + 1) * 64], pt[:, bk * 128:(bk + 1) * 128], first, not first)
                                            inst = mm(e, ad_[:, qi * 128:(qi + 1) * 128], ones_bf[:, 0:64], pt[:, bk * 128:(bk + 1) * 128], first, not first)
                                        return inst
                                    p.op("pe", fpv, r=[ptB, VB[bi]] + CB, w=[anB_, adB_])
                                    pv = lambda t: t[:, 0:256].rearrange("p (a j) -> p a j", a=2)
                                    if bi == 0:
                                        p.op("dve", lambda e, an_=an_, si=si: e.tensor_copy(out=accview(an, 0, si), in_=pv(an_)), r=[anB_], w=[anB])
                                        p.op("dve", lambda e, ad_=ad_, si=si: e.tensor_copy(out=accview(ad, 0, si), in_=pv(ad_)), r=[adB_], w=[adB])
                                    else:
                                        p.op("dve", lambda e, an_=an_, si=si, bi=bi: e.tensor_tensor(out=accview(an, bi, si), in0=pv(an_), in1=accview(an, bi, si), op=ALU.add),
                                             r=[anB_, anB], w=[anB])
                                        p.op("dve", lambda e, ad_=ad_, si=si, bi=bi: e.tensor_tensor(out=accview(ad, bi, si), in0=pv(ad_), in1=accview(ad, bi, si), op=ALU.add),
                                             r=[adB_, adB], w=[adB])
                            p.op("dve", lambda e: e.reciprocal(out=rec[:], in_=ad[:]), r=[adB], w=[recB])
                            p.op("dve", lambda e, r0=r0, c=c: e.tensor_tensor(out=oT[r0:r0 + 64, 4 + c, :], in0=an[:], in1=rec[:], op=ALU.mult),
                                 r=[anB, recB], w=[oTB[4 + c]])
                    if debug and s == 0:
                        dB = p.buf("dbg2", dma=True)
                        p.op("pool", lambda e: e.dma_start(out=dbg2[0:128, :].rearrange("p (a b) -> p a b", a=KC), in_=oT[:]), r=oTB, dsem=dB.dsem)
                        p.op("pool", lambda e: e.dma_start(out=dbg2[128:256, 0:S], in_=qT[:]), r=[qTB], dsem=dB.dsem)
                        p.op("pool", lambda e: e.dma_start(out=dbg2[128:256, S:2 * S], in_=kT[:]), r=[kTB], dsem=dB.dsem)
                        p.op("pool", lambda e: e.dma_start(out=dbg2[128:192, 2 * S:3 * S], in_=an), r=[anB], dsem=dB.dsem)
                        p.op("pool", lambda e: e.dma_start(out=dbg2[128:192, 3 * S:4 * S], in_=ad), r=[adB], dsem=dB.dsem)
                        p.op("pool", lambda e: e.dma_start(out=dbg2[128:256, 4 * S:5 * S], in_=Vt[:, 1, :, :].rearrange("p a b -> p (a b)")), r=VB, dsem=dB.dsem)
                    p.barrier()

            _ph2()
            def mix_stage(wname, li, dbg_i, row0=row0):
                with ExitStack() as st:
                    p.phase_begin()
                    L = ln_alloc(st, "m%d" % li)
                    L["eps"] = epsT
                    ln_load(L, li)
                    for tb in range(TB):
                        src = dr["x"][row0 + tb * 128: row0 + (tb + 1) * 128, :] if li == 0 else hsp[tb * 128:(tb + 1) * 128, :]
                        p.op("sp", lambda e, tb=tb, src=src: e.dma_start(out=h[:, tb, :], in_=src), w=[hB[tb]], dsem=hB[tb].dsem)
                    wo = sb(st, "wo", [128, KC, D], BF16)
                    woB = p.buf("wo", dma=True)
                    wload(wo[:, :, 0:512], wview(wname)[:, :, 0:512], woB)
                    wload(wo[:, :, 512:1024], wview(wname)[:, :, 512:1024], woB)
                    mixp = Rot([(ps(st, "mix%d" % i, [128, D]), p.buf("mix")) for i in range(2)])
                    for tb in range(TB):
                        mp, mpB = mixp.next()

                        def f(e, mp=mp, tb=tb):
                            inst = None
                            for half in range(2):
                                for c in range(KC):
                                    inst = mm(e, mp[:, half * 512:(half + 1) * 512], oT[:, c, tb * 128:(tb + 1) * 128],
                                              wo[:, c, half * 512:(half + 1) * 512], c == 0, c == KC - 1)
                            return inst
                        p.op("pe", f, r=[woB] + oTB, w=[mpB])
                        p.op("dve", lambda e, mp=mp, tb=tb: e.scalar_tensor_tensor(out=h[:, tb, :], in0=h[:, tb, :], scalar=ALPHA, in1=mp[:],
                                                                                   op0=ALU.mult, op1=ALU.add), r=[hB[tb], mpB], w=[hB[tb]])
                        ln_block(tb, L, to_hT=True, dbg_row0=(dbg_i * S if (debug and s == 0) else None))
                    p.barrier()
            mix_stage("wo0", 0, 0)
            if phases < 2:
                return

            def ffn_alloc(st):
                Fd = {}
                Fd["wg"] = Rot([(sb(st, "wg%d" % i, [128, KC, 512], BF16), p.buf("wg", dma=True)) for i in range(2)])
                Fd["wu"] = Rot([(sb(st, "wu%d" % i, [128, KC, 512], BF16), p.buf("wu", dma=True)) for i in range(2)])
                Fd["wd"] = Rot([(sb(st, "wd%d" % i, [128, 4, D], BF16), p.buf("wd", dma=True)) for i in range(2)])
                Fd["aT"] = Rot([(oT[:, 4 * i:4 * i + 4, :], p.buf("aT")) for i in range(2)])
                Fd["sg"] = Rot([(sb(st, "sg%d" % i, [128, 512], F32), p.buf("sg")) for i in range(2)])
                Fd["pg"] = Rot([(ps(st, "pg%d" % i, [128, 512]), p.buf("pg")) for i in range(2)])
                Fd["pu"] = Rot([(ps(st, "pu%d" % i, [128, 512]), p.buf("pu")) for i in range(2)])
                Fd["yp"] = Rot([(ps(st, "yp%d" % i, [128, D]), p.buf("yp")) for i in range(2)])
                return Fd

            def ffn(Fd, gname, uname, dname, grow0, drow0, F, gate_fn):
                f0 = 0
                while f0 < F:
                    gw = min(512, F - f0)
                    gc = gw // 128
                    wg, wgB = Fd["wg"].next()
                    wu, wuB = Fd["wu"].next()
                    wd, wdB = Fd["wd"].next()
                    aT, aTB = Fd["aT"].next()
                    wload(wg[:, :, 0:gw], wview(gname, grow0, D)[:, :, f0:f0 + gw], wgB)
                    wload(wu[:, :, 0:gw], wview(uname, grow0, D)[:, :, f0:f0 + gw], wuB)
                    wload(wd[:, 0:gc, :], wview(dname, drow0 + f0, gw), wdB)
                    for ci in range(gc):
                        for tc in range(4):
                            pg, pgB = Fd["pg"].next()
                            pu, puB = Fd["pu"].next()
                            sg, sgB = Fd["sg"].next()

                            def fg_(e, pg=pg, wg=wg, ci=ci, tc=tc):
                                inst = None
                                for kc in range(KC):
                                    inst = mm(e, pg[:], wg[:, kc, ci * 128:(ci + 1) * 128], hT[:, kc, tc * 512:(tc + 1) * 512], kc == 0, kc == KC - 1)
                                return inst

                            def fu_(e, pu=pu, wu=wu, ci=ci, tc=tc):
                                inst = None
                                for kc in range(KC):
                                    inst = mm(e, pu[:], wu[:, kc, ci * 128:(ci + 1) * 128], hT[:, kc, tc * 512:(tc + 1) * 512], kc == 0, kc == KC - 1)
                                return inst
                            p.op("pe", fg_, r=[wgB] + hTB[4 * tc:4 * tc + 4], w=[pgB])
                            p.op("pe", fu_, r=[wuB] + hTB[4 * tc:4 * tc + 4], w=[puB])
                            p.op("act", lambda e, sg=sg, pg=pg: e.activation(out=sg[:], in_=pg[:], func=AF.Silu), r=[pgB], w=[sgB])
                            p.op("dve", lambda e, sg=sg, pu=pu, aT=aT, ci=ci, tc=tc: e.tensor_tensor(
                                out=aT[:, ci, tc * 512:(tc + 1) * 512], in0=pu[:], in1=sg[:], op=ALU.mult), r=[puB, sgB], w=[aTB])
                    for tb in range(TB):
                        yp, ypB = Fd["yp"].next()

                        def fd_(e, yp=yp, aT=aT, wd=wd, tb=tb, gc=gc):
                            inst = None
                            for half in range(2):
                                for ci in range(gc):
                                    inst = mm(e, yp[:, half * 512:(half + 1) * 512], aT[:, ci, tb * 128:(tb + 1) * 128],
                                              wd[:, ci, half * 512:(half + 1) * 512], ci == 0, ci == gc - 1)
                            return inst
                        p.op("pe", fd_, r=[aTB, wdB], w=[ypB])
                        g_ap, gBs = gate_fn(tb)
                        p.op("dve", lambda e, yp=yp, tb=tb, g_ap=g_ap: e.scalar_tensor_tensor(out=h[:, tb, :], in0=yp[:], scalar=g_ap, in1=h[:, tb, :],
                                                                                            op0=ALU.mult, op1=ALU.add), r=[ypB, hB[tb]] + gBs, w=[hB[tb]])
                    f0 += gw

            def scale_h():
                for tb in range(TB):
                    p.op("act", lambda e, tb=tb: e.mul(out=h[:, tb, :], in_=h[:, tb, :], mul=ALPHA), r=[hB[tb]], w=[hB[tb]])

            def final_ln(li, dbg_i, to_hT, out_row0, spill=False):
                outs = []
                with ExitStack() as st:
                    p.phase_begin()
                    L = ln_alloc(st, "f%d" % li)
                    L["eps"] = epsT
                    ln_load(L, li)
                    for tb in range(TB):
                        o_ = ln_block(tb, L, to_hT=to_hT, out_row0=out_row0, dbg_row0=(dbg_i * S if (debug and s == 0) else None), spill=spill)
                        if o_ is not None:
                            outs.append(o_)
                    p.barrier()
                return outs

            def _ph3():
                with ExitStack() as st:
                    p.phase_begin()
                    Fd = ffn_alloc(st)
                    scale_h()
                    ffn(Fd, "fg", "fu", "fd", 0, 0, DFF, lambda tb: (1.0, []))
                    p.barrier()
            _ph3()
            final_ln(1, 1, True, None, spill=True)
            if phases < 3:
                return

            def _ph4():
                with ExitStack() as st:
                    p.phase_begin()
                    W = {}
                    W["wA"] = Rot([(sb(st, "wA%d" % i, [128, KC, 128], BF16), p.buf("wA", dma=True)) for i in range(3)])
                    W["pp"] = Rot([(ps(st, "pp%d" % i, [128, 512]), p.buf("pp")) for i in range(1)])
                    qT = sb(st, "qT", [128, S], BF16); qTB = p.buf("qT")
                    kT = sb(st, "kT", [128, S], BF16); kTB = p.buf("kT")
                    Vh = sb(st, "Vh", [128, TB, 128], BF16); VhB = p.buf("Vh")
                    sgo = sb(st, "sgo", [128, S], BF16); sgoB = p.buf("sgo")
                    pre = hs[:, 0:3 + S]; preB = p.buf("pre"); padB = p.buf("pad")
                    yv = hs[:, 2052:2052 + S]; yvB = p.buf("yv")
                    t0 = hs[:, 3:3 + S]; t0B = preB
                    t1 = hs[:, 4100:4100 + S]; t1B = p.buf("t1")
                    t2 = yv; t2B = yvB
                    hi = sb(st, "hi", [4, S], BF16); lo = sb(st, "lo", [4, S], BF16); hlB = p.buf("hl")
                    ua = hs[0:4, 9220:9220 + S]; uaB = p.buf("ua")
                    augQ = sb(st, "augQ", [4, S], BF16); augQB = p.buf("augQ")
                    augK = sb(st, "augK", [4, S], BF16); augKB = p.buf("augK")
                    tq = sb(st, "tq", [4, S], BF16); tqB = p.buf("tq")
                    pT = Rot([(sb(st, "pT%d" % i, [128, 512], BF16), p.buf("pT")) for i in range(2)])
                    Et = Rot([(hs[:, 6148 + i * 512:6148 + (i + 1) * 512], p.buf("Et")) for i in range(2)])
                    psA = Rot([(ps(st, "psA%d" % i, [128, 512]), p.buf("psA")) for i in range(2)])
                    psB = Rot([(ps(st, "psB%d" % i, [128, 512]), p.buf("psB")) for i in range(2)])
                    accn_t = ps(st, "accn", [128, 512]); accnB = p.buf("accn")
                    accd_t = ps(st, "accd", [128, 512]); accdB = p.buf("accd")
                    stat_t = ps(st, "stat", [128, 512]); statB = p.buf("stat")
                    f1 = hs[:, 7172:7684]; f1B = p.buf("f1")
                    f2 = hs[:, 7684:8196]; f2B = p.buf("f2")
                    f3 = hs[:, 8196:8708]; f3B = p.buf("f3")
                    f4 = hs[:, 8708:9220]; f4B = p.buf("f4")
                    p.op("dve", lambda e: e.memset(pre[:, 0:3], 0.0), w=[padB])

                    def make_aug(src_ap, srcB):
                        p.op("dve", lambda e: e.tensor_copy(out=hi[:], in_=src_ap), r=[srcB], w=[hlB])
                        p.op("dve", lambda e: e.tensor_tensor(out=lo[:], in0=src_ap, in1=hi[:], op=ALU.subtract), r=[srcB, hlB], w=[hlB])

                    def fin_aug(dst, dstB, c0):
                        p.op("dve", lambda e: e.tensor_scalar(out=tq[:], in0=hi[:], scalar1=augc[0:4, c0:c0 + 1], scalar2=augc[0:4, c0 + 2:c0 + 3],
                                                              op0=ALU.mult, op1=ALU.add), r=[hlB] + CB, w=[tqB])
                        p.op("dve", lambda e: e.scalar_tensor_tensor(out=dst[:], in0=lo[:], scalar=augc[0:4, c0 + 1:c0 + 2], in1=tq[:],
                                                                     op0=ALU.mult, op1=ALU.add), r=[hlB, tqB] + CB, w=[dstB])

                    for hd in range(8):
                        def conv_silu(wname, col0, chunk, dst, dstB):
                            def ev(tc, pp, ppB):
                                p.op("act", lambda e: e.activation(out=pre[:, 3 + tc * 512: 3 + (tc + 1) * 512], in_=pp[:], func=AF.Copy), r=[ppB], w=[preB])
                            proj_fm(W, wname, col0, ev)
                            p.op("dve", lambda e: e.tensor_scalar(out=yv[:], in0=pre[:, 3:3 + S], scalar1=wc[:, chunk, 3:4], scalar2=None, op0=ALU.mult),
                                 r=[preB, padB] + CB, w=[yvB])
                            for i in range(3):
                                p.op("dve", lambda e, i=i: e.scalar_tensor_tensor(out=yv[:], in0=pre[:, i:i + S], scalar=wc[:, chunk, i:i + 1], in1=yv[:],
                                                                                 op0=ALU.mult, op1=ALU.add), r=[preB, padB, yvB] + CB, w=[yvB])
                            p.op("act", lambda e: e.activation(out=dst[:], in_=yv[:], func=AF.Silu), r=[yvB], w=[dstB])
                        conv_silu("wq1", hd * 128, hd, qT, qTB)
                        conv_silu("wk1", hd * 128, 8 + hd, kT, kTB)

                        def ev_v(si, pp, ppB):
                            p.op("act", lambda e: e.activation(out=Vh[:, si, :], in_=pp[:, 0:128], func=AF.Copy), r=[ppB], w=[VhB])
                        proj_tm(W, "wv1", hd * 128, 128, [slice(tb * 128, (tb + 1) * 128) for tb in range(TB)], ev_v)

                        def ev_og(tc, pp, ppB):
                            p.op("act", lambda e: e.activation(out=sgo[:, tc * 512:(tc + 1) * 512], in_=pp[:], func=AF.Sigmoid), r=[ppB], w=[sgoB])
                        proj_fm(W, "wog", hd * 128, ev_og)

                        def ev_f(tc, pp, ppB, hd=hd):
                            p.op("act", lambda e: e.activation(out=t0[:, tc * 512:(tc + 1) * 512], in_=pp[:], func=AF.Exp,
                                                               bias=ngb[:, 16 + hd:17 + hd], scale=-1.0), r=[ppB] + CB, w=[t0B])
                        proj_fm(W, "wfg", hd * 128, ev_f)
                        p.op("act", lambda e: e.activation(out=t0[:], in_=t0[:], func=AF.Ln, bias=onesf_one[:, 0:1]), r=[t0B] + CB, w=[t0B])
                        p.op("dve", lambda e: e.tensor_tensor_scan(out=t1[:], data0=ones_bf[:], data1=t0[:], initial=0.0, op0=ALU.mult, op1=ALU.add),
                             r=[t0B] + CB, w=[t1B])

                        def ev_i(tc, pp, ppB, hd=hd):
                            p.op("dve", lambda e: e.scalar_tensor_tensor(out=t0[:, tc * 512:(tc + 1) * 512], in0=pp[:], scalar=gb[:, 8 + hd:9 + hd],
                                                                         in1=t1[:, tc * 512:(tc + 1) * 512], op0=ALU.add, op1=ALU.add),
                                 r=[ppB, t1B] + CB, w=[t0B])
                        proj_fm(W, "wig", hd * 128, ev_i)
                        p.op("dve", lambda e: e.tensor_tensor_scan(out=t2[:], data0=t0[:], data1=t0[:], initial=-1e30, op0=ALU.max, op1=ALU.max),
                             r=[t0B] + CB, w=[t2B])
                        p.op("dve", lambda e: e.tensor_tensor(out=t1[:], in0=t1[:], in1=t2[:], op=ALU.subtract), r=[t1B, t2B], w=[t1B])
                        p.op("act", lambda e: e.activation(out=t1[:], in_=t1[:], func=AF.Exp), r=[t1B], w=[t1B])
                        make_aug(t2[0:4, :], t2B)
                        fin_aug(augQ, augQB, 0)
                        p.op("dve", lambda e: e.tensor_scalar(out=ua[:], in0=t0[0:4, :], scalar1=LNS, scalar2=None, op0=ALU.add), r=[t0B], w=[uaB])
                        make_aug(ua[:], uaB)
                        fin_aug(augK, augKB, 3)

                        for qc in range(4):
                            nkb = 4 * qc + 4
                            for kb in range(nkb):
                                j0 = max(0, kb - 4 * qc)
                                c0 = j0 * 128
                                diag = kb >= 4 * qc
                                pa, paB = psA.next()
                                pb, pbB = psB.next()
                                pt, ptB = pT.next()
                                et, etB = Et.next()
                                kblk = slice(kb * 128, (kb + 1) * 128)
                                qs = slice(qc * 512 + c0, qc * 512 + 512)
                                p.op("pe", lambda e, pa=pa, kblk=kblk, qs=qs, c0=c0: mm(e, pa[:, c0:512], kT[:, kblk], qT[:, qs], True, True),
                                     r=[kTB, qTB], w=[paB])

                                def fd_(e, pb=pb, kblk=kblk, qc=qc, c0=c0, diag=diag):
                                    q0 = qc * 512
                                    inst = None
                                    c1 = c0
                                    if diag:
                                        qs1 = slice(q0 + c0, q0 + c0 + 128)
                                        mm(e, pb[:, c0:c0 + 128], augK[0:4, kblk], augQ[0:4, qs1], True, False)
                                        inst = mm(e, pb[:, c0:c0 + 128], ident[:], masks[:, 0, :], False, True)
                                        c1 = c0 + 128
                                    if c1 < 512:
                                        inst = mm(e, pb[:, c1:512], augK[0:4, kblk], augQ[0:4, slice(q0 + c1, q0 + 512)], True, True)
                                    return inst
                                p.op("pe", fd_, r=[augKB, augQB] + CB, w=[pbB])
                                p.op("act", lambda e, pb=pb, et=et, c0=c0: e.activation(out=et[:, c0:512], in_=pb[:, c0:512], func=AF.Exp), r=[pbB], w=[etB])
                                p.op("dve", lambda e, pa=pa, et=et, pt=pt, c0=c0: e.tensor_tensor(out=pt[:, c0:512], in0=pa[:, c0:512], in1=et[:, c0:512], op=ALU.mult),
                                     r=[paB, etB], w=[ptB])

                                def fpv(e, pt=pt, kb=kb, c0=c0, nkb=nkb):
                                    mm(e, accn_t[:, c0:512], Vh[:, kb, :], pt[:, c0:512], kb == 0, kb == nkb - 1)
                                    return mm(e, accd_t[:, c0:512], ones_bf[:, 0:128], pt[:, c0:512], kb == 0, kb == nkb - 1)
                                p.op("pe", fpv, r=[ptB, VhB] + CB, w=[accnB, accdB])
                            cs = slice(qc * 512, (qc + 1) * 512)
                            p.op("act", lambda e: e.activation(out=f1[:], in_=accd_t[:], func=AF.Abs), r=[accdB], w=[f1B])
                            p.op("dve", lambda e, cs=cs: e.tensor_tensor(out=f1[:], in0=f1[:], in1=t1[:, cs], op=ALU.max), r=[f1B, t1B], w=[f1B])
                            p.op("dve", lambda e: e.reciprocal(out=f1[:], in_=f1[:]), r=[f1B], w=[f1B])
                            p.op("dve", lambda e: e.tensor_tensor(out=f2[:], in0=accn_t[:], in1=f1[:], op=ALU.mult), r=[accnB, f1B], w=[f2B])
                            p.op("act", lambda e: e.activation(out=f3[:], in_=f2[:], func=AF.Square), r=[f2B], w=[f3B])
                            p.op("pe", lambda e: mm(e, stat_t[:], onesf[:], f2[:], True, True), r=[f2B] + CB, w=[statB])
                            p.op("act", lambda e: e.activation(out=f1[:], in_=stat_t[:], func=AF.Copy), r=[statB], w=[f1B])
                            p.op("pe", lambda e: mm(e, stat_t[:], onesf[:], f3[:], True, True), r=[f3B, f1B] + CB, w=[statB])
                            p.op("dve", lambda e: e.tensor_tensor(out=f4[:], in0=f1[:], in1=f1[:], op=ALU.mult), r=[f1B], w=[f4B])
                            p.op("dve", lambda e: e.tensor_tensor(out=f4[:], in0=stat_t[:], in1=f4[:], op=ALU.subtract), r=[statB, f4B], w=[f4B])
                            p.op("act", lambda e: e.activation(out=f4[:], in_=f4[:], func=AF.Ln, bias=epsT[:, 0:1]), r=[f4B] + CB, w=[f4B])
                            p.op("act", lambda e: e.activation(out=f4[:], in_=f4[:], func=AF.Exp, scale=-0.5), r=[f4B], w=[f4B])
                            p.op("dve", lambda e: e.tensor_tensor(out=f2[:], in0=f2[:], in1=f1[:], op=ALU.subtract), r=[f2B, f1B], w=[f2B])
                            p.op("dve", lambda e: e.tensor_tensor(out=f2[:], in0=f2[:], in1=f4[:], op=ALU.mult), r=[f2B, f4B], w=[f2B])
                            p.op("dve", lambda e, cs=cs, hd=hd: e.scalar_tensor_tensor(out=oT[:, hd, cs], in0=f2[:], scalar=ng[:, hd:hd + 1], in1=sgo[:, cs],
                                                                                       op0=ALU.mult, op1=ALU.mult), r=[f2B, sgoB] + CB, w=[oTB[hd]])
                    if debug and s == 0:
                        dB = p.buf("dbg2b", dma=True)
                        p.op("pool", lambda e: e.dma_start(out=dbg2[128:256, :].rearrange("p (a b) -> p a b", a=KC), in_=oT[:]), r=oTB, dsem=dB.dsem)
                    p.barrier()
            _ph4()
            mix_stage("wo1", 2, 2)
            if phases < 4:
                return

            def _ph5():
                with ExitStack() as st:
                    p.phase_begin()
                    gates = sb(st, "gates", [128, 128], F32); gatesB = p.buf("gates")
                    st_outer = st
                    st = st_outer.enter_context(ExitStack())
                    wr = sb(st, "wr", [128, KC, 8], BF16); wrB = p.buf("wr", dma=True)
                    wload(wr[:], wview("wr"), wrB)
                    lgp = ps(st, "lgp", [128, 128]); lgB = p.buf("lg")
                    Lg = sb(st, "Lg", [128, 128], F32); LgB = p.buf("Lg")
                    L2 = sb(st, "L2", [128, 128], F32)
                    e1 = sb(st, "e1", [128, 128], F32)
                    e2 = sb(st, "e2", [128, 128], F32)
                    m1 = sb(st, "m1", [128, 16], F32)
                    m2 = sb(st, "m2", [128, 16], F32)
                    w1 = sb(st, "w1", [128, 16], F32)
                    w2 = sb(st, "w2", [128, 16], F32)
                    v3 = lambda t: t[:, :].rearrange("p (a b) -> p a b", b=8)
                    bc = lambda t: t[:, :].unsqueeze(2).to_broadcast([128, 16, 8])

                    def flg(e):
                        inst = None
                        for tb in range(TB):
                            for kc in range(KC):
                                inst = mm(e, lgp[:, tb * 8:(tb + 1) * 8], hT[:, kc, tb * 128:(tb + 1) * 128], wr[:, kc, :], kc == 0, kc == KC - 1)
                        return inst
                    p.op("pe", flg, r=[wrB] + hTB, w=[lgB])
                    G = [LgB]
                    p.op("dve", lambda e: e.tensor_copy(out=Lg[:], in_=lgp[:]), r=[lgB], w=G)
                    p.op("dve", lambda e: e.tensor_reduce(out=m1[:], in_=v3(Lg), axis=AX.X, op=ALU.max), r=G, w=G)
                    p.op("dve", lambda e: e.tensor_tensor(out=v3(e1), in0=v3(Lg), in1=bc(m1), op=ALU.is_equal), r=G, w=G)
                    p.op("dve", lambda e: e.scalar_tensor_tensor(out=L2[:], in0=e1[:], scalar=-1e30, in1=Lg[:], op0=ALU.mult, op1=ALU.add), r=G, w=G)
                    p.op("dve", lambda e: e.tensor_reduce(out=m2[:], in_=v3(L2), axis=AX.X, op=ALU.max), r=G, w=G)
                    p.op("dve", lambda e: e.tensor_tensor(out=v3(e2), in0=v3(L2), in1=bc(m2), op=ALU.is_equal), r=G, w=G)
                    p.op("dve", lambda e: e.tensor_tensor(out=w2[:], in0=m2[:], in1=m1[:], op=ALU.subtract), r=G, w=G)
                    p.op("act", lambda e: e.activation(out=w2[:], in_=w2[:], func=AF.Exp), r=G, w=G)
                    p.op("dve", lambda e: e.tensor_scalar(out=w1[:], in0=w2[:], scalar1=1.0, scalar2=None, op0=ALU.add), r=G, w=G)
                    p.op("dve", lambda e: e.reciprocal(out=w1[:], in_=w1[:]), r=G, w=G)
                    p.op("dve", lambda e: e.tensor_tensor(out=w2[:], in0=w2[:], in1=w1[:], op=ALU.mult), r=G, w=G)
                    p.op("dve", lambda e: e.tensor_tensor(out=v3(e1), in0=v3(e1), in1=bc(w1), op=ALU.mult), r=G, w=G)
                    p.op("dve", lambda e: e.tensor_tensor(out=v3(e2), in0=v3(e2), in1=bc(w2), op=ALU.mult), r=G, w=G)
                    p.op("dve", lambda e: e.tensor_tensor(out=gates[:], in0=e1[:], in1=e2[:], op=ALU.add), r=G, w=[gatesB])
                    p.barrier()
                    st.close()
                    st = st_outer
                    Fd = ffn_alloc(st)
                    scale_h()
                    for ex in range(NE):
                        ffn(Fd, "mg", "mu", "md", ex * D, ex * DFE, DFE, lambda tb, ex=ex: (gates[:, tb * 8 + ex: tb * 8 + ex + 1], [gatesB]))
                    p.barrier()
            _ph5()
            outs = final_ln(3, 3, False, row0)

        for s_ in range(nseq):
            do_seq(s_)
        lasts = list(p.lastd.values())
        p.pend["sp"] = lasts
        p.op("sp", lambda e: None)
        block = es.enter_context(nc.Block())
        p.emit(block)
    return nc


def prep_shared(inp):
    f = lambda a: np.ascontiguousarray(a, dtype=np.float32)
    w_in_e = inp["w_in_e"][0]
    sh = {}
    sh["wqa"] = f(w_in_e[:, 0:512]); sh["wka"] = f(w_in_e[:, 512:1024]); sh["wva"] = f(w_in_e[:, 1024:1536])
    sh["wfa"] = f(np.repeat(w_in_e[:, 1536:1544], 128, axis=1))
    qd = w_in_e[:, 1544:2056]; kd = w_in_e[:, 2056:2568]
    sh["wqd"] = f(qd); sh["wkd"] = f(kd); sh["wvd"] = f(w_in_e[:, 2568:3080])
    perm = np.arange(512)
    for hh in range(8):
        for i in range(8):
            perm[hh * 64 + i] = hh * 64 + i + 8
            perm[hh * 64 + 8 + i] = hh * 64 + i
    sh["wqds"] = f(qd[:, perm]); sh["wkds"] = f(kd[:, perm])
    sh["wo0"] = f(inp["w_out_e"][0]); sh["fg"] = f(inp["ffn_w_gate_e"][0]); sh["fu"] = f(inp["ffn_w_up_e"][0]); sh["fd"] = f(inp["ffn_w_down_e"][0])
    w_in_o = inp["w_in_o"][0]
    sh["wq1"] = f(w_in_o[:, 0:1024]); sh["wk1"] = f(w_in_o[:, 1024:2048]); sh["wv1"] = f(w_in_o[:, 2048:3072])
    sh["wig"] = f(np.repeat(w_in_o[:, 3072:3080], 128, axis=1)); sh["wfg"] = f(np.repeat(w_in_o[:, 3080:3088], 128, axis=1))
    sh["wog"] = f(w_in_o[:, 3088:4112])
    sh["wo1"] = f(inp["w_out_o"][0]); sh["wr"] = f(inp["w_router_o"][0])
    sh["mg"] = f(inp["moe_w_gate_o"][0].reshape(NE * D, DFE)); sh["mu"] = f(inp["moe_w_up_o"][0].reshape(NE * D, DFE))
    sh["md"] = f(inp["moe_w_down_o"][0].reshape(NE * DFE, D))
    lnp = np.stack([np.stack([inp["ln_mix_g_e"][0], inp["ln_mix_b_e"][0]]), np.stack([inp["ln_ffn_g_e"][0], inp["ln_ffn_b_e"][0]]),
                    np.stack([inp["ln_mix_g_o"][0], inp["ln_mix_b_o"][0]]), np.stack([inp["ln_ffn_g_o"][0], inp["ln_ffn_b_o"][0]])])
    sh["lnp"] = f(np.broadcast_to(lnp[:, :, None, :], (4, 2, 128, D)).reshape(4 * 2 * 128, D))
    gbv = np.concatenate([inp["b_forget_e"][0], inp["b_igate_o"][0], inp["b_fgate_o"][0]])
    sh["gb"] = f(np.broadcast_to(gbv[None, :], (128, 24)))
    sh["ng"] = f(inp["mlstm_norm_g_o"][0].reshape(8, 128).T)
    sh["wc"] = f(inp["w_conv_o"][0].T.reshape(16, 128, 4).transpose(1, 0, 2).reshape(128, 64))
    sh["ident"] = np.eye(128, dtype=np.float32)
    k = np.arange(128)[:, None]; q = np.arange(128)[None, :]
    mC = np.where(k > q, NEG, 0.0).astype(np.float32)
    mU = np.where(k < q, NEG, 0.0).astype(np.float32)
    mA = np.full((128, 128), NEG, np.float32)
    sh["masks"] = f(np.concatenate([mC, mU, mA], axis=1))
    sh["mask4"] = f(np.concatenate([mU, mC, mU, mC, mA, mC, mU, mC, mA, mC, mA, mC], axis=1))
    half = 8
    inv = 500000.0 ** (-np.arange(half, dtype=np.float32) / half)
    ang = np.arange(S, dtype=np.float32)[None, :] * inv[:, None]
    cosT = np.ones((128, S), np.float32); sinT = np.zeros((128, S), np.float32)
    for hh in range(2):
        b = hh * 64
        cosT[b:b + 8] = np.cos(ang); cosT[b + 8:b + 16] = np.cos(ang)
        sinT[b:b + 8] = -np.sin(ang); sinT[b + 8:b + 16] = np.sin(ang)
    sh["rope"] = f(np.concatenate([cosT, sinT], axis=1))
    augc = np.zeros((128, 6), np.float32)
    augc[0:4, 0] = [-1, 0, 0, 0]; augc[0:4, 1] = [0, -1, 0, 0]; augc[0:4, 2] = [0, 0, 1, 1]
    augc[0:4, 3] = [0, 0, 1, 0]; augc[0:4, 4] = [0, 0, 0, 1]; augc[0:4, 5] = [1, 1, 0, 0]
    sh["augc"] = augc
    return sh


N_CORES = 8


def kernel(**inputs):
    x = np.ascontiguousarray(inputs["x"], dtype=np.float32)
    B = x.shape[0]
    nseq = B // N_CORES
    sh = prep_shared(inputs)
    nc = build_nc(nseq)
    in_maps = []
    for c in range(N_CORES):
        m = dict(sh)
        m["x"] = x[c * nseq:(c + 1) * nseq].reshape(nseq * S, D)
        in_maps.append(m)
    res = run_bass_kernel_spmd(nc, in_maps, core_ids=list(range(N_CORES)))
    outs = [np.asarray(r["out"]).reshape(nseq, S, D) for r in res.results]
    return np.concatenate(outs, axis=0).astype(np.float32)
```

```python
import numpy as np
from contextlib import ExitStack
import concourse.bass as bass
import concourse.mybir as mybir
from concourse.bass_utils import run_bass_kernel_spmd

F32 = mybir.dt.float32
BF16 = mybir.dt.bfloat16
AF = mybir.ActivationFunctionType
ALU = mybir.AluOpType
AX = mybir.AxisListType

S = 2048
D = 1024
TB = 16
KC = 8
ALPHA = 4.0 ** 0.25
EPS = 1e-5
NEG = -30000.0
DFF = 2816
DFE = 3584
NE = 8
LNS = float(np.log(128.0 ** -0.5))
ENGS = ("pe", "act", "dve", "pool", "sp")
I32 = mybir.dt.int32
NSEQ = 4
CAP = NSEQ * S
TS = 1024
NT = 2 * CAP // TS + 7
NROWS = 8 * CAP + TS


class Op:
    __slots__ = ("eng", "fn", "deps", "sig", "val", "dsem", "key")


class Buf:
    __slots__ = ("lastw", "readers", "dsem", "name")

    def __init__(self, name, dsem=None):
        self.lastw = None
        self.readers = {}
        self.dsem = dsem
        self.name = name


class Prog:
    def __init__(self, nc, es):
        self.nc = nc
        self.es = es
        self.ops = {e: [] for e in ENGS}
        self.esem = {e: es.enter_context(nc.semaphore("s_" + e)) for e in ENGS}
        self.dcnt = {}
        self.last = {e: None for e in ENGS}
        self.lastd = {}
        self.pend = {e: [] for e in ENGS}
        self.nsem = 0
        self.sem_pool = []
        self.pool_idx = None

    def newsem(self):
        if self.pool_idx is not None:
            if self.pool_idx >= len(self.sem_pool):
                self.nsem += 1
                self.sem_pool.append(self.es.enter_context(self.nc.semaphore("d%d" % self.nsem)))
            sm = self.sem_pool[self.pool_idx]
            self.pool_idx += 1
            return sm
        self.nsem += 1
        return self.es.enter_context(self.nc.semaphore("d%d" % self.nsem))

    def phase_begin(self):
        self.pool_idx = 0

    def buf(self, name, dma=False):
        return Buf(name, self.newsem() if dma else None)

    def op(self, eng, fn, r=(), w=(), dsem=None, force=False):
        o = Op()
        o.eng = eng
        o.fn = fn
        o.sig = False
        o.val = 0
        o.dsem = dsem
        o.key = ("d", id(dsem)) if dsem is not None else eng
        deps = []
        for b in r:
            if b.lastw is not None:
                deps.append((b.lastw, True))
        for b in w:
            if b.lastw is not None:
                deps.append((b.lastw, False))
            for rd in b.readers.values():
                deps.append((rd, False))
        for d in self.pend[eng]:
            deps.append((d, True))
        self.pend[eng] = []
        dd = []
        for d, raw in deps:
            if d is o:
                continue
            if d.key == o.key and (not raw or eng == "pe") and not (force and d.dsem is not None):
                continue
            if d not in dd:
                dd.append(d)
        o.deps = dd
        if dsem is not None:
            c = self.dcnt.get(id(dsem), 0) + 16
            self.dcnt[id(dsem)] = c
            o.val = c
            self.lastd[id(dsem)] = o
        for d in dd:
            if d.dsem is None:
                d.sig = True
        for b in r:
            b.readers[o.key] = o
        for b in w:
            b.lastw = o
            b.readers = {}
        self.ops[eng].append(o)
        self.last[eng] = o
        return o

    def barrier(self):
        lasts = [self.last[e] for e in ENGS if self.last[e] is not None] + list(self.lastd.values())
        for e in ENGS:
            self.pend[e] = list(lasts)

    def emit(self, block):
        for e in ENGS:
            c = 0
            for o in self.ops[e]:
                if o.dsem is None and o.sig:
                    c += 1
                    o.val = c

        def runner(ename):
            def f(eh):
                seen = {}
                for o in self.ops[ename]:
                    for d in o.deps:
                        if seen.get(d.key, 0) >= d.val:
                            continue
                        sem = d.dsem if d.dsem is not None else self.esem[d.eng]
                        eh.wait_ge(sem, d.val)
                        seen[d.key] = d.val
                    inst = o.fn(eh)
                    if inst is None:
                        continue
                    if o.dsem is not None:
                        inst.then_inc(o.dsem, 16)
                    elif o.sig:
                        inst.then_inc(self.esem[ename], 1)
            return f

        block.tensor(runner("pe"))
        block.scalar(runner("act"))
        block.vector(runner("dve"))
        block.gpsimd(runner("pool"))
        block.sync(runner("sp"))


class Rot:
    def __init__(self, items):
        self.items = items
        self.i = 0

    def next(self):
        it = self.items[self.i % len(self.items)]
        self.i += 1
        return it


WNAMES = {
    "wqa": (D, 512), "wka": (D, 512), "wva": (D, 512), "wfa": (D, 1024),
    "wqd": (D, 512), "wkd": (D, 512), "wqds": (D, 512), "wkds": (D, 512), "wvd": (D, 512),
    "wo0": (D, D), "fg": (D, DFF), "fu": (D, DFF), "fd": (DFF, D),
    "wq1": (D, D), "wk1": (D, D), "wv1": (D, D), "wog": (D, D), "wig": (D, D), "wfg": (D, D),
    "wo1": (D, D), "wr": (D, 8), "mg": (NE * 7 * D, 512), "mu": (NE * 7 * D, 512), "md": (NE * DFE, D),
    "lnp": (4 * 2 * 128, D), "gb": (128, 24), "ng": (128, 8), "wc": (128, 64),
    "ident": (128, 128), "masks": (128, 3 * 128), "mask4": (128, 3 * 512), "rope": (128, 2 * S), "augc": (128, 6),
    "cp": (128, 56), "tri": (128, 128), "eoff": (128, 128), "thr": (128, 64), "jc": (128, NT * 8),
}


def build_nc(nseq, debug=False, phases=5):
    nc = bass.Bass("TRN2", target_bir_lowering=False)
    dr = {}
    dr["x"] = nc.dram_tensor("x", [nseq * S, D], F32, kind="ExternalInput").ap()
    for k, shp in WNAMES.items():
        dr[k] = nc.dram_tensor(k, list(shp), F32, kind="ExternalInput").ap()
    out = nc.dram_tensor("out", [nseq * S, D], F32, kind="ExternalOutput").ap()
    hsp = nc.dram_tensor("hsp", [S, D], F32, kind="Internal").ap()
    h1sp = nc.dram_tensor("h1sp", [nseq * S, D], F32, kind="Internal").ap()
    XsH = [nc.dram_tensor("Xs%d" % i, [NROWS, 512], F32, kind="Internal").ap() for i in range(2)]
    YsH = [nc.dram_tensor("Ys%d" % i, [NROWS, 512], F32, kind="Internal").ap() for i in range(2)]
    dbg = None
    if debug:
        dbg = nc.dram_tensor("dbg", [4 * S, D], F32, kind="ExternalOutput").ap()
        dbg2 = nc.dram_tensor("dbg2", [2 * 128, KC * S], F32, kind="ExternalOutput").ap()

    with ExitStack() as es:
        p = Prog(nc, es)

        uid = [0]

        def sb(st, name, shape, dt):
            uid[0] += 1
            return st.enter_context(nc.sbuf_tensor("%s_s%d" % (name, uid[0]), shape, dt))

        def ps(st, name, shape, dt=F32):
            uid[0] += 1
            return st.enter_context(nc.psum_tensor("%s_p%d" % (name, uid[0]), shape, dt))

        h = sb(es, "h", [128, TB, D], F32)
        hB = [p.buf("h%d" % i, dma=True) for i in range(TB)]
        hs = h[:, :, :].rearrange("p a b -> p (a b)")
        hT = sb(es, "hT", [128, KC, S], BF16)
        hTB = [p.buf("hT%d" % i) for i in range(TB)]
        oT = sb(es, "oT", [128, KC, S], BF16)
        oTB = [p.buf("oT%d" % i) for i in range(KC)]
        ident = sb(es, "ident", [128, 128], BF16)
        identB = p.buf("ident", dma=True)
        masks = sb(es, "masks", [128, 3, 128], BF16)
        mask4 = sb(es, "mask4", [128, 3, 512], BF16)
        augc = sb(es, "augc", [128, 6], F32)
        gb = sb(es, "gb", [128, 24], F32)
        ngb = sb(es, "ngb", [128, 24], F32)
        ng = sb(es, "ng", [128, 8], F32)
        wc = sb(es, "wc", [128, 16, 4], F32)
        ones_bf = sb(es, "ones_bf", [128, S], BF16)
        onesf = sb(es, "onesf", [128, 128], F32)
        constB = p.buf("const", dma=True)
        const2B = p.buf("const2")

        p.op("pool", lambda e: e.dma_start(out=ident[:], in_=dr["ident"][:, :]), w=[identB], dsem=identB.dsem)
        p.op("pool", lambda e: e.dma_start(out=masks[:], in_=dr["masks"].rearrange("p (a b) -> p a b", a=3)), w=[constB], dsem=constB.dsem)
        p.op("pool", lambda e: e.dma_start(out=mask4[:], in_=dr["mask4"].rearrange("p (a b) -> p a b", a=3)), w=[constB], dsem=constB.dsem)
        p.op("sp", lambda e: e.dma_start(out=augc[:], in_=dr["augc"][:, :]), w=[constB], dsem=constB.dsem)
        p.op("sp", lambda e: e.dma_start(out=gb[:], in_=dr["gb"][:, :]), w=[constB], dsem=constB.dsem)
        p.op("sp", lambda e: e.dma_start(out=ng[:], in_=dr["ng"][:, :]), w=[constB], dsem=constB.dsem)
        p.op("sp", lambda e: e.dma_start(out=wc[:], in_=dr["wc"].rearrange("p (a b) -> p a b", b=4)), w=[constB], dsem=constB.dsem)
        p.op("dve", lambda e: e.memset(ones_bf[:], 1.0), w=[const2B])
        p.op("dve", lambda e: e.memset(onesf[:], 1.0 / 128.0), w=[const2B])
        p.op("dve", lambda e: e.tensor_scalar(out=ngb[:], in0=gb[:], scalar1=-1.0, scalar2=None, op0=ALU.mult), r=[constB], w=[const2B])
        CB = [constB, const2B, identB]
        tri = sb(es, "tri", [128, 128], BF16)
        eoff = sb(es, "eoff", [128, 128], F32)
        thr = sb(es, "thr", [128, 64], F32)
        jc = sb(es, "jc", [128, NT * 8], F32)
        posI = sb(es, "posI", [128, nseq * 32], I32); posB = p.buf("posI")
        gW = sb(es, "gW", [128, nseq * 32], F32); gWB = p.buf("gW")
        base = sb(es, "base", [128, 8], F32); baseB = p.buf("base")
        NBm = TS // 128
        XO, WO, DO = 0, NT * NBm, NT * NBm + NT * 56
        NTAB = DO + NT * 28
        tabI = sb(es, "tabI", [128, NTAB], I32); tabB = p.buf("tabI")
        cp = sb(es, "cp", [128, 56], F32)
        p.op("sp", lambda e: e.dma_start(out=cp[:], in_=dr["cp"][:, :]), w=[constB], dsem=constB.dsem)
        p.op("pool", lambda e: e.dma_start(out=tri[:], in_=dr["tri"][:, :]), w=[identB], dsem=identB.dsem)
        p.op("sp", lambda e: e.dma_start(out=eoff[:], in_=dr["eoff"][:, :]), w=[constB], dsem=constB.dsem)
        p.op("sp", lambda e: e.dma_start(out=thr[:], in_=dr["thr"][:, :]), w=[constB], dsem=constB.dsem)
        p.op("sp", lambda e: e.dma_start(out=jc[:], in_=dr["jc"][:, :]), w=[constB], dsem=constB.dsem)
        p.op("dve", lambda e: e.memset(base[:], 0.0), w=[baseB])
        IOA = bass.IndirectOffsetOnAxis

        def wview(name, rows_off=0, nrows=D):
            return dr[name][rows_off:rows_off + nrows, :].rearrange("(k p) n -> p k n", p=128)

        def mm(e, o, l, r_, st=True, sp=True):
            return e.matmul(o, l, r_, start=st, stop=sp)

        def build_hT(tb, L):
            xb, xbB = L["xb"].next()
            pst, pstB = L["pst"].next()
            p.op("act", lambda e: e.activation(out=xb[:], in_=h[:, tb, :], func=AF.Copy), r=[hB[tb]], w=[xbB])

            def tr(e):
                inst = None
                for kc in range(KC):
                    inst = e.transpose(pst[:, kc * 128:(kc + 1) * 128], xb[:, kc * 128:(kc + 1) * 128], ident[:])
                return inst
            p.op("pe", tr, r=[xbB, identB], w=[pstB])
            p.op("dve", lambda e: e.tensor_copy(out=hT[:, :, tb * 128:(tb + 1) * 128],
                                                in_=pst[:].rearrange("p (k t) -> p k t", k=KC)), r=[pstB], w=[hTB[tb]])

        def ln_alloc(st, tag):
            L = {}
            L["xb"] = Rot([(sb(st, "xb%s%d" % (tag, i), [128, D], BF16), p.buf("xb")) for i in range(2)])
            L["pst"] = Rot([(ps(st, "pst%s%d" % (tag, i), [128, D], BF16), p.buf("pst")) for i in range(2)])
            L["st6"] = Rot([(sb(st, "st6%s%d" % (tag, i), [128, 12], F32), p.buf("st6")) for i in range(2)])
            L["mv"] = Rot([(sb(st, "mv%s%d" % (tag, i), [128, 4], F32), p.buf("mv")) for i in range(2)])
            L["lnp"] = sb(st, "lnp%s" % tag, [128, 2, D], F32)
            L["lnpB"] = p.buf("lnp", dma=True)
            return L

        def ln_load(L, li):
            src = dr["lnp"][li * 256:(li + 1) * 256, :].rearrange("(a p) n -> p a n", a=2)
            p.op("sp", lambda e: e.dma_start(out=L["lnp"][:], in_=src), w=[L["lnpB"]], dsem=L["lnpB"].dsem)

        def ln_block(tb, L, to_hT=True, out_row0=None, dbg_row0=None, spill=False, gb_eng="pool"):
            st6, st6B = L["st6"].next()
            mv, mvB = L["mv"].next()
            lnp = L["lnp"]
            p.op("dve", lambda e: e.bn_stats(out=st6[:, 0:6], in_=h[:, tb, 0:512]), r=[hB[tb]], w=[st6B])
            p.op("dve", lambda e: e.bn_stats(out=st6[:, 6:12], in_=h[:, tb, 512:1024]), r=[hB[tb]], w=[st6B])
            p.op("dve", lambda e: e.bn_aggr(out=mv[:, 0:2], in_=st6[:]), r=[st6B], w=[mvB])
            p.op("act", lambda e: e.activation(out=mv[:, 2:3], in_=mv[:, 1:2], func=AF.Ln, bias=L["eps"][:, 0:1]), r=[mvB, const2B], w=[mvB])
            p.op("act", lambda e: e.activation(out=mv[:, 3:4], in_=mv[:, 2:3], func=AF.Exp, scale=-0.5), r=[mvB], w=[mvB])
            p.op("dve", lambda e: e.tensor_scalar(out=h[:, tb, :], in0=h[:, tb, :], scalar1=mv[:, 0:1], scalar2=mv[:, 3:4],
                                                  op0=ALU.subtract, op1=ALU.mult), r=[hB[tb], mvB], w=[hB[tb]])
            p.op(gb_eng, lambda e: e.tensor_tensor(out=h[:, tb, :], in0=h[:, tb, :], in1=lnp[:, 0, :], op=ALU.mult), r=[hB[tb], L["lnpB"]], w=[hB[tb]])
            p.op(gb_eng, lambda e: e.tensor_tensor(out=h[:, tb, :], in0=h[:, tb, :], in1=lnp[:, 1, :], op=ALU.add), r=[hB[tb], L["lnpB"]], w=[hB[tb]])
            if dbg_row0 is not None:
                p.op("sp", lambda e: e.dma_start(out=dbg[dbg_row0 + tb * 128: dbg_row0 + (tb + 1) * 128, :], in_=h[:, tb, :]),
                     r=[hB[tb]], dsem=hB[tb].dsem)
            if spill:
                p.op("sp", lambda e: e.dma_start(out=hsp[tb * 128:(tb + 1) * 128, :], in_=h[:, tb, :]), r=[hB[tb]], dsem=hB[tb].dsem)
            if out_row0 is not None:
                return p.op("sp", lambda e: e.dma_start(out=out[out_row0 + tb * 128: out_row0 + (tb + 1) * 128, :], in_=h[:, tb, :]),
                            r=[hB[tb]], dsem=hB[tb].dsem)
            if to_hT:
                build_hT(tb, L)
            return None

        epsT = sb(es, "epsT", [128, 1], F32)
        onesf_one = sb(es, "oneT", [128, 1], F32)
        p.op("dve", lambda e: e.memset(epsT[:], EPS), w=[const2B])
        p.op("dve", lambda e: e.memset(onesf_one[:], 1.0), w=[const2B])

        def wload(dst_ap, src_ap, B):
            return p.op("pool", lambda e: e.dma_start(out=dst_ap, in_=src_ap), w=[B], dsem=B.dsem)

        def proj_fm(W, wname, col0, evac, ncols=128):
            wt, wtB = W["wA"].next()
            wload(wt[:, :, 0:ncols], wview(wname)[:, :, col0:col0 + ncols], wtB)
            for tc in range(4):
                pp, ppB = W["pp"].next()

                def f(e, pp=pp, wt=wt, tc=tc):
                    inst = None
                    for kc in range(KC):
                        inst = mm(e, pp[0:ncols, :], wt[:, kc, 0:ncols], hT[:, kc, tc * 512:(tc + 1) * 512], kc == 0, kc == KC - 1)
                    return inst
                p.op("pe", f, r=[wtB] + hTB[4 * tc:4 * tc + 4], w=[ppB])
                evac(tc, pp, ppB)

        def proj_tm(W, wname, col0, ncols, sets, evac):
            wt, wtB = W["wA"].next()
            wload(wt[:, :, 0:ncols], wview(wname)[:, :, col0:col0 + ncols], wtB)
            for si, tsl in enumerate(sets):
                pp, ppB = W["pp"].next()

                def f(e, pp=pp, wt=wt, tsl=tsl):
                    inst = None
                    for kc in range(KC):
                        inst = mm(e, pp[:, 0:ncols], hT[:, kc, tsl], wt[:, kc, 0:ncols], kc == 0, kc == KC - 1)
                    return inst
                p.op("pe", f, r=[wtB] + hTB, w=[ppB])
                evac(si, pp, ppB)

        def do_seq(s):
            row0 = s * S
            def _ph1():
                with ExitStack() as st:
                    p.phase_begin()
                    L = ln_alloc(st, "p0")
                    L["eps"] = epsT
                    for tb in range(TB):
                        p.op("sp", lambda e, tb=tb, row0=row0: e.dma_start(out=h[:, tb, :], in_=dr["x"][row0 + tb * 128: row0 + (tb + 1) * 128, :]),
                             w=[hB[tb]], dsem=hB[tb].dsem)
                    for tb in range(TB):
                        build_hT(tb, L)
                    p.barrier()
            _ph1()
            if phases < 1:
                return

            def _ph2():
                with ExitStack() as st:
                    p.phase_begin()
                    W = {}
                    W["wA"] = Rot([(sb(st, "wA%d" % i, [128, KC, 128], BF16), p.buf("wA", dma=True)) for i in range(3)])
                    W["pp"] = Rot([(ps(st, "pp%d" % i, [128, 512]), p.buf("pp")) for i in range(2)])
                    qT = sb(st, "qT", [128, S], BF16); qTB = p.buf("qT")
                    kT = sb(st, "kT", [128, S], BF16); kTB = p.buf("kT")
                    Vt = sb(st, "Vt", [128, 3, TB, 128], BF16); VB = [p.buf("V%d" % i) for i in range(3)]
                    t0 = hs[:, 0:S]; t0B = p.buf("t0")
                    t1 = hs[:, S:2 * S]; t1B = p.buf("t1")
                    hi = sb(st, "hi", [4, S], BF16); lo = sb(st, "lo", [4, S], BF16); hlB = p.buf("hl")
                    augQ = sb(st, "augQ", [4, S], BF16); augQB = p.buf("augQ")
                    augK = sb(st, "augK", [4, S], BF16); augKB = p.buf("augK")
                    tq = sb(st, "tq", [4, S], BF16); tqB = p.buf("tq")
                    pT = Rot([(sb(st, "pT%d" % i, [128, 512], BF16), p.buf("pT")) for i in range(2)])
                    rec = hs[0:64, 2 * S:3 * S]; recB = p.buf("rec")
                    sts = Rot([(ps(st, "st%d" % i, [128, 512]), p.buf("st")) for i in range(2)])
                    W["pp"] = Rot(W["pp"].items + sts.items)
                    accn = Rot([(ps(st, "accn%d" % i, [64, 512]), p.buf("accn")) for i in range(2)])
                    accd = Rot([(ps(st, "accd%d" % i, [64, 512]), p.buf("accd")) for i in range(2)])
                    rope = hs[:, 3 * S:5 * S].rearrange("p (a b) -> p a b", a=2); ropeB = p.buf("rope", dma=True)
                    rt = Rot([(hs[:, 5 * S + i * 1024:5 * S + (i + 1) * 1024].rearrange("p (a b) -> p a b", a=2), p.buf("rt")) for i in range(2)])
                    an = hs[0:64, 6 * S:7 * S]; anB = p.buf("an")
                    ad = hs[0:64, 7 * S:8 * S]; adB = p.buf("ad")
                    p.op("sp", lambda e: e.dma_start(out=rope, in_=dr["rope"].rearrange("p (a b) -> p a b", a=2)), w=[ropeB], dsem=ropeB.dsem)

                    def make_aug(src_ap, srcB):
                        p.op("dve", lambda e: e.tensor_copy(out=hi[:], in_=src_ap), r=[srcB], w=[hlB])
                        p.op("dve", lambda e: e.tensor_tensor(out=lo[:], in0=src_ap, in1=hi[:], op=ALU.subtract), r=[srcB, hlB], w=[hlB])

                    def fin_aug(dst, dstB, c0):
                        p.op("dve", lambda e: e.tensor_scalar(out=tq[:], in0=hi[:], scalar1=augc[0:4, c0:c0 + 1], scalar2=augc[0:4, c0 + 2:c0 + 3],
                                                              op0=ALU.mult, op1=ALU.add), r=[hlB] + CB, w=[tqB])
                        p.op("dve", lambda e: e.scalar_tensor_tensor(out=dst[:], in0=lo[:], scalar=augc[0:4, c0 + 1:c0 + 2], in1=tq[:],
                                                                     op0=ALU.mult, op1=ALU.add), r=[hlB, tqB] + CB, w=[dstB])

                    for c in range(4):
                        def ev_q(tc, pp, ppB):
                            p.op("act", lambda e: e.activation(out=qT[:, tc * 512:(tc + 1) * 512], in_=pp[:], func=AF.Copy, scale=0.125), r=[ppB], w=[qTB])

                        def ev_k(tc, pp, ppB):
                            p.op("dve", lambda e: e.tensor_copy(out=kT[:, tc * 512:(tc + 1) * 512], in_=pp[:]), r=[ppB], w=[kTB])

                        def ev_v(si, pp, ppB):
                            p.op("act", lambda e: e.activation(out=Vt[:, 0, si, :], in_=pp[:, 0:128], func=AF.Copy), r=[ppB], w=[VB[0]])
                        proj_fm(W, "wqa", c * 128, ev_q)
                        proj_fm(W, "wka", c * 128, ev_k)
                        proj_tm(W, "wva", c * 128, 128, [slice(tb * 128, (tb + 1) * 128) for tb in range(TB)], ev_v)
                        for hh in range(2):
                            head = 2 * c + hh
                            r0 = 64 * hh

                            def ev_g(tc, pp, ppB, head=head):
                                p.op("act", lambda e: e.activation(out=t0[:, tc * 512:(tc + 1) * 512], in_=pp[:], func=AF.Exp,
                                                                   bias=ngb[:, head:head + 1], scale=-1.0), r=[ppB] + CB, w=[t0B])
                            proj_fm(W, "wfa", head * 128, ev_g)
                            p.op("act", lambda e: e.activation(out=t0[:], in_=t0[:], func=AF.Ln, bias=onesf_one[:, 0:1]), r=[t0B] + CB, w=[t0B])
                            p.op("dve", lambda e: e.tensor_tensor_scan(out=t1[:], data0=ones_bf[:], data1=t0[:], initial=0.0,
                                                                       op0=ALU.mult, op1=ALU.add), r=[t0B] + CB, w=[t1B])
                            make_aug(t1[0:4, :], t1B)
                            fin_aug(augQ, augQB, 0)
                            fin_aug(augK, augKB, 3)
                            for qc in range(4):
                                an_, anB_ = accn.next()
                                ad_, adB_ = accd.next()
                                nkb = 4 * qc + 4
                                for kb in range(nkb):
                                    j0 = max(0, kb - 4 * qc)
                                    c0 = j0 * 128
                                    stt, sttB = sts.next()
                                    pt, ptB = pT.next()
                                    diag = kb >= 4 * qc

                                    def fs(e, stt=stt, kb=kb, qc=qc, c0=c0, diag=diag, r0=r0):
                                        kblk = slice(kb * 128, (kb + 1) * 128)
                                        q0 = qc * 512
                                        inst = None
                                        if diag:
                                            qs = slice(q0 + c0, q0 + c0 + 128)
                                            mm(e, stt[:, c0:c0 + 128], kT[r0:r0 + 64, kblk], qT[r0:r0 + 64, qs], True, False)
                                            mm(e, stt[:, c0:c0 + 128], augK[0:4, kblk], augQ[0:4, qs], False, False)
                                            inst = mm(e, stt[:, c0:c0 + 128], ident[:], masks[:, 0, :], False, True)
                                            c1 = c0 + 128
                                        else:
                                            c1 = c0
                                        if c1 < 512:
                                            qs = slice(q0 + c1, q0 + 512)
                                            mm(e, stt[:, c1:512], kT[r0:r0 + 64, kblk], qT[r0:r0 + 64, qs], True, False)
                                            inst = mm(e, stt[:, c1:512], augK[0:4, kblk], augQ[0:4, qs], False, True)
                                        return inst
                                    p.op("pe", fs, r=[kTB, qTB, augKB, augQB] + CB, w=[sttB])
                                    p.op("act", lambda e, stt=stt, pt=pt, c0=c0: e.activation(out=pt[:, c0:512], in_=stt[:, c0:512], func=AF.Exp),
                                         r=[sttB], w=[ptB])

                                    def fpv(e, pt=pt, kb=kb, c0=c0, hh=hh, an_=an_, ad_=ad_, nkb=nkb):
                                        mm(e, an_[:, c0:512], Vt[:, 0, kb, hh * 64:(hh + 1) * 64], pt[:, c0:512], kb == 0, kb == nkb - 1)
                                        return mm(e, ad_[:, c0:512], ones_bf[:, 0:64], pt[:, c0:512], kb == 0, kb == nkb - 1)
                                    p.op("pe", fpv, r=[ptB, VB[0]] + CB, w=[anB_, adB_])
                                p.op("dve", lambda e, ad_=ad_, qc=qc: e.reciprocal(out=rec[:, qc * 512:(qc + 1) * 512], in_=ad_[:]), r=[adB_], w=[recB])
                                p.op("dve", lambda e, an_=an_, qc=qc, r0=r0, c=c: e.tensor_tensor(
                                    out=oT[r0:r0 + 64, c, qc * 512:(qc + 1) * 512], in0=an_[:], in1=rec[:, qc * 512:(qc + 1) * 512], op=ALU.mult),
                                    r=[anB_, recB], w=[oTB[c]])

                    def tokset(bi, si):
                        if bi == 0:
                            return slice(si * 128, (si + 1) * 128)
                        if bi == 1:
                            r_, n_ = si // 4, si % 4
                            return slice(512 * n_ + r_, 512 * (n_ + 1), 4)
                        return slice(si, S, 16)

                    def accview(t, bi, si):
                        if bi == 0:
                            return t[:, :].rearrange("p (n j) -> p n j", j=128)[:, si:si + 2, :]
                        if bi == 1:
                            r_, n_ = si // 4, si % 4
                            return t[:, :].rearrange("p (n j r) -> p n j r", n=4, j=128, r=4)[:, n_:n_ + 2, :, r_]
                        return t[:, :].rearrange("p (j r) -> p r j", r=16)[:, si:si + 2, :]

                    for c in range(4):
                        def mk_rope(dst, dstB, wn, wns, c=c):
                            store = {}

                            def ev_a(tc, pp, ppB):
                                rtt, rtB = rt.next()
                                store[tc] = (rtt, rtB)
                                p.op("dve", lambda e: e.tensor_tensor(out=rtt[:, 0, :], in0=pp[:], in1=rope[:, 0, tc * 512:(tc + 1) * 512], op=ALU.mult),
                                     r=[ppB, ropeB], w=[rtB])

                            def ev_b(tc, pp, ppB):
                                rtt, rtB = store[tc]
                                p.op("dve", lambda e: e.tensor_tensor(out=rtt[:, 1, :], in0=pp[:], in1=rope[:, 1, tc * 512:(tc + 1) * 512], op=ALU.mult),
                                     r=[ppB, ropeB], w=[rtB])
                                p.op("pool", lambda e: e.tensor_tensor(out=dst[:, tc * 512:(tc + 1) * 512], in0=rtt[:, 0, :], in1=rtt[:, 1, :], op=ALU.add),
                                     r=[rtB], w=[dstB])
                            wa, waB = W["wA"].next()
                            wb, wbB = W["wA"].next()
                            wload(wa[:], wview(wn)[:, :, c * 128:(c + 1) * 128], waB)
                            wload(wb[:], wview(wns)[:, :, c * 128:(c + 1) * 128], wbB)
                            for tc in range(4):
                                for (wt_, wtB_, ev) in ((wa, waB, ev_a), (wb, wbB, ev_b)):
                                    pp, ppB = W["pp"].next()

                                    def f(e, pp=pp, wt_=wt_, tc=tc):
                                        inst = None
                                        for kc in range(KC):
                                            inst = mm(e, pp[:], wt_[:, kc, :], hT[:, kc, tc * 512:(tc + 1) * 512], kc == 0, kc == KC - 1)
                                        return inst
                                    p.op("pe", f, r=[wtB_] + hTB[4 * tc:4 * tc + 4], w=[ppB])
                                    ev(tc, pp, ppB)
                        mk_rope(qT, qTB, "wqd", "wqds")
                        mk_rope(kT, kTB, "wkd", "wkds")
                        for bi in range(3):
                            def ev_v(si, pp, ppB, bi=bi):
                                p.op("act", lambda e: e.activation(out=Vt[:, bi, si, :], in_=pp[:, 0:128], func=AF.Copy), r=[ppB], w=[VB[bi]])
                            proj_tm(W, "wvd", c * 128, 128, [tokset(bi, si) for si in range(16)], ev_v)
                        for hh in range(2):
                            r0 = 64 * hh
                            for bi in range(3):
                                for si in range(0, 16, 2):
                                    blocks = []
                                    for sq in (si, si + 1):
                                        if bi == 0:
                                            prev = sq - 1 if sq >= 1 else None
                                        elif bi == 1:
                                            prev = sq - 1 if (sq % 4) >= 1 else None
                                        else:
                                            prev = None
                                        blocks.append((sq, prev if prev is not None else sq))
                                        blocks.append((sq, sq))
                                    if bi == 2:
                                        mi = 2
                                    elif (bi == 0 and si == 0) or (bi == 1 and si % 4 == 0):
                                        mi = 1
                                    else:
                                        mi = 0
                                    stt, sttB = sts.next()
                                    pt, ptB = pT.next()
                                    an_, anB_ = accn.next()
                                    ad_, adB_ = accd.next()

                                    def fs(e, stt=stt, blocks=blocks, mi=mi, bi=bi, r0=r0):
                                        inst = None
                                        for bk, (sq, sk) in enumerate(blocks):
                                            mm(e, stt[:, bk * 128:(bk + 1) * 128], kT[r0:r0 + 64, tokset(bi, sk)], qT[r0:r0 + 64, tokset(bi, sq)], True, False)
                                            inst = mm(e, stt[:, bk * 128:(bk + 1) * 128], ident[:], mask4[:, mi, bk * 128:(bk + 1) * 128], False, True)
                                        return inst
                                    p.op("pe", fs, r=[kTB, qTB] + CB, w=[sttB])
                                    p.op("act", lambda e, stt=stt, pt=pt: e.activation(out=pt[:], in_=stt[:], func=AF.Exp, scale=0.125), r=[sttB], w=[ptB])

                                    def fpv(e, pt=pt, blocks=blocks, bi=bi, hh=hh, an_=an_, ad_=ad_):
                                        inst = None
                                        for bk, (sq, sk) in enumerate(blocks):
                                            qi = bk // 2
                                            first = (bk % 2 == 0)
                                            mm(e, an_[:, qi * 128:(qi + 1) * 128], Vt[:, bi, sk, hh * 64:(hh + 1) * 64], pt[:, bk * 128:(bk + 1) * 128], first, not first)
                                            inst = mm(e, ad_[:, qi * 128:(qi + 1) * 128], ones_bf[:, 0:64], pt[:, bk * 128:(bk + 1) * 128], first, not first)
                                        return inst
                                    p.op("pe", fpv, r=[ptB, VB[bi]] + CB, w=[anB_, adB_])
                                    pv = lambda t: t[:, 0:256].rearrange("p (a j) -> p a j", a=2)
                                    if bi == 0:
                                        p.op("dve", lambda e, an_=an_, si=si: e.tensor_copy(out=accview(an, 0, si), in_=pv(an_)), r=[anB_], w=[anB])
                                        p.op("dve", lambda e, ad_=ad_, si=si: e.tensor_copy(out=accview(ad, 0, si), in_=pv(ad_)), r=[adB_], w=[adB])
                                    else:
                                        p.op("dve", lambda e, an_=an_, si=si, bi=bi: e.tensor_tensor(out=accview(an, bi, si), in0=pv(an_), in1=accview(an, bi, si), op=ALU.add),
                                             r=[anB_, anB], w=[anB])
                                        p.op("dve", lambda e, ad_=ad_, si=si, bi=bi: e.tensor_tensor(out=accview(ad, bi, si), in0=pv(ad_), in1=accview(ad, bi, si), op=ALU.add),
                                             r=[adB_, adB], w=[adB])
                            p.op("dve", lambda e: e.reciprocal(out=rec[:], in_=ad[:]), r=[adB], w=[recB])
                            p.op("dve", lambda e, r0=r0, c=c: e.tensor_tensor(out=oT[r0:r0 + 64, 4 + c, :], in0=an[:], in1=rec[:], op=ALU.mult),
                                 r=[anB, recB], w=[oTB[4 + c]])
                    if debug and s == 0:
                        dB = p.buf("dbg2", dma=True)
                        p.op("pool", lambda e: e.dma_start(out=dbg2[0:128, :].rearrange("p (a b) -> p a b", a=KC), in_=oT[:]), r=oTB, dsem=dB.dsem)
                        p.op("pool", lambda e: e.dma_start(out=dbg2[128:256, 0:S], in_=qT[:]), r=[qTB], dsem=dB.dsem)
                        p.op("pool", lambda e: e.dma_start(out=dbg2[128:256, S:2 * S], in_=kT[:]), r=[kTB], dsem=dB.dsem)
                        p.op("pool", lambda e: e.dma_start(out=dbg2[128:192, 2 * S:3 * S], in_=an), r=[anB], dsem=dB.dsem)
                        p.op("pool", lambda e: e.dma_start(out=dbg2[128:192, 3 * S:4 * S], in_=ad), r=[adB], dsem=dB.dsem)
                        p.op("pool", lambda e: e.dma_start(out=dbg2[128:256, 4 * S:5 * S], in_=Vt[:, 1, :, :].rearrange("p a b -> p (a b)")), r=VB, dsem=dB.dsem)
                    p.barrier()

            _ph2()
            def mix_stage(wname, li, dbg_i, row0=row0):
                with ExitStack() as st:
                    p.phase_begin()
                    L = ln_alloc(st, "m%d" % li)
                    L["eps"] = epsT
                    ln_load(L, li)
                    for tb in range(TB):
                        src = dr["x"][row0 + tb * 128: row0 + (tb + 1) * 128, :] if li == 0 else hsp[tb * 128:(tb + 1) * 128, :]
                        p.op("sp", lambda e, tb=tb, src=src: e.dma_start(out=h[:, tb, :], in_=src), w=[hB[tb]], dsem=hB[tb].dsem)
                    wo = sb(st, "wo", [128, KC, D], BF16)
                    woB = p.buf("wo", dma=True)
                    wload(wo[:, :, 0:512], wview(wname)[:, :, 0:512], woB)
                    wload(wo[:, :, 512:1024], wview(wname)[:, :, 512:1024], woB)
                    mixp = Rot([(ps(st, "mix%d" % i, [128, D]), p.buf("mix")) for i in range(2)])
                    for tb in range(TB):
                        mp, mpB = mixp.next()

                        def f(e, mp=mp, tb=tb):
                            inst = None
                            for half in range(2):
                                for c in range(KC):
                                    inst = mm(e, mp[:, half * 512:(half + 1) * 512], oT[:, c, tb * 128:(tb + 1) * 128],
                                              wo[:, c, half * 512:(half + 1) * 512], c == 0, c == KC - 1)
                            return inst
                        p.op("pe", f, r=[woB] + oTB, w=[mpB])
                        p.op("dve", lambda e, mp=mp, tb=tb: e.scalar_tensor_tensor(out=h[:, tb, :], in0=h[:, tb, :], scalar=ALPHA, in1=mp[:],
                                                                                   op0=ALU.mult, op1=ALU.add), r=[hB[tb], mpB], w=[hB[tb]])
                        ln_block(tb, L, to_hT=True, dbg_row0=(dbg_i * S if (debug and s == 0) else None))
                    p.barrier()
            mix_stage("wo0", 0, 0)
            if phases < 2:
                return

            def ffn_alloc(st):
                Fd = {}
                Fd["wg"] = Rot([(sb(st, "wg%d" % i, [128, KC, 512], BF16), p.buf("wg", dma=True)) for i in range(2)])
                Fd["wu"] = Rot([(sb(st, "wu%d" % i, [128, KC, 512], BF16), p.buf("wu", dma=True)) for i in range(2)])
                Fd["wd"] = Rot([(sb(st, "wd%d" % i, [128, 4, D], BF16), p.buf("wd", dma=True)) for i in range(2)])
                Fd["aT"] = Rot([(oT[:, 4 * i:4 * i + 4, :], p.buf("aT")) for i in range(2)])
                Fd["sg"] = Rot([(sb(st, "sg%d" % i, [128, 512], F32), p.buf("sg")) for i in range(2)])
                Fd["pg"] = Rot([(ps(st, "pg%d" % i, [128, 512]), p.buf("pg")) for i in range(2)])
                Fd["pu"] = Rot([(ps(st, "pu%d" % i, [128, 512]), p.buf("pu")) for i in range(2)])
                Fd["yp"] = Rot([(ps(st, "yp%d" % i, [128, D]), p.buf("yp")) for i in range(2)])
                return Fd

            def ffn(Fd, gname, uname, dname, grow0, drow0, F, gate_fn):
                f0 = 0
                while f0 < F:
                    gw = min(512, F - f0)
                    gc = gw // 128
                    wg, wgB = Fd["wg"].next()
                    wu, wuB = Fd["wu"].next()
                    wd, wdB = Fd["wd"].next()
                    aT, aTB = Fd["aT"].next()
                    wload(wg[:, :, 0:gw], wview(gname, grow0, D)[:, :, f0:f0 + gw], wgB)
                    wload(wu[:, :, 0:gw], wview(uname, grow0, D)[:, :, f0:f0 + gw], wuB)
                    wload(wd[:, 0:gc, :], wview(dname, drow0 + f0, gw), wdB)
                    for ci in range(gc):
                        for tc in range(4):
                            pg, pgB = Fd["pg"].next()
                            pu, puB = Fd["pu"].next()
                            sg, sgB = Fd["sg"].next()

                            def fg_(e, pg=pg, wg=wg, ci=ci, tc=tc):
                                inst = None
                                for kc in range(KC):
                                    inst = mm(e, pg[:], wg[:, kc, ci * 128:(ci + 1) * 128], hT[:, kc, tc * 512:(tc + 1) * 512], kc == 0, kc == KC - 1)
                                return inst

                            def fu_(e, pu=pu, wu=wu, ci=ci, tc=tc):
                                inst = None
                                for kc in range(KC):
                                    inst = mm(e, pu[:], wu[:, kc, ci * 128:(ci + 1) * 128], hT[:, kc, tc * 512:(tc + 1) * 512], kc == 0, kc == KC - 1)
                                return inst
                            p.op("pe", fg_, r=[wgB] + hTB[4 * tc:4 * tc + 4], w=[pgB])
                            p.op("pe", fu_, r=[wuB] + hTB[4 * tc:4 * tc + 4], w=[puB])
                            p.op("act", lambda e, sg=sg, pg=pg: e.activation(out=sg[:], in_=pg[:], func=AF.Silu), r=[pgB], w=[sgB])
                            p.op("dve", lambda e, sg=sg, pu=pu, aT=aT, ci=ci, tc=tc: e.tensor_tensor(
                                out=aT[:, ci, tc * 512:(tc + 1) * 512], in0=pu[:], in1=sg[:], op=ALU.mult), r=[puB, sgB], w=[aTB])
                    for tb in range(TB):
                        yp, ypB = Fd["yp"].next()

                        def fd_(e, yp=yp, aT=aT, wd=wd, tb=tb, gc=gc):
                            inst = None
                            for half in range(2):
                                for ci in range(gc):
                                    inst = mm(e, yp[:, half * 512:(half + 1) * 512], aT[:, ci, tb * 128:(tb + 1) * 128],
                                              wd[:, ci, half * 512:(half + 1) * 512], ci == 0, ci == gc - 1)
                            return inst
                        p.op("pe", fd_, r=[aTB, wdB], w=[ypB])
                        g_ap, gBs = gate_fn(tb)
                        p.op("dve", lambda e, yp=yp, tb=tb, g_ap=g_ap: e.scalar_tensor_tensor(out=h[:, tb, :], in0=yp[:], scalar=g_ap, in1=h[:, tb, :],
                                                                                            op0=ALU.mult, op1=ALU.add), r=[ypB, hB[tb]] + gBs, w=[hB[tb]])
                    f0 += gw

            def scale_h():
                for tb in range(TB):
                    p.op("act", lambda e, tb=tb: e.mul(out=h[:, tb, :], in_=h[:, tb, :], mul=ALPHA), r=[hB[tb]], w=[hB[tb]])

            def final_ln(li, dbg_i, to_hT, out_row0, spill=False):
                outs = []
                with ExitStack() as st:
                    p.phase_begin()
                    L = ln_alloc(st, "f%d" % li)
                    L["eps"] = epsT
                    ln_load(L, li)
                    for tb in range(TB):
                        o_ = ln_block(tb, L, to_hT=to_hT, out_row0=out_row0, dbg_row0=(dbg_i * S if (debug and s == 0) else None), spill=spill)
                        if o_ is not None:
                            outs.append(o_)
                    p.barrier()
                return outs

            def _ph3():
                with ExitStack() as st:
                    p.phase_begin()
                    Fd = ffn_alloc(st)
                    scale_h()
                    ffn(Fd, "fg", "fu", "fd", 0, 0, DFF, lambda tb: (1.0, []))
                    p.barrier()
            _ph3()
            final_ln(1, 1, True, None, spill=True)
            if phases < 3:
                return

            def _ph4():
                with ExitStack() as st:
                    p.phase_begin()
                    W = {}
                    W["wA"] = Rot([(sb(st, "wA%d" % i, [128, KC, 128], BF16), p.buf("wA", dma=True)) for i in range(3)])
                    W["pp"] = Rot([(ps(st, "pp%d" % i, [128, 512]), p.buf("pp")) for i in range(1)])
                    qT = sb(st, "qT", [128, S], BF16); qTB = p.buf("qT")
                    kT = sb(st, "kT", [128, S], BF16); kTB = p.buf("kT")
                    Vh = sb(st, "Vh", [128, TB, 128], BF16); VhB = p.buf("Vh")
                    sgo = sb(st, "sgo", [128, S], BF16); sgoB = p.buf("sgo")
                    pre = hs[:, 0:3 + S]; preB = p.buf("pre"); padB = p.buf("pad")
                    yv = hs[:, 2052:2052 + S]; yvB = p.buf("yv")
                    t0 = hs[:, 3:3 + S]; t0B = preB
                    t1 = hs[:, 4100:4100 + S]; t1B = p.buf("t1")
                    t2 = yv; t2B = yvB
                    hi = sb(st, "hi", [4, S], BF16); lo = sb(st, "lo", [4, S], BF16); hlB = p.buf("hl")
                    ua = hs[0:4, 9220:9220 + S]; uaB = p.buf("ua")
                    augQ = sb(st, "augQ", [4, S], BF16); augQB = p.buf("augQ")
                    augK = sb(st, "augK", [4, S], BF16); augKB = p.buf("augK")
                    tq = sb(st, "tq", [4, S], BF16); tqB = p.buf("tq")
                    pT = Rot([(sb(st, "pT%d" % i, [128, 512], BF16), p.buf("pT")) for i in range(2)])
                    Et = Rot([(hs[:, 6148 + i * 512:6148 + (i + 1) * 512], p.buf("Et")) for i in range(2)])
                    psA = Rot([(ps(st, "psA%d" % i, [128, 512]), p.buf("psA")) for i in range(2)])
                    psB = Rot([(ps(st, "psB%d" % i, [128, 512]), p.buf("psB")) for i in range(2)])
                    accn_t = ps(st, "accn", [128, 512]); accnB = p.buf("accn")
                    accd_t = ps(st, "accd", [128, 512]); accdB = p.buf("accd")
                    stat_t = ps(st, "stat", [128, 512]); statB = p.buf("stat")
                    W["pp"] = Rot(W["pp"].items + [(stat_t, statB)] + psA.items + psB.items)
                    f1 = hs[:, 7172:7684]; f1B = p.buf("f1")
                    f2 = hs[:, 7684:8196]; f2B = p.buf("f2")
                    f3 = hs[:, 8196:8708]; f3B = p.buf("f3")
                    f4 = hs[:, 8708:9220]; f4B = p.buf("f4")
                    p.op("dve", lambda e: e.memset(pre[:, 0:3], 0.0), w=[padB])

                    def make_aug(src_ap, srcB):
                        p.op("dve", lambda e: e.tensor_copy(out=hi[:], in_=src_ap), r=[srcB], w=[hlB])
                        p.op("dve", lambda e: e.tensor_tensor(out=lo[:], in0=src_ap, in1=hi[:], op=ALU.subtract), r=[srcB, hlB], w=[hlB])

                    def fin_aug(dst, dstB, c0):
                        p.op("dve", lambda e: e.tensor_scalar(out=tq[:], in0=hi[:], scalar1=augc[0:4, c0:c0 + 1], scalar2=augc[0:4, c0 + 2:c0 + 3],
                                                              op0=ALU.mult, op1=ALU.add), r=[hlB] + CB, w=[tqB])
                        p.op("dve", lambda e: e.scalar_tensor_tensor(out=dst[:], in0=lo[:], scalar=augc[0:4, c0 + 1:c0 + 2], in1=tq[:],
                                                                     op0=ALU.mult, op1=ALU.add), r=[hlB, tqB] + CB, w=[dstB])

                    for hd in range(8):
                        def conv_silu(wname, col0, chunk, dst, dstB):
                            def ev(tc, pp, ppB):
                                p.op("act", lambda e: e.activation(out=pre[:, 3 + tc * 512: 3 + (tc + 1) * 512], in_=pp[:], func=AF.Copy), r=[ppB], w=[preB])
                            proj_fm(W, wname, col0, ev)
                            p.op("dve", lambda e: e.tensor_scalar(out=yv[:], in0=pre[:, 3:3 + S], scalar1=wc[:, chunk, 3:4], scalar2=None, op0=ALU.mult),
                                 r=[preB, padB] + CB, w=[yvB])
                            for i in range(3):
                                p.op("dve", lambda e, i=i: e.scalar_tensor_tensor(out=yv[:], in0=pre[:, i:i + S], scalar=wc[:, chunk, i:i + 1], in1=yv[:],
                                                                                 op0=ALU.mult, op1=ALU.add), r=[preB, padB, yvB] + CB, w=[yvB])
                            p.op("act", lambda e: e.activation(out=dst[:], in_=yv[:], func=AF.Silu), r=[yvB], w=[dstB])
                        conv_silu("wq1", hd * 128, hd, qT, qTB)
                        conv_silu("wk1", hd * 128, 8 + hd, kT, kTB)

                        def ev_v(si, pp, ppB):
                            p.op("act", lambda e: e.activation(out=Vh[:, si, :], in_=pp[:, 0:128], func=AF.Copy), r=[ppB], w=[VhB])
                        proj_tm(W, "wv1", hd * 128, 128, [slice(tb * 128, (tb + 1) * 128) for tb in range(TB)], ev_v)

                        def ev_og(tc, pp, ppB):
                            p.op("act", lambda e: e.activation(out=sgo[:, tc * 512:(tc + 1) * 512], in_=pp[:], func=AF.Sigmoid), r=[ppB], w=[sgoB])
                        proj_fm(W, "wog", hd * 128, ev_og)

                        def ev_f(tc, pp, ppB, hd=hd):
                            p.op("act", lambda e: e.activation(out=t0[:, tc * 512:(tc + 1) * 512], in_=pp[:], func=AF.Exp,
                                                               bias=ngb[:, 16 + hd:17 + hd], scale=-1.0), r=[ppB] + CB, w=[t0B])
                        proj_fm(W, "wfg", hd * 128, ev_f)
                        p.op("act", lambda e: e.activation(out=t0[:], in_=t0[:], func=AF.Ln, bias=onesf_one[:, 0:1]), r=[t0B] + CB, w=[t0B])
                        p.op("dve", lambda e: e.tensor_tensor_scan(out=t1[:], data0=ones_bf[:], data1=t0[:], initial=0.0, op0=ALU.mult, op1=ALU.add),
                             r=[t0B] + CB, w=[t1B])

                        def ev_i(tc, pp, ppB, hd=hd):
                            p.op("dve", lambda e: e.scalar_tensor_tensor(out=t0[:, tc * 512:(tc + 1) * 512], in0=pp[:], scalar=gb[:, 8 + hd:9 + hd],
                                                                         in1=t1[:, tc * 512:(tc + 1) * 512], op0=ALU.add, op1=ALU.add),
                                 r=[ppB, t1B] + CB, w=[t0B])
                        proj_fm(W, "wig", hd * 128, ev_i)
                        p.op("dve", lambda e: e.tensor_tensor_scan(out=t2[:], data0=t0[:], data1=t0[:], initial=-1e30, op0=ALU.max, op1=ALU.max),
                             r=[t0B] + CB, w=[t2B])
                        p.op("dve", lambda e: e.tensor_tensor(out=t1[:], in0=t1[:], in1=t2[:], op=ALU.subtract), r=[t1B, t2B], w=[t1B])
                        p.op("act", lambda e: e.activation(out=t1[:], in_=t1[:], func=AF.Exp), r=[t1B], w=[t1B])
                        make_aug(t2[0:4, :], t2B)
                        fin_aug(augQ, augQB, 0)
                        p.op("dve", lambda e: e.tensor_scalar(out=ua[:], in0=t0[0:4, :], scalar1=LNS, scalar2=None, op0=ALU.add), r=[t0B], w=[uaB])
                        make_aug(ua[:], uaB)
                        fin_aug(augK, augKB, 3)

                        for qc in range(4):
                            nkb = 4 * qc + 4
                            for kb in range(nkb):
                                j0 = max(0, kb - 4 * qc)
                                c0 = j0 * 128
                                diag = kb >= 4 * qc
                                pa, paB = psA.next()
                                pb, pbB = psB.next()
                                pt, ptB = pT.next()
                                et, etB = Et.next()
                                kblk = slice(kb * 128, (kb + 1) * 128)
                                qs = slice(qc * 512 + c0, qc * 512 + 512)
                                p.op("pe", lambda e, pa=pa, kblk=kblk, qs=qs, c0=c0: mm(e, pa[:, c0:512], kT[:, kblk], qT[:, qs], True, True),
                                     r=[kTB, qTB], w=[paB])

                                def fd_(e, pb=pb, kblk=kblk, qc=qc, c0=c0, diag=diag):
                                    q0 = qc * 512
                                    inst = None
                                    c1 = c0
                                    if diag:
                                        qs1 = slice(q0 + c0, q0 + c0 + 128)
                                        mm(e, pb[:, c0:c0 + 128], augK[0:4, kblk], augQ[0:4, qs1], True, False)
                                        inst = mm(e, pb[:, c0:c0 + 128], ident[:], masks[:, 0, :], False, True)
                                        c1 = c0 + 128
                                    if c1 < 512:
                                        inst = mm(e, pb[:, c1:512], augK[0:4, kblk], augQ[0:4, slice(q0 + c1, q0 + 512)], True, True)
                                    return inst
                                p.op("pe", fd_, r=[augKB, augQB] + CB, w=[pbB])
                                p.op("act", lambda e, pb=pb, et=et, c0=c0: e.activation(out=et[:, c0:512], in_=pb[:, c0:512], func=AF.Exp), r=[pbB], w=[etB])
                                p.op("dve", lambda e, pa=pa, et=et, pt=pt, c0=c0: e.tensor_tensor(out=pt[:, c0:512], in0=pa[:, c0:512], in1=et[:, c0:512], op=ALU.mult),
                                     r=[paB, etB], w=[ptB])

                                def fpv(e, pt=pt, kb=kb, c0=c0, nkb=nkb):
                                    mm(e, accn_t[:, c0:512], Vh[:, kb, :], pt[:, c0:512], kb == 0, kb == nkb - 1)
                                    return mm(e, accd_t[:, c0:512], ones_bf[:, 0:128], pt[:, c0:512], kb == 0, kb == nkb - 1)
                                p.op("pe", fpv, r=[ptB, VhB] + CB, w=[accnB, accdB])
                            cs = slice(qc * 512, (qc + 1) * 512)
                            p.op("act", lambda e: e.activation(out=f1[:], in_=accd_t[:], func=AF.Abs), r=[accdB], w=[f1B])
                            p.op("dve", lambda e, cs=cs: e.tensor_tensor(out=f1[:], in0=f1[:], in1=t1[:, cs], op=ALU.max), r=[f1B, t1B], w=[f1B])
                            p.op("dve", lambda e: e.reciprocal(out=f1[:], in_=f1[:]), r=[f1B], w=[f1B])
                            p.op("dve", lambda e: e.tensor_tensor(out=f2[:], in0=accn_t[:], in1=f1[:], op=ALU.mult), r=[accnB, f1B], w=[f2B])
                            p.op("act", lambda e: e.activation(out=f3[:], in_=f2[:], func=AF.Square), r=[f2B], w=[f3B])
                            p.op("pe", lambda e: mm(e, stat_t[:], onesf[:], f2[:], True, True), r=[f2B] + CB, w=[statB])
                            p.op("act", lambda e: e.activation(out=f1[:], in_=stat_t[:], func=AF.Copy), r=[statB], w=[f1B])
                            p.op("pe", lambda e: mm(e, stat_t[:], onesf[:], f3[:], True, True), r=[f3B, f1B] + CB, w=[statB])
                            p.op("dve", lambda e: e.tensor_tensor(out=f4[:], in0=f1[:], in1=f1[:], op=ALU.mult), r=[f1B], w=[f4B])
                            p.op("dve", lambda e: e.tensor_tensor(out=f4[:], in0=stat_t[:], in1=f4[:], op=ALU.subtract), r=[statB, f4B], w=[f4B])
                            p.op("act", lambda e: e.activation(out=f4[:], in_=f4[:], func=AF.Ln, bias=epsT[:, 0:1]), r=[f4B] + CB, w=[f4B])
                            p.op("act", lambda e: e.activation(out=f4[:], in_=f4[:], func=AF.Exp, scale=-0.5), r=[f4B], w=[f4B])
                            p.op("dve", lambda e: e.tensor_tensor(out=f2[:], in0=f2[:], in1=f1[:], op=ALU.subtract), r=[f2B, f1B], w=[f2B])
                            p.op("dve", lambda e: e.tensor_tensor(out=f2[:], in0=f2[:], in1=f4[:], op=ALU.mult), r=[f2B, f4B], w=[f2B])
                            p.op("dve", lambda e, cs=cs, hd=hd: e.scalar_tensor_tensor(out=oT[:, hd, cs], in0=f2[:], scalar=ng[:, hd:hd + 1], in1=sgo[:, cs],
                                                                                       op0=ALU.mult, op1=ALU.mult), r=[f2B, sgoB] + CB, w=[oTB[hd]])
                    if debug and s == 0:
                        dB = p.buf("dbg2b", dma=True)
                        p.op("pool", lambda e: e.dma_start(out=dbg2[128:256, :].rearrange("p (a b) -> p a b", a=KC), in_=oT[:]), r=oTB, dsem=dB.dsem)
                    p.barrier()
            _ph4()
            mix_stage("wo1", 2, 2)
            if phases < 4:
                return

            def _ph5():
                with ExitStack() as st:
                    p.phase_begin()
                    wr = sb(st, "wr", [128, KC, 8], BF16); wrB = p.buf("wr", dma=True)
                    wload(wr[:], wview("wr"), wrB)
                    lgp = ps(st, "lgp", [128, 128]); lgB = p.buf("lg")
                    prp = ps(st, "prp", [128, 128]); prB = p.buf("pr")
                    ttp = ps(st, "ttp", [128, 128]); ttB = p.buf("tt")
                    Lg = sb(st, "Lg", [128, 128], F32); LgB = p.buf("Lg")
                    L2 = sb(st, "L2", [128, 128], F32)
                    e1 = sb(st, "e1", [128, 128], F32)
                    e2 = sb(st, "e2", [128, 128], F32)
                    Mb = sb(st, "Mb", [128, 128], BF16)
                    Tt = sb(st, "Tt", [128, 128], F32)
                    Sc = sb(st, "Sc", [128, 128], F32)
                    Rr = sb(st, "Rr", [128, 128], F32)
                    row = sb(st, "row", [128, 128], F32)
                    tmp = sb(st, "tmp", [128, 128], F32)
                    pf = sb(st, "pf", [128, 32], F32)
                    tsum = sb(st, "tsum", [128, 8], F32)
                    m1 = sb(st, "m1", [128, 16], F32)
                    m2 = sb(st, "m2", [128, 16], F32)
                    w1 = sb(st, "w1", [128, 16], F32)
                    w2 = sb(st, "w2", [128, 16], F32)
                    v3 = lambda t: t[:, :].rearrange("p (a b) -> p a b", b=8)
                    emT = lambda t: t[:, :].rearrange("p (e a) -> p a e", e=8)
                    em3 = lambda t: t[:, :].rearrange("p (e a) -> p e a", e=8)
                    bc = lambda t: t[:, :].unsqueeze(2).to_broadcast([128, 16, 8])

                    def flg(e):
                        inst = None
                        for tb in range(TB):
                            for kc in range(KC):
                                inst = mm(e, lgp[:, tb * 8:(tb + 1) * 8], hT[:, kc, tb * 128:(tb + 1) * 128], wr[:, kc, :], kc == 0, kc == KC - 1)
                        return inst
                    p.op("pe", flg, r=[wrB] + hTB, w=[lgB])
                    G = [LgB]
                    p.op("dve", lambda e: e.tensor_copy(out=Lg[:], in_=lgp[:]), r=[lgB], w=G)
                    p.op("dve", lambda e: e.tensor_reduce(out=m1[:], in_=v3(Lg), axis=AX.X, op=ALU.max), r=G, w=G)
                    p.op("dve", lambda e: e.tensor_tensor(out=v3(e1), in0=v3(Lg), in1=bc(m1), op=ALU.is_equal), r=G, w=G)
                    p.op("dve", lambda e: e.scalar_tensor_tensor(out=L2[:], in0=e1[:], scalar=-1e30, in1=Lg[:], op0=ALU.mult, op1=ALU.add), r=G, w=G)
                    p.op("dve", lambda e: e.tensor_reduce(out=m2[:], in_=v3(L2), axis=AX.X, op=ALU.max), r=G, w=G)
                    p.op("dve", lambda e: e.tensor_tensor(out=v3(e2), in0=v3(L2), in1=bc(m2), op=ALU.is_equal), r=G, w=G)
                    p.op("dve", lambda e: e.tensor_tensor(out=w2[:], in0=m2[:], in1=m1[:], op=ALU.subtract), r=G, w=G)
                    p.op("act", lambda e: e.activation(out=w2[:], in_=w2[:], func=AF.Exp), r=G, w=G)
                    p.op("dve", lambda e: e.tensor_scalar(out=w1[:], in0=w2[:], scalar1=1.0, scalar2=None, op0=ALU.add), r=G, w=G)
                    p.op("dve", lambda e: e.reciprocal(out=w1[:], in_=w1[:]), r=G, w=G)
                    p.op("dve", lambda e: e.tensor_tensor(out=w2[:], in0=w2[:], in1=w1[:], op=ALU.mult), r=G, w=G)
                    p.op("dve", lambda e: e.tensor_copy(out=gW[:, s * 32:s * 32 + 16], in_=w1[:]), r=G, w=[gWB])
                    p.op("dve", lambda e: e.tensor_copy(out=gW[:, s * 32 + 16:s * 32 + 32], in_=w2[:]), r=G, w=[gWB])
                    p.op("dve", lambda e: e.tensor_tensor(out=emT(Mb), in0=v3(e1), in1=v3(e2), op=ALU.add), r=G, w=G)

                    def fpr(e):
                        mm(e, prp[:], tri[:], Mb[:], True, True)
                        return mm(e, ttp[:], ones_bf[:, 0:128], Mb[:], True, True)
                    p.op("pe", fpr, r=G + CB, w=[prB, ttB])
                    p.op("dve", lambda e: e.tensor_copy(out=Tt[:], in_=ttp[:]), r=[ttB], w=G)
                    p.op("dve", lambda e: e.tensor_tensor_scan(out=Sc[:], data0=ones_bf[:, 0:128], data1=Tt[:], initial=0.0, op0=ALU.mult, op1=ALU.add),
                         r=G + CB, w=G)
                    p.op("dve", lambda e: e.tensor_tensor(out=Sc[:], in0=Sc[:], in1=Tt[:], op=ALU.subtract), r=G, w=G)
                    p.op("dve", lambda e: e.tensor_tensor(out=em3(Rr), in0=em3(Sc), in1=em3(Sc)[:, :, 0:1].to_broadcast([128, 8, 16]), op=ALU.subtract), r=G, w=G)
                    p.op("dve", lambda e: e.tensor_tensor(out=em3(Rr), in0=em3(Rr), in1=base[:, :].unsqueeze(2).to_broadcast([128, 8, 16]), op=ALU.add),
                         r=G + [baseB], w=G)
                    p.op("dve", lambda e: e.tensor_tensor(out=row[:], in0=prp[:], in1=Rr[:], op=ALU.add), r=G + [prB], w=G)
                    p.op("dve", lambda e: e.tensor_tensor(out=row[:], in0=row[:], in1=eoff[:], op=ALU.add), r=G + CB, w=G)
                    p.op("dve", lambda e: e.tensor_reduce(out=tsum[:], in_=em3(Tt), axis=AX.X, op=ALU.add), r=G, w=G)
                    p.op("dve", lambda e: e.tensor_tensor(out=base[:], in0=base[:], in1=tsum[:], op=ALU.add), r=G + [baseB], w=[baseB])
                    p.op("dve", lambda e: e.tensor_tensor(out=v3(tmp), in0=v3(e1), in1=emT(row), op=ALU.mult), r=G, w=G)
                    p.op("dve", lambda e: e.tensor_reduce(out=pf[:, 0:16], in_=v3(tmp), axis=AX.X, op=ALU.add), r=G, w=G)
                    p.op("dve", lambda e: e.tensor_tensor(out=v3(tmp), in0=v3(e2), in1=emT(row), op=ALU.mult), r=G, w=G)
                    p.op("dve", lambda e: e.tensor_reduce(out=pf[:, 16:32], in_=v3(tmp), axis=AX.X, op=ALU.add), r=G, w=G)
                    p.op("dve", lambda e: e.tensor_copy(out=posI[:, s * 32:(s + 1) * 32], in_=pf[:]), r=G, w=[posB])
                    for tb in range(TB):
                        p.op("sp", lambda e, tb=tb: e.dma_start(out=h1sp[row0 + tb * 128: row0 + (tb + 1) * 128, :], in_=h[:, tb, :]),
                             r=[hB[tb]], dsem=hB[tb].dsem)
                        for k in range(2):
                            col = s * 32 + k * 16 + tb
                            for hf in range(2):
                                p.op("pool", lambda e, tb=tb, col=col, hf=hf: e.indirect_dma_start(
                                    out=XsH[hf][:, :], out_offset=IOA(ap=posI[:, col:col + 1], axis=0),
                                    in_=hs[:, tb * D + hf * 512: tb * D + (hf + 1) * 512], in_offset=None),
                                    r=[hB[tb], posB], dsem=hB[tb].dsem)
                    p.barrier()
            _ph5()

        def moe_phase():
            NB = TS // 128
            NTC = TS // 512
            with ExitStack() as st:
                p.phase_begin()
                ntl = sb(st, "ntl", [128, 8], F32)
                cinc = sb(st, "cinc", [128, 8], F32)
                cmp3 = hs[:, NTAB:NTAB + 64]
                le = hs[:, NTAB + 64:NTAB + 64 + NT * 8]
                le2 = hs[:, NTAB + 64 + NT * 8:NTAB + 64 + 2 * NT * 8]
                ej = sb(st, "ej", [128, NT], F32)
                ub = sb(st, "ub", [128, NT], F32)
                tabF = hs[:, 0:NTAB]
                xrow = sb(st, "xrow", [128, NT], F32)
                ew = sb(st, "ew", [128, NT], F32)
                T = [p.buf("tabw")]
                c3 = lambda t: t.rearrange("p (a b) -> p a b", b=8)
                p.op("dve", lambda e: e.tensor_tensor(out=c3(cmp3), in0=base[:, :].unsqueeze(2).to_broadcast([128, 8, 8]), in1=c3(thr[:, :]), op=ALU.is_gt),
                     r=[baseB] + CB, w=T)
                p.op("dve", lambda e: e.tensor_reduce(out=ntl[:], in_=c3(cmp3), axis=AX.X, op=ALU.add), r=T, w=T)
                p.op("dve", lambda e: e.tensor_tensor_scan(out=cinc[:], data0=ones_bf[:, 0:8], data1=ntl[:], initial=0.0, op0=ALU.mult, op1=ALU.add),
                     r=T + CB, w=T)
                p.op("dve", lambda e: e.tensor_tensor(out=c3(le), in0=cinc[:, :].unsqueeze(1).to_broadcast([128, NT, 8]), in1=c3(jc[:, :]), op=ALU.is_le),
                     r=T + CB, w=T)
                p.op("dve", lambda e: e.tensor_reduce(out=ej[:], in_=c3(le), axis=AX.X, op=ALU.add), r=T, w=T)
                p.op("dve", lambda e: e.tensor_tensor(out=c3(le2), in0=c3(le), in1=ntl[:, :].unsqueeze(1).to_broadcast([128, NT, 8]), op=ALU.mult), r=T, w=T)
                p.op("dve", lambda e: e.tensor_reduce(out=ub[:], in_=c3(le2), axis=AX.X, op=ALU.add), r=T, w=T)
                p.op("dve", lambda e: e.tensor_tensor(out=ub[:], in0=c3(jc[:, :])[:, :, 0], in1=ub[:], op=ALU.subtract), r=T + CB, w=T)
                p.op("dve", lambda e: e.tensor_scalar(out=ub[:], in0=ub[:], scalar1=float(TS), scalar2=None, op0=ALU.mult), r=T, w=T)
                p.op("dve", lambda e: e.scalar_tensor_tensor(out=xrow[:], in0=ej[:], scalar=float(CAP), in1=ub[:], op0=ALU.mult, op1=ALU.add), r=T, w=T)
                p.op("dve", lambda e: e.tensor_scalar(out=xrow[:], in0=xrow[:], scalar1=float(8 * CAP), scalar2=None, op0=ALU.min), r=T, w=T)
                p.op("dve", lambda e: e.tensor_scalar(out=ej[:], in0=ej[:], scalar1=7.0, scalar2=None, op0=ALU.min), r=T, w=T)
                tv = lambda o, n: tabF[:, o:o + NT * n].rearrange("p (j c) -> p j c", c=n)
                bj = lambda t, n: t[:, :].unsqueeze(2).to_broadcast([128, NT, n])
                bcp = lambda n: cp[:, 0:n].unsqueeze(1).to_broadcast([128, NT, n])
                p.op("dve", lambda e: e.tensor_tensor(out=tv(XO, NBm), in0=bj(xrow, NBm), in1=bcp(NBm), op=ALU.add), r=T + CB, w=T)
                p.op("dve", lambda e: e.tensor_scalar(out=ew[:], in0=ej[:], scalar1=float(7 * D), scalar2=None, op0=ALU.mult), r=T, w=T)
                p.op("dve", lambda e: e.tensor_tensor(out=tv(WO, 56), in0=bj(ew, 56), in1=bcp(56), op=ALU.add), r=T + CB, w=T)
                p.op("dve", lambda e: e.tensor_scalar(out=ew[:], in0=ej[:], scalar1=float(DFE), scalar2=None, op0=ALU.mult), r=T, w=T)
                p.op("dve", lambda e: e.tensor_tensor(out=tv(DO, 28), in0=bj(ew, 28), in1=bcp(28), op=ALU.add), r=T + CB, w=T)
                p.op("dve", lambda e: e.tensor_copy(out=tabI[:], in_=tabF), r=T, w=[tabB])

                wgs = Rot([(sb(st, "wg%d" % i, [128, KC, 512], BF16), p.buf("wg", dma=True)) for i in range(2)])
                wus = Rot([(sb(st, "wu%d" % i, [128, KC, 512], BF16), p.buf("wu", dma=True)) for i in range(2)])
                wds = Rot([(sb(st, "wd%d" % i, [128, 4, D], BF16), p.buf("wd", dma=True)) for i in range(2)])
                xbs = Rot([(sb(st, "xbm%d" % i, [128, D], BF16), p.buf("xbm", dma=True)) for i in range(3)])
                sgs = Rot([(sb(st, "sg%d" % i, [128, 512], F32), p.buf("sg")) for i in range(2)])
                pgs = Rot([(ps(st, "pg%d" % i, [128, 512]), p.buf("pg")) for i in range(2)])
                pus = Rot([(ps(st, "pu%d" % i, [128, 512]), p.buf("pu")) for i in range(2)])
                yps = Rot([(ps(st, "yp%d" % i, [128, D]), p.buf("yp")) for i in range(2)])
                psts = Rot([(pu_[:].bitcast(BF16), puB_) for (pu_, puB_) in pus.items])
                xTs = Rot([(hT[:, :, i * TS:(i + 1) * TS], p.buf("xT")) for i in range(2)])
                aTs = Rot([(oT[:, 4 * i:4 * i + 4, 0:TS], p.buf("aTm")) for i in range(2)])
                yss = Rot([(h[:, NB * i:NB * (i + 1), :], p.buf("ys", dma=True)) for i in range(2)])

                def load_tile(j):
                    xT, xTB = xTs.next()
                    for a in range(NB):
                        xb, xbB = xbs.next()
                        pst, pstB = psts.next()
                        for hf in range(2):
                            p.op("pool", lambda e, xb=xb, a=a, hf=hf: e.indirect_dma_start(
                                out=xb[:, hf * 512:(hf + 1) * 512], out_offset=None, in_=XsH[hf][:, :],
                                in_offset=IOA(ap=tabI[:, XO + j * NB + a:XO + j * NB + a + 1], axis=0)), r=[tabB], w=[xbB], dsem=xbB.dsem)

                        def tr(e, xb=xb, pst=pst):
                            inst = None
                            for kc in range(KC):
                                inst = e.transpose(pst[:, kc * 128:(kc + 1) * 128], xb[:, kc * 128:(kc + 1) * 128], ident[:])
                            return inst
                        p.op("pe", tr, r=[xbB, identB], w=[pstB])
                        p.op("act", lambda e, xT=xT, pst=pst, a=a: e.activation(out=xT[:, :, a * 128:(a + 1) * 128],
                                                                                 in_=pst.rearrange("p (k t) -> p k t", k=KC), func=AF.Copy),
                             r=[pstB], w=[xTB])
                    return xT, xTB

                def ffn_group(j, g, xT, xTB, ys, ysB):
                    f0 = g * 512
                    wg, wgB = wgs.next()
                    wu, wuB = wus.next()
                    wd, wdB = wds.next()
                    aT, aTB = aTs.next()
                    for (wt_, wtB_, wn_) in ((wg, wgB, "mg"), (wu, wuB, "mu")):
                        for kc in range(KC):
                            c_ = WO + j * 56 + g * 8 + kc
                            p.op("pool", lambda e, wt_=wt_, wn_=wn_, kc=kc, c_=c_: e.indirect_dma_start(
                                out=wt_[:, kc, :], out_offset=None, in_=dr[wn_][:, :],
                                in_offset=IOA(ap=tabI[:, c_:c_ + 1], axis=0)), r=[tabB], w=[wtB_], dsem=wtB_.dsem)
                    for ci in range(4):
                        c_ = DO + j * 28 + g * 4 + ci
                        p.op("pool", lambda e, ci=ci, c_=c_: e.indirect_dma_start(
                            out=wd[:, ci, :], out_offset=None, in_=dr["md"][:, :],
                            in_offset=IOA(ap=tabI[:, c_:c_ + 1], axis=0)), r=[tabB], w=[wdB], dsem=wdB.dsem)
                    for ci in range(4):
                        for tc in range(NTC):
                            pg, pgB = pgs.next()
                            pu, puB = pus.next()
                            sg, sgB = sgs.next()

                            def fg_(e, pg=pg, ci=ci, tc=tc):
                                inst = None
                                for kc in range(KC):
                                    inst = mm(e, pg[:], wg[:, kc, ci * 128:(ci + 1) * 128], xT[:, kc, tc * 512:(tc + 1) * 512], kc == 0, kc == KC - 1)
                                return inst

                            def fu_(e, pu=pu, ci=ci, tc=tc):
                                inst = None
                                for kc in range(KC):
                                    inst = mm(e, pu[:], wu[:, kc, ci * 128:(ci + 1) * 128], xT[:, kc, tc * 512:(tc + 1) * 512], kc == 0, kc == KC - 1)
                                return inst
                            p.op("pe", fg_, r=[wgB, xTB], w=[pgB])
                            p.op("pe", fu_, r=[wuB, xTB], w=[puB])
                            p.op("act", lambda e, sg=sg, pg=pg: e.activation(out=sg[:], in_=pg[:], func=AF.Silu), r=[pgB], w=[sgB])
                            p.op("dve", lambda e, sg=sg, pu=pu, ci=ci, tc=tc: e.tensor_tensor(
                                out=aT[:, ci, tc * 512:(tc + 1) * 512], in0=pu[:], in1=sg[:], op=ALU.mult), r=[puB, sgB], w=[aTB])
                    for tb in range(NB):
                        yp, ypB = yps.next()

                        def fd_(e, yp=yp, tb=tb):
                            inst = None
                            for half in range(2):
                                for ci in range(4):
                                    inst = mm(e, yp[:, half * 512:(half + 1) * 512], aT[:, ci, tb * 128:(tb + 1) * 128],
                                              wd[:, ci, half * 512:(half + 1) * 512], ci == 0, ci == 3)
                            return inst
                        p.op("pe", fd_, r=[aTB, wdB], w=[ypB])
                        ydst = ys[:, tb, :]
                        if g == 0:
                            p.op("dve", lambda e, yp=yp, ydst=ydst: e.tensor_copy(out=ydst, in_=yp[:]), r=[ypB], w=[ysB])
                        else:
                            p.op("dve", lambda e, yp=yp, ydst=ydst: e.tensor_tensor(out=ydst, in0=yp[:], in1=ydst, op=ALU.add), r=[ypB, ysB], w=[ysB])

                NG = DFE // 512
                nxt = load_tile(0)
                for j in range(NT):
                    xT, xTB = nxt
                    ys, ysB = yss.next()
                    for g in range(NG):
                        ffn_group(j, g, xT, xTB, ys, ysB)
                        if g == 3 and j + 1 < NT:
                            nxt = load_tile(j + 1)
                    for a in range(NB):
                        for hf in range(2):
                            p.op("pool", lambda e, ys=ys, j=j, hf=hf, a=a: e.indirect_dma_start(
                                out=YsH[hf][:, :], out_offset=IOA(ap=tabI[:, XO + j * NB + a:XO + j * NB + a + 1], axis=0),
                                in_=ys[:, a, hf * 512:(hf + 1) * 512], in_offset=None), r=[ysB, tabB], dsem=ysB.dsem)
                p.barrier()

        def combine_phase():
            with ExitStack() as st:
                p.phase_begin()
                L = ln_alloc(st, "fin")
                L["eps"] = epsT
                ln_load(L, 3)
                y1s = Rot([(sb(st, "y1_%d" % i, [128, D], F32), p.buf("y1", dma=True)) for i in range(3)])
                y2s = Rot([(sb(st, "y2_%d" % i, [128, D], F32), p.buf("y2", dma=True)) for i in range(3)])
                for s in range(nseq):
                    for tb in range(TB):
                        col = s * 32 + tb
                        r0 = s * S + tb * 128
                        p.op("sp", lambda e, tb=tb, r0=r0: e.dma_start(out=h[:, tb, :], in_=h1sp[r0:r0 + 128, :]), w=[hB[tb]], dsem=hB[tb].dsem, force=True)
                        y1, y1B = y1s.next()
                        y2, y2B = y2s.next()
                        for hf in range(2):
                            p.op("pool", lambda e, y1=y1, col=col, hf=hf: e.indirect_dma_start(
                                out=y1[:, hf * 512:(hf + 1) * 512], out_offset=None, in_=YsH[hf][:, :],
                                in_offset=IOA(ap=posI[:, col:col + 1], axis=0)),
                                r=[posB], w=[y1B], dsem=y1B.dsem)
                            p.op("pool", lambda e, y2=y2, col=col, hf=hf: e.indirect_dma_start(
                                out=y2[:, hf * 512:(hf + 1) * 512], out_offset=None, in_=YsH[hf][:, :],
                                in_offset=IOA(ap=posI[:, col + 16:col + 17], axis=0)),
                                r=[posB], w=[y2B], dsem=y2B.dsem)
                        p.op("act", lambda e, tb=tb: e.mul(out=h[:, tb, :], in_=h[:, tb, :], mul=ALPHA), r=[hB[tb]], w=[hB[tb]])
                        p.op("dve", lambda e, tb=tb, y1=y1, col=col: e.scalar_tensor_tensor(out=h[:, tb, :], in0=y1[:], scalar=gW[:, col:col + 1], in1=h[:, tb, :],
                                                                                         op0=ALU.mult, op1=ALU.add), r=[y1B, hB[tb], gWB], w=[hB[tb]])
                        p.op("dve", lambda e, tb=tb, y2=y2, col=col: e.scalar_tensor_tensor(out=h[:, tb, :], in0=y2[:], scalar=gW[:, col + 16:col + 17], in1=h[:, tb, :],
                                                                                         op0=ALU.mult, op1=ALU.add), r=[y2B, hB[tb], gWB], w=[hB[tb]])
                        ln_block(tb, L, to_hT=False, out_row0=s * S, gb_eng="dve")
                p.barrier()

        for s_ in range(nseq):
            do_seq(s_)
        moe_phase()
        combine_phase()
        lasts = list(p.lastd.values())
        p.pend["sp"] = lasts
        p.op("sp", lambda e: None)
        block = es.enter_context(nc.Block())
        p.emit(block)
    return nc


def prep_shared(inp):
    f = lambda a: np.ascontiguousarray(a, dtype=np.float32)
    w_in_e = inp["w_in_e"][0]
    sh = {}
    sh["wqa"] = f(w_in_e[:, 0:512]); sh["wka"] = f(w_in_e[:, 512:1024]); sh["wva"] = f(w_in_e[:, 1024:1536])
    sh["wfa"] = f(np.repeat(w_in_e[:, 1536:1544], 128, axis=1))
    qd = w_in_e[:, 1544:2056]; kd = w_in_e[:, 2056:2568]
    sh["wqd"] = f(qd); sh["wkd"] = f(kd); sh["wvd"] = f(w_in_e[:, 2568:3080])
    perm = np.arange(512)
    for hh in range(8):
        for i in range(8):
            perm[hh * 64 + i] = hh * 64 + i + 8
            perm[hh * 64 + 8 + i] = hh * 64 + i
    sh["wqds"] = f(qd[:, perm]); sh["wkds"] = f(kd[:, perm])
    sh["wo0"] = f(inp["w_out_e"][0]); sh["fg"] = f(inp["ffn_w_gate_e"][0]); sh["fu"] = f(inp["ffn_w_up_e"][0]); sh["fd"] = f(inp["ffn_w_down_e"][0])
    w_in_o = inp["w_in_o"][0]
    sh["wq1"] = f(w_in_o[:, 0:1024]); sh["wk1"] = f(w_in_o[:, 1024:2048]); sh["wv1"] = f(w_in_o[:, 2048:3072])
    sh["wig"] = f(np.repeat(w_in_o[:, 3072:3080], 128, axis=1)); sh["wfg"] = f(np.repeat(w_in_o[:, 3080:3088], 128, axis=1))
    sh["wog"] = f(w_in_o[:, 3088:4112])
    sh["wo1"] = f(inp["w_out_o"][0]); sh["wr"] = f(inp["w_router_o"][0])
    relay = lambda w: f(w.reshape(NE, D, 7, 512).transpose(0, 2, 1, 3).reshape(NE * 7 * D, 512))
    sh["mg"] = relay(inp["moe_w_gate_o"][0]); sh["mu"] = relay(inp["moe_w_up_o"][0])
    sh["md"] = f(inp["moe_w_down_o"][0].reshape(NE * DFE, D))
    lnp = np.stack([np.stack([inp["ln_mix_g_e"][0], inp["ln_mix_b_e"][0]]), np.stack([inp["ln_ffn_g_e"][0], inp["ln_ffn_b_e"][0]]),
                    np.stack([inp["ln_mix_g_o"][0], inp["ln_mix_b_o"][0]]), np.stack([inp["ln_ffn_g_o"][0], inp["ln_ffn_b_o"][0]])])
    sh["lnp"] = f(np.broadcast_to(lnp[:, :, None, :], (4, 2, 128, D)).reshape(4 * 2 * 128, D))
    gbv = np.concatenate([inp["b_forget_e"][0], inp["b_igate_o"][0], inp["b_fgate_o"][0]])
    sh["gb"] = f(np.broadcast_to(gbv[None, :], (128, 24)))
    sh["ng"] = f(inp["mlstm_norm_g_o"][0].reshape(8, 128).T)
    sh["wc"] = f(inp["w_conv_o"][0].T.reshape(16, 128, 4).transpose(1, 0, 2).reshape(128, 64))
    sh["ident"] = np.eye(128, dtype=np.float32)
    k = np.arange(128)[:, None]; q = np.arange(128)[None, :]
    mC = np.where(k > q, NEG, 0.0).astype(np.float32)
    mU = np.where(k < q, NEG, 0.0).astype(np.float32)
    mA = np.full((128, 128), NEG, np.float32)
    sh["masks"] = f(np.concatenate([mC, mU, mA], axis=1))
    sh["mask4"] = f(np.concatenate([mU, mC, mU, mC, mA, mC, mU, mC, mA, mC, mA, mC], axis=1))
    half = 8
    inv = 500000.0 ** (-np.arange(half, dtype=np.float32) / half)
    ang = np.arange(S, dtype=np.float32)[None, :] * inv[:, None]
    cosT = np.ones((128, S), np.float32); sinT = np.zeros((128, S), np.float32)
    for hh in range(2):
        b = hh * 64
        cosT[b:b + 8] = np.cos(ang); cosT[b + 8:b + 16] = np.cos(ang)
        sinT[b:b + 8] = -np.sin(ang); sinT[b + 8:b + 16] = np.sin(ang)
    sh["rope"] = f(np.concatenate([cosT, sinT], axis=1))
    augc = np.zeros((128, 6), np.float32)
    augc[0:4, 0] = [-1, 0, 0, 0]; augc[0:4, 1] = [0, -1, 0, 0]; augc[0:4, 2] = [0, 0, 1, 1]
    augc[0:4, 3] = [0, 0, 1, 0]; augc[0:4, 4] = [0, 0, 0, 1]; augc[0:4, 5] = [1, 1, 0, 0]
    sh["augc"] = augc
    sh["cp"] = f(np.arange(56, dtype=np.float32)[None, :] * 128.0 + np.arange(128, dtype=np.float32)[:, None])
    sh["tri"] = np.triu(np.ones((128, 128), np.float32))
    eo = np.zeros((128, 128), np.float32)
    for e_ in range(8):
        eo[:, e_ * 16:(e_ + 1) * 16] = e_ * CAP - 1.0
    sh["eoff"] = eo
    sh["thr"] = f(np.broadcast_to((np.arange(8, dtype=np.float32) * TS)[None, None, :], (128, 8, 8)).reshape(128, 64))
    sh["jc"] = f(np.broadcast_to(np.arange(NT, dtype=np.float32)[None, :, None], (128, NT, 8)).reshape(128, NT * 8))
    return sh


N_CORES = 8


def kernel(**inputs):
    x = np.ascontiguousarray(inputs["x"], dtype=np.float32)
    B = x.shape[0]
    nseq = B // N_CORES
    assert nseq == NSEQ
    sh = prep_shared(inputs)
    nc = build_nc(nseq)
    in_maps = []
    for c in range(N_CORES):
        m = dict(sh)
        m["x"] = x[c * nseq:(c + 1) * nseq].reshape(nseq * S, D)
        in_maps.append(m)
    res = run_bass_kernel_spmd(nc, in_maps, core_ids=list(range(N_CORES)))
    outs = [np.asarray(r["out"]).reshape(nseq, S, D) for r in res.results]
    return np.concatenate(outs, axis=0).astype(np.float32)
```

```python
import numpy as np
from contextlib import ExitStack
import concourse.bass as bass
import concourse.mybir as mybir
from concourse.bass_utils import run_bass_kernel_spmd

F32 = mybir.dt.float32
BF16 = mybir.dt.bfloat16
AF = mybir.ActivationFunctionType
ALU = mybir.AluOpType
AX = mybir.AxisListType

S = 2048
D = 1024
TB = 16
KC = 8
ALPHA = 4.0 ** 0.25
EPS = 1e-5
NEG = -30000.0
DFF = 2816
DFE = 3584
NE = 8
LNS = float(np.log(128.0 ** -0.5))
ENGS = ("pe", "act", "dve", "pool", "sp")
I32 = mybir.dt.int32
NSEQ = 4
CAP = NSEQ * S
TS = 1024
NT = 2 * CAP // TS + 7
NROWS = 8 * CAP + TS


class Op:
    __slots__ = ("eng", "fn", "deps", "sig", "val", "dsem", "key")


class Buf:
    __slots__ = ("lastw", "readers", "dsem", "name")

    def __init__(self, name, dsem=None):
        self.lastw = None
        self.readers = {}
        self.dsem = dsem
        self.name = name


class Prog:
    def __init__(self, nc, es):
        self.nc = nc
        self.es = es
        self.ops = {e: [] for e in ENGS}
        self.esem = {e: es.enter_context(nc.semaphore("s_" + e)) for e in ENGS}
        self.dcnt = {}
        self.last = {e: None for e in ENGS}
        self.lastd = {}
        self.pend = {e: [] for e in ENGS}
        self.nsem = 0
        self.sem_pool = []
        self.pool_idx = None

    def newsem(self):
        if self.pool_idx is not None:
            if self.pool_idx >= len(self.sem_pool):
                self.nsem += 1
                self.sem_pool.append(self.es.enter_context(self.nc.semaphore("d%d" % self.nsem)))
            sm = self.sem_pool[self.pool_idx]
            self.pool_idx += 1
            return sm
        self.nsem += 1
        return self.es.enter_context(self.nc.semaphore("d%d" % self.nsem))

    def phase_begin(self):
        self.pool_idx = 0

    def buf(self, name, dma=False):
        return Buf(name, self.newsem() if dma else None)

    def op(self, eng, fn, r=(), w=(), dsem=None, force=False):
        o = Op()
        o.eng = eng
        o.fn = fn
        o.sig = False
        o.val = 0
        o.dsem = dsem
        o.key = ("d", id(dsem)) if dsem is not None else eng
        deps = []
        for b in r:
            if b.lastw is not None:
                deps.append((b.lastw, True))
        for b in w:
            if b.lastw is not None:
                deps.append((b.lastw, False))
            for rd in b.readers.values():
                deps.append((rd, False))
        for d in self.pend[eng]:
            deps.append((d, True))
        self.pend[eng] = []
        dd = []
        for d, raw in deps:
            if d is o:
                continue
            if d.key == o.key and (not raw or eng == "pe") and not (force and d.dsem is not None):
                continue
            if d not in dd:
                dd.append(d)
        o.deps = dd
        if dsem is not None:
            c = self.dcnt.get(id(dsem), 0) + 16
            self.dcnt[id(dsem)] = c
            o.val = c
            self.lastd[id(dsem)] = o
        for d in dd:
            if d.dsem is None:
                d.sig = True
        for b in r:
            b.readers[o.key] = o
        for b in w:
            b.lastw = o
            b.readers = {}
        self.ops[eng].append(o)
        self.last[eng] = o
        return o

    def barrier(self):
        lasts = [self.last[e] for e in ENGS if self.last[e] is not None] + list(self.lastd.values())
        for e in ENGS:
            self.pend[e] = list(lasts)

    def emit(self, block):
        for e in ENGS:
            c = 0
            for o in self.ops[e]:
                if o.dsem is None and o.sig:
                    c += 1
                    o.val = c

        def runner(ename):
            def f(eh):
                seen = {}
                for o in self.ops[ename]:
                    for d in o.deps:
                        if seen.get(d.key, 0) >= d.val:
                            continue
                        sem = d.dsem if d.dsem is not None else self.esem[d.eng]
                        eh.wait_ge(sem, d.val)
                        seen[d.key] = d.val
                    inst = o.fn(eh)
                    if inst is None:
                        continue
                    if o.dsem is not None:
                        inst.then_inc(o.dsem, 16)
                    elif o.sig:
                        inst.then_inc(self.esem[ename], 1)
            return f

        block.tensor(runner("pe"))
        block.scalar(runner("act"))
        block.vector(runner("dve"))
        block.gpsimd(runner("pool"))
        block.sync(runner("sp"))


class Rot:
    def __init__(self, items):
        self.items = items
        self.i = 0

    def next(self):
        it = self.items[self.i % len(self.items)]
        self.i += 1
        return it


WNAMES = {
    "wqa": (D, 512), "wka": (D, 512), "wva": (D, 512), "wfa": (D, 1024),
    "wqd": (D, 512), "wkd": (D, 512), "wqds": (D, 512), "wkds": (D, 512), "wvd": (D, 512),
    "wo0": (D, D), "fg": (D, DFF), "fu": (D, DFF), "fd": (DFF, D),
    "wq1": (D, D), "wk1": (D, D), "wv1": (D, D), "wog": (D, D), "wig": (D, D), "wfg": (D, D),
    "wo1": (D, D), "wr": (D, 8), "mg": (NE * 7 * D, 512), "mu": (NE * 7 * D, 512), "md": (NE * DFE, D),
    "lnp": (4 * 2 * 128, D), "gb": (128, 24), "ng": (128, 8), "wc": (128, 64),
    "ident": (128, 128), "masks": (128, 3 * 128), "mask4": (128, 3 * 512), "rope": (128, 2 * S), "augc": (128, 6),
    "cp": (128, 56), "tri": (128, 128), "eoff": (128, 128), "thr": (128, 64), "jc": (128, NT * 8),
}


def build_nc(nseq, debug=False, phases=5):
    nc = bass.Bass("TRN2", target_bir_lowering=False)
    dr = {}
    dr["x"] = nc.dram_tensor("x", [nseq * S, D], F32, kind="ExternalInput").ap()
    for k, shp in WNAMES.items():
        dr[k] = nc.dram_tensor(k, list(shp), F32, kind="ExternalInput").ap()
    out = nc.dram_tensor("out", [nseq * S, D], F32, kind="ExternalOutput").ap()
    hsp = nc.dram_tensor("hsp", [S, D], F32, kind="Internal").ap()
    h1sp = nc.dram_tensor("h1sp", [nseq * S, D], F32, kind="Internal").ap()
    XsH = [nc.dram_tensor("Xs%d" % i, [NROWS, 512], F32, kind="Internal").ap() for i in range(2)]
    YsH = [nc.dram_tensor("Ys%d" % i, [NROWS, 512], F32, kind="Internal").ap() for i in range(2)]
    dbg = None
    if debug:
        dbg = nc.dram_tensor("dbg", [4 * S, D], F32, kind="ExternalOutput").ap()
        dbg2 = nc.dram_tensor("dbg2", [2 * 128, KC * S], F32, kind="ExternalOutput").ap()

    with ExitStack() as es:
        p = Prog(nc, es)

        uid = [0]

        def sb(st, name, shape, dt):
            uid[0] += 1
            return st.enter_context(nc.sbuf_tensor("%s_s%d" % (name, uid[0]), shape, dt))

        def ps(st, name, shape, dt=F32):
            uid[0] += 1
            return st.enter_context(nc.psum_tensor("%s_p%d" % (name, uid[0]), shape, dt))

        h = sb(es, "h", [128, TB, D], F32)
        hB = [p.buf("h%d" % i, dma=True) for i in range(TB)]
        hs = h[:, :, :].rearrange("p a b -> p (a b)")
        hT = sb(es, "hT", [128, KC, S], BF16)
        hTB = [p.buf("hT%d" % i) for i in range(TB)]
        oT = sb(es, "oT", [128, KC, S], BF16)
        oTB = [p.buf("oT%d" % i) for i in range(KC)]
        ident = sb(es, "ident", [128, 128], BF16)
        identB = p.buf("ident", dma=True)
        masks = sb(es, "masks", [128, 3, 128], BF16)
        mask4 = sb(es, "mask4", [128, 3, 512], BF16)
        augc = sb(es, "augc", [128, 6], F32)
        gb = sb(es, "gb", [128, 24], F32)
        ngb = sb(es, "ngb", [128, 24], F32)
        ng = sb(es, "ng", [128, 8], F32)
        wc = sb(es, "wc", [128, 16, 4], F32)
        ones_bf = sb(es, "ones_bf", [128, S], BF16)
        onesf = sb(es, "onesf", [128, 128], F32)
        constB = p.buf("const", dma=True)
        const2B = p.buf("const2")

        p.op("pool", lambda e: e.dma_start(out=ident[:], in_=dr["ident"][:, :]), w=[identB], dsem=identB.dsem)
        p.op("pool", lambda e: e.dma_start(out=masks[:], in_=dr["masks"].rearrange("p (a b) -> p a b", a=3)), w=[constB], dsem=constB.dsem)
        p.op("pool", lambda e: e.dma_start(out=mask4[:], in_=dr["mask4"].rearrange("p (a b) -> p a b", a=3)), w=[constB], dsem=constB.dsem)
        p.op("sp", lambda e: e.dma_start(out=augc[:], in_=dr["augc"][:, :]), w=[constB], dsem=constB.dsem)
        p.op("sp", lambda e: e.dma_start(out=gb[:], in_=dr["gb"][:, :]), w=[constB], dsem=constB.dsem)
        p.op("sp", lambda e: e.dma_start(out=ng[:], in_=dr["ng"][:, :]), w=[constB], dsem=constB.dsem)
        p.op("sp", lambda e: e.dma_start(out=wc[:], in_=dr["wc"].rearrange("p (a b) -> p a b", b=4)), w=[constB], dsem=constB.dsem)
        p.op("dve", lambda e: e.memset(ones_bf[:], 1.0), w=[const2B])
        p.op("dve", lambda e: e.memset(onesf[:], 1.0 / 128.0), w=[const2B])
        p.op("dve", lambda e: e.tensor_scalar(out=ngb[:], in0=gb[:], scalar1=-1.0, scalar2=None, op0=ALU.mult), r=[constB], w=[const2B])
        CB = [constB, const2B, identB]
        tri = sb(es, "tri", [128, 128], BF16)
        eoff = sb(es, "eoff", [128, 128], F32)
        thr = sb(es, "thr", [128, 64], F32)
        jc = sb(es, "jc", [128, NT * 8], F32)
        posI = sb(es, "posI", [128, nseq * 32], I32); posB = p.buf("posI")
        gW = sb(es, "gW", [128, nseq * 32], F32); gWB = p.buf("gW")
        base = sb(es, "base", [128, 8], F32); baseB = p.buf("base")
        NBm = TS // 128
        XO, WO, DO = 0, NT * NBm, NT * NBm + NT * 56
        NTAB = DO + NT * 28
        tabI = sb(es, "tabI", [128, NTAB], I32); tabB = p.buf("tabI")
        cp = sb(es, "cp", [128, 56], F32)
        p.op("sp", lambda e: e.dma_start(out=cp[:], in_=dr["cp"][:, :]), w=[constB], dsem=constB.dsem)
        p.op("pool", lambda e: e.dma_start(out=tri[:], in_=dr["tri"][:, :]), w=[identB], dsem=identB.dsem)
        p.op("sp", lambda e: e.dma_start(out=eoff[:], in_=dr["eoff"][:, :]), w=[constB], dsem=constB.dsem)
        p.op("sp", lambda e: e.dma_start(out=thr[:], in_=dr["thr"][:, :]), w=[constB], dsem=constB.dsem)
        p.op("sp", lambda e: e.dma_start(out=jc[:], in_=dr["jc"][:, :]), w=[constB], dsem=constB.dsem)
        p.op("dve", lambda e: e.memset(base[:], 0.0), w=[baseB])
        IOA = bass.IndirectOffsetOnAxis

        def wview(name, rows_off=0, nrows=D):
            return dr[name][rows_off:rows_off + nrows, :].rearrange("(k p) n -> p k n", p=128)

        def mm(e, o, l, r_, st=True, sp=True):
            return e.matmul(o, l, r_, start=st, stop=sp)

        def build_hT(tb, L):
            xb, xbB = L["xb"].next()
            pst, pstB = L["pst"].next()
            p.op("act", lambda e: e.activation(out=xb[:], in_=h[:, tb, :], func=AF.Copy), r=[hB[tb]], w=[xbB])

            def tr(e):
                inst = None
                for kc in range(KC):
                    inst = e.transpose(pst[:, kc * 128:(kc + 1) * 128], xb[:, kc * 128:(kc + 1) * 128], ident[:])
                return inst
            p.op("pe", tr, r=[xbB, identB], w=[pstB])
            p.op("dve", lambda e: e.tensor_copy(out=hT[:, :, tb * 128:(tb + 1) * 128],
                                                in_=pst[:].rearrange("p (k t) -> p k t", k=KC)), r=[pstB], w=[hTB[tb]])

        def ln_alloc(st, tag):
            L = {}
            L["xb"] = Rot([(sb(st, "xb%s%d" % (tag, i), [128, D], BF16), p.buf("xb")) for i in range(2)])
            L["pst"] = Rot([(ps(st, "pst%s%d" % (tag, i), [128, D], BF16), p.buf("pst")) for i in range(2)])
            L["st6"] = Rot([(sb(st, "st6%s%d" % (tag, i), [128, 12], F32), p.buf("st6")) for i in range(2)])
            L["mv"] = Rot([(sb(st, "mv%s%d" % (tag, i), [128, 4], F32), p.buf("mv")) for i in range(2)])
            L["lnp"] = sb(st, "lnp%s" % tag, [128, 2, D], F32)
            L["lnpB"] = p.buf("lnp", dma=True)
            return L

        def ln_load(L, li):
            src = dr["lnp"][li * 256:(li + 1) * 256, :].rearrange("(a p) n -> p a n", a=2)
            p.op("sp", lambda e: e.dma_start(out=L["lnp"][:], in_=src), w=[L["lnpB"]], dsem=L["lnpB"].dsem)

        def ln_block(tb, L, to_hT=True, out_row0=None, dbg_row0=None, spill=False, gb_eng="pool"):
            st6, st6B = L["st6"].next()
            mv, mvB = L["mv"].next()
            lnp = L["lnp"]
            p.op("dve", lambda e: e.bn_stats(out=st6[:, 0:6], in_=h[:, tb, 0:512]), r=[hB[tb]], w=[st6B])
            p.op("dve", lambda e: e.bn_stats(out=st6[:, 6:12], in_=h[:, tb, 512:1024]), r=[hB[tb]], w=[st6B])
            p.op("dve", lambda e: e.bn_aggr(out=mv[:, 0:2], in_=st6[:]), r=[st6B], w=[mvB])
            p.op("act", lambda e: e.activation(out=mv[:, 2:3], in_=mv[:, 1:2], func=AF.Ln, bias=L["eps"][:, 0:1]), r=[mvB, const2B], w=[mvB])
            p.op("act", lambda e: e.activation(out=mv[:, 3:4], in_=mv[:, 2:3], func=AF.Exp, scale=-0.5), r=[mvB], w=[mvB])

            def fin():
                p.op("dve", lambda e: e.tensor_scalar(out=h[:, tb, :], in0=h[:, tb, :], scalar1=mv[:, 0:1], scalar2=mv[:, 3:4],
                                                      op0=ALU.subtract, op1=ALU.mult), r=[hB[tb], mvB], w=[hB[tb]])
                p.op(gb_eng, lambda e: e.tensor_tensor(out=h[:, tb, :], in0=h[:, tb, :], in1=lnp[:, 0, :], op=ALU.mult), r=[hB[tb], L["lnpB"]], w=[hB[tb]])
                p.op(gb_eng, lambda e: e.tensor_tensor(out=h[:, tb, :], in0=h[:, tb, :], in1=lnp[:, 1, :], op=ALU.add), r=[hB[tb], L["lnpB"]], w=[hB[tb]])
                if dbg_row0 is not None:
                    p.op("sp", lambda e: e.dma_start(out=dbg[dbg_row0 + tb * 128: dbg_row0 + (tb + 1) * 128, :], in_=h[:, tb, :]),
                         r=[hB[tb]], dsem=hB[tb].dsem)
                if spill:
                    p.op("sp", lambda e: e.dma_start(out=hsp[tb * 128:(tb + 1) * 128, :], in_=h[:, tb, :]), r=[hB[tb]], dsem=hB[tb].dsem)
                if out_row0 is not None:
                    p.op("sp", lambda e: e.dma_start(out=out[out_row0 + tb * 128: out_row0 + (tb + 1) * 128, :], in_=h[:, tb, :]),
                         r=[hB[tb]], dsem=hB[tb].dsem)
                elif to_hT:
                    build_hT(tb, L)
            return fin

        class LnPipe:
            def __init__(self):
                self.pend = None

            def push(self, fin):
                if self.pend is not None:
                    self.pend()
                self.pend = fin

            def flush(self):
                if self.pend is not None:
                    self.pend()
                self.pend = None

        epsT = sb(es, "epsT", [128, 1], F32)
        onesf_one = sb(es, "oneT", [128, 1], F32)
        p.op("dve", lambda e: e.memset(epsT[:], EPS), w=[const2B])
        p.op("dve", lambda e: e.memset(onesf_one[:], 1.0), w=[const2B])

        def wload(dst_ap, src_ap, B):
            return p.op("pool", lambda e: e.dma_start(out=dst_ap, in_=src_ap), w=[B], dsem=B.dsem)

        def proj_fm(W, wname, col0, evac, ncols=128):
            wt, wtB = W["wA"].next()
            wload(wt[:, :, 0:ncols], wview(wname)[:, :, col0:col0 + ncols], wtB)
            for tc in range(4):
                pp, ppB = W["pp"].next()

                def f(e, pp=pp, wt=wt, tc=tc):
                    inst = None
                    for kc in range(KC):
                        inst = mm(e, pp[0:ncols, :], wt[:, kc, 0:ncols], hT[:, kc, tc * 512:(tc + 1) * 512], kc == 0, kc == KC - 1)
                    return inst
                p.op("pe", f, r=[wtB] + hTB[4 * tc:4 * tc + 4], w=[ppB])
                evac(tc, pp, ppB)

        def proj_tm(W, wname, col0, ncols, sets, evac):
            wt, wtB = W["wA"].next()
            wload(wt[:, :, 0:ncols], wview(wname)[:, :, col0:col0 + ncols], wtB)
            for si, tsl in enumerate(sets):
                pp, ppB = W["pp"].next()

                def f(e, pp=pp, wt=wt, tsl=tsl):
                    inst = None
                    for kc in range(KC):
                        inst = mm(e, pp[:, 0:ncols], hT[:, kc, tsl], wt[:, kc, 0:ncols], kc == 0, kc == KC - 1)
                    return inst
                p.op("pe", f, r=[wtB] + hTB, w=[ppB])
                evac(si, pp, ppB)

        def do_seq(s):
            row0 = s * S
            def _ph1():
                with ExitStack() as st:
                    p.phase_begin()
                    L = ln_alloc(st, "p0")
                    L["eps"] = epsT
                    for tb in range(TB):
                        p.op("sp", lambda e, tb=tb, row0=row0: e.dma_start(out=h[:, tb, :], in_=dr["x"][row0 + tb * 128: row0 + (tb + 1) * 128, :]),
                             w=[hB[tb]], dsem=hB[tb].dsem)
                    for tb in range(TB):
                        build_hT(tb, L)
                    p.barrier()
            _ph1()
            if phases < 1:
                return

            def _ph2():
                with ExitStack() as st:
                    p.phase_begin()
                    W = {}
                    W["wA"] = Rot([(sb(st, "wA%d" % i, [128, KC, 128], BF16), p.buf("wA", dma=True)) for i in range(3)])
                    W["pp"] = Rot([(ps(st, "pp%d" % i, [128, 512]), p.buf("pp")) for i in range(2)])
                    qT = sb(st, "qT", [128, S], BF16); qTB = p.buf("qT")
                    kT = sb(st, "kT", [128, S], BF16); kTB = p.buf("kT")
                    Vt = sb(st, "Vt", [128, 3, TB, 128], BF16); VB = [p.buf("V%d" % i) for i in range(3)]
                    t0 = hs[:, 0:S]; t0B = p.buf("t0")
                    t1 = hs[:, S:2 * S]; t1B = p.buf("t1")
                    hi = sb(st, "hi", [4, S], BF16); lo = sb(st, "lo", [4, S], BF16); hlB = p.buf("hl")
                    augQ = sb(st, "augQ", [4, S], BF16); augQB = p.buf("augQ")
                    augK = sb(st, "augK", [4, S], BF16); augKB = p.buf("augK")
                    tq = sb(st, "tq", [4, S], BF16); tqB = p.buf("tq")
                    pT = Rot([(sb(st, "pT%d" % i, [128, 512], BF16), p.buf("pT")) for i in range(2)])
                    rec = hs[0:64, 2 * S:3 * S]; recB = p.buf("rec")
                    sts = Rot([(ps(st, "st%d" % i, [128, 512]), p.buf("st")) for i in range(2)])
                    W["pp"] = Rot(W["pp"].items + sts.items)
                    accn = Rot([(ps(st, "accn%d" % i, [64, 512]), p.buf("accn")) for i in range(2)])
                    accd = Rot([(ps(st, "accd%d" % i, [64, 512]), p.buf("accd")) for i in range(2)])
                    rope = hs[:, 3 * S:5 * S].rearrange("p (a b) -> p a b", a=2); ropeB = p.buf("rope", dma=True)
                    rt = Rot([(hs[:, 5 * S + i * 1024:5 * S + (i + 1) * 1024].rearrange("p (a b) -> p a b", a=2), p.buf("rt")) for i in range(2)])
                    an = hs[0:64, 6 * S:7 * S]; anB = p.buf("an")
                    ad = hs[0:64, 7 * S:8 * S]; adB = p.buf("ad")
                    p.op("sp", lambda e: e.dma_start(out=rope, in_=dr["rope"].rearrange("p (a b) -> p a b", a=2)), w=[ropeB], dsem=ropeB.dsem)

                    def make_aug(src_ap, srcB):
                        p.op("dve", lambda e: e.tensor_copy(out=hi[:], in_=src_ap), r=[srcB], w=[hlB])
                        p.op("dve", lambda e: e.tensor_tensor(out=lo[:], in0=src_ap, in1=hi[:], op=ALU.subtract), r=[srcB, hlB], w=[hlB])

                    def fin_aug(dst, dstB, c0):
                        p.op("dve", lambda e: e.tensor_scalar(out=tq[:], in0=hi[:], scalar1=augc[0:4, c0:c0 + 1], scalar2=augc[0:4, c0 + 2:c0 + 3],
                                                              op0=ALU.mult, op1=ALU.add), r=[hlB] + CB, w=[tqB])
                        p.op("dve", lambda e: e.scalar_tensor_tensor(out=dst[:], in0=lo[:], scalar=augc[0:4, c0 + 1:c0 + 2], in1=tq[:],
                                                                     op0=ALU.mult, op1=ALU.add), r=[hlB, tqB] + CB, w=[dstB])

                    for c in range(4):
                        def ev_q(tc, pp, ppB):
                            p.op("act", lambda e: e.activation(out=qT[:, tc * 512:(tc + 1) * 512], in_=pp[:], func=AF.Copy, scale=0.125), r=[ppB], w=[qTB])

                        def ev_k(tc, pp, ppB):
                            p.op("dve", lambda e: e.tensor_copy(out=kT[:, tc * 512:(tc + 1) * 512], in_=pp[:]), r=[ppB], w=[kTB])

                        def ev_v(si, pp, ppB):
                            p.op("act", lambda e: e.activation(out=Vt[:, 0, si, :], in_=pp[:, 0:128], func=AF.Copy), r=[ppB], w=[VB[0]])
                        proj_fm(W, "wqa", c * 128, ev_q)
                        proj_fm(W, "wka", c * 128, ev_k)
                        proj_tm(W, "wva", c * 128, 128, [slice(tb * 128, (tb + 1) * 128) for tb in range(TB)], ev_v)
                        for hh in range(2):
                            head = 2 * c + hh
                            r0 = 64 * hh

                            def ev_g(tc, pp, ppB, head=head):
                                p.op("act", lambda e: e.activation(out=t0[:, tc * 512:(tc + 1) * 512], in_=pp[:], func=AF.Exp,
                                                                   bias=ngb[:, head:head + 1], scale=-1.0), r=[ppB] + CB, w=[t0B])
                            proj_fm(W, "wfa", head * 128, ev_g)
                            p.op("act", lambda e: e.activation(out=t0[:], in_=t0[:], func=AF.Ln, bias=onesf_one[:, 0:1]), r=[t0B] + CB, w=[t0B])
                            p.op("dve", lambda e: e.tensor_tensor_scan(out=t1[:], data0=ones_bf[:], data1=t0[:], initial=0.0,
                                                                       op0=ALU.mult, op1=ALU.add), r=[t0B] + CB, w=[t1B])
                            make_aug(t1[0:4, :], t1B)
                            fin_aug(augQ, augQB, 0)
                            fin_aug(augK, augKB, 3)
                            for qc in range(4):
                                an_, anB_ = accn.next()
                                ad_, adB_ = accd.next()
                                nkb = 4 * qc + 4
                                for kb in range(nkb):
                                    j0 = max(0, kb - 4 * qc)
                                    c0 = j0 * 128
                                    stt, sttB = sts.next()
                                    pt, ptB = pT.next()
                                    diag = kb >= 4 * qc

                                    def fs(e, stt=stt, kb=kb, qc=qc, c0=c0, diag=diag, r0=r0):
                                        kblk = slice(kb * 128, (kb + 1) * 128)
                                        q0 = qc * 512
                                        inst = None
                                        if diag:
                                            qs = slice(q0 + c0, q0 + c0 + 128)
                                            mm(e, stt[:, c0:c0 + 128], kT[r0:r0 + 64, kblk], qT[r0:r0 + 64, qs], True, False)
                                            mm(e, stt[:, c0:c0 + 128], augK[0:4, kblk], augQ[0:4, qs], False, False)
                                            inst = mm(e, stt[:, c0:c0 + 128], ident[:], masks[:, 0, :], False, True)
                                            c1 = c0 + 128
                                        else:
                                            c1 = c0
                                        if c1 < 512:
                                            qs = slice(q0 + c1, q0 + 512)
                                            mm(e, stt[:, c1:512], kT[r0:r0 + 64, kblk], qT[r0:r0 + 64, qs], True, False)
                                            inst = mm(e, stt[:, c1:512], augK[0:4, kblk], augQ[0:4, qs], False, True)
                                        return inst
                                    p.op("pe", fs, r=[kTB, qTB, augKB, augQB] + CB, w=[sttB])
                                    p.op("act", lambda e, stt=stt, pt=pt, c0=c0: e.activation(out=pt[:, c0:512], in_=stt[:, c0:512], func=AF.Exp),
                                         r=[sttB], w=[ptB])

                                    def fpv(e, pt=pt, kb=kb, c0=c0, hh=hh, an_=an_, ad_=ad_, nkb=nkb):
                                        mm(e, an_[:, c0:512], Vt[:, 0, kb, hh * 64:(hh + 1) * 64], pt[:, c0:512], kb == 0, kb == nkb - 1)
                                        return mm(e, ad_[:, c0:512], ones_bf[:, 0:64], pt[:, c0:512], kb == 0, kb == nkb - 1)
                                    p.op("pe", fpv, r=[ptB, VB[0]] + CB, w=[anB_, adB_])
                                p.op("dve", lambda e, ad_=ad_, qc=qc: e.reciprocal(out=rec[:, qc * 512:(qc + 1) * 512], in_=ad_[:]), r=[adB_], w=[recB])
                                p.op("dve", lambda e, an_=an_, qc=qc, r0=r0, c=c: e.tensor_tensor(
                                    out=oT[r0:r0 + 64, c, qc * 512:(qc + 1) * 512], in0=an_[:], in1=rec[:, qc * 512:(qc + 1) * 512], op=ALU.mult),
                                    r=[anB_, recB], w=[oTB[c]])

                    def tokset(bi, si):
                        if bi == 0:
                            return slice(si * 128, (si + 1) * 128)
                        if bi == 1:
                            r_, n_ = si // 4, si % 4
                            return slice(512 * n_ + r_, 512 * (n_ + 1), 4)
                        return slice(si, S, 16)

                    def accview(t, bi, si):
                        if bi == 0:
                            return t[:, :].rearrange("p (n j) -> p n j", j=128)[:, si:si + 2, :]
                        if bi == 1:
                            r_, n_ = si // 4, si % 4
                            return t[:, :].rearrange("p (n j r) -> p n j r", n=4, j=128, r=4)[:, n_:n_ + 2, :, r_]
                        return t[:, :].rearrange("p (j r) -> p r j", r=16)[:, si:si + 2, :]

                    for c in range(4):
                        def mk_rope(dst, dstB, wn, wns, c=c):
                            store = {}

                            def ev_a(tc, pp, ppB):
                                rtt, rtB = rt.next()
                                store[tc] = (rtt, rtB)
                                p.op("dve", lambda e: e.tensor_tensor(out=rtt[:, 0, :], in0=pp[:], in1=rope[:, 0, tc * 512:(tc + 1) * 512], op=ALU.mult),
                                     r=[ppB, ropeB], w=[rtB])

                            def ev_b(tc, pp, ppB):
                                rtt, rtB = store[tc]
                                p.op("dve", lambda e: e.tensor_tensor(out=rtt[:, 1, :], in0=pp[:], in1=rope[:, 1, tc * 512:(tc + 1) * 512], op=ALU.mult),
                                     r=[ppB, ropeB], w=[rtB])
                                p.op("pool", lambda e: e.tensor_tensor(out=dst[:, tc * 512:(tc + 1) * 512], in0=rtt[:, 0, :], in1=rtt[:, 1, :], op=ALU.add),
                                     r=[rtB], w=[dstB])
                            wa, waB = W["wA"].next()
                            wb, wbB = W["wA"].next()
                            wload(wa[:], wview(wn)[:, :, c * 128:(c + 1) * 128], waB)
                            wload(wb[:], wview(wns)[:, :, c * 128:(c + 1) * 128], wbB)
                            for tc in range(4):
                                for (wt_, wtB_, ev) in ((wa, waB, ev_a), (wb, wbB, ev_b)):
                                    pp, ppB = W["pp"].next()

                                    def f(e, pp=pp, wt_=wt_, tc=tc):
                                        inst = None
                                        for kc in range(KC):
                                            inst = mm(e, pp[:], wt_[:, kc, :], hT[:, kc, tc * 512:(tc + 1) * 512], kc == 0, kc == KC - 1)
                                        return inst
                                    p.op("pe", f, r=[wtB_] + hTB[4 * tc:4 * tc + 4], w=[ppB])
                                    ev(tc, pp, ppB)
                        mk_rope(qT, qTB, "wqd", "wqds")
                        mk_rope(kT, kTB, "wkd", "wkds")
                        for bi in range(3):
                            def ev_v(si, pp, ppB, bi=bi):
                                p.op("act", lambda e: e.activation(out=Vt[:, bi, si, :], in_=pp[:, 0:128], func=AF.Copy), r=[ppB], w=[VB[bi]])
                            proj_tm(W, "wvd", c * 128, 128, [tokset(bi, si) for si in range(16)], ev_v)
                        for hh in range(2):
                            r0 = 64 * hh
                            for bi in range(3):
                                for si in range(0, 16, 2):
                                    blocks = []
                                    for sq in (si, si + 1):
                                        if bi == 0:
                                            prev = sq - 1 if sq >= 1 else None
                                        elif bi == 1:
                                            prev = sq - 1 if (sq % 4) >= 1 else None
                                        else:
                                            prev = None
                                        blocks.append((sq, prev if prev is not None else sq))
                                        blocks.append((sq, sq))
                                    if bi == 2:
                                        mi = 2
                                    elif (bi == 0 and si == 0) or (bi == 1 and si % 4 == 0):
                                        mi = 1
                                    else:
                                        mi = 0
                                    stt, sttB = sts.next()
                                    pt, ptB = pT.next()
                                    an_, anB_ = accn.next()
                                    ad_, adB_ = accd.next()

                                    def fs(e, stt=stt, blocks=blocks, mi=mi, bi=bi, r0=r0):
                                        inst = None
                                        for bk, (sq, sk) in enumerate(blocks):
                                            mm(e, stt[:, bk * 128:(bk + 1) * 128], kT[r0:r0 + 64, tokset(bi, sk)], qT[r0:r0 + 64, tokset(bi, sq)], True, False)
                                            inst = mm(e, stt[:, bk * 128:(bk + 1) * 128], ident[:], mask4[:, mi, bk * 128:(bk + 1) * 128], False, True)
                                        return inst
                                    p.op("pe", fs, r=[kTB, qTB] + CB, w=[sttB])
                                    p.op("act", lambda e, stt=stt, pt=pt: e.activation(out=pt[:], in_=stt[:], func=AF.Exp, scale=0.125), r=[sttB], w=[ptB])

                                    def fpv(e, pt=pt, blocks=blocks, bi=bi, hh=hh, an_=an_, ad_=ad_):
                                        inst = None
                                        for bk, (sq, sk) in enumerate(blocks):
                                            qi = bk // 2
                                            first = (bk % 2 == 0)
                                            mm(e, an_[:, qi * 128:(qi + 1) * 128], Vt[:, bi, sk, hh * 64:(hh + 1) * 64], pt[:, bk * 128:(bk + 1) * 128], first, not first)
                                            inst = mm(e, ad_[:, qi * 128:(qi + 1) * 128], ones_bf[:, 0:64], pt[:, bk * 128:(bk + 1) * 128], first, not first)
                                        return inst
                                    p.op("pe", fpv, r=[ptB, VB[bi]] + CB, w=[anB_, adB_])
                                    pv = lambda t: t[:, 0:256].rearrange("p (a j) -> p a j", a=2)
                                    if bi == 0:
                                        p.op("dve", lambda e, an_=an_, si=si: e.tensor_copy(out=accview(an, 0, si), in_=pv(an_)), r=[anB_], w=[anB])
                                        p.op("dve", lambda e, ad_=ad_, si=si: e.tensor_copy(out=accview(ad, 0, si), in_=pv(ad_)), r=[adB_], w=[adB])
                                    else:
                                        p.op("dve", lambda e, an_=an_, si=si, bi=bi: e.tensor_tensor(out=accview(an, bi, si), in0=pv(an_), in1=accview(an, bi, si), op=ALU.add),
                                             r=[anB_, anB], w=[anB])
                                        p.op("dve", lambda e, ad_=ad_, si=si, bi=bi: e.tensor_tensor(out=accview(ad, bi, si), in0=pv(ad_), in1=accview(ad, bi, si), op=ALU.add),
                                             r=[adB_, adB], w=[adB])
                            p.op("dve", lambda e: e.reciprocal(out=rec[:], in_=ad[:]), r=[adB], w=[recB])
                            p.op("dve", lambda e, r0=r0, c=c: e.tensor_tensor(out=oT[r0:r0 + 64, 4 + c, :], in0=an[:], in1=rec[:], op=ALU.mult),
                                 r=[anB, recB], w=[oTB[4 + c]])
                    if debug and s == 0:
                        dB = p.buf("dbg2", dma=True)
                        p.op("pool", lambda e: e.dma_start(out=dbg2[0:128, :].rearrange("p (a b) -> p a b", a=KC), in_=oT[:]), r=oTB, dsem=dB.dsem)
                        p.op("pool", lambda e: e.dma_start(out=dbg2[128:256, 0:S], in_=qT[:]), r=[qTB], dsem=dB.dsem)
                        p.op("pool", lambda e: e.dma_start(out=dbg2[128:256, S:2 * S], in_=kT[:]), r=[kTB], dsem=dB.dsem)
                        p.op("pool", lambda e: e.dma_start(out=dbg2[128:192, 2 * S:3 * S], in_=an), r=[anB], dsem=dB.dsem)
                        p.op("pool", lambda e: e.dma_start(out=dbg2[128:192, 3 * S:4 * S], in_=ad), r=[adB], dsem=dB.dsem)
                        p.op("pool", lambda e: e.dma_start(out=dbg2[128:256, 4 * S:5 * S], in_=Vt[:, 1, :, :].rearrange("p a b -> p (a b)")), r=VB, dsem=dB.dsem)
                    p.barrier()

            _ph2()
            def mix_stage(wname, li, dbg_i, row0=row0):
                with ExitStack() as st:
                    p.phase_begin()
                    L = ln_alloc(st, "m%d" % li)
                    L["eps"] = epsT
                    ln_load(L, li)
                    for tb in range(TB):
                        src = dr["x"][row0 + tb * 128: row0 + (tb + 1) * 128, :] if li == 0 else hsp[tb * 128:(tb + 1) * 128, :]
                        p.op("sp", lambda e, tb=tb, src=src: e.dma_start(out=h[:, tb, :], in_=src), w=[hB[tb]], dsem=hB[tb].dsem)
                    wo = sb(st, "wo", [128, KC, D], BF16)
                    woB = p.buf("wo", dma=True)
                    wload(wo[:, :, 0:512], wview(wname)[:, :, 0:512], woB)
                    wload(wo[:, :, 512:1024], wview(wname)[:, :, 512:1024], woB)
                    mixp = Rot([(ps(st, "mix%d" % i, [128, D]), p.buf("mix")) for i in range(2)])
                    lp = LnPipe()
                    for tb in range(TB):
                        mp, mpB = mixp.next()

                        def f(e, mp=mp, tb=tb):
                            inst = None
                            for half in range(2):
                                for c in range(KC):
                                    inst = mm(e, mp[:, half * 512:(half + 1) * 512], oT[:, c, tb * 128:(tb + 1) * 128],
                                              wo[:, c, half * 512:(half + 1) * 512], c == 0, c == KC - 1)
                            return inst
                        p.op("pe", f, r=[woB] + oTB, w=[mpB])
                        p.op("dve", lambda e, mp=mp, tb=tb: e.scalar_tensor_tensor(out=h[:, tb, :], in0=h[:, tb, :], scalar=ALPHA, in1=mp[:],
                                                                                   op0=ALU.mult, op1=ALU.add), r=[hB[tb], mpB], w=[hB[tb]])
                        lp.push(ln_block(tb, L, to_hT=True, dbg_row0=(dbg_i * S if (debug and s == 0) else None)))
                    lp.flush()
                    p.barrier()
            mix_stage("wo0", 0, 0)
            if phases < 2:
                return

            def ffn_alloc(st):
                Fd = {}
                Fd["wg"] = Rot([(sb(st, "wg%d" % i, [128, KC, 512], BF16), p.buf("wg", dma=True)) for i in range(2)])
                Fd["wu"] = Rot([(sb(st, "wu%d" % i, [128, KC, 512], BF16), p.buf("wu", dma=True)) for i in range(2)])
                Fd["wd"] = Rot([(sb(st, "wd%d" % i, [128, 4, D], BF16), p.buf("wd", dma=True)) for i in range(2)])
                Fd["aT"] = Rot([(oT[:, 4 * i:4 * i + 4, :], p.buf("aT")) for i in range(2)])
                Fd["sg"] = Rot([(sb(st, "sg%d" % i, [128, 512], F32), p.buf("sg")) for i in range(2)])
                Fd["pg"] = Rot([(ps(st, "pg%d" % i, [128, 512]), p.buf("pg")) for i in range(2)])
                Fd["pu"] = Rot([(ps(st, "pu%d" % i, [128, 512]), p.buf("pu")) for i in range(2)])
                Fd["yp"] = Rot([(ps(st, "yp%d" % i, [128, D]), p.buf("yp")) for i in range(2)])
                return Fd

            def ffn(Fd, gname, uname, dname, grow0, drow0, F, gate_fn):
                f0 = 0
                while f0 < F:
                    gw = min(512, F - f0)
                    gc = gw // 128
                    wg, wgB = Fd["wg"].next()
                    wu, wuB = Fd["wu"].next()
                    wd, wdB = Fd["wd"].next()
                    aT, aTB = Fd["aT"].next()
                    wload(wg[:, :, 0:gw], wview(gname, grow0, D)[:, :, f0:f0 + gw], wgB)
                    wload(wu[:, :, 0:gw], wview(uname, grow0, D)[:, :, f0:f0 + gw], wuB)
                    wload(wd[:, 0:gc, :], wview(dname, drow0 + f0, gw), wdB)
                    for ci in range(gc):
                        for tc in range(4):
                            pg, pgB = Fd["pg"].next()
                            pu, puB = Fd["pu"].next()
                            sg, sgB = Fd["sg"].next()

                            def fg_(e, pg=pg, wg=wg, ci=ci, tc=tc):
                                inst = None
                                for kc in range(KC):
                                    inst = mm(e, pg[:], wg[:, kc, ci * 128:(ci + 1) * 128], hT[:, kc, tc * 512:(tc + 1) * 512], kc == 0, kc == KC - 1)
                                return inst

                            def fu_(e, pu=pu, wu=wu, ci=ci, tc=tc):
                                inst = None
                                for kc in range(KC):
                                    inst = mm(e, pu[:], wu[:, kc, ci * 128:(ci + 1) * 128], hT[:, kc, tc * 512:(tc + 1) * 512], kc == 0, kc == KC - 1)
                                return inst
                            p.op("pe", fg_, r=[wgB] + hTB[4 * tc:4 * tc + 4], w=[pgB])
                            p.op("pe", fu_, r=[wuB] + hTB[4 * tc:4 * tc + 4], w=[puB])
                            p.op("act", lambda e, sg=sg, pg=pg: e.activation(out=sg[:], in_=pg[:], func=AF.Silu), r=[pgB], w=[sgB])
                            p.op("dve", lambda e, sg=sg, pu=pu, aT=aT, ci=ci, tc=tc: e.tensor_tensor(
                                out=aT[:, ci, tc * 512:(tc + 1) * 512], in0=pu[:], in1=sg[:], op=ALU.mult), r=[puB, sgB], w=[aTB])
                    for tb in range(TB):
                        yp, ypB = Fd["yp"].next()

                        def fd_(e, yp=yp, aT=aT, wd=wd, tb=tb, gc=gc):
                            inst = None
                            for half in range(2):
                                for ci in range(gc):
                                    inst = mm(e, yp[:, half * 512:(half + 1) * 512], aT[:, ci, tb * 128:(tb + 1) * 128],
                                              wd[:, ci, half * 512:(half + 1) * 512], ci == 0, ci == gc - 1)
                            return inst
                        p.op("pe", fd_, r=[aTB, wdB], w=[ypB])
                        g_ap, gBs = gate_fn(tb)
                        p.op("dve", lambda e, yp=yp, tb=tb, g_ap=g_ap: e.scalar_tensor_tensor(out=h[:, tb, :], in0=yp[:], scalar=g_ap, in1=h[:, tb, :],
                                                                                            op0=ALU.mult, op1=ALU.add), r=[ypB, hB[tb]] + gBs, w=[hB[tb]])
                    f0 += gw

            def scale_h():
                for tb in range(TB):
                    p.op("act", lambda e, tb=tb: e.mul(out=h[:, tb, :], in_=h[:, tb, :], mul=ALPHA), r=[hB[tb]], w=[hB[tb]])

            def final_ln(li, dbg_i, to_hT, out_row0, spill=False):
                outs = []
                with ExitStack() as st:
                    p.phase_begin()
                    L = ln_alloc(st, "f%d" % li)
                    L["eps"] = epsT
                    ln_load(L, li)
                    lp = LnPipe()
                    for tb in range(TB):
                        lp.push(ln_block(tb, L, to_hT=to_hT, out_row0=out_row0, dbg_row0=(dbg_i * S if (debug and s == 0) else None), spill=spill))
                    lp.flush()
                    p.barrier()
                return outs

            def _ph3():
                with ExitStack() as st:
                    p.phase_begin()
                    Fd = ffn_alloc(st)
                    scale_h()
                    ffn(Fd, "fg", "fu", "fd", 0, 0, DFF, lambda tb: (1.0, []))
                    p.barrier()
            _ph3()
            final_ln(1, 1, True, None, spill=True)
            if phases < 3:
                return

            def _ph4():
                with ExitStack() as st:
                    p.phase_begin()
                    W = {}
                    W["wA"] = Rot([(sb(st, "wA%d" % i, [128, KC, 128], BF16), p.buf("wA", dma=True)) for i in range(3)])
                    W["pp"] = Rot([(ps(st, "pp%d" % i, [128, 512]), p.buf("pp")) for i in range(1)])
                    qT = sb(st, "qT", [128, S], BF16); qTB = p.buf("qT")
                    kT = sb(st, "kT", [128, S], BF16); kTB = p.buf("kT")
                    Vh = sb(st, "Vh", [128, TB, 128], BF16); VhB = p.buf("Vh")
                    sgo = sb(st, "sgo", [128, S], BF16); sgoB = p.buf("sgo")
                    pre = hs[:, 0:3 + S]; preB = p.buf("pre"); padB = p.buf("pad")
                    yv = hs[:, 2052:2052 + S]; yvB = p.buf("yv")
                    t0 = hs[:, 3:3 + S]; t0B = preB
                    t1 = hs[:, 4100:4100 + S]; t1B = p.buf("t1")
                    t2 = yv; t2B = yvB
                    hi = sb(st, "hi", [4, S], BF16); lo = sb(st, "lo", [4, S], BF16); hlB = p.buf("hl")
                    ua = hs[0:4, 9220:9220 + S]; uaB = p.buf("ua")
                    augQ = sb(st, "augQ", [4, S], BF16); augQB = p.buf("augQ")
                    augK = sb(st, "augK", [4, S], BF16); augKB = p.buf("augK")
                    tq = sb(st, "tq", [4, S], BF16); tqB = p.buf("tq")
                    pT = Rot([(sb(st, "pT%d" % i, [128, 512], BF16), p.buf("pT")) for i in range(2)])
                    Et = Rot([(hs[:, 6148 + i * 512:6148 + (i + 1) * 512], p.buf("Et")) for i in range(2)])
                    psA = Rot([(ps(st, "psA%d" % i, [128, 512]), p.buf("psA")) for i in range(2)])
                    psB = Rot([(ps(st, "psB%d" % i, [128, 512]), p.buf("psB")) for i in range(2)])
                    accn_t = ps(st, "accn", [128, 512]); accnB = p.buf("accn")
                    accd_t = ps(st, "accd", [128, 512]); accdB = p.buf("accd")
                    stat_t = ps(st, "stat", [128, 512]); statB = p.buf("stat")
                    W["pp"] = Rot(W["pp"].items + [(stat_t, statB)] + psA.items + psB.items)
                    f1 = hs[:, 7172:7684]; f1B = p.buf("f1")
                    f2 = hs[:, 7684:8196]; f2B = p.buf("f2")
                    f3 = hs[:, 8196:8708]; f3B = p.buf("f3")
                    f4 = hs[:, 8708:9220]; f4B = p.buf("f4")
                    p.op("dve", lambda e: e.memset(pre[:, 0:3], 0.0), w=[padB])

                    def make_aug(src_ap, srcB):
                        p.op("dve", lambda e: e.tensor_copy(out=hi[:], in_=src_ap), r=[srcB], w=[hlB])
                        p.op("dve", lambda e: e.tensor_tensor(out=lo[:], in0=src_ap, in1=hi[:], op=ALU.subtract), r=[srcB, hlB], w=[hlB])

                    def fin_aug(dst, dstB, c0):
                        p.op("dve", lambda e: e.tensor_scalar(out=tq[:], in0=hi[:], scalar1=augc[0:4, c0:c0 + 1], scalar2=augc[0:4, c0 + 2:c0 + 3],
                                                              op0=ALU.mult, op1=ALU.add), r=[hlB] + CB, w=[tqB])
                        p.op("dve", lambda e: e.scalar_tensor_tensor(out=dst[:], in0=lo[:], scalar=augc[0:4, c0 + 1:c0 + 2], in1=tq[:],
                                                                     op0=ALU.mult, op1=ALU.add), r=[hlB, tqB] + CB, w=[dstB])

                    for hd in range(8):
                        def conv_silu(wname, col0, chunk, dst, dstB):
                            def ev(tc, pp, ppB):
                                p.op("act", lambda e: e.activation(out=pre[:, 3 + tc * 512: 3 + (tc + 1) * 512], in_=pp[:], func=AF.Copy), r=[ppB], w=[preB])
                            proj_fm(W, wname, col0, ev)
                            p.op("dve", lambda e: e.tensor_scalar(out=yv[:], in0=pre[:, 3:3 + S], scalar1=wc[:, chunk, 3:4], scalar2=None, op0=ALU.mult),
                                 r=[preB, padB] + CB, w=[yvB])
                            for i in range(3):
                                p.op("dve", lambda e, i=i: e.scalar_tensor_tensor(out=yv[:], in0=pre[:, i:i + S], scalar=wc[:, chunk, i:i + 1], in1=yv[:],
                                                                                 op0=ALU.mult, op1=ALU.add), r=[preB, padB, yvB] + CB, w=[yvB])
                            p.op("act", lambda e: e.activation(out=dst[:], in_=yv[:], func=AF.Silu), r=[yvB], w=[dstB])
                        conv_silu("wq1", hd * 128, hd, qT, qTB)
                        conv_silu("wk1", hd * 128, 8 + hd, kT, kTB)

                        def ev_v(si, pp, ppB):
                            p.op("act", lambda e: e.activation(out=Vh[:, si, :], in_=pp[:, 0:128], func=AF.Copy), r=[ppB], w=[VhB])
                        proj_tm(W, "wv1", hd * 128, 128, [slice(tb * 128, (tb + 1) * 128) for tb in range(TB)], ev_v)

                        def ev_og(tc, pp, ppB):
                            p.op("act", lambda e: e.activation(out=sgo[:, tc * 512:(tc + 1) * 512], in_=pp[:], func=AF.Sigmoid), r=[ppB], w=[sgoB])
                        proj_fm(W, "wog", hd * 128, ev_og)

                        def ev_f(tc, pp, ppB, hd=hd):
                            p.op("act", lambda e: e.activation(out=t0[:, tc * 512:(tc + 1) * 512], in_=pp[:], func=AF.Exp,
                                                               bias=ngb[:, 16 + hd:17 + hd], scale=-1.0), r=[ppB] + CB, w=[t0B])
                        proj_fm(W, "wfg", hd * 128, ev_f)
                        p.op("act", lambda e: e.activation(out=t0[:], in_=t0[:], func=AF.Ln, bias=onesf_one[:, 0:1]), r=[t0B] + CB, w=[t0B])
                        p.op("dve", lambda e: e.tensor_tensor_scan(out=t1[:], data0=ones_bf[:], data1=t0[:], initial=0.0, op0=ALU.mult, op1=ALU.add),
                             r=[t0B] + CB, w=[t1B])

                        def ev_i(tc, pp, ppB, hd=hd):
                            p.op("dve", lambda e: e.scalar_tensor_tensor(out=t0[:, tc * 512:(tc + 1) * 512], in0=pp[:], scalar=gb[:, 8 + hd:9 + hd],
                                                                         in1=t1[:, tc * 512:(tc + 1) * 512], op0=ALU.add, op1=ALU.add),
                                 r=[ppB, t1B] + CB, w=[t0B])
                        proj_fm(W, "wig", hd * 128, ev_i)
                        p.op("dve", lambda e: e.tensor_tensor_scan(out=t2[:], data0=t0[:], data1=t0[:], initial=-1e30, op0=ALU.max, op1=ALU.max),
                             r=[t0B] + CB, w=[t2B])
                        p.op("dve", lambda e: e.tensor_tensor(out=t1[:], in0=t1[:], in1=t2[:], op=ALU.subtract), r=[t1B, t2B], w=[t1B])
                        p.op("act", lambda e: e.activation(out=t1[:], in_=t1[:], func=AF.Exp), r=[t1B], w=[t1B])
                        make_aug(t2[0:4, :], t2B)
                        fin_aug(augQ, augQB, 0)
                        p.op("dve", lambda e: e.tensor_scalar(out=ua[:], in0=t0[0:4, :], scalar1=LNS, scalar2=None, op0=ALU.add), r=[t0B], w=[uaB])
                        make_aug(ua[:], uaB)
                        fin_aug(augK, augKB, 3)

                        for qc in range(4):
                            nkb = 4 * qc + 4
                            for kb in range(nkb):
                                j0 = max(0, kb - 4 * qc)
                                c0 = j0 * 128
                                diag = kb >= 4 * qc
                                pa, paB = psA.next()
                                pb, pbB = psB.next()
                                pt, ptB = pT.next()
                                et, etB = Et.next()
                                kblk = slice(kb * 128, (kb + 1) * 128)
                                qs = slice(qc * 512 + c0, qc * 512 + 512)
                                p.op("pe", lambda e, pa=pa, kblk=kblk, qs=qs, c0=c0: mm(e, pa[:, c0:512], kT[:, kblk], qT[:, qs], True, True),
                                     r=[kTB, qTB], w=[paB])

                                def fd_(e, pb=pb, kblk=kblk, qc=qc, c0=c0, diag=diag):
                                    q0 = qc * 512
                                    inst = None
                                    c1 = c0
                                    if diag:
                                        qs1 = slice(q0 + c0, q0 + c0 + 128)
                                        mm(e, pb[:, c0:c0 + 128], augK[0:4, kblk], augQ[0:4, qs1], True, False)
                                        inst = mm(e, pb[:, c0:c0 + 128], ident[:], masks[:, 0, :], False, True)
                                        c1 = c0 + 128
                                    if c1 < 512:
                                        inst = mm(e, pb[:, c1:512], augK[0:4, kblk], augQ[0:4, slice(q0 + c1, q0 + 512)], True, True)
                                    return inst
                                p.op("pe", fd_, r=[augKB, augQB] + CB, w=[pbB])
                                p.op("act", lambda e, pb=pb, et=et, c0=c0: e.activation(out=et[:, c0:512], in_=pb[:, c0:512], func=AF.Exp), r=[pbB], w=[etB])
                                p.op("dve", lambda e, pa=pa, et=et, pt=pt, c0=c0: e.tensor_tensor(out=pt[:, c0:512], in0=pa[:, c0:512], in1=et[:, c0:512], op=ALU.mult),
                                     r=[paB, etB], w=[ptB])

                                def fpv(e, pt=pt, kb=kb, c0=c0, nkb=nkb):
                                    mm(e, accn_t[:, c0:512], Vh[:, kb, :], pt[:, c0:512], kb == 0, kb == nkb - 1)
                                    return mm(e, accd_t[:, c0:512], ones_bf[:, 0:128], pt[:, c0:512], kb == 0, kb == nkb - 1)
                                p.op("pe", fpv, r=[ptB, VhB] + CB, w=[accnB, accdB])
                            cs = slice(qc * 512, (qc + 1) * 512)
                            p.op("act", lambda e: e.activation(out=f1[:], in_=accd_t[:], func=AF.Abs), r=[accdB], w=[f1B])
                            p.op("dve", lambda e, cs=cs: e.tensor_tensor(out=f1[:], in0=f1[:], in1=t1[:, cs], op=ALU.max), r=[f1B, t1B], w=[f1B])
                            p.op("dve", lambda e: e.reciprocal(out=f1[:], in_=f1[:]), r=[f1B], w=[f1B])
                            p.op("dve", lambda e: e.tensor_tensor(out=f2[:], in0=accn_t[:], in1=f1[:], op=ALU.mult), r=[accnB, f1B], w=[f2B])
                            p.op("act", lambda e: e.activation(out=f3[:], in_=f2[:], func=AF.Square), r=[f2B], w=[f3B])
                            p.op("pe", lambda e: mm(e, stat_t[:], onesf[:], f2[:], True, True), r=[f2B] + CB, w=[statB])
                            p.op("act", lambda e: e.activation(out=f1[:], in_=stat_t[:], func=AF.Copy), r=[statB], w=[f1B])
                            p.op("pe", lambda e: mm(e, stat_t[:], onesf[:], f3[:], True, True), r=[f3B, f1B] + CB, w=[statB])
                            p.op("dve", lambda e: e.tensor_tensor(out=f4[:], in0=f1[:], in1=f1[:], op=ALU.mult), r=[f1B], w=[f4B])
                            p.op("dve", lambda e: e.tensor_tensor(out=f4[:], in0=stat_t[:], in1=f4[:], op=ALU.subtract), r=[statB, f4B], w=[f4B])
                            p.op("act", lambda e: e.activation(out=f4[:], in_=f4[:], func=AF.Ln, bias=epsT[:, 0:1]), r=[f4B] + CB, w=[f4B])
                            p.op("act", lambda e: e.activation(out=f4[:], in_=f4[:], func=AF.Exp, scale=-0.5), r=[f4B], w=[f4B])
                            p.op("dve", lambda e: e.tensor_tensor(out=f2[:], in0=f2[:], in1=f1[:], op=ALU.subtract), r=[f2B, f1B], w=[f2B])
                            p.op("dve", lambda e: e.tensor_tensor(out=f2[:], in0=f2[:], in1=f4[:], op=ALU.mult), r=[f2B, f4B], w=[f2B])
                            p.op("dve", lambda e, cs=cs, hd=hd: e.scalar_tensor_tensor(out=oT[:, hd, cs], in0=f2[:], scalar=ng[:, hd:hd + 1], in1=sgo[:, cs],
                                                                                       op0=ALU.mult, op1=ALU.mult), r=[f2B, sgoB] + CB, w=[oTB[hd]])
                    if debug and s == 0:
                        dB = p.buf("dbg2b", dma=True)
                        p.op("pool", lambda e: e.dma_start(out=dbg2[128:256, :].rearrange("p (a b) -> p a b", a=KC), in_=oT[:]), r=oTB, dsem=dB.dsem)
                    p.barrier()
            _ph4()
            mix_stage("wo1", 2, 2)
            if phases < 4:
                return

            def _ph5():
                with ExitStack() as st:
                    p.phase_begin()
                    wr = sb(st, "wr", [128, KC, 8], BF16); wrB = p.buf("wr", dma=True)
                    wload(wr[:], wview("wr"), wrB)
                    lgp = ps(st, "lgp", [128, 128]); lgB = p.buf("lg")
                    prp = ps(st, "prp", [128, 128]); prB = p.buf("pr")
                    ttp = ps(st, "ttp", [128, 128]); ttB = p.buf("tt")
                    Lg = sb(st, "Lg", [128, 128], F32); LgB = p.buf("Lg")
                    L2 = sb(st, "L2", [128, 128], F32)
                    e1 = sb(st, "e1", [128, 128], F32)
                    e2 = sb(st, "e2", [128, 128], F32)
                    Mb = sb(st, "Mb", [128, 128], BF16)
                    Tt = sb(st, "Tt", [128, 128], F32)
                    Sc = sb(st, "Sc", [128, 128], F32)
                    Rr = sb(st, "Rr", [128, 128], F32)
                    row = sb(st, "row", [128, 128], F32)
                    tmp = sb(st, "tmp", [128, 128], F32)
                    pf = sb(st, "pf", [128, 32], F32)
                    tsum = sb(st, "tsum", [128, 8], F32)
                    m1 = sb(st, "m1", [128, 16], F32)
                    m2 = sb(st, "m2", [128, 16], F32)
                    w1 = sb(st, "w1", [128, 16], F32)
                    w2 = sb(st, "w2", [128, 16], F32)
                    v3 = lambda t: t[:, :].rearrange("p (a b) -> p a b", b=8)
                    emT = lambda t: t[:, :].rearrange("p (e a) -> p a e", e=8)
                    em3 = lambda t: t[:, :].rearrange("p (e a) -> p e a", e=8)
                    bc = lambda t: t[:, :].unsqueeze(2).to_broadcast([128, 16, 8])

                    def flg(e):
                        inst = None
                        for tb in range(TB):
                            for kc in range(KC):
                                inst = mm(e, lgp[:, tb * 8:(tb + 1) * 8], hT[:, kc, tb * 128:(tb + 1) * 128], wr[:, kc, :], kc == 0, kc == KC - 1)
                        return inst
                    p.op("pe", flg, r=[wrB] + hTB, w=[lgB])
                    G = [LgB]
                    p.op("dve", lambda e: e.tensor_copy(out=Lg[:], in_=lgp[:]), r=[lgB], w=G)
                    p.op("dve", lambda e: e.tensor_reduce(out=m1[:], in_=v3(Lg), axis=AX.X, op=ALU.max), r=G, w=G)
                    p.op("dve", lambda e: e.tensor_tensor(out=v3(e1), in0=v3(Lg), in1=bc(m1), op=ALU.is_equal), r=G, w=G)
                    p.op("dve", lambda e: e.scalar_tensor_tensor(out=L2[:], in0=e1[:], scalar=-1e30, in1=Lg[:], op0=ALU.mult, op1=ALU.add), r=G, w=G)
                    p.op("dve", lambda e: e.tensor_reduce(out=m2[:], in_=v3(L2), axis=AX.X, op=ALU.max), r=G, w=G)
                    p.op("dve", lambda e: e.tensor_tensor(out=v3(e2), in0=v3(L2), in1=bc(m2), op=ALU.is_equal), r=G, w=G)
                    p.op("dve", lambda e: e.tensor_tensor(out=w2[:], in0=m2[:], in1=m1[:], op=ALU.subtract), r=G, w=G)
                    p.op("act", lambda e: e.activation(out=w2[:], in_=w2[:], func=AF.Exp), r=G, w=G)
                    p.op("dve", lambda e: e.tensor_scalar(out=w1[:], in0=w2[:], scalar1=1.0, scalar2=None, op0=ALU.add), r=G, w=G)
                    p.op("dve", lambda e: e.reciprocal(out=w1[:], in_=w1[:]), r=G, w=G)
                    p.op("dve", lambda e: e.tensor_tensor(out=w2[:], in0=w2[:], in1=w1[:], op=ALU.mult), r=G, w=G)
                    p.op("dve", lambda e: e.tensor_copy(out=gW[:, s * 32:s * 32 + 16], in_=w1[:]), r=G, w=[gWB])
                    p.op("dve", lambda e: e.tensor_copy(out=gW[:, s * 32 + 16:s * 32 + 32], in_=w2[:]), r=G, w=[gWB])
                    p.op("dve", lambda e: e.tensor_tensor(out=emT(Mb), in0=v3(e1), in1=v3(e2), op=ALU.add), r=G, w=G)

                    def fpr(e):
                        mm(e, prp[:], tri[:], Mb[:], True, True)
                        return mm(e, ttp[:], ones_bf[:, 0:128], Mb[:], True, True)
                    p.op("pe", fpr, r=G + CB, w=[prB, ttB])
                    p.op("dve", lambda e: e.tensor_copy(out=Tt[:], in_=ttp[:]), r=[ttB], w=G)
                    p.op("dve", lambda e: e.tensor_tensor_scan(out=Sc[:], data0=ones_bf[:, 0:128], data1=Tt[:], initial=0.0, op0=ALU.mult, op1=ALU.add),
                         r=G + CB, w=G)
                    p.op("dve", lambda e: e.tensor_tensor(out=Sc[:], in0=Sc[:], in1=Tt[:], op=ALU.subtract), r=G, w=G)
                    p.op("dve", lambda e: e.tensor_tensor(out=em3(Rr), in0=em3(Sc), in1=em3(Sc)[:, :, 0:1].to_broadcast([128, 8, 16]), op=ALU.subtract), r=G, w=G)
                    p.op("dve", lambda e: e.tensor_tensor(out=em3(Rr), in0=em3(Rr), in1=base[:, :].unsqueeze(2).to_broadcast([128, 8, 16]), op=ALU.add),
                         r=G + [baseB], w=G)
                    p.op("dve", lambda e: e.tensor_tensor(out=row[:], in0=prp[:], in1=Rr[:], op=ALU.add), r=G + [prB], w=G)
                    p.op("dve", lambda e: e.tensor_tensor(out=row[:], in0=row[:], in1=eoff[:], op=ALU.add), r=G + CB, w=G)
                    p.op("dve", lambda e: e.tensor_reduce(out=tsum[:], in_=em3(Tt), axis=AX.X, op=ALU.add), r=G, w=G)
                    p.op("dve", lambda e: e.tensor_tensor(out=base[:], in0=base[:], in1=tsum[:], op=ALU.add), r=G + [baseB], w=[baseB])
                    p.op("dve", lambda e: e.tensor_tensor(out=v3(tmp), in0=v3(e1), in1=emT(row), op=ALU.mult), r=G, w=G)
                    p.op("dve", lambda e: e.tensor_reduce(out=pf[:, 0:16], in_=v3(tmp), axis=AX.X, op=ALU.add), r=G, w=G)
                    p.op("dve", lambda e: e.tensor_tensor(out=v3(tmp), in0=v3(e2), in1=emT(row), op=ALU.mult), r=G, w=G)
                    p.op("dve", lambda e: e.tensor_reduce(out=pf[:, 16:32], in_=v3(tmp), axis=AX.X, op=ALU.add), r=G, w=G)
                    p.op("dve", lambda e: e.tensor_copy(out=posI[:, s * 32:(s + 1) * 32], in_=pf[:]), r=G, w=[posB])
                    for tb in range(TB):
                        p.op("sp", lambda e, tb=tb: e.dma_start(out=h1sp[row0 + tb * 128: row0 + (tb + 1) * 128, :], in_=h[:, tb, :]),
                             r=[hB[tb]], dsem=hB[tb].dsem)
                        for k in range(2):
                            col = s * 32 + k * 16 + tb
                            for hf in range(2):
                                p.op("pool", lambda e, tb=tb, col=col, hf=hf: e.indirect_dma_start(
                                    out=XsH[hf][:, :], out_offset=IOA(ap=posI[:, col:col + 1], axis=0),
                                    in_=hs[:, tb * D + hf * 512: tb * D + (hf + 1) * 512], in_offset=None),
                                    r=[hB[tb], posB], dsem=hB[tb].dsem)
                    p.barrier()
            _ph5()

        def moe_phase():
            NB = TS // 128
            NTC = TS // 512
            with ExitStack() as st:
                p.phase_begin()
                ntl = sb(st, "ntl", [128, 8], F32)
                cinc = sb(st, "cinc", [128, 8], F32)
                cmp3 = hs[:, NTAB:NTAB + 64]
                le = hs[:, NTAB + 64:NTAB + 64 + NT * 8]
                le2 = hs[:, NTAB + 64 + NT * 8:NTAB + 64 + 2 * NT * 8]
                ej = sb(st, "ej", [128, NT], F32)
                ub = sb(st, "ub", [128, NT], F32)
                tabF = hs[:, 0:NTAB]
                xrow = sb(st, "xrow", [128, NT], F32)
                ew = sb(st, "ew", [128, NT], F32)
                T = [p.buf("tabw")]
                c3 = lambda t: t.rearrange("p (a b) -> p a b", b=8)
                p.op("dve", lambda e: e.tensor_tensor(out=c3(cmp3), in0=base[:, :].unsqueeze(2).to_broadcast([128, 8, 8]), in1=c3(thr[:, :]), op=ALU.is_gt),
                     r=[baseB] + CB, w=T)
                p.op("dve", lambda e: e.tensor_reduce(out=ntl[:], in_=c3(cmp3), axis=AX.X, op=ALU.add), r=T, w=T)
                p.op("dve", lambda e: e.tensor_tensor_scan(out=cinc[:], data0=ones_bf[:, 0:8], data1=ntl[:], initial=0.0, op0=ALU.mult, op1=ALU.add),
                     r=T + CB, w=T)
                p.op("dve", lambda e: e.tensor_tensor(out=c3(le), in0=cinc[:, :].unsqueeze(1).to_broadcast([128, NT, 8]), in1=c3(jc[:, :]), op=ALU.is_le),
                     r=T + CB, w=T)
                p.op("dve", lambda e: e.tensor_reduce(out=ej[:], in_=c3(le), axis=AX.X, op=ALU.add), r=T, w=T)
                p.op("dve", lambda e: e.tensor_tensor(out=c3(le2), in0=c3(le), in1=ntl[:, :].unsqueeze(1).to_broadcast([128, NT, 8]), op=ALU.mult), r=T, w=T)
                p.op("dve", lambda e: e.tensor_reduce(out=ub[:], in_=c3(le2), axis=AX.X, op=ALU.add), r=T, w=T)
                p.op("dve", lambda e: e.tensor_tensor(out=ub[:], in0=c3(jc[:, :])[:, :, 0], in1=ub[:], op=ALU.subtract), r=T + CB, w=T)
                p.op("dve", lambda e: e.tensor_scalar(out=ub[:], in0=ub[:], scalar1=float(TS), scalar2=None, op0=ALU.mult), r=T, w=T)
                p.op("dve", lambda e: e.scalar_tensor_tensor(out=xrow[:], in0=ej[:], scalar=float(CAP), in1=ub[:], op0=ALU.mult, op1=ALU.add), r=T, w=T)
                p.op("dve", lambda e: e.tensor_scalar(out=xrow[:], in0=xrow[:], scalar1=float(8 * CAP), scalar2=None, op0=ALU.min), r=T, w=T)
                p.op("dve", lambda e: e.tensor_scalar(out=ej[:], in0=ej[:], scalar1=7.0, scalar2=None, op0=ALU.min), r=T, w=T)
                tv = lambda o, n: tabF[:, o:o + NT * n].rearrange("p (j c) -> p j c", c=n)
                bj = lambda t, n: t[:, :].unsqueeze(2).to_broadcast([128, NT, n])
                bcp = lambda n: cp[:, 0:n].unsqueeze(1).to_broadcast([128, NT, n])
                p.op("dve", lambda e: e.tensor_tensor(out=tv(XO, NBm), in0=bj(xrow, NBm), in1=bcp(NBm), op=ALU.add), r=T + CB, w=T)
                p.op("dve", lambda e: e.tensor_scalar(out=ew[:], in0=ej[:], scalar1=float(7 * D), scalar2=None, op0=ALU.mult), r=T, w=T)
                p.op("dve", lambda e: e.tensor_tensor(out=tv(WO, 56), in0=bj(ew, 56), in1=bcp(56), op=ALU.add), r=T + CB, w=T)
                p.op("dve", lambda e: e.tensor_scalar(out=ew[:], in0=ej[:], scalar1=float(DFE), scalar2=None, op0=ALU.mult), r=T, w=T)
                p.op("dve", lambda e: e.tensor_tensor(out=tv(DO, 28), in0=bj(ew, 28), in1=bcp(28), op=ALU.add), r=T + CB, w=T)
                p.op("dve", lambda e: e.tensor_copy(out=tabI[:], in_=tabF), r=T, w=[tabB])

                wgs = Rot([(sb(st, "wg%d" % i, [128, KC, 512], BF16), p.buf("wg", dma=True)) for i in range(2)])
                wus = Rot([(sb(st, "wu%d" % i, [128, KC, 512], BF16), p.buf("wu", dma=True)) for i in range(2)])
                wds = Rot([(sb(st, "wd%d" % i, [128, 4, D], BF16), p.buf("wd", dma=True)) for i in range(2)])
                xbs = Rot([(sb(st, "xbm%d" % i, [128, D], BF16), p.buf("xbm", dma=True)) for i in range(3)])
                sgs = Rot([(sb(st, "sg%d" % i, [128, 512], F32), p.buf("sg")) for i in range(2)])
                pgs = Rot([(ps(st, "pg%d" % i, [128, 512]), p.buf("pg")) for i in range(2)])
                pus = Rot([(ps(st, "pu%d" % i, [128, 512]), p.buf("pu")) for i in range(2)])
                yps = Rot([(ps(st, "yp%d" % i, [128, D]), p.buf("yp")) for i in range(2)])
                psts = Rot([(pu_[:].bitcast(BF16), puB_) for (pu_, puB_) in pus.items])
                xTs = Rot([(hT[:, :, i * TS:(i + 1) * TS], p.buf("xT")) for i in range(2)])
                aTs = Rot([(oT[:, 4 * i:4 * i + 4, 0:TS], p.buf("aTm")) for i in range(2)])
                yss = Rot([(h[:, NB * i:NB * (i + 1), :], p.buf("ys", dma=True)) for i in range(2)])

                def load_tile(j):
                    xT, xTB = xTs.next()
                    for a in range(NB):
                        xb, xbB = xbs.next()
                        pst, pstB = psts.next()
                        for hf in range(2):
                            p.op("pool", lambda e, xb=xb, a=a, hf=hf: e.indirect_dma_start(
                                out=xb[:, hf * 512:(hf + 1) * 512], out_offset=None, in_=XsH[hf][:, :],
                                in_offset=IOA(ap=tabI[:, XO + j * NB + a:XO + j * NB + a + 1], axis=0)), r=[tabB], w=[xbB], dsem=xbB.dsem)

                        def tr(e, xb=xb, pst=pst):
                            inst = None
                            for kc in range(KC):
                                inst = e.transpose(pst[:, kc * 128:(kc + 1) * 128], xb[:, kc * 128:(kc + 1) * 128], ident[:])
                            return inst
                        p.op("pe", tr, r=[xbB, identB], w=[pstB])
                        p.op("act", lambda e, xT=xT, pst=pst, a=a: e.activation(out=xT[:, :, a * 128:(a + 1) * 128],
                                                                                 in_=pst.rearrange("p (k t) -> p k t", k=KC), func=AF.Copy),
                             r=[pstB], w=[xTB])
                    return xT, xTB

                def ffn_load(j, g):
                    wg, wgB = wgs.next()
                    wu, wuB = wus.next()
                    wd, wdB = wds.next()
                    for (wt_, wtB_, wn_) in ((wg, wgB, "mg"), (wu, wuB, "mu")):
                        for kc in range(KC):
                            c_ = WO + j * 56 + g * 8 + kc
                            p.op("pool", lambda e, wt_=wt_, wn_=wn_, kc=kc, c_=c_: e.indirect_dma_start(
                                out=wt_[:, kc, :], out_offset=None, in_=dr[wn_][:, :],
                                in_offset=IOA(ap=tabI[:, c_:c_ + 1], axis=0)), r=[tabB], w=[wtB_], dsem=wtB_.dsem)
                    for ci in range(4):
                        c_ = DO + j * 28 + g * 4 + ci
                        p.op("pool", lambda e, ci=ci, c_=c_: e.indirect_dma_start(
                            out=wd[:, ci, :], out_offset=None, in_=dr["md"][:, :],
                            in_offset=IOA(ap=tabI[:, c_:c_ + 1], axis=0)), r=[tabB], w=[wdB], dsem=wdB.dsem)
                    return (wg, wgB, wu, wuB, wd, wdB)

                def ffn_group(j, g, xT, xTB, ys, ysB, wts):
                    wg, wgB, wu, wuB, wd, wdB = wts
                    aT, aTB = aTs.next()
                    for ci in range(4):
                        for tc in range(NTC):
                            pg, pgB = pgs.next()
                            pu, puB = pus.next()
                            sg, sgB = sgs.next()

                            def fg_(e, pg=pg, ci=ci, tc=tc):
                                inst = None
                                for kc in range(KC):
                                    inst = mm(e, pg[:], wg[:, kc, ci * 128:(ci + 1) * 128], xT[:, kc, tc * 512:(tc + 1) * 512], kc == 0, kc == KC - 1)
                                return inst

                            def fu_(e, pu=pu, ci=ci, tc=tc):
                                inst = None
                                for kc in range(KC):
                                    inst = mm(e, pu[:], wu[:, kc, ci * 128:(ci + 1) * 128], xT[:, kc, tc * 512:(tc + 1) * 512], kc == 0, kc == KC - 1)
                                return inst
                            p.op("pe", fg_, r=[wgB, xTB], w=[pgB])
                            p.op("pe", fu_, r=[wuB, xTB], w=[puB])
                            p.op("act", lambda e, sg=sg, pg=pg: e.activation(out=sg[:], in_=pg[:], func=AF.Silu), r=[pgB], w=[sgB])
                            p.op("dve", lambda e, sg=sg, pu=pu, ci=ci, tc=tc: e.tensor_tensor(
                                out=aT[:, ci, tc * 512:(tc + 1) * 512], in0=pu[:], in1=sg[:], op=ALU.mult), r=[puB, sgB], w=[aTB])
                    for tb in range(NB):
                        yp, ypB = yps.next()

                        def fd_(e, yp=yp, tb=tb):
                            inst = None
                            for half in range(2):
                                for ci in range(4):
                                    inst = mm(e, yp[:, half * 512:(half + 1) * 512], aT[:, ci, tb * 128:(tb + 1) * 128],
                                              wd[:, ci, half * 512:(half + 1) * 512], ci == 0, ci == 3)
                            return inst
                        p.op("pe", fd_, r=[aTB, wdB], w=[ypB])
                        ydst = ys[:, tb, :]
                        if g == 0:
                            p.op("dve", lambda e, yp=yp, ydst=ydst: e.tensor_copy(out=ydst, in_=yp[:]), r=[ypB], w=[ysB])
                        else:
                            p.op("dve", lambda e, yp=yp, ydst=ydst: e.tensor_tensor(out=ydst, in0=yp[:], in1=ydst, op=ALU.add), r=[ypB, ysB], w=[ysB])

                NG = DFE // 512
                nxt = load_tile(0)
                wnext = ffn_load(0, 0)
                for j in range(NT):
                    xT, xTB = nxt
                    ys, ysB = yss.next()
                    for g in range(NG):
                        wcur = wnext
                        if g + 1 < NG:
                            wnext = ffn_load(j, g + 1)
                        elif j + 1 < NT:
                            wnext = ffn_load(j + 1, 0)
                        ffn_group(j, g, xT, xTB, ys, ysB, wcur)
                        if g == 3 and j + 1 < NT:
                            nxt = load_tile(j + 1)
                    for a in range(NB):
                        for hf in range(2):
                            p.op("pool", lambda e, ys=ys, j=j, hf=hf, a=a: e.indirect_dma_start(
                                out=YsH[hf][:, :], out_offset=IOA(ap=tabI[:, XO + j * NB + a:XO + j * NB + a + 1], axis=0),
                                in_=ys[:, a, hf * 512:(hf + 1) * 512], in_offset=None), r=[ysB, tabB], dsem=ysB.dsem)
                p.barrier()

        def combine_phase():
            with ExitStack() as st:
                p.phase_begin()
                L = ln_alloc(st, "fin")
                L["eps"] = epsT
                ln_load(L, 3)
                y1s = Rot([(sb(st, "y1_%d" % i, [128, D], F32), p.buf("y1", dma=True)) for i in range(3)])
                y2s = Rot([(sb(st, "y2_%d" % i, [128, D], F32), p.buf("y2", dma=True)) for i in range(3)])
                lp = LnPipe()
                for s in range(nseq):
                    for tb in range(TB):
                        col = s * 32 + tb
                        r0 = s * S + tb * 128
                        p.op("sp", lambda e, tb=tb, r0=r0: e.dma_start(out=h[:, tb, :], in_=h1sp[r0:r0 + 128, :]), w=[hB[tb]], dsem=hB[tb].dsem, force=True)
                        y1, y1B = y1s.next()
                        y2, y2B = y2s.next()
                        for hf in range(2):
                            p.op("pool", lambda e, y1=y1, col=col, hf=hf: e.indirect_dma_start(
                                out=y1[:, hf * 512:(hf + 1) * 512], out_offset=None, in_=YsH[hf][:, :],
                                in_offset=IOA(ap=posI[:, col:col + 1], axis=0)),
                                r=[posB], w=[y1B], dsem=y1B.dsem)
                            p.op("pool", lambda e, y2=y2, col=col, hf=hf: e.indirect_dma_start(
                                out=y2[:, hf * 512:(hf + 1) * 512], out_offset=None, in_=YsH[hf][:, :],
                                in_offset=IOA(ap=posI[:, col + 16:col + 17], axis=0)),
                                r=[posB], w=[y2B], dsem=y2B.dsem)
                        p.op("act", lambda e, tb=tb: e.mul(out=h[:, tb, :], in_=h[:, tb, :], mul=ALPHA), r=[hB[tb]], w=[hB[tb]])
                        p.op("dve", lambda e, tb=tb, y1=y1, col=col: e.scalar_tensor_tensor(out=h[:, tb, :], in0=y1[:], scalar=gW[:, col:col + 1], in1=h[:, tb, :],
                                                                                         op0=ALU.mult, op1=ALU.add), r=[y1B, hB[tb], gWB], w=[hB[tb]])
                        p.op("dve", lambda e, tb=tb, y2=y2, col=col: e.scalar_tensor_tensor(out=h[:, tb, :], in0=y2[:], scalar=gW[:, col + 16:col + 17], in1=h[:, tb, :],
                                                                                         op0=ALU.mult, op1=ALU.add), r=[y2B, hB[tb], gWB], w=[hB[tb]])
                        lp.push(ln_block(tb, L, to_hT=False, out_row0=s * S, gb_eng="dve"))
                lp.flush()
                p.barrier()

        for s_ in range(nseq):
            do_seq(s_)
        moe_phase()
        combine_phase()
        lasts = list(p.lastd.values())
        p.pend["sp"] = lasts
        p.op("sp", lambda e: None)
        block = es.enter_context(nc.Block())
        p.emit(block)
    return nc


def prep_shared(inp):
    f = lambda a: np.ascontiguousarray(a, dtype=np.float32)
    w_in_e = inp["w_in_e"][0]
    sh = {}
    sh["wqa"] = f(w_in_e[:, 0:512]); sh["wka"] = f(w_in_e[:, 512:1024]); sh["wva"] = f(w_in_e[:, 1024:1536])
    sh["wfa"] = f(np.repeat(w_in_e[:, 1536:1544], 128, axis=1))
    qd = w_in_e[:, 1544:2056]; kd = w_in_e[:, 2056:2568]
    sh["wqd"] = f(qd); sh["wkd"] = f(kd); sh["wvd"] = f(w_in_e[:, 2568:3080])
    perm = np.arange(512)
    for hh in range(8):
        for i in range(8):
            perm[hh * 64 + i] = hh * 64 + i + 8
            perm[hh * 64 + 8 + i] = hh * 64 + i
    sh["wqds"] = f(qd[:, perm]); sh["wkds"] = f(kd[:, perm])
    sh["wo0"] = f(inp["w_out_e"][0]); sh["fg"] = f(inp["ffn_w_gate_e"][0]); sh["fu"] = f(inp["ffn_w_up_e"][0]); sh["fd"] = f(inp["ffn_w_down_e"][0])
    w_in_o = inp["w_in_o"][0]
    sh["wq1"] = f(w_in_o[:, 0:1024]); sh["wk1"] = f(w_in_o[:, 1024:2048]); sh["wv1"] = f(w_in_o[:, 2048:3072])
    sh["wig"] = f(np.repeat(w_in_o[:, 3072:3080], 128, axis=1)); sh["wfg"] = f(np.repeat(w_in_o[:, 3080:3088], 128, axis=1))
    sh["wog"] = f(w_in_o[:, 3088:4112])
    sh["wo1"] = f(inp["w_out_o"][0]); sh["wr"] = f(inp["w_router_o"][0])
    relay = lambda w: f(w.reshape(NE, D, 7, 512).transpose(0, 2, 1, 3).reshape(NE * 7 * D, 512))
    sh["mg"] = relay(inp["moe_w_gate_o"][0]); sh["mu"] = relay(inp["moe_w_up_o"][0])
    sh["md"] = f(inp["moe_w_down_o"][0].reshape(NE * DFE, D))
    lnp = np.stack([np.stack([inp["ln_mix_g_e"][0], inp["ln_mix_b_e"][0]]), np.stack([inp["ln_ffn_g_e"][0], inp["ln_ffn_b_e"][0]]),
                    np.stack([inp["ln_mix_g_o"][0], inp["ln_mix_b_o"][0]]), np.stack([inp["ln_ffn_g_o"][0], inp["ln_ffn_b_o"][0]])])
    sh["lnp"] = f(np.broadcast_to(lnp[:, :, None, :], (4, 2, 128, D)).reshape(4 * 2 * 128, D))
    gbv = np.concatenate([inp["b_forget_e"][0], inp["b_igate_o"][0], inp["b_fgate_o"][0]])
    sh["gb"] = f(np.broadcast_to(gbv[None, :], (128, 24)))
    sh["ng"] = f(inp["mlstm_norm_g_o"][0].reshape(8, 128).T)
    sh["wc"] = f(inp["w_conv_o"][0].T.reshape(16, 128, 4).transpose(1, 0, 2).reshape(128, 64))
    sh["ident"] = np.eye(128, dtype=np.float32)
    k = np.arange(128)[:, None]; q = np.arange(128)[None, :]
    mC = np.where(k > q, NEG, 0.0).astype(np.float32)
    mU = np.where(k < q, NEG, 0.0).astype(np.float32)
    mA = np.full((128, 128), NEG, np.float32)
    sh["masks"] = f(np.concatenate([mC, mU, mA], axis=1))
    sh["mask4"] = f(np.concatenate([mU, mC, mU, mC, mA, mC, mU, mC, mA, mC, mA, mC], axis=1))
    half = 8
    inv = 500000.0 ** (-np.arange(half, dtype=np.float32) / half)
    ang = np.arange(S, dtype=np.float32)[None, :] * inv[:, None]
    cosT = np.ones((128, S), np.float32); sinT = np.zeros((128, S), np.float32)
    for hh in range(2):
        b = hh * 64
        cosT[b:b + 8] = np.cos(ang); cosT[b + 8:b + 16] = np.cos(ang)
        sinT[b:b + 8] = -np.sin(ang); sinT[b + 8:b + 16] = np.sin(ang)
    sh["rope"] = f(np.concatenate([cosT, sinT], axis=1))
    augc = np.zeros((128, 6), np.float32)
    augc[0:4, 0] = [-1, 0, 0, 0]; augc[0:4, 1] = [0, -1, 0, 0]; augc[0:4, 2] = [0, 0, 1, 1]
    augc[0:4, 3] = [0, 0, 1, 0]; augc[0:4, 4] = [0, 0, 0, 1]; augc[0:4, 5] = [1, 1, 0, 0]
    sh["augc"] = augc
    sh["cp"] = f(np.arange(56, dtype=np.float32)[None, :] * 128.0 + np.arange(128, dtype=np.float32)[:, None])
    sh["tri"] = np.triu(np.ones((128, 128), np.float32))
    eo = np.zeros((128, 128), np.float32)
    for e_ in range(8):
        eo[:, e_ * 16:(e_ + 1) * 16] = e_ * CAP - 1.0
    sh["eoff"] = eo
    sh["thr"] = f(np.broadcast_to((np.arange(8, dtype=np.float32) * TS)[None, None, :], (128, 8, 8)).reshape(128, 64))
    sh["jc"] = f(np.broadcast_to(np.arange(NT, dtype=np.float32)[None, :, None], (128, NT, 8)).reshape(128, NT * 8))
    return sh


N_CORES = 8


def kernel(**inputs):
    x = np.ascontiguousarray(inputs["x"], dtype=np.float32)
    B = x.shape[0]
    nseq = B // N_CORES
    assert nseq == NSEQ
    sh = prep_shared(inputs)
    nc = build_nc(nseq)
    in_maps = []
    for c in range(N_CORES):
        m = dict(sh)
        m["x"] = x[c * nseq:(c + 1) * nseq].reshape(nseq * S, D)
        in_maps.append(m)
    res = run_bass_kernel_spmd(nc, in_maps, core_ids=list(range(N_CORES)))
    outs = [np.asarray(r["out"]).reshape(nseq, S, D) for r in res.results]
    return np.concatenate(outs, axis=0).astype(np.float32)
```

```python
import numpy as np
from contextlib import ExitStack
import concourse.bass as bass
import concourse.mybir as mybir
from concourse.bass_utils import run_bass_kernel_spmd

F32 = mybir.dt.float32
BF16 = mybir.dt.bfloat16
AF = mybir.ActivationFunctionType
ALU = mybir.AluOpType
AX = mybir.AxisListType

S = 2048
D = 1024
TB = 16
KC = 8
ALPHA = 4.0 ** 0.25
EPS = 1e-5
NEG = -30000.0
DFF = 2816
DFE = 3584
NE = 8
LNS = float(np.log(128.0 ** -0.5))
ENGS = ("pe", "act", "dve", "pool", "sp")
I32 = mybir.dt.int32
NSEQ = 4
CAP = NSEQ * S
TS = 1024
NT = 2 * CAP // TS + 7
NROWS = 8 * CAP + TS


class Op:
    __slots__ = ("eng", "fn", "deps", "sig", "val", "dsem", "key")


class Buf:
    __slots__ = ("lastw", "readers", "dsem", "name")

    def __init__(self, name, dsem=None):
        self.lastw = None
        self.readers = {}
        self.dsem = dsem
        self.name = name


class Prog:
    def __init__(self, nc, es):
        self.nc = nc
        self.es = es
        self.ops = {e: [] for e in ENGS}
        self.esem = {e: es.enter_context(nc.semaphore("s_" + e)) for e in ENGS}
        self.dcnt = {}
        self.last = {e: None for e in ENGS}
        self.lastd = {}
        self.pend = {e: [] for e in ENGS}
        self.nsem = 0
        self.sem_pool = []
        self.pool_idx = None

    def newsem(self):
        if self.pool_idx is not None:
            if self.pool_idx >= len(self.sem_pool):
                self.nsem += 1
                self.sem_pool.append(self.es.enter_context(self.nc.semaphore("d%d" % self.nsem)))
            sm = self.sem_pool[self.pool_idx]
            self.pool_idx += 1
            return sm
        self.nsem += 1
        return self.es.enter_context(self.nc.semaphore("d%d" % self.nsem))

    def phase_begin(self):
        self.pool_idx = 0

    def buf(self, name, dma=False):
        return Buf(name, self.newsem() if dma else None)

    def op(self, eng, fn, r=(), w=(), dsem=None, force=False):
        o = Op()
        o.eng = eng
        o.fn = fn
        o.sig = False
        o.val = 0
        o.dsem = dsem
        o.key = ("d", id(dsem)) if dsem is not None else eng
        deps = []
        for b in r:
            if b.lastw is not None:
                deps.append((b.lastw, True))
        for b in w:
            if b.lastw is not None:
                deps.append((b.lastw, False))
            for rd in b.readers.values():
                deps.append((rd, False))
        for d in self.pend[eng]:
            deps.append((d, True))
        self.pend[eng] = []
        dd = []
        for d, raw in deps:
            if d is o:
                continue
            if d.key == o.key and (not raw or eng == "pe") and not (force and d.dsem is not None):
                continue
            if d not in dd:
                dd.append(d)
        o.deps = dd
        if dsem is not None:
            c = self.dcnt.get(id(dsem), 0) + 16
            self.dcnt[id(dsem)] = c
            o.val = c
            self.lastd[id(dsem)] = o
        for d in dd:
            if d.dsem is None:
                d.sig = True
        for b in r:
            b.readers[o.key] = o
        for b in w:
            b.lastw = o
            b.readers = {}
        self.ops[eng].append(o)
        self.last[eng] = o
        return o

    def barrier(self):
        lasts = [self.last[e] for e in ENGS if self.last[e] is not None] + list(self.lastd.values())
        for e in ENGS:
            self.pend[e] = list(lasts)

    def emit(self, block):
        for e in ENGS:
            c = 0
            for o in self.ops[e]:
                if o.dsem is None and o.sig:
                    c += 1
                    o.val = c

        def runner(ename):
            def f(eh):
                seen = {}
                for o in self.ops[ename]:
                    for d in o.deps:
                        if seen.get(d.key, 0) >= d.val:
                            continue
                        sem = d.dsem if d.dsem is not None else self.esem[d.eng]
                        eh.wait_ge(sem, d.val)
                        seen[d.key] = d.val
                    inst = o.fn(eh)
                    if inst is None:
                        continue
                    if o.dsem is not None:
                        inst.then_inc(o.dsem, 16)
                    elif o.sig:
                        inst.then_inc(self.esem[ename], 1)
            return f

        block.tensor(runner("pe"))
        block.scalar(runner("act"))
        block.vector(runner("dve"))
        block.gpsimd(runner("pool"))
        block.sync(runner("sp"))


class Rot:
    def __init__(self, items):
        self.items = items
        self.i = 0

    def next(self):
        it = self.items[self.i % len(self.items)]
        self.i += 1
        return it


WNAMES = {
    "wqa": (D, 512), "wka": (D, 512), "wva": (D, 512), "wfa": (D, 1024),
    "wqd": (D, 512), "wkd": (D, 512), "wqds": (D, 512), "wkds": (D, 512), "wvd": (D, 512),
    "wo0": (D, D), "fg": (D, DFF), "fu": (D, DFF), "fd": (DFF, D),
    "wq1": (D, D), "wk1": (D, D), "wv1": (D, D), "wog": (D, D), "wig": (D, D), "wfg": (D, D),
    "wo1": (D, D), "wr": (D, 8), "mg": (NE * 7 * D, 512), "mu": (NE * 7 * D, 512), "md": (NE * DFE, D),
    "lnp": (4 * 2 * 128, D), "gb": (128, 24), "ng": (128, 8), "wc": (128, 64),
    "ident": (128, 128), "masks": (128, 3 * 128), "mask4": (128, 3 * 512), "rope": (128, 2 * S), "augc": (128, 6),
    "cp": (128, 56), "tri": (128, 128), "eoff": (128, 128), "thr": (128, 64), "jc": (128, NT * 8),
}


def build_nc(nseq, debug=False, phases=5):
    nc = bass.Bass("TRN2", target_bir_lowering=False)
    dr = {}
    dr["x"] = nc.dram_tensor("x", [nseq * S, D], F32, kind="ExternalInput").ap()
    for k, shp in WNAMES.items():
        dr[k] = nc.dram_tensor(k, list(shp), F32, kind="ExternalInput").ap()
    out = nc.dram_tensor("out", [nseq * S, D], F32, kind="ExternalOutput").ap()
    hsp = nc.dram_tensor("hsp", [S, D], F32, kind="Internal").ap()
    h1sp = nc.dram_tensor("h1sp", [nseq * S, D], F32, kind="Internal").ap()
    XsH = [nc.dram_tensor("Xs%d" % i, [NROWS, 512], F32, kind="Internal").ap() for i in range(2)]
    YsH = [nc.dram_tensor("Ys%d" % i, [NROWS, 512], F32, kind="Internal").ap() for i in range(2)]
    dbg = None
    if debug:
        dbg = nc.dram_tensor("dbg", [4 * S, D], F32, kind="ExternalOutput").ap()
        dbg2 = nc.dram_tensor("dbg2", [2 * 128, KC * S], F32, kind="ExternalOutput").ap()

    with ExitStack() as es:
        p = Prog(nc, es)

        uid = [0]

        def sb(st, name, shape, dt):
            uid[0] += 1
            return st.enter_context(nc.sbuf_tensor("%s_s%d" % (name, uid[0]), shape, dt))

        def ps(st, name, shape, dt=F32):
            uid[0] += 1
            return st.enter_context(nc.psum_tensor("%s_p%d" % (name, uid[0]), shape, dt))

        h = sb(es, "h", [128, TB, D], F32)
        hB = [p.buf("h%d" % i, dma=True) for i in range(TB)]
        hs = h[:, :, :].rearrange("p a b -> p (a b)")
        hT = sb(es, "hT", [128, KC, S], BF16)
        hTB = [p.buf("hT%d" % i) for i in range(TB)]
        oT = sb(es, "oT", [128, KC, S], BF16)
        oTB = [p.buf("oT%d" % i) for i in range(KC)]
        ident = sb(es, "ident", [128, 128], BF16)
        identB = p.buf("ident", dma=True)
        masks = sb(es, "masks", [128, 3, 128], BF16)
        mask4 = sb(es, "mask4", [128, 3, 512], BF16)
        augc = sb(es, "augc", [128, 6], F32)
        gb = sb(es, "gb", [128, 24], F32)
        ngb = sb(es, "ngb", [128, 24], F32)
        ng = sb(es, "ng", [128, 8], F32)
        wc = sb(es, "wc", [128, 16, 4], F32)
        ones_bf = sb(es, "ones_bf", [128, S], BF16)
        onesf = sb(es, "onesf", [128, 128], F32)
        constB = p.buf("const", dma=True)
        const2B = p.buf("const2")

        p.op("pool", lambda e: e.dma_start(out=ident[:], in_=dr["ident"][:, :]), w=[identB], dsem=identB.dsem)
        p.op("pool", lambda e: e.dma_start(out=masks[:], in_=dr["masks"].rearrange("p (a b) -> p a b", a=3)), w=[constB], dsem=constB.dsem)
        p.op("pool", lambda e: e.dma_start(out=mask4[:], in_=dr["mask4"].rearrange("p (a b) -> p a b", a=3)), w=[constB], dsem=constB.dsem)
        p.op("sp", lambda e: e.dma_start(out=augc[:], in_=dr["augc"][:, :]), w=[constB], dsem=constB.dsem)
        p.op("sp", lambda e: e.dma_start(out=gb[:], in_=dr["gb"][:, :]), w=[constB], dsem=constB.dsem)
        p.op("sp", lambda e: e.dma_start(out=ng[:], in_=dr["ng"][:, :]), w=[constB], dsem=constB.dsem)
        p.op("sp", lambda e: e.dma_start(out=wc[:], in_=dr["wc"].rearrange("p (a b) -> p a b", b=4)), w=[constB], dsem=constB.dsem)
        p.op("dve", lambda e: e.memset(ones_bf[:], 1.0), w=[const2B])
        p.op("dve", lambda e: e.memset(onesf[:], 1.0 / 128.0), w=[const2B])
        p.op("dve", lambda e: e.tensor_scalar(out=ngb[:], in0=gb[:], scalar1=-1.0, scalar2=None, op0=ALU.mult), r=[constB], w=[const2B])
        CB = [constB, const2B, identB]
        tri = sb(es, "tri", [128, 128], BF16)
        eoff = sb(es, "eoff", [128, 128], F32)
        thr = sb(es, "thr", [128, 64], F32)
        jc = sb(es, "jc", [128, NT * 8], F32)
        posI = sb(es, "posI", [128, nseq * 32], I32); posB = p.buf("posI")
        gW = sb(es, "gW", [128, nseq * 32], F32); gWB = p.buf("gW")
        base = sb(es, "base", [128, 8], F32); baseB = p.buf("base")
        NBm = TS // 128
        XO, WO, DO = 0, NT * NBm, NT * NBm + NT * 56
        NTAB = DO + NT * 28
        tabI = sb(es, "tabI", [128, NTAB], I32); tabB = p.buf("tabI")
        cp = sb(es, "cp", [128, 56], F32)
        p.op("sp", lambda e: e.dma_start(out=cp[:], in_=dr["cp"][:, :]), w=[constB], dsem=constB.dsem)
        p.op("pool", lambda e: e.dma_start(out=tri[:], in_=dr["tri"][:, :]), w=[identB], dsem=identB.dsem)
        p.op("sp", lambda e: e.dma_start(out=eoff[:], in_=dr["eoff"][:, :]), w=[constB], dsem=constB.dsem)
        p.op("sp", lambda e: e.dma_start(out=thr[:], in_=dr["thr"][:, :]), w=[constB], dsem=constB.dsem)
        p.op("sp", lambda e: e.dma_start(out=jc[:], in_=dr["jc"][:, :]), w=[constB], dsem=constB.dsem)
        p.op("dve", lambda e: e.memset(base[:], 0.0), w=[baseB])
        IOA = bass.IndirectOffsetOnAxis

        def wview(name, rows_off=0, nrows=D):
            return dr[name][rows_off:rows_off + nrows, :].rearrange("(k p) n -> p k n", p=128)

        def mm(e, o, l, r_, st=True, sp=True):
            return e.matmul(o, l, r_, start=st, stop=sp)

        def build_hT(tb, L):
            xb, xbB = L["xb"].next()
            pst, pstB = L["pst"].next()
            p.op("act", lambda e: e.activation(out=xb[:], in_=h[:, tb, :], func=AF.Copy), r=[hB[tb]], w=[xbB])

            def tr(e):
                inst = None
                for kc in range(KC):
                    inst = e.transpose(pst[:, kc * 128:(kc + 1) * 128], xb[:, kc * 128:(kc + 1) * 128], ident[:])
                return inst
            p.op("pe", tr, r=[xbB, identB], w=[pstB])
            p.op("dve", lambda e: e.tensor_copy(out=hT[:, :, tb * 128:(tb + 1) * 128],
                                                in_=pst[:].rearrange("p (k t) -> p k t", k=KC)), r=[pstB], w=[hTB[tb]])

        def ln_alloc(st, tag):
            L = {}
            L["xb"] = Rot([(sb(st, "xb%s%d" % (tag, i), [128, D], BF16), p.buf("xb")) for i in range(2)])
            L["pst"] = Rot([(ps(st, "pst%s%d" % (tag, i), [128, D], BF16), p.buf("pst")) for i in range(2)])
            L["st6"] = Rot([(sb(st, "st6%s%d" % (tag, i), [128, 12], F32), p.buf("st6")) for i in range(2)])
            L["mv"] = Rot([(sb(st, "mv%s%d" % (tag, i), [128, 4], F32), p.buf("mv")) for i in range(2)])
            L["lnp"] = sb(st, "lnp%s" % tag, [128, 2, D], F32)
            L["lnpB"] = p.buf("lnp", dma=True)
            return L

        def ln_load(L, li):
            src = dr["lnp"][li * 256:(li + 1) * 256, :].rearrange("(a p) n -> p a n", a=2)
            p.op("sp", lambda e: e.dma_start(out=L["lnp"][:], in_=src), w=[L["lnpB"]], dsem=L["lnpB"].dsem)

        def ln_block(tb, L, to_hT=True, out_row0=None, dbg_row0=None, spill=False, gb_eng="pool"):
            st6, st6B = L["st6"].next()
            mv, mvB = L["mv"].next()
            lnp = L["lnp"]
            p.op("dve", lambda e: e.bn_stats(out=st6[:, 0:6], in_=h[:, tb, 0:512]), r=[hB[tb]], w=[st6B])
            p.op("dve", lambda e: e.bn_stats(out=st6[:, 6:12], in_=h[:, tb, 512:1024]), r=[hB[tb]], w=[st6B])
            p.op("dve", lambda e: e.bn_aggr(out=mv[:, 0:2], in_=st6[:]), r=[st6B], w=[mvB])
            p.op("act", lambda e: e.activation(out=mv[:, 2:3], in_=mv[:, 1:2], func=AF.Ln, bias=L["eps"][:, 0:1]), r=[mvB, const2B], w=[mvB])
            p.op("act", lambda e: e.activation(out=mv[:, 3:4], in_=mv[:, 2:3], func=AF.Exp, scale=-0.5), r=[mvB], w=[mvB])

            def fin():
                p.op("dve", lambda e: e.tensor_scalar(out=h[:, tb, :], in0=h[:, tb, :], scalar1=mv[:, 0:1], scalar2=mv[:, 3:4],
                                                      op0=ALU.subtract, op1=ALU.mult), r=[hB[tb], mvB], w=[hB[tb]])
                p.op(gb_eng, lambda e: e.tensor_tensor(out=h[:, tb, :], in0=h[:, tb, :], in1=lnp[:, 0, :], op=ALU.mult), r=[hB[tb], L["lnpB"]], w=[hB[tb]])
                p.op(gb_eng, lambda e: e.tensor_tensor(out=h[:, tb, :], in0=h[:, tb, :], in1=lnp[:, 1, :], op=ALU.add), r=[hB[tb], L["lnpB"]], w=[hB[tb]])
                if dbg_row0 is not None:
                    p.op("sp", lambda e: e.dma_start(out=dbg[dbg_row0 + tb * 128: dbg_row0 + (tb + 1) * 128, :], in_=h[:, tb, :]),
                         r=[hB[tb]], dsem=hB[tb].dsem)
                if spill:
                    p.op("sp", lambda e: e.dma_start(out=hsp[tb * 128:(tb + 1) * 128, :], in_=h[:, tb, :]), r=[hB[tb]], dsem=hB[tb].dsem)
                if out_row0 is not None:
                    p.op("sp", lambda e: e.dma_start(out=out[out_row0 + tb * 128: out_row0 + (tb + 1) * 128, :], in_=h[:, tb, :]),
                         r=[hB[tb]], dsem=hB[tb].dsem)
                elif to_hT:
                    build_hT(tb, L)
            return fin

        class LnPipe:
            def __init__(self):
                self.pend = None

            def push(self, fin):
                if self.pend is not None:
                    self.pend()
                self.pend = fin

            def flush(self):
                if self.pend is not None:
                    self.pend()
                self.pend = None

        epsT = sb(es, "epsT", [128, 1], F32)
        onesf_one = sb(es, "oneT", [128, 1], F32)
        p.op("dve", lambda e: e.memset(epsT[:], EPS), w=[const2B])
        p.op("dve", lambda e: e.memset(onesf_one[:], 1.0), w=[const2B])

        def wload(dst_ap, src_ap, B):
            return p.op("pool", lambda e: e.dma_start(out=dst_ap, in_=src_ap), w=[B], dsem=B.dsem)

        def proj_fm(W, wname, col0, evac, ncols=128):
            wt, wtB = W["wA"].next()
            wload(wt[:, :, 0:ncols], wview(wname)[:, :, col0:col0 + ncols], wtB)
            for tc in range(4):
                pp, ppB = W["pp"].next()

                def f(e, pp=pp, wt=wt, tc=tc):
                    inst = None
                    for kc in range(KC):
                        inst = mm(e, pp[0:ncols, :], wt[:, kc, 0:ncols], hT[:, kc, tc * 512:(tc + 1) * 512], kc == 0, kc == KC - 1)
                    return inst
                p.op("pe", f, r=[wtB] + hTB[4 * tc:4 * tc + 4], w=[ppB])
                evac(tc, pp, ppB)

        def proj_tm(W, wname, col0, ncols, sets, evac):
            wt, wtB = W["wA"].next()
            wload(wt[:, :, 0:ncols], wview(wname)[:, :, col0:col0 + ncols], wtB)
            for si, tsl in enumerate(sets):
                pp, ppB = W["pp"].next()

                def f(e, pp=pp, wt=wt, tsl=tsl):
                    inst = None
                    for kc in range(KC):
                        inst = mm(e, pp[:, 0:ncols], hT[:, kc, tsl], wt[:, kc, 0:ncols], kc == 0, kc == KC - 1)
                    return inst
                p.op("pe", f, r=[wtB] + hTB, w=[ppB])
                evac(si, pp, ppB)

        def do_seq(s):
            row0 = s * S
            def _ph1():
                with ExitStack() as st:
                    p.phase_begin()
                    L = ln_alloc(st, "p0")
                    L["eps"] = epsT
                    for tb in range(TB):
                        p.op("sp", lambda e, tb=tb, row0=row0: e.dma_start(out=h[:, tb, :], in_=dr["x"][row0 + tb * 128: row0 + (tb + 1) * 128, :]),
                             w=[hB[tb]], dsem=hB[tb].dsem)
                    for tb in range(TB):
                        build_hT(tb, L)
                    p.barrier()
            _ph1()
            if phases < 1:
                return

            def _ph2():
                with ExitStack() as st:
                    p.phase_begin()
                    W = {}
                    W["wA"] = Rot([(sb(st, "wA%d" % i, [128, KC, 128], BF16), p.buf("wA", dma=True)) for i in range(3)])
                    W["pp"] = Rot([(ps(st, "pp%d" % i, [128, 512]), p.buf("pp")) for i in range(2)])
                    qT = sb(st, "qT", [128, S], BF16); qTB = p.buf("qT")
                    kT = sb(st, "kT", [128, S], BF16); kTB = p.buf("kT")
                    Vt = sb(st, "Vt", [128, 3, TB, 128], BF16); VB = [p.buf("V%d" % i) for i in range(3)]
                    t0 = hs[:, 0:S]; t0B = p.buf("t0")
                    t1 = hs[:, S:2 * S]; t1B = p.buf("t1")
                    hi = sb(st, "hi", [4, S], BF16); lo = sb(st, "lo", [4, S], BF16); hlB = p.buf("hl")
                    augQ = sb(st, "augQ", [4, S], BF16); augQB = p.buf("augQ")
                    augK = sb(st, "augK", [4, S], BF16); augKB = p.buf("augK")
                    tq = sb(st, "tq", [4, S], BF16); tqB = p.buf("tq")
                    pT = Rot([(sb(st, "pT%d" % i, [128, 512], BF16), p.buf("pT")) for i in range(2)])
                    rec = hs[0:64, 2 * S:3 * S]; recB = p.buf("rec")
                    sts = Rot([(ps(st, "st%d" % i, [128, 512]), p.buf("st")) for i in range(2)])
                    W["pp"] = Rot(W["pp"].items + sts.items)
                    accn = Rot([(ps(st, "accn%d" % i, [64, 512]), p.buf("accn")) for i in range(2)])
                    accd = Rot([(ps(st, "accd%d" % i, [64, 512]), p.buf("accd")) for i in range(2)])
                    rope = hs[:, 3 * S:5 * S].rearrange("p (a b) -> p a b", a=2); ropeB = p.buf("rope", dma=True)
                    rt = Rot([(hs[:, 5 * S + i * 1024:5 * S + (i + 1) * 1024].rearrange("p (a b) -> p a b", a=2), p.buf("rt")) for i in range(2)])
                    an = hs[0:64, 6 * S:7 * S]; anB = p.buf("an")
                    ad = hs[0:64, 7 * S:8 * S]; adB = p.buf("ad")
                    p.op("sp", lambda e: e.dma_start(out=rope, in_=dr["rope"].rearrange("p (a b) -> p a b", a=2)), w=[ropeB], dsem=ropeB.dsem)

                    def make_aug(src_ap, srcB):
                        p.op("dve", lambda e: e.tensor_copy(out=hi[:], in_=src_ap), r=[srcB], w=[hlB])
                        p.op("dve", lambda e: e.tensor_tensor(out=lo[:], in0=src_ap, in1=hi[:], op=ALU.subtract), r=[srcB, hlB], w=[hlB])

                    def fin_aug(dst, dstB, c0):
                        p.op("dve", lambda e: e.tensor_scalar(out=tq[:], in0=hi[:], scalar1=augc[0:4, c0:c0 + 1], scalar2=augc[0:4, c0 + 2:c0 + 3],
                                                              op0=ALU.mult, op1=ALU.add), r=[hlB] + CB, w=[tqB])
                        p.op("dve", lambda e: e.scalar_tensor_tensor(out=dst[:], in0=lo[:], scalar=augc[0:4, c0 + 1:c0 + 2], in1=tq[:],
                                                                     op0=ALU.mult, op1=ALU.add), r=[hlB, tqB] + CB, w=[dstB])

                    for c in range(4):
                        def ev_q(tc, pp, ppB):
                            p.op("act", lambda e: e.activation(out=qT[:, tc * 512:(tc + 1) * 512], in_=pp[:], func=AF.Copy, scale=0.125), r=[ppB], w=[qTB])

                        def ev_k(tc, pp, ppB):
                            p.op("dve", lambda e: e.tensor_copy(out=kT[:, tc * 512:(tc + 1) * 512], in_=pp[:]), r=[ppB], w=[kTB])

                        def ev_v(si, pp, ppB):
                            p.op("act", lambda e: e.activation(out=Vt[:, 0, si, :], in_=pp[:, 0:128], func=AF.Copy), r=[ppB], w=[VB[0]])
                        proj_fm(W, "wqa", c * 128, ev_q)
                        proj_fm(W, "wka", c * 128, ev_k)
                        proj_tm(W, "wva", c * 128, 128, [slice(tb * 128, (tb + 1) * 128) for tb in range(TB)], ev_v)
                        for hh in range(2):
                            head = 2 * c + hh
                            r0 = 64 * hh

                            def ev_g(tc, pp, ppB, head=head):
                                p.op("act", lambda e: e.activation(out=t0[:, tc * 512:(tc + 1) * 512], in_=pp[:], func=AF.Exp,
                                                                   bias=ngb[:, head:head + 1], scale=-1.0), r=[ppB] + CB, w=[t0B])
                            proj_fm(W, "wfa", head * 128, ev_g)
                            p.op("act", lambda e: e.activation(out=t0[:], in_=t0[:], func=AF.Ln, bias=onesf_one[:, 0:1]), r=[t0B] + CB, w=[t0B])
                            p.op("dve", lambda e: e.tensor_tensor_scan(out=t1[:], data0=ones_bf[:], data1=t0[:], initial=0.0,
                                                                       op0=ALU.mult, op1=ALU.add), r=[t0B] + CB, w=[t1B])
                            make_aug(t1[0:4, :], t1B)
                            fin_aug(augQ, augQB, 0)
                            fin_aug(augK, augKB, 3)
                            for qc in range(4):
                                an_, anB_ = accn.next()
                                ad_, adB_ = accd.next()
                                nkb = 4 * qc + 4
                                for kb in range(nkb):
                                    j0 = max(0, kb - 4 * qc)
                                    c0 = j0 * 128
                                    stt, sttB = sts.next()
                                    pt, ptB = pT.next()
                                    diag = kb >= 4 * qc

                                    def fs(e, stt=stt, kb=kb, qc=qc, c0=c0, diag=diag, r0=r0):
                                        kblk = slice(kb * 128, (kb + 1) * 128)
                                        q0 = qc * 512
                                        inst = None
                                        if diag:
                                            qs = slice(q0 + c0, q0 + c0 + 128)
                                            mm(e, stt[:, c0:c0 + 128], kT[r0:r0 + 64, kblk], qT[r0:r0 + 64, qs], True, False)
                                            mm(e, stt[:, c0:c0 + 128], augK[0:4, kblk], augQ[0:4, qs], False, False)
                                            inst = mm(e, stt[:, c0:c0 + 128], ident[:], masks[:, 0, :], False, True)
                                            c1 = c0 + 128
                                        else:
                                            c1 = c0
                                        if c1 < 512:
                                            qs = slice(q0 + c1, q0 + 512)
                                            mm(e, stt[:, c1:512], kT[r0:r0 + 64, kblk], qT[r0:r0 + 64, qs], True, False)
                                            inst = mm(e, stt[:, c1:512], augK[0:4, kblk], augQ[0:4, qs], False, True)
                                        return inst
                                    p.op("pe", fs, r=[kTB, qTB, augKB, augQB] + CB, w=[sttB])
                                    p.op("act", lambda e, stt=stt, pt=pt, c0=c0: e.activation(out=pt[:, c0:512], in_=stt[:, c0:512], func=AF.Exp),
                                         r=[sttB], w=[ptB])

                                    def fpv(e, pt=pt, kb=kb, c0=c0, hh=hh, an_=an_, ad_=ad_, nkb=nkb):
                                        mm(e, an_[:, c0:512], Vt[:, 0, kb, hh * 64:(hh + 1) * 64], pt[:, c0:512], kb == 0, kb == nkb - 1)
                                        return mm(e, ad_[:, c0:512], ones_bf[:, 0:64], pt[:, c0:512], kb == 0, kb == nkb - 1)
                                    p.op("pe", fpv, r=[ptB, VB[0]] + CB, w=[anB_, adB_])
                                p.op("dve", lambda e, ad_=ad_, qc=qc: e.reciprocal(out=rec[:, qc * 512:(qc + 1) * 512], in_=ad_[:]), r=[adB_], w=[recB])
                                p.op("dve", lambda e, an_=an_, qc=qc, r0=r0, c=c: e.tensor_tensor(
                                    out=oT[r0:r0 + 64, c, qc * 512:(qc + 1) * 512], in0=an_[:], in1=rec[:, qc * 512:(qc + 1) * 512], op=ALU.mult),
                                    r=[anB_, recB], w=[oTB[c]])

                    def tokset(bi, si):
                        if bi == 0:
                            return slice(si * 128, (si + 1) * 128)
                        if bi == 1:
                            r_, n_ = si // 4, si % 4
                            return slice(512 * n_ + r_, 512 * (n_ + 1), 4)
                        return slice(si, S, 16)

                    def accview(t, bi, si):
                        if bi == 0:
                            return t[:, :].rearrange("p (n j) -> p n j", j=128)[:, si:si + 2, :]
                        if bi == 1:
                            r_, n_ = si // 4, si % 4
                            return t[:, :].rearrange("p (n j r) -> p n j r", n=4, j=128, r=4)[:, n_:n_ + 2, :, r_]
                        return t[:, :].rearrange("p (j r) -> p r j", r=16)[:, si:si + 2, :]

                    for c in range(4):
                        def mk_rope(dst, dstB, wn, wns, c=c):
                            store = {}

                            def ev_a(tc, pp, ppB):
                                rtt, rtB = rt.next()
                                store[tc] = (rtt, rtB)
                                p.op("dve", lambda e: e.tensor_tensor(out=rtt[:, 0, :], in0=pp[:], in1=rope[:, 0, tc * 512:(tc + 1) * 512], op=ALU.mult),
                                     r=[ppB, ropeB], w=[rtB])

                            def ev_b(tc, pp, ppB):
                                rtt, rtB = store[tc]
                                p.op("dve", lambda e: e.tensor_tensor(out=rtt[:, 1, :], in0=pp[:], in1=rope[:, 1, tc * 512:(tc + 1) * 512], op=ALU.mult),
                                     r=[ppB, ropeB], w=[rtB])
                                p.op("pool", lambda e: e.tensor_tensor(out=dst[:, tc * 512:(tc + 1) * 512], in0=rtt[:, 0, :], in1=rtt[:, 1, :], op=ALU.add),
                                     r=[rtB], w=[dstB])
                            wa, waB = W["wA"].next()
                            wb, wbB = W["wA"].next()
                            wload(wa[:], wview(wn)[:, :, c * 128:(c + 1) * 128], waB)
                            wload(wb[:], wview(wns)[:, :, c * 128:(c + 1) * 128], wbB)
                            for tc in range(4):
                                for (wt_, wtB_, ev) in ((wa, waB, ev_a), (wb, wbB, ev_b)):
                                    pp, ppB = W["pp"].next()

                                    def f(e, pp=pp, wt_=wt_, tc=tc):
                                        inst = None
                                        for kc in range(KC):
                                            inst = mm(e, pp[:], wt_[:, kc, :], hT[:, kc, tc * 512:(tc + 1) * 512], kc == 0, kc == KC - 1)
                                        return inst
                                    p.op("pe", f, r=[wtB_] + hTB[4 * tc:4 * tc + 4], w=[ppB])
                                    ev(tc, pp, ppB)
                        mk_rope(qT, qTB, "wqd", "wqds")
                        mk_rope(kT, kTB, "wkd", "wkds")
                        for bi in range(3):
                            def ev_v(si, pp, ppB, bi=bi):
                                p.op("act", lambda e: e.activation(out=Vt[:, bi, si, :], in_=pp[:, 0:128], func=AF.Copy), r=[ppB], w=[VB[bi]])
                            proj_tm(W, "wvd", c * 128, 128, [tokset(bi, si) for si in range(16)], ev_v)
                        for hh in range(2):
                            r0 = 64 * hh
                            for bi in range(3):
                                for si in range(0, 16, 2):
                                    blocks = []
                                    for sq in (si, si + 1):
                                        if bi == 0:
                                            prev = sq - 1 if sq >= 1 else None
                                        elif bi == 1:
                                            prev = sq - 1 if (sq % 4) >= 1 else None
                                        else:
                                            prev = None
                                        blocks.append((sq, prev if prev is not None else sq))
                                        blocks.append((sq, sq))
                                    if bi == 2:
                                        mi = 2
                                    elif (bi == 0 and si == 0) or (bi == 1 and si % 4 == 0):
                                        mi = 1
                                    else:
                                        mi = 0
                                    stt, sttB = sts.next()
                                    pt, ptB = pT.next()
                                    an_, anB_ = accn.next()
                                    ad_, adB_ = accd.next()

                                    def fs(e, stt=stt, blocks=blocks, mi=mi, bi=bi, r0=r0):
                                        inst = None
                                        for bk, (sq, sk) in enumerate(blocks):
                                            mm(e, stt[:, bk * 128:(bk + 1) * 128], kT[r0:r0 + 64, tokset(bi, sk)], qT[r0:r0 + 64, tokset(bi, sq)], True, False)
                                            inst = mm(e, stt[:, bk * 128:(bk + 1) * 128], ident[:], mask4[:, mi, bk * 128:(bk + 1) * 128], False, True)
                                        return inst
                                    p.op("pe", fs, r=[kTB, qTB] + CB, w=[sttB])
                                    p.op("act", lambda e, stt=stt, pt=pt: e.activation(out=pt[:], in_=stt[:], func=AF.Exp, scale=0.125), r=[sttB], w=[ptB])

                                    def fpv(e, pt=pt, blocks=blocks, bi=bi, hh=hh, an_=an_, ad_=ad_):
                                        inst = None
                                        for bk, (sq, sk) in enumerate(blocks):
                                            qi = bk // 2
                                            first = (bk % 2 == 0)
                                            mm(e, an_[:, qi * 128:(qi + 1) * 128], Vt[:, bi, sk, hh * 64:(hh + 1) * 64], pt[:, bk * 128:(bk + 1) * 128], first, not first)
                                            inst = mm(e, ad_[:, qi * 128:(qi + 1) * 128], ones_bf[:, 0:64], pt[:, bk * 128:(bk + 1) * 128], first, not first)
                                        return inst
                                    p.op("pe", fpv, r=[ptB, VB[bi]] + CB, w=[anB_, adB_])
                                    pv = lambda t: t[:, 0:256].rearrange("p (a j) -> p a j", a=2)
                                    if bi == 0:
                                        p.op("dve", lambda e, an_=an_, si=si: e.tensor_copy(out=accview(an, 0, si), in_=pv(an_)), r=[anB_], w=[anB])
                                        p.op("dve", lambda e, ad_=ad_, si=si: e.tensor_copy(out=accview(ad, 0, si), in_=pv(ad_)), r=[adB_], w=[adB])
                                    else:
                                        p.op("dve", lambda e, an_=an_, si=si, bi=bi: e.tensor_tensor(out=accview(an, bi, si), in0=pv(an_), in1=accview(an, bi, si), op=ALU.add),
                                             r=[anB_, anB], w=[anB])
                                        p.op("dve", lambda e, ad_=ad_, si=si, bi=bi: e.tensor_tensor(out=accview(ad, bi, si), in0=pv(ad_), in1=accview(ad, bi, si), op=ALU.add),
                                             r=[adB_, adB], w=[adB])
                            p.op("dve", lambda e: e.reciprocal(out=rec[:], in_=ad[:]), r=[adB], w=[recB])
                            p.op("dve", lambda e, r0=r0, c=c: e.tensor_tensor(out=oT[r0:r0 + 64, 4 + c, :], in0=an[:], in1=rec[:], op=ALU.mult),
                                 r=[anB, recB], w=[oTB[4 + c]])
                    if debug and s == 0:
                        dB = p.buf("dbg2", dma=True)
                        p.op("pool", lambda e: e.dma_start(out=dbg2[0:128, :].rearrange("p (a b) -> p a b", a=KC), in_=oT[:]), r=oTB, dsem=dB.dsem)
                        p.op("pool", lambda e: e.dma_start(out=dbg2[128:256, 0:S], in_=qT[:]), r=[qTB], dsem=dB.dsem)
                        p.op("pool", lambda e: e.dma_start(out=dbg2[128:256, S:2 * S], in_=kT[:]), r=[kTB], dsem=dB.dsem)
                        p.op("pool", lambda e: e.dma_start(out=dbg2[128:192, 2 * S:3 * S], in_=an), r=[anB], dsem=dB.dsem)
                        p.op("pool", lambda e: e.dma_start(out=dbg2[128:192, 3 * S:4 * S], in_=ad), r=[adB], dsem=dB.dsem)
                        p.op("pool", lambda e: e.dma_start(out=dbg2[128:256, 4 * S:5 * S], in_=Vt[:, 1, :, :].rearrange("p a b -> p (a b)")), r=VB, dsem=dB.dsem)
                    p.barrier()

            _ph2()
            def mix_stage(wname, li, dbg_i, row0=row0):
                with ExitStack() as st:
                    p.phase_begin()
                    L = ln_alloc(st, "m%d" % li)
                    L["eps"] = epsT
                    ln_load(L, li)
                    for tb in range(TB):
                        src = dr["x"][row0 + tb * 128: row0 + (tb + 1) * 128, :] if li == 0 else hsp[tb * 128:(tb + 1) * 128, :]
                        p.op("sp", lambda e, tb=tb, src=src: e.dma_start(out=h[:, tb, :], in_=src), w=[hB[tb]], dsem=hB[tb].dsem)
                    wo = sb(st, "wo", [128, KC, D], BF16)
                    woB = p.buf("wo", dma=True)
                    wload(wo[:, :, 0:512], wview(wname)[:, :, 0:512], woB)
                    wload(wo[:, :, 512:1024], wview(wname)[:, :, 512:1024], woB)
                    mixp = Rot([(ps(st, "mix%d" % i, [128, D]), p.buf("mix")) for i in range(2)])
                    lp = LnPipe()
                    for tb in range(TB):
                        mp, mpB = mixp.next()

                        def f(e, mp=mp, tb=tb):
                            inst = None
                            for half in range(2):
                                for c in range(KC):
                                    inst = mm(e, mp[:, half * 512:(half + 1) * 512], oT[:, c, tb * 128:(tb + 1) * 128],
                                              wo[:, c, half * 512:(half + 1) * 512], c == 0, c == KC - 1)
                            return inst
                        p.op("pe", f, r=[woB] + oTB, w=[mpB])
                        p.op("dve", lambda e, mp=mp, tb=tb: e.scalar_tensor_tensor(out=h[:, tb, :], in0=h[:, tb, :], scalar=ALPHA, in1=mp[:],
                                                                                   op0=ALU.mult, op1=ALU.add), r=[hB[tb], mpB], w=[hB[tb]])
                        lp.push(ln_block(tb, L, to_hT=True, dbg_row0=(dbg_i * S if (debug and s == 0) else None)))
                    lp.flush()
                    p.barrier()
            mix_stage("wo0", 0, 0)
            if phases < 2:
                return

            def ffn_alloc(st):
                Fd = {}
                Fd["wg"] = Rot([(sb(st, "wg%d" % i, [128, KC, 512], BF16), p.buf("wg", dma=True)) for i in range(2)])
                Fd["wu"] = Rot([(sb(st, "wu%d" % i, [128, KC, 512], BF16), p.buf("wu", dma=True)) for i in range(2)])
                Fd["wd"] = Rot([(sb(st, "wd%d" % i, [128, 4, D], BF16), p.buf("wd", dma=True)) for i in range(2)])
                Fd["aT"] = Rot([(oT[:, 4 * i:4 * i + 4, :], p.buf("aT")) for i in range(2)])
                Fd["sg"] = Rot([(sb(st, "sg%d" % i, [128, 512], F32), p.buf("sg")) for i in range(2)])
                Fd["pg"] = Rot([(ps(st, "pg%d" % i, [128, 512]), p.buf("pg")) for i in range(2)])
                Fd["pu"] = Rot([(ps(st, "pu%d" % i, [128, 512]), p.buf("pu")) for i in range(2)])
                Fd["yp"] = Rot([(ps(st, "yp%d" % i, [128, D]), p.buf("yp")) for i in range(2)])
                return Fd

            def ffn(Fd, gname, uname, dname, grow0, drow0, F, gate_fn):
                f0 = 0
                while f0 < F:
                    gw = min(512, F - f0)
                    gc = gw // 128
                    wg, wgB = Fd["wg"].next()
                    wu, wuB = Fd["wu"].next()
                    wd, wdB = Fd["wd"].next()
                    aT, aTB = Fd["aT"].next()
                    wload(wg[:, :, 0:gw], wview(gname, grow0, D)[:, :, f0:f0 + gw], wgB)
                    wload(wu[:, :, 0:gw], wview(uname, grow0, D)[:, :, f0:f0 + gw], wuB)
                    wload(wd[:, 0:gc, :], wview(dname, drow0 + f0, gw), wdB)
                    for ci in range(gc):
                        for tc in range(4):
                            pg, pgB = Fd["pg"].next()
                            pu, puB = Fd["pu"].next()
                            sg, sgB = Fd["sg"].next()

                            def fg_(e, pg=pg, wg=wg, ci=ci, tc=tc):
                                inst = None
                                for kc in range(KC):
                                    inst = mm(e, pg[:], wg[:, kc, ci * 128:(ci + 1) * 128], hT[:, kc, tc * 512:(tc + 1) * 512], kc == 0, kc == KC - 1)
                                return inst

                            def fu_(e, pu=pu, wu=wu, ci=ci, tc=tc):
                                inst = None
                                for kc in range(KC):
                                    inst = mm(e, pu[:], wu[:, kc, ci * 128:(ci + 1) * 128], hT[:, kc, tc * 512:(tc + 1) * 512], kc == 0, kc == KC - 1)
                                return inst
                            p.op("pe", fg_, r=[wgB] + hTB[4 * tc:4 * tc + 4], w=[pgB])
                            p.op("pe", fu_, r=[wuB] + hTB[4 * tc:4 * tc + 4], w=[puB])
                            p.op("act", lambda e, sg=sg, pg=pg: e.activation(out=sg[:], in_=pg[:], func=AF.Silu), r=[pgB], w=[sgB])
                            p.op("dve", lambda e, sg=sg, pu=pu, aT=aT, ci=ci, tc=tc: e.tensor_tensor(
                                out=aT[:, ci, tc * 512:(tc + 1) * 512], in0=pu[:], in1=sg[:], op=ALU.mult), r=[puB, sgB], w=[aTB])
                    for tb in range(TB):
                        yp, ypB = Fd["yp"].next()

                        def fd_(e, yp=yp, aT=aT, wd=wd, tb=tb, gc=gc):
                            inst = None
                            for half in range(2):
                                for ci in range(gc):
                                    inst = mm(e, yp[:, half * 512:(half + 1) * 512], aT[:, ci, tb * 128:(tb + 1) * 128],
                                              wd[:, ci, half * 512:(half + 1) * 512], ci == 0, ci == gc - 1)
                            return inst
                        p.op("pe", fd_, r=[aTB, wdB], w=[ypB])
                        g_ap, gBs = gate_fn(tb)
                        p.op("dve", lambda e, yp=yp, tb=tb, g_ap=g_ap: e.scalar_tensor_tensor(out=h[:, tb, :], in0=yp[:], scalar=g_ap, in1=h[:, tb, :],
                                                                                            op0=ALU.mult, op1=ALU.add), r=[ypB, hB[tb]] + gBs, w=[hB[tb]])
                    f0 += gw

            def scale_h():
                for tb in range(TB):
                    p.op("act", lambda e, tb=tb: e.mul(out=h[:, tb, :], in_=h[:, tb, :], mul=ALPHA), r=[hB[tb]], w=[hB[tb]])

            def final_ln(li, dbg_i, to_hT, out_row0, spill=False):
                outs = []
                with ExitStack() as st:
                    p.phase_begin()
                    L = ln_alloc(st, "f%d" % li)
                    L["eps"] = epsT
                    ln_load(L, li)
                    lp = LnPipe()
                    for tb in range(TB):
                        lp.push(ln_block(tb, L, to_hT=to_hT, out_row0=out_row0, dbg_row0=(dbg_i * S if (debug and s == 0) else None), spill=spill))
                    lp.flush()
                    p.barrier()
                return outs

            def _ph3():
                with ExitStack() as st:
                    p.phase_begin()
                    Fd = ffn_alloc(st)
                    scale_h()
                    ffn(Fd, "fg", "fu", "fd", 0, 0, DFF, lambda tb: (1.0, []))
                    p.barrier()
            _ph3()
            final_ln(1, 1, True, None, spill=True)
            if phases < 3:
                return

            def _ph4():
                with ExitStack() as st:
                    p.phase_begin()
                    W = {}
                    W["wA"] = Rot([(sb(st, "wA%d" % i, [128, KC, 128], BF16), p.buf("wA", dma=True)) for i in range(3)])
                    qT = sb(st, "qT", [128, S], BF16); qTB = p.buf("qT")
                    kT = sb(st, "kT", [128, S], BF16); kTB = p.buf("kT")
                    Vh = sb(st, "Vh", [128, TB, 128], BF16); VhB = p.buf("Vh")
                    sgo = sb(st, "sgo", [128, S], BF16); sgoB = p.buf("sgo")
                    pre = hs[:, 0:3 + S]; preB = p.buf("pre"); padB = p.buf("pad")
                    yv = hs[:, 2052:2052 + S]; yvB = p.buf("yv")
                    t0 = hs[:, 3:3 + S]; t0B = preB
                    t1 = hs[:, 4100:4100 + S]; t1B = p.buf("t1")
                    t2 = yv; t2B = yvB
                    hi = sb(st, "hi", [4, S], BF16); lo = sb(st, "lo", [4, S], BF16); hlB = p.buf("hl")
                    ua = hs[0:4, 9220:9220 + S]; uaB = p.buf("ua")
                    augQ = sb(st, "augQ", [4, S], BF16); augQB = p.buf("augQ")
                    augK = sb(st, "augK", [4, S], BF16); augKB = p.buf("augK")
                    tq = sb(st, "tq", [4, S], BF16); tqB = p.buf("tq")
                    pT = Rot([(sb(st, "pT%d" % i, [128, 512], BF16), p.buf("pT")) for i in range(3)])
                    Et = Rot([(hs[:, 12288 + i * 512:12288 + (i + 1) * 512], p.buf("Et")) for i in range(3)])
                    psA = Rot([(ps(st, "psA%d" % i, [128, 512]), p.buf("psA")) for i in range(3)])
                    psB = Rot([(ps(st, "psB%d" % i, [128, 512]), p.buf("psB")) for i in range(3)])
                    accn_t = ps(st, "accn", [128, 512]); accnB = p.buf("accn")
                    accd_t = ps(st, "accd", [128, 512]); accdB = p.buf("accd")
                    W["pp"] = Rot(psA.items + psB.items)
                    f1 = hs[:, 7172:7684]; f1B = p.buf("f1")
                    f2 = hs[:, 7684:8196]; f2B = p.buf("f2")
                    f3 = hs[:, 8196:8708]; f3B = p.buf("f3")
                    f4 = hs[:, 8708:9220]; f4B = p.buf("f4")
                    p.op("dve", lambda e: e.memset(pre[:, 0:3], 0.0), w=[padB])

                    def make_aug(src_ap, srcB):
                        p.op("dve", lambda e: e.tensor_copy(out=hi[:], in_=src_ap), r=[srcB], w=[hlB])
                        p.op("dve", lambda e: e.tensor_tensor(out=lo[:], in0=src_ap, in1=hi[:], op=ALU.subtract), r=[srcB, hlB], w=[hlB])

                    def fin_aug(dst, dstB, c0):
                        p.op("dve", lambda e: e.tensor_scalar(out=tq[:], in0=hi[:], scalar1=augc[0:4, c0:c0 + 1], scalar2=augc[0:4, c0 + 2:c0 + 3],
                                                              op0=ALU.mult, op1=ALU.add), r=[hlB] + CB, w=[tqB])
                        p.op("dve", lambda e: e.scalar_tensor_tensor(out=dst[:], in0=lo[:], scalar=augc[0:4, c0 + 1:c0 + 2], in1=tq[:],
                                                                     op0=ALU.mult, op1=ALU.add), r=[hlB, tqB] + CB, w=[dstB])

                    for hd in range(8):
                        def conv_silu(wname, col0, chunk, dst, dstB):
                            def ev(tc, pp, ppB):
                                p.op("act", lambda e: e.activation(out=pre[:, 3 + tc * 512: 3 + (tc + 1) * 512], in_=pp[:], func=AF.Copy), r=[ppB], w=[preB])
                            proj_fm(W, wname, col0, ev)
                            p.op("dve", lambda e: e.tensor_scalar(out=yv[:], in0=pre[:, 3:3 + S], scalar1=wc[:, chunk, 3:4], scalar2=None, op0=ALU.mult),
                                 r=[preB, padB] + CB, w=[yvB])
                            for i in range(3):
                                p.op("dve", lambda e, i=i: e.scalar_tensor_tensor(out=yv[:], in0=pre[:, i:i + S], scalar=wc[:, chunk, i:i + 1], in1=yv[:],
                                                                                 op0=ALU.mult, op1=ALU.add), r=[preB, padB, yvB] + CB, w=[yvB])
                            p.op("act", lambda e: e.activation(out=dst[:], in_=yv[:], func=AF.Silu), r=[yvB], w=[dstB])
                        conv_silu("wq1", hd * 128, hd, qT, qTB)
                        conv_silu("wk1", hd * 128, 8 + hd, kT, kTB)

                        def ev_v(si, pp, ppB):
                            p.op("act", lambda e: e.activation(out=Vh[:, si, :], in_=pp[:, 0:128], func=AF.Copy), r=[ppB], w=[VhB])
                        proj_tm(W, "wv1", hd * 128, 128, [slice(tb * 128, (tb + 1) * 128) for tb in range(TB)], ev_v)

                        def ev_og(tc, pp, ppB):
                            p.op("act", lambda e: e.activation(out=sgo[:, tc * 512:(tc + 1) * 512], in_=pp[:], func=AF.Sigmoid), r=[ppB], w=[sgoB])
                        proj_fm(W, "wog", hd * 128, ev_og)

                        def ev_f(tc, pp, ppB, hd=hd):
                            p.op("act", lambda e: e.activation(out=t0[:, tc * 512:(tc + 1) * 512], in_=pp[:], func=AF.Exp,
                                                               bias=ngb[:, 16 + hd:17 + hd], scale=-1.0), r=[ppB] + CB, w=[t0B])
                        proj_fm(W, "wfg", hd * 128, ev_f)
                        p.op("act", lambda e: e.activation(out=t0[:], in_=t0[:], func=AF.Ln, bias=onesf_one[:, 0:1]), r=[t0B] + CB, w=[t0B])
                        p.op("dve", lambda e: e.tensor_tensor_scan(out=t1[:], data0=ones_bf[:], data1=t0[:], initial=0.0, op0=ALU.mult, op1=ALU.add),
                             r=[t0B] + CB, w=[t1B])

                        def ev_i(tc, pp, ppB, hd=hd):
                            p.op("dve", lambda e: e.scalar_tensor_tensor(out=t0[:, tc * 512:(tc + 1) * 512], in0=pp[:], scalar=gb[:, 8 + hd:9 + hd],
                                                                         in1=t1[:, tc * 512:(tc + 1) * 512], op0=ALU.add, op1=ALU.add),
                                 r=[ppB, t1B] + CB, w=[t0B])
                        proj_fm(W, "wig", hd * 128, ev_i)
                        p.op("dve", lambda e: e.tensor_tensor_scan(out=t2[:], data0=t0[:], data1=t0[:], initial=-1e30, op0=ALU.max, op1=ALU.max),
                             r=[t0B] + CB, w=[t2B])
                        p.op("dve", lambda e: e.tensor_tensor(out=t1[:], in0=t1[:], in1=t2[:], op=ALU.subtract), r=[t1B, t2B], w=[t1B])
                        p.op("act", lambda e: e.activation(out=t1[:], in_=t1[:], func=AF.Exp), r=[t1B], w=[t1B])
                        make_aug(t2[0:4, :], t2B)
                        fin_aug(augQ, augQB, 0)
                        p.op("dve", lambda e: e.tensor_scalar(out=ua[:], in0=t0[0:4, :], scalar1=LNS, scalar2=None, op0=ALU.add), r=[t0B], w=[uaB])
                        make_aug(ua[:], uaB)
                        fin_aug(augK, augKB, 3)

                        def stage1(qc, kb):
                            nkb = 4 * qc + 4
                            j0 = max(0, kb - 4 * qc)
                            c0 = j0 * 128
                            diag = kb >= 4 * qc
                            pa, paB = psA.next()
                            pb, pbB = psB.next()
                            pt, ptB = pT.next()
                            et, etB = Et.next()
                            kblk = slice(kb * 128, (kb + 1) * 128)
                            qs = slice(qc * 512 + c0, qc * 512 + 512)
                            p.op("pe", lambda e: mm(e, pa[:, c0:512], kT[:, kblk], qT[:, qs], True, True), r=[kTB, qTB], w=[paB])

                            def fd_(e):
                                q0 = qc * 512
                                inst = None
                                c1 = c0
                                if diag:
                                    qs1 = slice(q0 + c0, q0 + c0 + 128)
                                    mm(e, pb[:, c0:c0 + 128], augK[0:4, kblk], augQ[0:4, qs1], True, False)
                                    inst = mm(e, pb[:, c0:c0 + 128], ident[:], masks[:, 0, :], False, True)
                                    c1 = c0 + 128
                                if c1 < 512:
                                    inst = mm(e, pb[:, c1:512], augK[0:4, kblk], augQ[0:4, slice(q0 + c1, q0 + 512)], True, True)
                                return inst
                            p.op("pe", fd_, r=[augKB, augQB] + CB, w=[pbB])
                            p.op("act", lambda e: e.activation(out=et[:, c0:512], in_=pb[:, c0:512], func=AF.Exp), r=[pbB], w=[etB])
                            p.op("dve", lambda e: e.tensor_tensor(out=pt[:, c0:512], in0=pa[:, c0:512], in1=et[:, c0:512], op=ALU.mult), r=[paB, etB], w=[ptB])
                            return (qc, kb, nkb, c0, pt, ptB)

                        def finalize(qc, hd=hd):
                            cs = slice(qc * 512, (qc + 1) * 512)
                            s1, s1B = W["pp"].next()
                            s2, s2B = W["pp"].next()
                            p.op("act", lambda e: e.activation(out=f1[:], in_=accd_t[:], func=AF.Abs), r=[accdB], w=[f1B])
                            p.op("dve", lambda e: e.tensor_tensor(out=f1[:], in0=f1[:], in1=t1[:, cs], op=ALU.max), r=[f1B, t1B], w=[f1B])
                            p.op("dve", lambda e: e.reciprocal(out=f1[:], in_=f1[:]), r=[f1B], w=[f1B])
                            p.op("dve", lambda e: e.tensor_tensor(out=f2[:], in0=accn_t[:], in1=f1[:], op=ALU.mult), r=[accnB, f1B], w=[f2B])
                            p.op("act", lambda e: e.activation(out=f3[:], in_=f2[:], func=AF.Square), r=[f2B], w=[f3B])
                            p.op("pe", lambda e: mm(e, s1[:], onesf[:], f2[:], True, True), r=[f2B] + CB, w=[s1B])
                            p.op("act", lambda e: e.activation(out=f1[:], in_=s1[:], func=AF.Copy), r=[s1B], w=[f1B])
                            p.op("pe", lambda e: mm(e, s2[:], onesf[:], f3[:], True, True), r=[f3B] + CB, w=[s2B])
                            p.op("dve", lambda e: e.tensor_tensor(out=f4[:], in0=f1[:], in1=f1[:], op=ALU.mult), r=[f1B], w=[f4B])
                            p.op("dve", lambda e: e.tensor_tensor(out=f4[:], in0=s2[:], in1=f4[:], op=ALU.subtract), r=[s2B, f4B], w=[f4B])
                            p.op("act", lambda e: e.activation(out=f4[:], in_=f4[:], func=AF.Ln, bias=epsT[:, 0:1]), r=[f4B] + CB, w=[f4B])
                            p.op("act", lambda e: e.activation(out=f4[:], in_=f4[:], func=AF.Exp, scale=-0.5), r=[f4B], w=[f4B])
                            p.op("dve", lambda e: e.tensor_tensor(out=f2[:], in0=f2[:], in1=f1[:], op=ALU.subtract), r=[f2B, f1B], w=[f2B])
                            p.op("dve", lambda e: e.tensor_tensor(out=f2[:], in0=f2[:], in1=f4[:], op=ALU.mult), r=[f2B, f4B], w=[f2B])
                            p.op("dve", lambda e: e.scalar_tensor_tensor(out=oT[:, hd, cs], in0=f2[:], scalar=ng[:, hd:hd + 1], in1=sgo[:, cs],
                                                                         op0=ALU.mult, op1=ALU.mult), r=[f2B, sgoB] + CB, w=[oTB[hd]])

                        def stage2(info):
                            qc, kb, nkb, c0, pt, ptB = info

                            def fpv(e):
                                mm(e, accn_t[:, c0:512], Vh[:, kb, :], pt[:, c0:512], kb == 0, kb == nkb - 1)
                                return mm(e, accd_t[:, c0:512], ones_bf[:, 0:128], pt[:, c0:512], kb == 0, kb == nkb - 1)
                            p.op("pe", fpv, r=[ptB, VhB] + CB, w=[accnB, accdB])
                            if kb == nkb - 1:
                                finalize(qc)

                        LA = 2
                        pend = []
                        for qc in range(4):
                            for kb in range(4 * qc + 4):
                                pend.append(stage1(qc, kb))
                                if len(pend) > LA:
                                    stage2(pend.pop(0))
                        while pend:
                            stage2(pend.pop(0))
                    if debug and s == 0:
                        dB = p.buf("dbg2b", dma=True)
                        p.op("pool", lambda e: e.dma_start(out=dbg2[128:256, :].rearrange("p (a b) -> p a b", a=KC), in_=oT[:]), r=oTB, dsem=dB.dsem)
                    p.barrier()
            _ph4()
            mix_stage("wo1", 2, 2)
            if phases < 4:
                return

            def _ph5():
                with ExitStack() as st:
                    p.phase_begin()
                    wr = sb(st, "wr", [128, KC, 8], BF16); wrB = p.buf("wr", dma=True)
                    wload(wr[:], wview("wr"), wrB)
                    lgp = ps(st, "lgp", [128, 128]); lgB = p.buf("lg")
                    prp = ps(st, "prp", [128, 128]); prB = p.buf("pr")
                    ttp = ps(st, "ttp", [128, 128]); ttB = p.buf("tt")
                    Lg = sb(st, "Lg", [128, 128], F32); LgB = p.buf("Lg")
                    L2 = sb(st, "L2", [128, 128], F32)
                    e1 = sb(st, "e1", [128, 128], F32)
                    e2 = sb(st, "e2", [128, 128], F32)
                    Mb = sb(st, "Mb", [128, 128], BF16)
                    Tt = sb(st, "Tt", [128, 128], F32)
                    Sc = sb(st, "Sc", [128, 128], F32)
                    Rr = sb(st, "Rr", [128, 128], F32)
                    row = sb(st, "row", [128, 128], F32)
                    tmp = sb(st, "tmp", [128, 128], F32)
                    pf = sb(st, "pf", [128, 32], F32)
                    tsum = sb(st, "tsum", [128, 8], F32)
                    m1 = sb(st, "m1", [128, 16], F32)
                    m2 = sb(st, "m2", [128, 16], F32)
                    w1 = sb(st, "w1", [128, 16], F32)
                    w2 = sb(st, "w2", [128, 16], F32)
                    v3 = lambda t: t[:, :].rearrange("p (a b) -> p a b", b=8)
                    emT = lambda t: t[:, :].rearrange("p (e a) -> p a e", e=8)
                    em3 = lambda t: t[:, :].rearrange("p (e a) -> p e a", e=8)
                    bc = lambda t: t[:, :].unsqueeze(2).to_broadcast([128, 16, 8])

                    def flg(e):
                        inst = None
                        for tb in range(TB):
                            for kc in range(KC):
                                inst = mm(e, lgp[:, tb * 8:(tb + 1) * 8], hT[:, kc, tb * 128:(tb + 1) * 128], wr[:, kc, :], kc == 0, kc == KC - 1)
                        return inst
                    p.op("pe", flg, r=[wrB] + hTB, w=[lgB])
                    G = [LgB]
                    p.op("dve", lambda e: e.tensor_copy(out=Lg[:], in_=lgp[:]), r=[lgB], w=G)
                    p.op("dve", lambda e: e.tensor_reduce(out=m1[:], in_=v3(Lg), axis=AX.X, op=ALU.max), r=G, w=G)
                    p.op("dve", lambda e: e.tensor_tensor(out=v3(e1), in0=v3(Lg), in1=bc(m1), op=ALU.is_equal), r=G, w=G)
                    p.op("dve", lambda e: e.scalar_tensor_tensor(out=L2[:], in0=e1[:], scalar=-1e30, in1=Lg[:], op0=ALU.mult, op1=ALU.add), r=G, w=G)
                    p.op("dve", lambda e: e.tensor_reduce(out=m2[:], in_=v3(L2), axis=AX.X, op=ALU.max), r=G, w=G)
                    p.op("dve", lambda e: e.tensor_tensor(out=v3(e2), in0=v3(L2), in1=bc(m2), op=ALU.is_equal), r=G, w=G)
                    p.op("dve", lambda e: e.tensor_tensor(out=w2[:], in0=m2[:], in1=m1[:], op=ALU.subtract), r=G, w=G)
                    p.op("act", lambda e: e.activation(out=w2[:], in_=w2[:], func=AF.Exp), r=G, w=G)
                    p.op("dve", lambda e: e.tensor_scalar(out=w1[:], in0=w2[:], scalar1=1.0, scalar2=None, op0=ALU.add), r=G, w=G)
                    p.op("dve", lambda e: e.reciprocal(out=w1[:], in_=w1[:]), r=G, w=G)
                    p.op("dve", lambda e: e.tensor_tensor(out=w2[:], in0=w2[:], in1=w1[:], op=ALU.mult), r=G, w=G)
                    p.op("dve", lambda e: e.tensor_copy(out=gW[:, s * 32:s * 32 + 16], in_=w1[:]), r=G, w=[gWB])
                    p.op("dve", lambda e: e.tensor_copy(out=gW[:, s * 32 + 16:s * 32 + 32], in_=w2[:]), r=G, w=[gWB])
                    p.op("dve", lambda e: e.tensor_tensor(out=emT(Mb), in0=v3(e1), in1=v3(e2), op=ALU.add), r=G, w=G)

                    def fpr(e):
                        mm(e, prp[:], tri[:], Mb[:], True, True)
                        return mm(e, ttp[:], ones_bf[:, 0:128], Mb[:], True, True)
                    p.op("pe", fpr, r=G + CB, w=[prB, ttB])
                    p.op("dve", lambda e: e.tensor_copy(out=Tt[:], in_=ttp[:]), r=[ttB], w=G)
                    p.op("dve", lambda e: e.tensor_tensor_scan(out=Sc[:], data0=ones_bf[:, 0:128], data1=Tt[:], initial=0.0, op0=ALU.mult, op1=ALU.add),
                         r=G + CB, w=G)
                    p.op("dve", lambda e: e.tensor_tensor(out=Sc[:], in0=Sc[:], in1=Tt[:], op=ALU.subtract), r=G, w=G)
                    p.op("dve", lambda e: e.tensor_tensor(out=em3(Rr), in0=em3(Sc), in1=em3(Sc)[:, :, 0:1].to_broadcast([128, 8, 16]), op=ALU.subtract), r=G, w=G)
                    p.op("dve", lambda e: e.tensor_tensor(out=em3(Rr), in0=em3(Rr), in1=base[:, :].unsqueeze(2).to_broadcast([128, 8, 16]), op=ALU.add),
                         r=G + [baseB], w=G)
                    p.op("dve", lambda e: e.tensor_tensor(out=row[:], in0=prp[:], in1=Rr[:], op=ALU.add), r=G + [prB], w=G)
                    p.op("dve", lambda e: e.tensor_tensor(out=row[:], in0=row[:], in1=eoff[:], op=ALU.add), r=G + CB, w=G)
                    p.op("dve", lambda e: e.tensor_reduce(out=tsum[:], in_=em3(Tt), axis=AX.X, op=ALU.add), r=G, w=G)
                    p.op("dve", lambda e: e.tensor_tensor(out=base[:], in0=base[:], in1=tsum[:], op=ALU.add), r=G + [baseB], w=[baseB])
                    p.op("dve", lambda e: e.tensor_tensor(out=v3(tmp), in0=v3(e1), in1=emT(row), op=ALU.mult), r=G, w=G)
                    p.op("dve", lambda e: e.tensor_reduce(out=pf[:, 0:16], in_=v3(tmp), axis=AX.X, op=ALU.add), r=G, w=G)
                    p.op("dve", lambda e: e.tensor_tensor(out=v3(tmp), in0=v3(e2), in1=emT(row), op=ALU.mult), r=G, w=G)
                    p.op("dve", lambda e: e.tensor_reduce(out=pf[:, 16:32], in_=v3(tmp), axis=AX.X, op=ALU.add), r=G, w=G)
                    p.op("dve", lambda e: e.tensor_copy(out=posI[:, s * 32:(s + 1) * 32], in_=pf[:]), r=G, w=[posB])
                    for tb in range(TB):
                        p.op("sp", lambda e, tb=tb: e.dma_start(out=h1sp[row0 + tb * 128: row0 + (tb + 1) * 128, :], in_=h[:, tb, :]),
                             r=[hB[tb]], dsem=hB[tb].dsem)
                        for k in range(2):
                            col = s * 32 + k * 16 + tb
                            for hf in range(2):
                                p.op("pool", lambda e, tb=tb, col=col, hf=hf: e.indirect_dma_start(
                                    out=XsH[hf][:, :], out_offset=IOA(ap=posI[:, col:col + 1], axis=0),
                                    in_=hs[:, tb * D + hf * 512: tb * D + (hf + 1) * 512], in_offset=None),
                                    r=[hB[tb], posB], dsem=hB[tb].dsem)
                    p.barrier()
            _ph5()

        def moe_phase():
            NB = TS // 128
            NTC = TS // 512
            with ExitStack() as st:
                p.phase_begin()
                ntl = sb(st, "ntl", [128, 8], F32)
                cinc = sb(st, "cinc", [128, 8], F32)
                cmp3 = hs[:, NTAB:NTAB + 64]
                le = hs[:, NTAB + 64:NTAB + 64 + NT * 8]
                le2 = hs[:, NTAB + 64 + NT * 8:NTAB + 64 + 2 * NT * 8]
                ej = sb(st, "ej", [128, NT], F32)
                ub = sb(st, "ub", [128, NT], F32)
                tabF = hs[:, 0:NTAB]
                xrow = sb(st, "xrow", [128, NT], F32)
                ew = sb(st, "ew", [128, NT], F32)
                T = [p.buf("tabw")]
                c3 = lambda t: t.rearrange("p (a b) -> p a b", b=8)
                p.op("dve", lambda e: e.tensor_tensor(out=c3(cmp3), in0=base[:, :].unsqueeze(2).to_broadcast([128, 8, 8]), in1=c3(thr[:, :]), op=ALU.is_gt),
                     r=[baseB] + CB, w=T)
                p.op("dve", lambda e: e.tensor_reduce(out=ntl[:], in_=c3(cmp3), axis=AX.X, op=ALU.add), r=T, w=T)
                p.op("dve", lambda e: e.tensor_tensor_scan(out=cinc[:], data0=ones_bf[:, 0:8], data1=ntl[:], initial=0.0, op0=ALU.mult, op1=ALU.add),
                     r=T + CB, w=T)
                p.op("dve", lambda e: e.tensor_tensor(out=c3(le), in0=cinc[:, :].unsqueeze(1).to_broadcast([128, NT, 8]), in1=c3(jc[:, :]), op=ALU.is_le),
                     r=T + CB, w=T)
                p.op("dve", lambda e: e.tensor_reduce(out=ej[:], in_=c3(le), axis=AX.X, op=ALU.add), r=T, w=T)
                p.op("dve", lambda e: e.tensor_tensor(out=c3(le2), in0=c3(le), in1=ntl[:, :].unsqueeze(1).to_broadcast([128, NT, 8]), op=ALU.mult), r=T, w=T)
                p.op("dve", lambda e: e.tensor_reduce(out=ub[:], in_=c3(le2), axis=AX.X, op=ALU.add), r=T, w=T)
                p.op("dve", lambda e: e.tensor_tensor(out=ub[:], in0=c3(jc[:, :])[:, :, 0], in1=ub[:], op=ALU.subtract), r=T + CB, w=T)
                p.op("dve", lambda e: e.tensor_scalar(out=ub[:], in0=ub[:], scalar1=float(TS), scalar2=None, op0=ALU.mult), r=T, w=T)
                p.op("dve", lambda e: e.scalar_tensor_tensor(out=xrow[:], in0=ej[:], scalar=float(CAP), in1=ub[:], op0=ALU.mult, op1=ALU.add), r=T, w=T)
                p.op("dve", lambda e: e.tensor_scalar(out=xrow[:], in0=xrow[:], scalar1=float(8 * CAP), scalar2=None, op0=ALU.min), r=T, w=T)
                p.op("dve", lambda e: e.tensor_scalar(out=ej[:], in0=ej[:], scalar1=7.0, scalar2=None, op0=ALU.min), r=T, w=T)
                tv = lambda o, n: tabF[:, o:o + NT * n].rearrange("p (j c) -> p j c", c=n)
                bj = lambda t, n: t[:, :].unsqueeze(2).to_broadcast([128, NT, n])
                bcp = lambda n: cp[:, 0:n].unsqueeze(1).to_broadcast([128, NT, n])
                p.op("dve", lambda e: e.tensor_tensor(out=tv(XO, NBm), in0=bj(xrow, NBm), in1=bcp(NBm), op=ALU.add), r=T + CB, w=T)
                p.op("dve", lambda e: e.tensor_scalar(out=ew[:], in0=ej[:], scalar1=float(7 * D), scalar2=None, op0=ALU.mult), r=T, w=T)
                p.op("dve", lambda e: e.tensor_tensor(out=tv(WO, 56), in0=bj(ew, 56), in1=bcp(56), op=ALU.add), r=T + CB, w=T)
                p.op("dve", lambda e: e.tensor_scalar(out=ew[:], in0=ej[:], scalar1=float(DFE), scalar2=None, op0=ALU.mult), r=T, w=T)
                p.op("dve", lambda e: e.tensor_tensor(out=tv(DO, 28), in0=bj(ew, 28), in1=bcp(28), op=ALU.add), r=T + CB, w=T)
                p.op("dve", lambda e: e.tensor_copy(out=tabI[:], in_=tabF), r=T, w=[tabB])

                wgs = Rot([(sb(st, "wg%d" % i, [128, KC, 512], BF16), p.buf("wg", dma=True)) for i in range(2)])
                wus = Rot([(sb(st, "wu%d" % i, [128, KC, 512], BF16), p.buf("wu", dma=True)) for i in range(2)])
                wds = Rot([(sb(st, "wd%d" % i, [128, 4, D], BF16), p.buf("wd", dma=True)) for i in range(2)])
                xbs = Rot([(sb(st, "xbm%d" % i, [128, D], BF16), p.buf("xbm", dma=True)) for i in range(3)])
                sgs = Rot([(sb(st, "sg%d" % i, [128, 512], F32), p.buf("sg")) for i in range(2)])
                pgs = Rot([(ps(st, "pg%d" % i, [128, 512]), p.buf("pg")) for i in range(2)])
                pus = Rot([(ps(st, "pu%d" % i, [128, 512]), p.buf("pu")) for i in range(2)])
                yps = Rot([(ps(st, "yp%d" % i, [128, D]), p.buf("yp")) for i in range(2)])
                psts = Rot([(pu_[:].bitcast(BF16), puB_) for (pu_, puB_) in pus.items])
                xTs = Rot([(hT[:, :, i * TS:(i + 1) * TS], p.buf("xT")) for i in range(2)])
                aTs = Rot([(oT[:, 4 * i:4 * i + 4, 0:TS], p.buf("aTm")) for i in range(2)])
                yss = Rot([(h[:, NB * i:NB * (i + 1), :], p.buf("ys", dma=True)) for i in range(2)])

                def load_tile(j):
                    xT, xTB = xTs.next()
                    for a in range(NB):
                        xb, xbB = xbs.next()
                        pst, pstB = psts.next()
                        for hf in range(2):
                            p.op("pool", lambda e, xb=xb, a=a, hf=hf: e.indirect_dma_start(
                                out=xb[:, hf * 512:(hf + 1) * 512], out_offset=None, in_=XsH[hf][:, :],
                                in_offset=IOA(ap=tabI[:, XO + j * NB + a:XO + j * NB + a + 1], axis=0)), r=[tabB], w=[xbB], dsem=xbB.dsem)

                        def tr(e, xb=xb, pst=pst):
                            inst = None
                            for kc in range(KC):
                                inst = e.transpose(pst[:, kc * 128:(kc + 1) * 128], xb[:, kc * 128:(kc + 1) * 128], ident[:])
                            return inst
                        p.op("pe", tr, r=[xbB, identB], w=[pstB])
                        p.op("act", lambda e, xT=xT, pst=pst, a=a: e.activation(out=xT[:, :, a * 128:(a + 1) * 128],
                                                                                 in_=pst.rearrange("p (k t) -> p k t", k=KC), func=AF.Copy),
                             r=[pstB], w=[xTB])
                    return xT, xTB

                def ffn_load(j, g):
                    wg, wgB = wgs.next()
                    wu, wuB = wus.next()
                    wd, wdB = wds.next()
                    for (wt_, wtB_, wn_) in ((wg, wgB, "mg"), (wu, wuB, "mu")):
                        for kc in range(KC):
                            c_ = WO + j * 56 + g * 8 + kc
                            p.op("pool", lambda e, wt_=wt_, wn_=wn_, kc=kc, c_=c_: e.indirect_dma_start(
                                out=wt_[:, kc, :], out_offset=None, in_=dr[wn_][:, :],
                                in_offset=IOA(ap=tabI[:, c_:c_ + 1], axis=0)), r=[tabB], w=[wtB_], dsem=wtB_.dsem)
                    for ci in range(4):
                        c_ = DO + j * 28 + g * 4 + ci
                        p.op("pool", lambda e, ci=ci, c_=c_: e.indirect_dma_start(
                            out=wd[:, ci, :], out_offset=None, in_=dr["md"][:, :],
                            in_offset=IOA(ap=tabI[:, c_:c_ + 1], axis=0)), r=[tabB], w=[wdB], dsem=wdB.dsem)
                    return (wg, wgB, wu, wuB, wd, wdB)

                def ffn_group(j, g, xT, xTB, ys, ysB, wts):
                    wg, wgB, wu, wuB, wd, wdB = wts
                    aT, aTB = aTs.next()
                    for ci in range(4):
                        for tc in range(NTC):
                            pg, pgB = pgs.next()
                            pu, puB = pus.next()
                            sg, sgB = sgs.next()

                            def fg_(e, pg=pg, ci=ci, tc=tc):
                                inst = None
                                for kc in range(KC):
                                    inst = mm(e, pg[:], wg[:, kc, ci * 128:(ci + 1) * 128], xT[:, kc, tc * 512:(tc + 1) * 512], kc == 0, kc == KC - 1)
                                return inst

                            def fu_(e, pu=pu, ci=ci, tc=tc):
                                inst = None
                                for kc in range(KC):
                                    inst = mm(e, pu[:], wu[:, kc, ci * 128:(ci + 1) * 128], xT[:, kc, tc * 512:(tc + 1) * 512], kc == 0, kc == KC - 1)
                                return inst
                            p.op("pe", fg_, r=[wgB, xTB], w=[pgB])
                            p.op("pe", fu_, r=[wuB, xTB], w=[puB])
                            p.op("act", lambda e, sg=sg, pg=pg: e.activation(out=sg[:], in_=pg[:], func=AF.Silu), r=[pgB], w=[sgB])
                            p.op("dve", lambda e, sg=sg, pu=pu, ci=ci, tc=tc: e.tensor_tensor(
                                out=aT[:, ci, tc * 512:(tc + 1) * 512], in0=pu[:], in1=sg[:], op=ALU.mult), r=[puB, sgB], w=[aTB])
                    for tb in range(NB):
                        yp, ypB = yps.next()

                        def fd_(e, yp=yp, tb=tb):
                            inst = None
                            for half in range(2):
                                for ci in range(4):
                                    inst = mm(e, yp[:, half * 512:(half + 1) * 512], aT[:, ci, tb * 128:(tb + 1) * 128],
                                              wd[:, ci, half * 512:(half + 1) * 512], ci == 0, ci == 3)
                            return inst
                        p.op("pe", fd_, r=[aTB, wdB], w=[ypB])
                        ydst = ys[:, tb, :]
                        if g == 0:
                            p.op("dve", lambda e, yp=yp, ydst=ydst: e.tensor_copy(out=ydst, in_=yp[:]), r=[ypB], w=[ysB])
                        else:
                            p.op("dve", lambda e, yp=yp, ydst=ydst: e.tensor_tensor(out=ydst, in0=yp[:], in1=ydst, op=ALU.add), r=[ypB, ysB], w=[ysB])

                NG = DFE // 512
                nxt = load_tile(0)
                wnext = ffn_load(0, 0)
                for j in range(NT):
                    xT, xTB = nxt
                    ys, ysB = yss.next()
                    for g in range(NG):
                        wcur = wnext
                        if g + 1 < NG:
                            wnext = ffn_load(j, g + 1)
                        elif j + 1 < NT:
                            wnext = ffn_load(j + 1, 0)
                        ffn_group(j, g, xT, xTB, ys, ysB, wcur)
                        if g == 3 and j + 1 < NT:
                            nxt = load_tile(j + 1)
                    for a in range(NB):
                        for hf in range(2):
                            p.op("pool", lambda e, ys=ys, j=j, hf=hf, a=a: e.indirect_dma_start(
                                out=YsH[hf][:, :], out_offset=IOA(ap=tabI[:, XO + j * NB + a:XO + j * NB + a + 1], axis=0),
                                in_=ys[:, a, hf * 512:(hf + 1) * 512], in_offset=None), r=[ysB, tabB], dsem=ysB.dsem)
                p.barrier()

        def combine_phase():
            with ExitStack() as st:
                p.phase_begin()
                L = ln_alloc(st, "fin")
                L["eps"] = epsT
                ln_load(L, 3)
                y1s = Rot([(sb(st, "y1_%d" % i, [128, D], F32), p.buf("y1", dma=True)) for i in range(3)])
                y2s = Rot([(sb(st, "y2_%d" % i, [128, D], F32), p.buf("y2", dma=True)) for i in range(3)])
                lp = LnPipe()
                for s in range(nseq):
                    for tb in range(TB):
                        col = s * 32 + tb
                        r0 = s * S + tb * 128
                        p.op("sp", lambda e, tb=tb, r0=r0: e.dma_start(out=h[:, tb, :], in_=h1sp[r0:r0 + 128, :]), w=[hB[tb]], dsem=hB[tb].dsem, force=True)
                        y1, y1B = y1s.next()
                        y2, y2B = y2s.next()
                        for hf in range(2):
                            p.op("pool", lambda e, y1=y1, col=col, hf=hf: e.indirect_dma_start(
                                out=y1[:, hf * 512:(hf + 1) * 512], out_offset=None, in_=YsH[hf][:, :],
                                in_offset=IOA(ap=posI[:, col:col + 1], axis=0)),
                                r=[posB], w=[y1B], dsem=y1B.dsem)
                            p.op("pool", lambda e, y2=y2, col=col, hf=hf: e.indirect_dma_start(
                                out=y2[:, hf * 512:(hf + 1) * 512], out_offset=None, in_=YsH[hf][:, :],
                                in_offset=IOA(ap=posI[:, col + 16:col + 17], axis=0)),
                                r=[posB], w=[y2B], dsem=y2B.dsem)
                        p.op("act", lambda e, tb=tb: e.mul(out=h[:, tb, :], in_=h[:, tb, :], mul=ALPHA), r=[hB[tb]], w=[hB[tb]])
                        p.op("dve", lambda e, tb=tb, y1=y1, col=col: e.scalar_tensor_tensor(out=h[:, tb, :], in0=y1[:], scalar=gW[:, col:col + 1], in1=h[:, tb, :],
                                                                                         op0=ALU.mult, op1=ALU.add), r=[y1B, hB[tb], gWB], w=[hB[tb]])
                        p.op("dve", lambda e, tb=tb, y2=y2, col=col: e.scalar_tensor_tensor(out=h[:, tb, :], in0=y2[:], scalar=gW[:, col + 16:col + 17], in1=h[:, tb, :],
                                                                                         op0=ALU.mult, op1=ALU.add), r=[y2B, hB[tb], gWB], w=[hB[tb]])
                        lp.push(ln_block(tb, L, to_hT=False, out_row0=s * S, gb_eng="dve"))
                lp.flush()
                p.barrier()

        for s_ in range(nseq):
            do_seq(s_)
        moe_phase()
        combine_phase()
        lasts = list(p.lastd.values())
        p.pend["sp"] = lasts
        p.op("sp", lambda e: None)
        block = es.enter_context(nc.Block())
        p.emit(block)
    return nc


def prep_shared(inp):
    f = lambda a: np.ascontiguousarray(a, dtype=np.float32)
    w_in_e = inp["w_in_e"][0]
    sh = {}
    sh["wqa"] = f(w_in_e[:, 0:512]); sh["wka"] = f(w_in_e[:, 512:1024]); sh["wva"] = f(w_in_e[:, 1024:1536])
    sh["wfa"] = f(np.repeat(w_in_e[:, 1536:1544], 128, axis=1))
    qd = w_in_e[:, 1544:2056]; kd = w_in_e[:, 2056:2568]
    sh["wqd"] = f(qd); sh["wkd"] = f(kd); sh["wvd"] = f(w_in_e[:, 2568:3080])
    perm = np.arange(512)
    for hh in range(8):
        for i in range(8):
            perm[hh * 64 + i] = hh * 64 + i + 8
            perm[hh * 64 + 8 + i] = hh * 64 + i
    sh["wqds"] = f(qd[:, perm]); sh["wkds"] = f(kd[:, perm])
    sh["wo0"] = f(inp["w_out_e"][0]); sh["fg"] = f(inp["ffn_w_gate_e"][0]); sh["fu"] = f(inp["ffn_w_up_e"][0]); sh["fd"] = f(inp["ffn_w_down_e"][0])
    w_in_o = inp["w_in_o"][0]
    sh["wq1"] = f(w_in_o[:, 0:1024]); sh["wk1"] = f(w_in_o[:, 1024:2048]); sh["wv1"] = f(w_in_o[:, 2048:3072])
    sh["wig"] = f(np.repeat(w_in_o[:, 3072:3080], 128, axis=1)); sh["wfg"] = f(np.repeat(w_in_o[:, 3080:3088], 128, axis=1))
    sh["wog"] = f(w_in_o[:, 3088:4112])
    sh["wo1"] = f(inp["w_out_o"][0]); sh["wr"] = f(inp["w_router_o"][0])
    relay = lambda w: f(w.reshape(NE, D, 7, 512).transpose(0, 2, 1, 3).reshape(NE * 7 * D, 512))
    sh["mg"] = relay(inp["moe_w_gate_o"][0]); sh["mu"] = relay(inp["moe_w_up_o"][0])
    sh["md"] = f(inp["moe_w_down_o"][0].reshape(NE * DFE, D))
    lnp = np.stack([np.stack([inp["ln_mix_g_e"][0], inp["ln_mix_b_e"][0]]), np.stack([inp["ln_ffn_g_e"][0], inp["ln_ffn_b_e"][0]]),
                    np.stack([inp["ln_mix_g_o"][0], inp["ln_mix_b_o"][0]]), np.stack([inp["ln_ffn_g_o"][0], inp["ln_ffn_b_o"][0]])])
    sh["lnp"] = f(np.broadcast_to(lnp[:, :, None, :], (4, 2, 128, D)).reshape(4 * 2 * 128, D))
    gbv = np.concatenate([inp["b_forget_e"][0], inp["b_igate_o"][0], inp["b_fgate_o"][0]])
    sh["gb"] = f(np.broadcast_to(gbv[None, :], (128, 24)))
    sh["ng"] = f(inp["mlstm_norm_g_o"][0].reshape(8, 128).T)
    sh["wc"] = f(inp["w_conv_o"][0].T.reshape(16, 128, 4).transpose(1, 0, 2).reshape(128, 64))
    sh["ident"] = np.eye(128, dtype=np.float32)
    k = np.arange(128)[:, None]; q = np.arange(128)[None, :]
    mC = np.where(k > q, NEG, 0.0).astype(np.float32)
    mU = np.where(k < q, NEG, 0.0).astype(np.float32)
    mA = np.full((128, 128), NEG, np.float32)
    sh["masks"] = f(np.concatenate([mC, mU, mA], axis=1))
    sh["mask4"] = f(np.concatenate([mU, mC, mU, mC, mA, mC, mU, mC, mA, mC, mA, mC], axis=1))
    half = 8
    inv = 500000.0 ** (-np.arange(half, dtype=np.float32) / half)
    ang = np.arange(S, dtype=np.float32)[None, :] * inv[:, None]
    cosT = np.ones((128, S), np.float32); sinT = np.zeros((128, S), np.float32)
    for hh in range(2):
        b = hh * 64
        cosT[b:b + 8] = np.cos(ang); cosT[b + 8:b + 16] = np.cos(ang)
        sinT[b:b + 8] = -np.sin(ang); sinT[b + 8:b + 16] = np.sin(ang)
    sh["rope"] = f(np.concatenate([cosT, sinT], axis=1))
    augc = np.zeros((128, 6), np.float32)
    augc[0:4, 0] = [-1, 0, 0, 0]; augc[0:4, 1] = [0, -1, 0, 0]; augc[0:4, 2] = [0, 0, 1, 1]
    augc[0:4, 3] = [0, 0, 1, 0]; augc[0:4, 4] = [0, 0, 0, 1]; augc[0:4, 5] = [1, 1, 0, 0]
    sh["augc"] = augc
    sh["cp"] = f(np.arange(56, dtype=np.float32)[None, :] * 128.0 + np.arange(128, dtype=np.float32)[:, None])
    sh["tri"] = np.triu(np.ones((128, 128), np.float32))
    eo = np.zeros((128, 128), np.float32)
    for e_ in range(8):
        eo[:, e_ * 16:(e_ + 1) * 16] = e_ * CAP - 1.0
    sh["eoff"] = eo
    sh["thr"] = f(np.broadcast_to((np.arange(8, dtype=np.float32) * TS)[None, None, :], (128, 8, 8)).reshape(128, 64))
    sh["jc"] = f(np.broadcast_to(np.arange(NT, dtype=np.float32)[None, :, None], (128, NT, 8)).reshape(128, NT * 8))
    return sh


N_CORES = 8


def kernel(**inputs):
    x = np.ascontiguousarray(inputs["x"], dtype=np.float32)
    B = x.shape[0]
    nseq = B // N_CORES
    assert nseq == NSEQ
    sh = prep_shared(inputs)
    nc = build_nc(nseq)
    in_maps = []
    for c in range(N_CORES):
        m = dict(sh)
        m["x"] = x[c * nseq:(c + 1) * nseq].reshape(nseq * S, D)
        in_maps.append(m)
    res = run_bass_kernel_spmd(nc, in_maps, core_ids=list(range(N_CORES)))
    outs = [np.asarray(r["out"]).reshape(nseq, S, D) for r in res.results]
    return np.concatenate(outs, axis=0).astype(np.float32)
```

```python
import numpy as np
from contextlib import ExitStack
import concourse.bass as bass
import concourse.mybir as mybir
from concourse.bass_utils import run_bass_kernel_spmd

F32 = mybir.dt.float32
BF16 = mybir.dt.bfloat16
AF = mybir.ActivationFunctionType
ALU = mybir.AluOpType
AX = mybir.AxisListType

S = 2048
D = 1024
TB = 16
KC = 8
ALPHA = 4.0 ** 0.25
EPS = 1e-5
NEG = -30000.0
DFF = 2816
DFE = 3584
NE = 8
LNS = float(np.log(128.0 ** -0.5))
ENGS = ("pe", "act", "dve", "pool", "sp")
I32 = mybir.dt.int32
NSEQ = 4
CAP = NSEQ * S
TS = 1024
NT = 2 * CAP // TS + 7
NROWS = 8 * CAP + TS


class Op:
    __slots__ = ("eng", "fn", "deps", "sig", "val", "dsem", "key")


class Buf:
    __slots__ = ("lastw", "readers", "dsem", "name")

    def __init__(self, name, dsem=None):
        self.lastw = None
        self.readers = {}
        self.dsem = dsem
        self.name = name


class Prog:
    def __init__(self, nc, es):
        self.nc = nc
        self.es = es
        self.ops = {e: [] for e in ENGS}
        self.esem = {e: es.enter_context(nc.semaphore("s_" + e)) for e in ENGS}
        self.dcnt = {}
        self.last = {e: None for e in ENGS}
        self.lastd = {}
        self.pend = {e: [] for e in ENGS}
        self.nsem = 0
        self.sem_pool = []
        self.pool_idx = None

    def newsem(self):
        if self.pool_idx is not None:
            if self.pool_idx >= len(self.sem_pool):
                self.nsem += 1
                self.sem_pool.append(self.es.enter_context(self.nc.semaphore("d%d" % self.nsem)))
            sm = self.sem_pool[self.pool_idx]
            self.pool_idx += 1
            return sm
        self.nsem += 1
        return self.es.enter_context(self.nc.semaphore("d%d" % self.nsem))

    def phase_begin(self):
        self.pool_idx = 0

    def buf(self, name, dma=False):
        return Buf(name, self.newsem() if dma else None)

    def op(self, eng, fn, r=(), w=(), dsem=None, force=False):
        o = Op()
        o.eng = eng
        o.fn = fn
        o.sig = False
        o.val = 0
        o.dsem = dsem
        o.key = ("d", id(dsem)) if dsem is not None else eng
        deps = []
        for b in r:
            if b.lastw is not None:
                deps.append((b.lastw, True))
        for b in w:
            if b.lastw is not None:
                deps.append((b.lastw, False))
            for rd in b.readers.values():
                deps.append((rd, False))
        for d in self.pend[eng]:
            deps.append((d, True))
        self.pend[eng] = []
        dd = []
        for d, raw in deps:
            if d is o:
                continue
            if d.key == o.key and (not raw or eng == "pe") and not (force and d.dsem is not None):
                continue
            if d not in dd:
                dd.append(d)
        o.deps = dd
        if dsem is not None:
            c = self.dcnt.get(id(dsem), 0) + 16
            self.dcnt[id(dsem)] = c
            o.val = c
            self.lastd[id(dsem)] = o
        for d in dd:
            if d.dsem is None:
                d.sig = True
        for b in r:
            b.readers[o.key] = o
        for b in w:
            b.lastw = o
            b.readers = {}
        self.ops[eng].append(o)
        self.last[eng] = o
        return o

    def barrier(self):
        lasts = [self.last[e] for e in ENGS if self.last[e] is not None] + list(self.lastd.values())
        for e in ENGS:
            self.pend[e] = list(lasts)

    def emit(self, block):
        for e in ENGS:
            c = 0
            for o in self.ops[e]:
                if o.dsem is None and o.sig:
                    c += 1
                    o.val = c

        def runner(ename):
            def f(eh):
                seen = {}
                for o in self.ops[ename]:
                    for d in o.deps:
                        if seen.get(d.key, 0) >= d.val:
                            continue
                        sem = d.dsem if d.dsem is not None else self.esem[d.eng]
                        eh.wait_ge(sem, d.val)
                        seen[d.key] = d.val
                    inst = o.fn(eh)
                    if inst is None:
                        continue
                    if o.dsem is not None:
                        inst.then_inc(o.dsem, 16)
                    elif o.sig:
                        inst.then_inc(self.esem[ename], 1)
            return f

        block.tensor(runner("pe"))
        block.scalar(runner("act"))
        block.vector(runner("dve"))
        block.gpsimd(runner("pool"))
        block.sync(runner("sp"))


class Rot:
    def __init__(self, items):
        self.items = items
        self.i = 0

    def next(self):
        it = self.items[self.i % len(self.items)]
        self.i += 1
        return it


WNAMES = {
    "wqa": (D, 512), "wka": (D, 512), "wva": (D, 512), "wfa": (D, 1024),
    "wqd": (D, 512), "wkd": (D, 512), "wqds": (D, 512), "wkds": (D, 512), "wvd": (D, 512),
    "wo0": (D, D), "fg": (D, DFF), "fu": (D, DFF), "fd": (DFF, D),
    "wq1": (D, D), "wk1": (D, D), "wv1": (D, D), "wog": (D, D), "wig": (D, D), "wfg": (D, D),
    "wo1": (D, D), "wr": (D, 8), "mg": (NE * 7 * D, 512), "mu": (NE * 7 * D, 512), "md": (NE * DFE, D),
    "lnp": (4 * 2 * 128, D), "gb": (128, 24), "ng": (128, 8), "wc": (128, 64),
    "ident": (128, 128), "masks": (128, 3 * 128), "mask4": (128, 3 * 512), "rope": (128, 2 * S), "augc": (128, 6),
    "cp": (128, 56), "tri": (128, 128), "eoff": (128, 128), "thr": (128, 64), "jc": (128, NT * 8),
}


def build_nc(nseq, debug=False, phases=5):
    nc = bass.Bass("TRN2", target_bir_lowering=False)
    dr = {}
    dr["x"] = nc.dram_tensor("x", [nseq * S, D], F32, kind="ExternalInput").ap()
    for k, shp in WNAMES.items():
        dr[k] = nc.dram_tensor(k, list(shp), F32, kind="ExternalInput").ap()
    out = nc.dram_tensor("out", [nseq * S, D], F32, kind="ExternalOutput").ap()
    hsp = nc.dram_tensor("hsp", [S, D], F32, kind="Internal").ap()
    h1sp = nc.dram_tensor("h1sp", [nseq * S, D], F32, kind="Internal").ap()
    XsH = [nc.dram_tensor("Xs%d" % i, [NROWS, 512], F32, kind="Internal").ap() for i in range(2)]
    YsH = [nc.dram_tensor("Ys%d" % i, [NROWS, 512], F32, kind="Internal").ap() for i in range(2)]
    dbg = None
    if debug:
        dbg = nc.dram_tensor("dbg", [4 * S, D], F32, kind="ExternalOutput").ap()
        dbg2 = nc.dram_tensor("dbg2", [2 * 128, KC * S], F32, kind="ExternalOutput").ap()

    with ExitStack() as es:
        p = Prog(nc, es)

        uid = [0]

        def sb(st, name, shape, dt):
            uid[0] += 1
            return st.enter_context(nc.sbuf_tensor("%s_s%d" % (name, uid[0]), shape, dt))

        def ps(st, name, shape, dt=F32):
            uid[0] += 1
            return st.enter_context(nc.psum_tensor("%s_p%d" % (name, uid[0]), shape, dt))

        h = sb(es, "h", [128, TB, D], F32)
        hB = [p.buf("h%d" % i, dma=True) for i in range(TB)]
        hs = h[:, :, :].rearrange("p a b -> p (a b)")
        hT = sb(es, "hT", [128, KC, S], BF16)
        hTB = [p.buf("hT%d" % i) for i in range(TB)]
        oT = sb(es, "oT", [128, KC, S], BF16)
        oTB = [p.buf("oT%d" % i) for i in range(KC)]
        ident = sb(es, "ident", [128, 128], BF16)
        identB = p.buf("ident", dma=True)
        masks = sb(es, "masks", [128, 3, 128], BF16)
        mask4 = sb(es, "mask4", [128, 3, 512], BF16)
        augc = sb(es, "augc", [128, 6], F32)
        gb = sb(es, "gb", [128, 24], F32)
        ngb = sb(es, "ngb", [128, 24], F32)
        ng = sb(es, "ng", [128, 8], F32)
        wc = sb(es, "wc", [128, 16, 4], F32)
        ones_bf = sb(es, "ones_bf", [128, S], BF16)
        onesf = sb(es, "onesf", [128, 128], F32)
        constB = p.buf("const", dma=True)
        const2B = p.buf("const2")

        p.op("pool", lambda e: e.dma_start(out=ident[:], in_=dr["ident"][:, :]), w=[identB], dsem=identB.dsem)
        p.op("pool", lambda e: e.dma_start(out=masks[:], in_=dr["masks"].rearrange("p (a b) -> p a b", a=3)), w=[constB], dsem=constB.dsem)
        p.op("pool", lambda e: e.dma_start(out=mask4[:], in_=dr["mask4"].rearrange("p (a b) -> p a b", a=3)), w=[constB], dsem=constB.dsem)
        p.op("sp", lambda e: e.dma_start(out=augc[:], in_=dr["augc"][:, :]), w=[constB], dsem=constB.dsem)
        p.op("sp", lambda e: e.dma_start(out=gb[:], in_=dr["gb"][:, :]), w=[constB], dsem=constB.dsem)
        p.op("sp", lambda e: e.dma_start(out=ng[:], in_=dr["ng"][:, :]), w=[constB], dsem=constB.dsem)
        p.op("sp", lambda e: e.dma_start(out=wc[:], in_=dr["wc"].rearrange("p (a b) -> p a b", b=4)), w=[constB], dsem=constB.dsem)
        p.op("dve", lambda e: e.memset(ones_bf[:], 1.0), w=[const2B])
        p.op("dve", lambda e: e.memset(onesf[:], 1.0 / 128.0), w=[const2B])
        p.op("dve", lambda e: e.tensor_scalar(out=ngb[:], in0=gb[:], scalar1=-1.0, scalar2=None, op0=ALU.mult), r=[constB], w=[const2B])
        CB = [constB, const2B, identB]
        tri = sb(es, "tri", [128, 128], BF16)
        eoff = sb(es, "eoff", [128, 128], F32)
        thr = sb(es, "thr", [128, 64], F32)
        jc = sb(es, "jc", [128, NT * 8], F32)
        posI = sb(es, "posI", [128, nseq * 32], I32); posB = p.buf("posI")
        gW = sb(es, "gW", [128, nseq * 32], F32); gWB = p.buf("gW")
        base = sb(es, "base", [128, 8], F32); baseB = p.buf("base")
        NBm = TS // 128
        XO, WO, DO = 0, NT * NBm, NT * NBm + NT * 56
        NTAB = DO + NT * 28
        tabI = sb(es, "tabI", [128, NTAB], I32); tabB = p.buf("tabI")
        cp = sb(es, "cp", [128, 56], F32)
        p.op("sp", lambda e: e.dma_start(out=cp[:], in_=dr["cp"][:, :]), w=[constB], dsem=constB.dsem)
        p.op("pool", lambda e: e.dma_start(out=tri[:], in_=dr["tri"][:, :]), w=[identB], dsem=identB.dsem)
        p.op("sp", lambda e: e.dma_start(out=eoff[:], in_=dr["eoff"][:, :]), w=[constB], dsem=constB.dsem)
        p.op("sp", lambda e: e.dma_start(out=thr[:], in_=dr["thr"][:, :]), w=[constB], dsem=constB.dsem)
        p.op("sp", lambda e: e.dma_start(out=jc[:], in_=dr["jc"][:, :]), w=[constB], dsem=constB.dsem)
        p.op("dve", lambda e: e.memset(base[:], 0.0), w=[baseB])
        IOA = bass.IndirectOffsetOnAxis

        def wview(name, rows_off=0, nrows=D):
            return dr[name][rows_off:rows_off + nrows, :].rearrange("(k p) n -> p k n", p=128)

        def mm(e, o, l, r_, st=True, sp=True):
            return e.matmul(o, l, r_, start=st, stop=sp)

        def build_hT(tb, L):
            xb, xbB = L["xb"].next()
            pst, pstB = L["pst"].next()
            p.op("act", lambda e: e.activation(out=xb[:], in_=h[:, tb, :], func=AF.Copy), r=[hB[tb]], w=[xbB])

            def tr(e):
                inst = None
                for kc in range(KC):
                    inst = e.transpose(pst[:, kc * 128:(kc + 1) * 128], xb[:, kc * 128:(kc + 1) * 128], ident[:])
                return inst
            p.op("pe", tr, r=[xbB, identB], w=[pstB])
            p.op("dve", lambda e: e.tensor_copy(out=hT[:, :, tb * 128:(tb + 1) * 128],
                                                in_=pst[:].rearrange("p (k t) -> p k t", k=KC)), r=[pstB], w=[hTB[tb]])

        def ln_alloc(st, tag):
            L = {}
            L["xb"] = Rot([(sb(st, "xb%s%d" % (tag, i), [128, D], BF16), p.buf("xb")) for i in range(2)])
            L["pst"] = Rot([(ps(st, "pst%s%d" % (tag, i), [128, D], BF16), p.buf("pst")) for i in range(2)])
            L["st6"] = Rot([(sb(st, "st6%s%d" % (tag, i), [128, 12], F32), p.buf("st6")) for i in range(2)])
            L["mv"] = Rot([(sb(st, "mv%s%d" % (tag, i), [128, 4], F32), p.buf("mv")) for i in range(2)])
            L["lnp"] = sb(st, "lnp%s" % tag, [128, 2, D], F32)
            L["lnpB"] = p.buf("lnp", dma=True)
            return L

        def ln_load(L, li):
            src = dr["lnp"][li * 256:(li + 1) * 256, :].rearrange("(a p) n -> p a n", a=2)
            p.op("sp", lambda e: e.dma_start(out=L["lnp"][:], in_=src), w=[L["lnpB"]], dsem=L["lnpB"].dsem)

        def ln_block(tb, L, to_hT=True, out_row0=None, dbg_row0=None, spill=False, gb_eng="pool"):
            st6, st6B = L["st6"].next()
            mv, mvB = L["mv"].next()
            lnp = L["lnp"]
            p.op("dve", lambda e: e.bn_stats(out=st6[:, 0:6], in_=h[:, tb, 0:512]), r=[hB[tb]], w=[st6B])
            p.op("dve", lambda e: e.bn_stats(out=st6[:, 6:12], in_=h[:, tb, 512:1024]), r=[hB[tb]], w=[st6B])
            p.op("dve", lambda e: e.bn_aggr(out=mv[:, 0:2], in_=st6[:]), r=[st6B], w=[mvB])
            p.op("act", lambda e: e.activation(out=mv[:, 2:3], in_=mv[:, 1:2], func=AF.Ln, bias=L["eps"][:, 0:1]), r=[mvB, const2B], w=[mvB])
            p.op("act", lambda e: e.activation(out=mv[:, 3:4], in_=mv[:, 2:3], func=AF.Exp, scale=-0.5), r=[mvB], w=[mvB])

            def fin():
                p.op("dve", lambda e: e.tensor_scalar(out=h[:, tb, :], in0=h[:, tb, :], scalar1=mv[:, 0:1], scalar2=mv[:, 3:4],
                                                      op0=ALU.subtract, op1=ALU.mult), r=[hB[tb], mvB], w=[hB[tb]])
                p.op(gb_eng, lambda e: e.tensor_tensor(out=h[:, tb, :], in0=h[:, tb, :], in1=lnp[:, 0, :], op=ALU.mult), r=[hB[tb], L["lnpB"]], w=[hB[tb]])
                p.op(gb_eng, lambda e: e.tensor_tensor(out=h[:, tb, :], in0=h[:, tb, :], in1=lnp[:, 1, :], op=ALU.add), r=[hB[tb], L["lnpB"]], w=[hB[tb]])
                if dbg_row0 is not None:
                    p.op("sp", lambda e: e.dma_start(out=dbg[dbg_row0 + tb * 128: dbg_row0 + (tb + 1) * 128, :], in_=h[:, tb, :]),
                         r=[hB[tb]], dsem=hB[tb].dsem)
                if spill:
                    p.op("sp", lambda e: e.dma_start(out=hsp[tb * 128:(tb + 1) * 128, :], in_=h[:, tb, :]), r=[hB[tb]], dsem=hB[tb].dsem)
                if out_row0 is not None:
                    p.op("sp", lambda e: e.dma_start(out=out[out_row0 + tb * 128: out_row0 + (tb + 1) * 128, :], in_=h[:, tb, :]),
                         r=[hB[tb]], dsem=hB[tb].dsem)
                elif to_hT:
                    build_hT(tb, L)
            return fin

        class LnPipe:
            def __init__(self):
                self.pend = None

            def push(self, fin):
                if self.pend is not None:
                    self.pend()
                self.pend = fin

            def flush(self):
                if self.pend is not None:
                    self.pend()
                self.pend = None

        epsT = sb(es, "epsT", [128, 1], F32)
        onesf_one = sb(es, "oneT", [128, 1], F32)
        p.op("dve", lambda e: e.memset(epsT[:], EPS), w=[const2B])
        p.op("dve", lambda e: e.memset(onesf_one[:], 1.0), w=[const2B])

        def wload(dst_ap, src_ap, B):
            return p.op("pool", lambda e: e.dma_start(out=dst_ap, in_=src_ap), w=[B], dsem=B.dsem)

        def proj_fm(W, wname, col0, evac, ncols=128):
            wt, wtB = W["wA"].next()
            wload(wt[:, :, 0:ncols], wview(wname)[:, :, col0:col0 + ncols], wtB)
            for tc in range(4):
                pp, ppB = W["pp"].next()

                def f(e, pp=pp, wt=wt, tc=tc):
                    inst = None
                    for kc in range(KC):
                        inst = mm(e, pp[0:ncols, :], wt[:, kc, 0:ncols], hT[:, kc, tc * 512:(tc + 1) * 512], kc == 0, kc == KC - 1)
                    return inst
                p.op("pe", f, r=[wtB] + hTB[4 * tc:4 * tc + 4], w=[ppB])
                evac(tc, pp, ppB)

        def proj_tm(W, wname, col0, ncols, sets, evac):
            wt, wtB = W["wA"].next()
            wload(wt[:, :, 0:ncols], wview(wname)[:, :, col0:col0 + ncols], wtB)
            for si, tsl in enumerate(sets):
                pp, ppB = W["pp"].next()

                def f(e, pp=pp, wt=wt, tsl=tsl):
                    inst = None
                    for kc in range(KC):
                        inst = mm(e, pp[:, 0:ncols], hT[:, kc, tsl], wt[:, kc, 0:ncols], kc == 0, kc == KC - 1)
                    return inst
                p.op("pe", f, r=[wtB] + hTB, w=[ppB])
                evac(si, pp, ppB)

        def do_seq(s):
            row0 = s * S
            def _ph1():
                with ExitStack() as st:
                    p.phase_begin()
                    L = ln_alloc(st, "p0")
                    L["eps"] = epsT
                    for tb in range(TB):
                        p.op("sp", lambda e, tb=tb, row0=row0: e.dma_start(out=h[:, tb, :], in_=dr["x"][row0 + tb * 128: row0 + (tb + 1) * 128, :]),
                             w=[hB[tb]], dsem=hB[tb].dsem)
                    for tb in range(TB):
                        build_hT(tb, L)
                    p.barrier()
            _ph1()
            if phases < 1:
                return

            def _ph2():
                with ExitStack() as st:
                    p.phase_begin()
                    W = {}
                    W["wA"] = Rot([(sb(st, "wA%d" % i, [128, KC, 128], BF16), p.buf("wA", dma=True)) for i in range(3)])
                    qT = sb(st, "qT", [128, S], BF16); qTB = p.buf("qT")
                    kT = sb(st, "kT", [128, S], BF16); kTB = p.buf("kT")
                    Vt = sb(st, "Vt", [128, 3, TB, 128], BF16); VB = [p.buf("V%d" % i) for i in range(3)]
                    t0 = hs[:, 0:S]; t0B = p.buf("t0")
                    t1 = hs[:, S:2 * S]; t1B = p.buf("t1")
                    hi = sb(st, "hi", [4, S], BF16); lo = sb(st, "lo", [4, S], BF16); hlB = p.buf("hl")
                    augQ = sb(st, "augQ", [4, S], BF16); augQB = p.buf("augQ")
                    augK = sb(st, "augK", [4, S], BF16); augKB = p.buf("augK")
                    tq = sb(st, "tq", [4, S], BF16); tqB = p.buf("tq")
                    pT = Rot([(sb(st, "pT%d" % i, [128, 512], BF16), p.buf("pT")) for i in range(4)])
                    rec = hs[0:64, 2 * S:3 * S]; recB = p.buf("rec")
                    sts = Rot([(ps(st, "st%d" % i, [128, 512]), p.buf("st")) for i in range(4)])
                    W["pp"] = Rot(sts.items)
                    LA = 2
                    accn = Rot([(ps(st, "accn%d" % i, [64, 512]), p.buf("accn")) for i in range(2)])
                    accd = Rot([(ps(st, "accd%d" % i, [64, 512]), p.buf("accd")) for i in range(2)])
                    rope = hs[:, 3 * S:5 * S].rearrange("p (a b) -> p a b", a=2); ropeB = p.buf("rope", dma=True)
                    rt = Rot([(hs[:, 5 * S + i * 1024:5 * S + (i + 1) * 1024].rearrange("p (a b) -> p a b", a=2), p.buf("rt")) for i in range(2)])
                    an = hs[0:64, 6 * S:7 * S]; anB = p.buf("an")
                    ad = hs[0:64, 7 * S:8 * S]; adB = p.buf("ad")
                    p.op("sp", lambda e: e.dma_start(out=rope, in_=dr["rope"].rearrange("p (a b) -> p a b", a=2)), w=[ropeB], dsem=ropeB.dsem)

                    def make_aug(src_ap, srcB):
                        p.op("dve", lambda e: e.tensor_copy(out=hi[:], in_=src_ap), r=[srcB], w=[hlB])
                        p.op("dve", lambda e: e.tensor_tensor(out=lo[:], in0=src_ap, in1=hi[:], op=ALU.subtract), r=[srcB, hlB], w=[hlB])

                    def fin_aug(dst, dstB, c0):
                        p.op("dve", lambda e: e.tensor_scalar(out=tq[:], in0=hi[:], scalar1=augc[0:4, c0:c0 + 1], scalar2=augc[0:4, c0 + 2:c0 + 3],
                                                              op0=ALU.mult, op1=ALU.add), r=[hlB] + CB, w=[tqB])
                        p.op("dve", lambda e: e.scalar_tensor_tensor(out=dst[:], in0=lo[:], scalar=augc[0:4, c0 + 1:c0 + 2], in1=tq[:],
                                                                     op0=ALU.mult, op1=ALU.add), r=[hlB, tqB] + CB, w=[dstB])

                    for c in range(4):
                        def ev_q(tc, pp, ppB):
                            p.op("act", lambda e: e.activation(out=qT[:, tc * 512:(tc + 1) * 512], in_=pp[:], func=AF.Copy, scale=0.125), r=[ppB], w=[qTB])

                        def ev_k(tc, pp, ppB):
                            p.op("dve", lambda e: e.tensor_copy(out=kT[:, tc * 512:(tc + 1) * 512], in_=pp[:]), r=[ppB], w=[kTB])

                        def ev_v(si, pp, ppB):
                            p.op("act", lambda e: e.activation(out=Vt[:, 0, si, :], in_=pp[:, 0:128], func=AF.Copy), r=[ppB], w=[VB[0]])
                        proj_fm(W, "wqa", c * 128, ev_q)
                        proj_fm(W, "wka", c * 128, ev_k)
                        proj_tm(W, "wva", c * 128, 128, [slice(tb * 128, (tb + 1) * 128) for tb in range(TB)], ev_v)
                        for hh in range(2):
                            head = 2 * c + hh
                            r0 = 64 * hh

                            def ev_g(tc, pp, ppB, head=head):
                                p.op("act", lambda e: e.activation(out=t0[:, tc * 512:(tc + 1) * 512], in_=pp[:], func=AF.Exp,
                                                                   bias=ngb[:, head:head + 1], scale=-1.0), r=[ppB] + CB, w=[t0B])
                            proj_fm(W, "wfa", head * 128, ev_g)
                            p.op("act", lambda e: e.activation(out=t0[:], in_=t0[:], func=AF.Ln, bias=onesf_one[:, 0:1]), r=[t0B] + CB, w=[t0B])
                            p.op("dve", lambda e: e.tensor_tensor_scan(out=t1[:], data0=ones_bf[:], data1=t0[:], initial=0.0,
                                                                       op0=ALU.mult, op1=ALU.add), r=[t0B] + CB, w=[t1B])
                            make_aug(t1[0:4, :], t1B)
                            fin_aug(augQ, augQB, 0)
                            fin_aug(augK, augKB, 3)
                            acc = {}

                            def fox1(qc, kb, r0=r0):
                                if kb == 0:
                                    acc[qc] = accn.next() + accd.next()
                                nkb = 4 * qc + 4
                                j0 = max(0, kb - 4 * qc)
                                c0 = j0 * 128
                                stt, sttB = sts.next()
                                pt, ptB = pT.next()
                                diag = kb >= 4 * qc

                                def fs(e):
                                    kblk = slice(kb * 128, (kb + 1) * 128)
                                    q0 = qc * 512
                                    inst = None
                                    if diag:
                                        qs = slice(q0 + c0, q0 + c0 + 128)
                                        mm(e, stt[:, c0:c0 + 128], kT[r0:r0 + 64, kblk], qT[r0:r0 + 64, qs], True, False)
                                        mm(e, stt[:, c0:c0 + 128], augK[0:4, kblk], augQ[0:4, qs], False, False)
                                        inst = mm(e, stt[:, c0:c0 + 128], ident[:], masks[:, 0, :], False, True)
                                        c1 = c0 + 128
                                    else:
                                        c1 = c0
                                    if c1 < 512:
                                        qs = slice(q0 + c1, q0 + 512)
                                        mm(e, stt[:, c1:512], kT[r0:r0 + 64, kblk], qT[r0:r0 + 64, qs], True, False)
                                        inst = mm(e, stt[:, c1:512], augK[0:4, kblk], augQ[0:4, qs], False, True)
                                    return inst
                                p.op("pe", fs, r=[kTB, qTB, augKB, augQB] + CB, w=[sttB])
                                p.op("act", lambda e: e.activation(out=pt[:, c0:512], in_=stt[:, c0:512], func=AF.Exp), r=[sttB], w=[ptB])
                                return (qc, kb, nkb, c0, pt, ptB)

                            def fox2(info, r0=r0, hh=hh, c=c):
                                qc, kb, nkb, c0, pt, ptB = info
                                an_, anB_, ad_, adB_ = acc[qc]

                                def fpv(e):
                                    mm(e, an_[:, c0:512], Vt[:, 0, kb, hh * 64:(hh + 1) * 64], pt[:, c0:512], kb == 0, kb == nkb - 1)
                                    return mm(e, ad_[:, c0:512], ones_bf[:, 0:64], pt[:, c0:512], kb == 0, kb == nkb - 1)
                                p.op("pe", fpv, r=[ptB, VB[0]] + CB, w=[anB_, adB_])
                                if kb == nkb - 1:
                                    p.op("dve", lambda e: e.reciprocal(out=rec[:, qc * 512:(qc + 1) * 512], in_=ad_[:]), r=[adB_], w=[recB])
                                    p.op("dve", lambda e: e.tensor_tensor(
                                        out=oT[r0:r0 + 64, c, qc * 512:(qc + 1) * 512], in0=an_[:], in1=rec[:, qc * 512:(qc + 1) * 512], op=ALU.mult),
                                        r=[anB_, recB], w=[oTB[c]])

                            pend = []
                            for qc in range(4):
                                for kb in range(4 * qc + 4):
                                    pend.append(fox1(qc, kb))
                                    if len(pend) > LA:
                                        fox2(pend.pop(0))
                            while pend:
                                fox2(pend.pop(0))

                    def tokset(bi, si):
                        if bi == 0:
                            return slice(si * 128, (si + 1) * 128)
                        if bi == 1:
                            r_, n_ = si // 4, si % 4
                            return slice(512 * n_ + r_, 512 * (n_ + 1), 4)
                        return slice(si, S, 16)

                    def accview(t, bi, si):
                        if bi == 0:
                            return t[:, :].rearrange("p (n j) -> p n j", j=128)[:, si:si + 2, :]
                        if bi == 1:
                            r_, n_ = si // 4, si % 4
                            return t[:, :].rearrange("p (n j r) -> p n j r", n=4, j=128, r=4)[:, n_:n_ + 2, :, r_]
                        return t[:, :].rearrange("p (j r) -> p r j", r=16)[:, si:si + 2, :]

                    for c in range(4):
                        def mk_rope(dst, dstB, wn, wns, c=c):
                            store = {}

                            def ev_a(tc, pp, ppB):
                                rtt, rtB = rt.next()
                                store[tc] = (rtt, rtB)
                                p.op("dve", lambda e: e.tensor_tensor(out=rtt[:, 0, :], in0=pp[:], in1=rope[:, 0, tc * 512:(tc + 1) * 512], op=ALU.mult),
                                     r=[ppB, ropeB], w=[rtB])

                            def ev_b(tc, pp, ppB):
                                rtt, rtB = store[tc]
                                p.op("dve", lambda e: e.tensor_tensor(out=rtt[:, 1, :], in0=pp[:], in1=rope[:, 1, tc * 512:(tc + 1) * 512], op=ALU.mult),
                                     r=[ppB, ropeB], w=[rtB])
                                p.op("pool", lambda e: e.tensor_tensor(out=dst[:, tc * 512:(tc + 1) * 512], in0=rtt[:, 0, :], in1=rtt[:, 1, :], op=ALU.add),
                                     r=[rtB], w=[dstB])
                            wa, waB = W["wA"].next()
                            wb, wbB = W["wA"].next()
                            wload(wa[:], wview(wn)[:, :, c * 128:(c + 1) * 128], waB)
                            wload(wb[:], wview(wns)[:, :, c * 128:(c + 1) * 128], wbB)
                            for tc in range(4):
                                for (wt_, wtB_, ev) in ((wa, waB, ev_a), (wb, wbB, ev_b)):
                                    pp, ppB = W["pp"].next()

                                    def f(e, pp=pp, wt_=wt_, tc=tc):
                                        inst = None
                                        for kc in range(KC):
                                            inst = mm(e, pp[:], wt_[:, kc, :], hT[:, kc, tc * 512:(tc + 1) * 512], kc == 0, kc == KC - 1)
                                        return inst
                                    p.op("pe", f, r=[wtB_] + hTB[4 * tc:4 * tc + 4], w=[ppB])
                                    ev(tc, pp, ppB)
                        mk_rope(qT, qTB, "wqd", "wqds")
                        mk_rope(kT, kTB, "wkd", "wkds")
                        for bi in range(3):
                            def ev_v(si, pp, ppB, bi=bi):
                                p.op("act", lambda e: e.activation(out=Vt[:, bi, si, :], in_=pp[:, 0:128], func=AF.Copy), r=[ppB], w=[VB[bi]])
                            proj_tm(W, "wvd", c * 128, 128, [tokset(bi, si) for si in range(16)], ev_v)
                        def dil1(blocks, mi, bi, si, r0):
                            stt, sttB = sts.next()
                            pt, ptB = pT.next()

                            def fs(e):
                                inst = None
                                for bk, (sq, sk) in enumerate(blocks):
                                    mm(e, stt[:, bk * 128:(bk + 1) * 128], kT[r0:r0 + 64, tokset(bi, sk)], qT[r0:r0 + 64, tokset(bi, sq)], True, False)
                                    inst = mm(e, stt[:, bk * 128:(bk + 1) * 128], ident[:], mask4[:, mi, bk * 128:(bk + 1) * 128], False, True)
                                return inst
                            p.op("pe", fs, r=[kTB, qTB] + CB, w=[sttB])
                            p.op("act", lambda e: e.activation(out=pt[:], in_=stt[:], func=AF.Exp, scale=0.125), r=[sttB], w=[ptB])
                            return (blocks, bi, si, pt, ptB)

                        def dil2(info, hh):
                            blocks, bi, si, pt, ptB = info
                            an_, anB_ = accn.next()
                            ad_, adB_ = accd.next()

                            def fpv(e):
                                inst = None
                                for bk, (sq, sk) in enumerate(blocks):
                                    qi = bk // 2
                                    first = (bk % 2 == 0)
                                    mm(e, an_[:, qi * 128:(qi + 1) * 128], Vt[:, bi, sk, hh * 64:(hh + 1) * 64], pt[:, bk * 128:(bk + 1) * 128], first, not first)
                                    inst = mm(e, ad_[:, qi * 128:(qi + 1) * 128], ones_bf[:, 0:64], pt[:, bk * 128:(bk + 1) * 128], first, not first)
                                return inst
                            p.op("pe", fpv, r=[ptB, VB[bi]] + CB, w=[anB_, adB_])
                            pv = lambda t: t[:, 0:256].rearrange("p (a j) -> p a j", a=2)
                            if bi == 0:
                                p.op("dve", lambda e: e.tensor_copy(out=accview(an, 0, si), in_=pv(an_)), r=[anB_], w=[anB])
                                p.op("dve", lambda e: e.tensor_copy(out=accview(ad, 0, si), in_=pv(ad_)), r=[adB_], w=[adB])
                            else:
                                p.op("dve", lambda e: e.tensor_tensor(out=accview(an, bi, si), in0=pv(an_), in1=accview(an, bi, si), op=ALU.add),
                                     r=[anB_, anB], w=[anB])
                                p.op("dve", lambda e: e.tensor_tensor(out=accview(ad, bi, si), in0=pv(ad_), in1=accview(ad, bi, si), op=ALU.add),
                                     r=[adB_, adB], w=[adB])

                        for hh in range(2):
                            r0 = 64 * hh
                            dpend = []
                            for bi in range(3):
                                for si in range(0, 16, 2):
                                    blocks = []
                                    for sq in (si, si + 1):
                                        if bi == 0:
                                            prev = sq - 1 if sq >= 1 else None
                                        elif bi == 1:
                                            prev = sq - 1 if (sq % 4) >= 1 else None
                                        else:
                                            prev = None
                                        blocks.append((sq, prev if prev is not None else sq))
                                        blocks.append((sq, sq))
                                    if bi == 2:
                                        mi = 2
                                    elif (bi == 0 and si == 0) or (bi == 1 and si % 4 == 0):
                                        mi = 1
                                    else:
                                        mi = 0
                                    dpend.append(dil1(blocks, mi, bi, si, r0))
                                    if len(dpend) > LA:
                                        dil2(dpend.pop(0), hh)
                            while dpend:
                                dil2(dpend.pop(0), hh)
                            p.op("dve", lambda e: e.reciprocal(out=rec[:], in_=ad[:]), r=[adB], w=[recB])
                            p.op("dve", lambda e, r0=r0, c=c: e.tensor_tensor(out=oT[r0:r0 + 64, 4 + c, :], in0=an[:], in1=rec[:], op=ALU.mult),
                                 r=[anB, recB], w=[oTB[4 + c]])
                    if debug and s == 0:
                        dB = p.buf("dbg2", dma=True)
                        p.op("pool", lambda e: e.dma_start(out=dbg2[0:128, :].rearrange("p (a b) -> p a b", a=KC), in_=oT[:]), r=oTB, dsem=dB.dsem)
                        p.op("pool", lambda e: e.dma_start(out=dbg2[128:256, 0:S], in_=qT[:]), r=[qTB], dsem=dB.dsem)
                        p.op("pool", lambda e: e.dma_start(out=dbg2[128:256, S:2 * S], in_=kT[:]), r=[kTB], dsem=dB.dsem)
                        p.op("pool", lambda e: e.dma_start(out=dbg2[128:192, 2 * S:3 * S], in_=an), r=[anB], dsem=dB.dsem)
                        p.op("pool", lambda e: e.dma_start(out=dbg2[128:192, 3 * S:4 * S], in_=ad), r=[adB], dsem=dB.dsem)
                        p.op("pool", lambda e: e.dma_start(out=dbg2[128:256, 4 * S:5 * S], in_=Vt[:, 1, :, :].rearrange("p a b -> p (a b)")), r=VB, dsem=dB.dsem)
                    p.barrier()

            _ph2()
            def mix_stage(wname, li, dbg_i, row0=row0):
                with ExitStack() as st:
                    p.phase_begin()
                    L = ln_alloc(st, "m%d" % li)
                    L["eps"] = epsT
                    ln_load(L, li)
                    for tb in range(TB):
                        src = dr["x"][row0 + tb * 128: row0 + (tb + 1) * 128, :] if li == 0 else hsp[tb * 128:(tb + 1) * 128, :]
                        p.op("sp", lambda e, tb=tb, src=src: e.dma_start(out=h[:, tb, :], in_=src), w=[hB[tb]], dsem=hB[tb].dsem)
                    wo = sb(st, "wo", [128, KC, D], BF16)
                    woB = p.buf("wo", dma=True)
                    wload(wo[:, :, 0:512], wview(wname)[:, :, 0:512], woB)
                    wload(wo[:, :, 512:1024], wview(wname)[:, :, 512:1024], woB)
                    mixp = Rot([(ps(st, "mix%d" % i, [128, D]), p.buf("mix")) for i in range(2)])
                    lp = LnPipe()
                    for tb in range(TB):
                        mp, mpB = mixp.next()

                        def f(e, mp=mp, tb=tb):
                            inst = None
                            for half in range(2):
                                for c in range(KC):
                                    inst = mm(e, mp[:, half * 512:(half + 1) * 512], oT[:, c, tb * 128:(tb + 1) * 128],
                                              wo[:, c, half * 512:(half + 1) * 512], c == 0, c == KC - 1)
                            return inst
                        p.op("pe", f, r=[woB] + oTB, w=[mpB])
                        p.op("dve", lambda e, mp=mp, tb=tb: e.scalar_tensor_tensor(out=h[:, tb, :], in0=h[:, tb, :], scalar=ALPHA, in1=mp[:],
                                                                                   op0=ALU.mult, op1=ALU.add), r=[hB[tb], mpB], w=[hB[tb]])
                        lp.push(ln_block(tb, L, to_hT=True, dbg_row0=(dbg_i * S if (debug and s == 0) else None)))
                    lp.flush()
                    p.barrier()
            mix_stage("wo0", 0, 0)
            if phases < 2:
                return

            def ffn_alloc(st):
                Fd = {}
                Fd["wg"] = Rot([(sb(st, "wg%d" % i, [128, KC, 512], BF16), p.buf("wg", dma=True)) for i in range(2)])
                Fd["wu"] = Rot([(sb(st, "wu%d" % i, [128, KC, 512], BF16), p.buf("wu", dma=True)) for i in range(2)])
                Fd["wd"] = Rot([(sb(st, "wd%d" % i, [128, 4, D], BF16), p.buf("wd", dma=True)) for i in range(2)])
                Fd["aT"] = Rot([(oT[:, 4 * i:4 * i + 4, :], p.buf("aT")) for i in range(2)])
                Fd["sg"] = Rot([(sb(st, "sg%d" % i, [128, 512], F32), p.buf("sg")) for i in range(2)])
                Fd["pg"] = Rot([(ps(st, "pg%d" % i, [128, 512]), p.buf("pg")) for i in range(2)])
                Fd["pu"] = Rot([(ps(st, "pu%d" % i, [128, 512]), p.buf("pu")) for i in range(2)])
                Fd["yp"] = Rot([(ps(st, "yp%d" % i, [128, D]), p.buf("yp")) for i in range(2)])
                return Fd

            def ffn(Fd, gname, uname, dname, grow0, drow0, F, gate_fn):
                f0 = 0
                while f0 < F:
                    gw = min(512, F - f0)
                    gc = gw // 128
                    wg, wgB = Fd["wg"].next()
                    wu, wuB = Fd["wu"].next()
                    wd, wdB = Fd["wd"].next()
                    aT, aTB = Fd["aT"].next()
                    wload(wg[:, :, 0:gw], wview(gname, grow0, D)[:, :, f0:f0 + gw], wgB)
                    wload(wu[:, :, 0:gw], wview(uname, grow0, D)[:, :, f0:f0 + gw], wuB)
                    wload(wd[:, 0:gc, :], wview(dname, drow0 + f0, gw), wdB)
                    for ci in range(gc):
                        for tc in range(4):
                            pg, pgB = Fd["pg"].next()
                            pu, puB = Fd["pu"].next()
                            sg, sgB = Fd["sg"].next()

                            def fg_(e, pg=pg, wg=wg, ci=ci, tc=tc):
                                inst = None
                                for kc in range(KC):
                                    inst = mm(e, pg[:], wg[:, kc, ci * 128:(ci + 1) * 128], hT[:, kc, tc * 512:(tc + 1) * 512], kc == 0, kc == KC - 1)
                                return inst

                            def fu_(e, pu=pu, wu=wu, ci=ci, tc=tc):
                                inst = None
                                for kc in range(KC):
                                    inst = mm(e, pu[:], wu[:, kc, ci * 128:(ci + 1) * 128], hT[:, kc, tc * 512:(tc + 1) * 512], kc == 0, kc == KC - 1)
                                return inst
                            p.op("pe", fg_, r=[wgB] + hTB[4 * tc:4 * tc + 4], w=[pgB])
                            p.op("pe", fu_, r=[wuB] + hTB[4 * tc:4 * tc + 4], w=[puB])
                            p.op("act", lambda e, sg=sg, pg=pg: e.activation(out=sg[:], in_=pg[:], func=AF.Silu), r=[pgB], w=[sgB])
                            p.op("dve", lambda e, sg=sg, pu=pu, aT=aT, ci=ci, tc=tc: e.tensor_tensor(
                                out=aT[:, ci, tc * 512:(tc + 1) * 512], in0=pu[:], in1=sg[:], op=ALU.mult), r=[puB, sgB], w=[aTB])
                    for tb in range(TB):
                        yp, ypB = Fd["yp"].next()

                        def fd_(e, yp=yp, aT=aT, wd=wd, tb=tb, gc=gc):
                            inst = None
                            for half in range(2):
                                for ci in range(gc):
                                    inst = mm(e, yp[:, half * 512:(half + 1) * 512], aT[:, ci, tb * 128:(tb + 1) * 128],
                                              wd[:, ci, half * 512:(half + 1) * 512], ci == 0, ci == gc - 1)
                            return inst
                        p.op("pe", fd_, r=[aTB, wdB], w=[ypB])
                        g_ap, gBs = gate_fn(tb)
                        p.op("dve", lambda e, yp=yp, tb=tb, g_ap=g_ap: e.scalar_tensor_tensor(out=h[:, tb, :], in0=yp[:], scalar=g_ap, in1=h[:, tb, :],
                                                                                            op0=ALU.mult, op1=ALU.add), r=[ypB, hB[tb]] + gBs, w=[hB[tb]])
                    f0 += gw

            def scale_h():
                for tb in range(TB):
                    p.op("act", lambda e, tb=tb: e.mul(out=h[:, tb, :], in_=h[:, tb, :], mul=ALPHA), r=[hB[tb]], w=[hB[tb]])

            def final_ln(li, dbg_i, to_hT, out_row0, spill=False):
                outs = []
                with ExitStack() as st:
                    p.phase_begin()
                    L = ln_alloc(st, "f%d" % li)
                    L["eps"] = epsT
                    ln_load(L, li)
                    lp = LnPipe()
                    for tb in range(TB):
                        lp.push(ln_block(tb, L, to_hT=to_hT, out_row0=out_row0, dbg_row0=(dbg_i * S if (debug and s == 0) else None), spill=spill))
                    lp.flush()
                    p.barrier()
                return outs

            def _ph3():
                with ExitStack() as st:
                    p.phase_begin()
                    Fd = ffn_alloc(st)
                    scale_h()
                    ffn(Fd, "fg", "fu", "fd", 0, 0, DFF, lambda tb: (1.0, []))
                    p.barrier()
            _ph3()
            final_ln(1, 1, True, None, spill=True)
            if phases < 3:
                return

            def _ph4():
                with ExitStack() as st:
                    p.phase_begin()
                    W = {}
                    W["wA"] = Rot([(sb(st, "wA%d" % i, [128, KC, 128], BF16), p.buf("wA", dma=True)) for i in range(3)])
                    qT = sb(st, "qT", [128, S], BF16); qTB = p.buf("qT")
                    kT = sb(st, "kT", [128, S], BF16); kTB = p.buf("kT")
                    Vh = sb(st, "Vh", [128, TB, 128], BF16); VhB = p.buf("Vh")
                    sgo = sb(st, "sgo", [128, S], BF16); sgoB = p.buf("sgo")
                    pre = hs[:, 0:3 + S]; preB = p.buf("pre"); padB = p.buf("pad")
                    yv = hs[:, 2052:2052 + S]; yvB = p.buf("yv")
                    t0 = hs[:, 3:3 + S]; t0B = preB
                    t1 = hs[:, 4100:4100 + S]; t1B = p.buf("t1")
                    t2 = yv; t2B = yvB
                    hi = sb(st, "hi", [4, S], BF16); lo = sb(st, "lo", [4, S], BF16); hlB = p.buf("hl")
                    ua = hs[0:4, 9220:9220 + S]; uaB = p.buf("ua")
                    augQ = sb(st, "augQ", [4, S], BF16); augQB = p.buf("augQ")
                    augK = sb(st, "augK", [4, S], BF16); augKB = p.buf("augK")
                    tq = sb(st, "tq", [4, S], BF16); tqB = p.buf("tq")
                    pT = Rot([(sb(st, "pT%d" % i, [128, 512], BF16), p.buf("pT")) for i in range(3)])
                    Et = Rot([(hs[:, 12288 + i * 512:12288 + (i + 1) * 512], p.buf("Et")) for i in range(3)])
                    psA = Rot([(ps(st, "psA%d" % i, [128, 512]), p.buf("psA")) for i in range(3)])
                    psB = Rot([(ps(st, "psB%d" % i, [128, 512]), p.buf("psB")) for i in range(3)])
                    accn_t = ps(st, "accn", [128, 512]); accnB = p.buf("accn")
                    accd_t = ps(st, "accd", [128, 512]); accdB = p.buf("accd")
                    W["pp"] = Rot(psA.items + psB.items)
                    f1 = hs[:, 7172:7684]; f1B = p.buf("f1")
                    f2 = hs[:, 7684:8196]; f2B = p.buf("f2")
                    f3 = hs[:, 8196:8708]; f3B = p.buf("f3")
                    f4 = hs[:, 8708:9220]; f4B = p.buf("f4")
                    p.op("dve", lambda e: e.memset(pre[:, 0:3], 0.0), w=[padB])

                    def make_aug(src_ap, srcB):
                        p.op("dve", lambda e: e.tensor_copy(out=hi[:], in_=src_ap), r=[srcB], w=[hlB])
                        p.op("dve", lambda e: e.tensor_tensor(out=lo[:], in0=src_ap, in1=hi[:], op=ALU.subtract), r=[srcB, hlB], w=[hlB])

                    def fin_aug(dst, dstB, c0):
                        p.op("dve", lambda e: e.tensor_scalar(out=tq[:], in0=hi[:], scalar1=augc[0:4, c0:c0 + 1], scalar2=augc[0:4, c0 + 2:c0 + 3],
                                                              op0=ALU.mult, op1=ALU.add), r=[hlB] + CB, w=[tqB])
                        p.op("dve", lambda e: e.scalar_tensor_tensor(out=dst[:], in0=lo[:], scalar=augc[0:4, c0 + 1:c0 + 2], in1=tq[:],
                                                                     op0=ALU.mult, op1=ALU.add), r=[hlB, tqB] + CB, w=[dstB])

                    for hd in range(8):
                        def conv_silu(wname, col0, chunk, dst, dstB):
                            def ev(tc, pp, ppB):
                                p.op("act", lambda e: e.activation(out=pre[:, 3 + tc * 512: 3 + (tc + 1) * 512], in_=pp[:], func=AF.Copy), r=[ppB], w=[preB])
                            proj_fm(W, wname, col0, ev)
                            p.op("dve", lambda e: e.tensor_scalar(out=yv[:], in0=pre[:, 3:3 + S], scalar1=wc[:, chunk, 3:4], scalar2=None, op0=ALU.mult),
                                 r=[preB, padB] + CB, w=[yvB])
                            for i in range(3):
                                p.op("dve", lambda e, i=i: e.scalar_tensor_tensor(out=yv[:], in0=pre[:, i:i + S], scalar=wc[:, chunk, i:i + 1], in1=yv[:],
                                                                                 op0=ALU.mult, op1=ALU.add), r=[preB, padB, yvB] + CB, w=[yvB])
                            p.op("act", lambda e: e.activation(out=dst[:], in_=yv[:], func=AF.Silu), r=[yvB], w=[dstB])
                        conv_silu("wq1", hd * 128, hd, qT, qTB)
                        conv_silu("wk1", hd * 128, 8 + hd, kT, kTB)

                        def ev_v(si, pp, ppB):
                            p.op("act", lambda e: e.activation(out=Vh[:, si, :], in_=pp[:, 0:128], func=AF.Copy), r=[ppB], w=[VhB])
                        proj_tm(W, "wv1", hd * 128, 128, [slice(tb * 128, (tb + 1) * 128) for tb in range(TB)], ev_v)

                        def ev_og(tc, pp, ppB):
                            p.op("act", lambda e: e.activation(out=sgo[:, tc * 512:(tc + 1) * 512], in_=pp[:], func=AF.Sigmoid), r=[ppB], w=[sgoB])
                        proj_fm(W, "wog", hd * 128, ev_og)

                        def ev_f(tc, pp, ppB, hd=hd):
                            p.op("act", lambda e: e.activation(out=t0[:, tc * 512:(tc + 1) * 512], in_=pp[:], func=AF.Exp,
                                                               bias=ngb[:, 16 + hd:17 + hd], scale=-1.0), r=[ppB] + CB, w=[t0B])
                        proj_fm(W, "wfg", hd * 128, ev_f)
                        p.op("act", lambda e: e.activation(out=t0[:], in_=t0[:], func=AF.Ln, bias=onesf_one[:, 0:1]), r=[t0B] + CB, w=[t0B])
                        p.op("dve", lambda e: e.tensor_tensor_scan(out=t1[:], data0=ones_bf[:], data1=t0[:], initial=0.0, op0=ALU.mult, op1=ALU.add),
                             r=[t0B] + CB, w=[t1B])

                        def ev_i(tc, pp, ppB, hd=hd):
                            p.op("dve", lambda e: e.scalar_tensor_tensor(out=t0[:, tc * 512:(tc + 1) * 512], in0=pp[:], scalar=gb[:, 8 + hd:9 + hd],
                                                                         in1=t1[:, tc * 512:(tc + 1) * 512], op0=ALU.add, op1=ALU.add),
                                 r=[ppB, t1B] + CB, w=[t0B])
                        proj_fm(W, "wig", hd * 128, ev_i)
                        p.op("dve", lambda e: e.tensor_tensor_scan(out=t2[:], data0=t0[:], data1=t0[:], initial=-1e30, op0=ALU.max, op1=ALU.max),
                             r=[t0B] + CB, w=[t2B])
                        p.op("dve", lambda e: e.tensor_tensor(out=t1[:], in0=t1[:], in1=t2[:], op=ALU.subtract), r=[t1B, t2B], w=[t1B])
                        p.op("act", lambda e: e.activation(out=t1[:], in_=t1[:], func=AF.Exp), r=[t1B], w=[t1B])
                        make_aug(t2[0:4, :], t2B)
                        fin_aug(augQ, augQB, 0)
                        p.op("dve", lambda e: e.tensor_scalar(out=ua[:], in0=t0[0:4, :], scalar1=LNS, scalar2=None, op0=ALU.add), r=[t0B], w=[uaB])
                        make_aug(ua[:], uaB)
                        fin_aug(augK, augKB, 3)

                        def stage1(qc, kb):
                            nkb = 4 * qc + 4
                            j0 = max(0, kb - 4 * qc)
                            c0 = j0 * 128
                            diag = kb >= 4 * qc
                            pa, paB = psA.next()
                            pb, pbB = psB.next()
                            pt, ptB = pT.next()
                            et, etB = Et.next()
                            kblk = slice(kb * 128, (kb + 1) * 128)
                            qs = slice(qc * 512 + c0, qc * 512 + 512)
                            p.op("pe", lambda e: mm(e, pa[:, c0:512], kT[:, kblk], qT[:, qs], True, True), r=[kTB, qTB], w=[paB])

                            def fd_(e):
                                q0 = qc * 512
                                inst = None
                                c1 = c0
                                if diag:
                                    qs1 = slice(q0 + c0, q0 + c0 + 128)
                                    mm(e, pb[:, c0:c0 + 128], augK[0:4, kblk], augQ[0:4, qs1], True, False)
                                    inst = mm(e, pb[:, c0:c0 + 128], ident[:], masks[:, 0, :], False, True)
                                    c1 = c0 + 128
                                if c1 < 512:
                                    inst = mm(e, pb[:, c1:512], augK[0:4, kblk], augQ[0:4, slice(q0 + c1, q0 + 512)], True, True)
                                return inst
                            p.op("pe", fd_, r=[augKB, augQB] + CB, w=[pbB])
                            p.op("act", lambda e: e.activation(out=et[:, c0:512], in_=pb[:, c0:512], func=AF.Exp), r=[pbB], w=[etB])
                            p.op("dve", lambda e: e.tensor_tensor(out=pt[:, c0:512], in0=pa[:, c0:512], in1=et[:, c0:512], op=ALU.mult), r=[paB, etB], w=[ptB])
                            return (qc, kb, nkb, c0, pt, ptB)

                        def finalize(qc, hd=hd):
                            cs = slice(qc * 512, (qc + 1) * 512)
                            s1, s1B = W["pp"].next()
                            s2, s2B = W["pp"].next()
                            p.op("act", lambda e: e.activation(out=f1[:], in_=accd_t[:], func=AF.Abs), r=[accdB], w=[f1B])
                            p.op("dve", lambda e: e.tensor_tensor(out=f1[:], in0=f1[:], in1=t1[:, cs], op=ALU.max), r=[f1B, t1B], w=[f1B])
                            p.op("dve", lambda e: e.reciprocal(out=f1[:], in_=f1[:]), r=[f1B], w=[f1B])
                            p.op("dve", lambda e: e.tensor_tensor(out=f2[:], in0=accn_t[:], in1=f1[:], op=ALU.mult), r=[accnB, f1B], w=[f2B])
                            p.op("act", lambda e: e.activation(out=f3[:], in_=f2[:], func=AF.Square), r=[f2B], w=[f3B])
                            p.op("pe", lambda e: mm(e, s1[:], onesf[:], f2[:], True, True), r=[f2B] + CB, w=[s1B])
                            p.op("act", lambda e: e.activation(out=f1[:], in_=s1[:], func=AF.Copy), r=[s1B], w=[f1B])
                            p.op("pe", lambda e: mm(e, s2[:], onesf[:], f3[:], True, True), r=[f3B] + CB, w=[s2B])
                            p.op("dve", lambda e: e.tensor_tensor(out=f4[:], in0=f1[:], in1=f1[:], op=ALU.mult), r=[f1B], w=[f4B])
                            p.op("dve", lambda e: e.tensor_tensor(out=f4[:], in0=s2[:], in1=f4[:], op=ALU.subtract), r=[s2B, f4B], w=[f4B])
                            p.op("act", lambda e: e.activation(out=f4[:], in_=f4[:], func=AF.Ln, bias=epsT[:, 0:1]), r=[f4B] + CB, w=[f4B])
                            p.op("act", lambda e: e.activation(out=f4[:], in_=f4[:], func=AF.Exp, scale=-0.5), r=[f4B], w=[f4B])
                            p.op("dve", lambda e: e.tensor_tensor(out=f2[:], in0=f2[:], in1=f1[:], op=ALU.subtract), r=[f2B, f1B], w=[f2B])
                            p.op("dve", lambda e: e.tensor_tensor(out=f2[:], in0=f2[:], in1=f4[:], op=ALU.mult), r=[f2B, f4B], w=[f2B])
                            p.op("dve", lambda e: e.scalar_tensor_tensor(out=oT[:, hd, cs], in0=f2[:], scalar=ng[:, hd:hd + 1], in1=sgo[:, cs],
                                                                         op0=ALU.mult, op1=ALU.mult), r=[f2B, sgoB] + CB, w=[oTB[hd]])

                        def stage2(info):
                            qc, kb, nkb, c0, pt, ptB = info

                            def fpv(e):
                                mm(e, accn_t[:, c0:512], Vh[:, kb, :], pt[:, c0:512], kb == 0, kb == nkb - 1)
                                return mm(e, accd_t[:, c0:512], ones_bf[:, 0:128], pt[:, c0:512], kb == 0, kb == nkb - 1)
                            p.op("pe", fpv, r=[ptB, VhB] + CB, w=[accnB, accdB])
                            if kb == nkb - 1:
                                finalize(qc)

                        LA = 2
                        pend = []
                        for qc in range(4):
                            for kb in range(4 * qc + 4):
                                pend.append(stage1(qc, kb))
                                if len(pend) > LA:
                                    stage2(pend.pop(0))
                        while pend:
                            stage2(pend.pop(0))
                    if debug and s == 0:
                        dB = p.buf("dbg2b", dma=True)
                        p.op("pool", lambda e: e.dma_start(out=dbg2[128:256, :].rearrange("p (a b) -> p a b", a=KC), in_=oT[:]), r=oTB, dsem=dB.dsem)
                    p.barrier()
            _ph4()
            mix_stage("wo1", 2, 2)
            if phases < 4:
                return

            def _ph5():
                with ExitStack() as st:
                    p.phase_begin()
                    wr = sb(st, "wr", [128, KC, 8], BF16); wrB = p.buf("wr", dma=True)
                    wload(wr[:], wview("wr"), wrB)
                    lgp = ps(st, "lgp", [128, 128]); lgB = p.buf("lg")
                    prp = ps(st, "prp", [128, 128]); prB = p.buf("pr")
                    ttp = ps(st, "ttp", [128, 128]); ttB = p.buf("tt")
                    Lg = sb(st, "Lg", [128, 128], F32); LgB = p.buf("Lg")
                    L2 = sb(st, "L2", [128, 128], F32)
                    e1 = sb(st, "e1", [128, 128], F32)
                    e2 = sb(st, "e2", [128, 128], F32)
                    Mb = sb(st, "Mb", [128, 128], BF16)
                    Tt = sb(st, "Tt", [128, 128], F32)
                    Sc = sb(st, "Sc", [128, 128], F32)
                    Rr = sb(st, "Rr", [128, 128], F32)
                    row = sb(st, "row", [128, 128], F32)
                    tmp = sb(st, "tmp", [128, 128], F32)
                    pf = sb(st, "pf", [128, 32], F32)
                    tsum = sb(st, "tsum", [128, 8], F32)
                    m1 = sb(st, "m1", [128, 16], F32)
                    m2 = sb(st, "m2", [128, 16], F32)
                    w1 = sb(st, "w1", [128, 16], F32)
                    w2 = sb(st, "w2", [128, 16], F32)
                    v3 = lambda t: t[:, :].rearrange("p (a b) -> p a b", b=8)
                    emT = lambda t: t[:, :].rearrange("p (e a) -> p a e", e=8)
                    em3 = lambda t: t[:, :].rearrange("p (e a) -> p e a", e=8)
                    bc = lambda t: t[:, :].unsqueeze(2).to_broadcast([128, 16, 8])

                    def flg(e):
                        inst = None
                        for tb in range(TB):
                            for kc in range(KC):
                                inst = mm(e, lgp[:, tb * 8:(tb + 1) * 8], hT[:, kc, tb * 128:(tb + 1) * 128], wr[:, kc, :], kc == 0, kc == KC - 1)
                        return inst
                    p.op("pe", flg, r=[wrB] + hTB, w=[lgB])
                    G = [LgB]
                    p.op("dve", lambda e: e.tensor_copy(out=Lg[:], in_=lgp[:]), r=[lgB], w=G)
                    p.op("dve", lambda e: e.tensor_reduce(out=m1[:], in_=v3(Lg), axis=AX.X, op=ALU.max), r=G, w=G)
                    p.op("dve", lambda e: e.tensor_tensor(out=v3(e1), in0=v3(Lg), in1=bc(m1), op=ALU.is_equal), r=G, w=G)
                    p.op("dve", lambda e: e.scalar_tensor_tensor(out=L2[:], in0=e1[:], scalar=-1e30, in1=Lg[:], op0=ALU.mult, op1=ALU.add), r=G, w=G)
                    p.op("dve", lambda e: e.tensor_reduce(out=m2[:], in_=v3(L2), axis=AX.X, op=ALU.max), r=G, w=G)
                    p.op("dve", lambda e: e.tensor_tensor(out=v3(e2), in0=v3(L2), in1=bc(m2), op=ALU.is_equal), r=G, w=G)
                    p.op("dve", lambda e: e.tensor_tensor(out=w2[:], in0=m2[:], in1=m1[:], op=ALU.subtract), r=G, w=G)
                    p.op("act", lambda e: e.activation(out=w2[:], in_=w2[:], func=AF.Exp), r=G, w=G)
                    p.op("dve", lambda e: e.tensor_scalar(out=w1[:], in0=w2[:], scalar1=1.0, scalar2=None, op0=ALU.add), r=G, w=G)
                    p.op("dve", lambda e: e.reciprocal(out=w1[:], in_=w1[:]), r=G, w=G)
                    p.op("dve", lambda e: e.tensor_tensor(out=w2[:], in0=w2[:], in1=w1[:], op=ALU.mult), r=G, w=G)
                    p.op("dve", lambda e: e.tensor_copy(out=gW[:, s * 32:s * 32 + 16], in_=w1[:]), r=G, w=[gWB])
                    p.op("dve", lambda e: e.tensor_copy(out=gW[:, s * 32 + 16:s * 32 + 32], in_=w2[:]), r=G, w=[gWB])
                    p.op("dve", lambda e: e.tensor_tensor(out=emT(Mb), in0=v3(e1), in1=v3(e2), op=ALU.add), r=G, w=G)

                    def fpr(e):
                        mm(e, prp[:], tri[:], Mb[:], True, True)
                        return mm(e, ttp[:], ones_bf[:, 0:128], Mb[:], True, True)
                    p.op("pe", fpr, r=G + CB, w=[prB, ttB])
                    p.op("dve", lambda e: e.tensor_copy(out=Tt[:], in_=ttp[:]), r=[ttB], w=G)
                    p.op("dve", lambda e: e.tensor_tensor_scan(out=Sc[:], data0=ones_bf[:, 0:128], data1=Tt[:], initial=0.0, op0=ALU.mult, op1=ALU.add),
                         r=G + CB, w=G)
                    p.op("dve", lambda e: e.tensor_tensor(out=Sc[:], in0=Sc[:], in1=Tt[:], op=ALU.subtract), r=G, w=G)
                    p.op("dve", lambda e: e.tensor_tensor(out=em3(Rr), in0=em3(Sc), in1=em3(Sc)[:, :, 0:1].to_broadcast([128, 8, 16]), op=ALU.subtract), r=G, w=G)
                    p.op("dve", lambda e: e.tensor_tensor(out=em3(Rr), in0=em3(Rr), in1=base[:, :].unsqueeze(2).to_broadcast([128, 8, 16]), op=ALU.add),
                         r=G + [baseB], w=G)
                    p.op("dve", lambda e: e.tensor_tensor(out=row[:], in0=prp[:], in1=Rr[:], op=ALU.add), r=G + [prB], w=G)
                    p.op("dve", lambda e: e.tensor_tensor(out=row[:], in0=row[:], in1=eoff[:], op=ALU.add), r=G + CB, w=G)
                    p.op("dve", lambda e: e.tensor_reduce(out=tsum[:], in_=em3(Tt), axis=AX.X, op=ALU.add), r=G, w=G)
                    p.op("dve", lambda e: e.tensor_tensor(out=base[:], in0=base[:], in1=tsum[:], op=ALU.add), r=G + [baseB], w=[baseB])
                    p.op("dve", lambda e: e.tensor_tensor(out=v3(tmp), in0=v3(e1), in1=emT(row), op=ALU.mult), r=G, w=G)
                    p.op("dve", lambda e: e.tensor_reduce(out=pf[:, 0:16], in_=v3(tmp), axis=AX.X, op=ALU.add), r=G, w=G)
                    p.op("dve", lambda e: e.tensor_tensor(out=v3(tmp), in0=v3(e2), in1=emT(row), op=ALU.mult), r=G, w=G)
                    p.op("dve", lambda e: e.tensor_reduce(out=pf[:, 16:32], in_=v3(tmp), axis=AX.X, op=ALU.add), r=G, w=G)
                    p.op("dve", lambda e: e.tensor_copy(out=posI[:, s * 32:(s + 1) * 32], in_=pf[:]), r=G, w=[posB])
                    for tb in range(TB):
                        p.op("sp", lambda e, tb=tb: e.dma_start(out=h1sp[row0 + tb * 128: row0 + (tb + 1) * 128, :], in_=h[:, tb, :]),
                             r=[hB[tb]], dsem=hB[tb].dsem)
                        for k in range(2):
                            col = s * 32 + k * 16 + tb
                            for hf in range(2):
                                p.op("pool", lambda e, tb=tb, col=col, hf=hf: e.indirect_dma_start(
                                    out=XsH[hf][:, :], out_offset=IOA(ap=posI[:, col:col + 1], axis=0),
                                    in_=hs[:, tb * D + hf * 512: tb * D + (hf + 1) * 512], in_offset=None),
                                    r=[hB[tb], posB], dsem=hB[tb].dsem)
                    p.barrier()
            _ph5()

        def moe_phase():
            NB = TS // 128
            NTC = TS // 512
            with ExitStack() as st:
                p.phase_begin()
                ntl = sb(st, "ntl", [128, 8], F32)
                cinc = sb(st, "cinc", [128, 8], F32)
                cmp3 = hs[:, NTAB:NTAB + 64]
                le = hs[:, NTAB + 64:NTAB + 64 + NT * 8]
                le2 = hs[:, NTAB + 64 + NT * 8:NTAB + 64 + 2 * NT * 8]
                ej = sb(st, "ej", [128, NT], F32)
                ub = sb(st, "ub", [128, NT], F32)
                tabF = hs[:, 0:NTAB]
                xrow = sb(st, "xrow", [128, NT], F32)
                ew = sb(st, "ew", [128, NT], F32)
                T = [p.buf("tabw")]
                c3 = lambda t: t.rearrange("p (a b) -> p a b", b=8)
                p.op("dve", lambda e: e.tensor_tensor(out=c3(cmp3), in0=base[:, :].unsqueeze(2).to_broadcast([128, 8, 8]), in1=c3(thr[:, :]), op=ALU.is_gt),
                     r=[baseB] + CB, w=T)
                p.op("dve", lambda e: e.tensor_reduce(out=ntl[:], in_=c3(cmp3), axis=AX.X, op=ALU.add), r=T, w=T)
                p.op("dve", lambda e: e.tensor_tensor_scan(out=cinc[:], data0=ones_bf[:, 0:8], data1=ntl[:], initial=0.0, op0=ALU.mult, op1=ALU.add),
                     r=T + CB, w=T)
                p.op("dve", lambda e: e.tensor_tensor(out=c3(le), in0=cinc[:, :].unsqueeze(1).to_broadcast([128, NT, 8]), in1=c3(jc[:, :]), op=ALU.is_le),
                     r=T + CB, w=T)
                p.op("dve", lambda e: e.tensor_reduce(out=ej[:], in_=c3(le), axis=AX.X, op=ALU.add), r=T, w=T)
                p.op("dve", lambda e: e.tensor_tensor(out=c3(le2), in0=c3(le), in1=ntl[:, :].unsqueeze(1).to_broadcast([128, NT, 8]), op=ALU.mult), r=T, w=T)
                p.op("dve", lambda e: e.tensor_reduce(out=ub[:], in_=c3(le2), axis=AX.X, op=ALU.add), r=T, w=T)
                p.op("dve", lambda e: e.tensor_tensor(out=ub[:], in0=c3(jc[:, :])[:, :, 0], in1=ub[:], op=ALU.subtract), r=T + CB, w=T)
                p.op("dve", lambda e: e.tensor_scalar(out=ub[:], in0=ub[:], scalar1=float(TS), scalar2=None, op0=ALU.mult), r=T, w=T)
                p.op("dve", lambda e: e.scalar_tensor_tensor(out=xrow[:], in0=ej[:], scalar=float(CAP), in1=ub[:], op0=ALU.mult, op1=ALU.add), r=T, w=T)
                p.op("dve", lambda e: e.tensor_scalar(out=xrow[:], in0=xrow[:], scalar1=float(8 * CAP), scalar2=None, op0=ALU.min), r=T, w=T)
                p.op("dve", lambda e: e.tensor_scalar(out=ej[:], in0=ej[:], scalar1=7.0, scalar2=None, op0=ALU.min), r=T, w=T)
                tv = lambda o, n: tabF[:, o:o + NT * n].rearrange("p (j c) -> p j c", c=n)
                bj = lambda t, n: t[:, :].unsqueeze(2).to_broadcast([128, NT, n])
                bcp = lambda n: cp[:, 0:n].unsqueeze(1).to_broadcast([128, NT, n])
                p.op("dve", lambda e: e.tensor_tensor(out=tv(XO, NBm), in0=bj(xrow, NBm), in1=bcp(NBm), op=ALU.add), r=T + CB, w=T)
                p.op("dve", lambda e: e.tensor_scalar(out=ew[:], in0=ej[:], scalar1=float(7 * D), scalar2=None, op0=ALU.mult), r=T, w=T)
                p.op("dve", lambda e: e.tensor_tensor(out=tv(WO, 56), in0=bj(ew, 56), in1=bcp(56), op=ALU.add), r=T + CB, w=T)
                p.op("dve", lambda e: e.tensor_scalar(out=ew[:], in0=ej[:], scalar1=float(DFE), scalar2=None, op0=ALU.mult), r=T, w=T)
                p.op("dve", lambda e: e.tensor_tensor(out=tv(DO, 28), in0=bj(ew, 28), in1=bcp(28), op=ALU.add), r=T + CB, w=T)
                p.op("dve", lambda e: e.tensor_copy(out=tabI[:], in_=tabF), r=T, w=[tabB])

                wgs = Rot([(sb(st, "wg%d" % i, [128, KC, 512], BF16), p.buf("wg", dma=True)) for i in range(2)])
                wus = Rot([(sb(st, "wu%d" % i, [128, KC, 512], BF16), p.buf("wu", dma=True)) for i in range(2)])
                wds = Rot([(sb(st, "wd%d" % i, [128, 4, D], BF16), p.buf("wd", dma=True)) for i in range(2)])
                xbs = Rot([(sb(st, "xbm%d" % i, [128, D], BF16), p.buf("xbm", dma=True)) for i in range(3)])
                sgs = Rot([(sb(st, "sg%d" % i, [128, 512], F32), p.buf("sg")) for i in range(2)])
                pgs = Rot([(ps(st, "pg%d" % i, [128, 512]), p.buf("pg")) for i in range(2)])
                pus = Rot([(ps(st, "pu%d" % i, [128, 512]), p.buf("pu")) for i in range(2)])
                yps = Rot([(ps(st, "yp%d" % i, [128, D]), p.buf("yp")) for i in range(2)])
                psts = Rot([(pu_[:].bitcast(BF16), puB_) for (pu_, puB_) in pus.items])
                xTs = Rot([(hT[:, :, i * TS:(i + 1) * TS], p.buf("xT")) for i in range(2)])
                aTs = Rot([(oT[:, 4 * i:4 * i + 4, 0:TS], p.buf("aTm")) for i in range(2)])
                yss = Rot([(h[:, NB * i:NB * (i + 1), :], p.buf("ys", dma=True)) for i in range(2)])

                def load_tile(j):
                    xT, xTB = xTs.next()
                    for a in range(NB):
                        xb, xbB = xbs.next()
                        pst, pstB = psts.next()
                        for hf in range(2):
                            p.op("pool", lambda e, xb=xb, a=a, hf=hf: e.indirect_dma_start(
                                out=xb[:, hf * 512:(hf + 1) * 512], out_offset=None, in_=XsH[hf][:, :],
                                in_offset=IOA(ap=tabI[:, XO + j * NB + a:XO + j * NB + a + 1], axis=0)), r=[tabB], w=[xbB], dsem=xbB.dsem)

                        def tr(e, xb=xb, pst=pst):
                            inst = None
                            for kc in range(KC):
                                inst = e.transpose(pst[:, kc * 128:(kc + 1) * 128], xb[:, kc * 128:(kc + 1) * 128], ident[:])
                            return inst
                        p.op("pe", tr, r=[xbB, identB], w=[pstB])
                        p.op("act", lambda e, xT=xT, pst=pst, a=a: e.activation(out=xT[:, :, a * 128:(a + 1) * 128],
                                                                                 in_=pst.rearrange("p (k t) -> p k t", k=KC), func=AF.Copy),
                             r=[pstB], w=[xTB])
                    return xT, xTB

                def ffn_load(j, g):
                    wg, wgB = wgs.next()
                    wu, wuB = wus.next()
                    wd, wdB = wds.next()
                    for (wt_, wtB_, wn_) in ((wg, wgB, "mg"), (wu, wuB, "mu")):
                        for kc in range(KC):
                            c_ = WO + j * 56 + g * 8 + kc
                            p.op("pool", lambda e, wt_=wt_, wn_=wn_, kc=kc, c_=c_: e.indirect_dma_start(
                                out=wt_[:, kc, :], out_offset=None, in_=dr[wn_][:, :],
                                in_offset=IOA(ap=tabI[:, c_:c_ + 1], axis=0)), r=[tabB], w=[wtB_], dsem=wtB_.dsem)
                    for ci in range(4):
                        c_ = DO + j * 28 + g * 4 + ci
                        p.op("pool", lambda e, ci=ci, c_=c_: e.indirect_dma_start(
                            out=wd[:, ci, :], out_offset=None, in_=dr["md"][:, :],
                            in_offset=IOA(ap=tabI[:, c_:c_ + 1], axis=0)), r=[tabB], w=[wdB], dsem=wdB.dsem)
                    return (wg, wgB, wu, wuB, wd, wdB)

                def ffn_group(j, g, xT, xTB, ys, ysB, wts):
                    wg, wgB, wu, wuB, wd, wdB = wts
                    aT, aTB = aTs.next()
                    for ci in range(4):
                        for tc in range(NTC):
                            pg, pgB = pgs.next()
                            pu, puB = pus.next()
                            sg, sgB = sgs.next()

                            def fg_(e, pg=pg, ci=ci, tc=tc):
                                inst = None
                                for kc in range(KC):
                                    inst = mm(e, pg[:], wg[:, kc, ci * 128:(ci + 1) * 128], xT[:, kc, tc * 512:(tc + 1) * 512], kc == 0, kc == KC - 1)
                                return inst

                            def fu_(e, pu=pu, ci=ci, tc=tc):
                                inst = None
                                for kc in range(KC):
                                    inst = mm(e, pu[:], wu[:, kc, ci * 128:(ci + 1) * 128], xT[:, kc, tc * 512:(tc + 1) * 512], kc == 0, kc == KC - 1)
                                return inst
                            p.op("pe", fg_, r=[wgB, xTB], w=[pgB])
                            p.op("pe", fu_, r=[wuB, xTB], w=[puB])
                            p.op("act", lambda e, sg=sg, pg=pg: e.activation(out=sg[:], in_=pg[:], func=AF.Silu), r=[pgB], w=[sgB])
                            p.op("dve", lambda e, sg=sg, pu=pu, ci=ci, tc=tc: e.tensor_tensor(
                                out=aT[:, ci, tc * 512:(tc + 1) * 512], in0=pu[:], in1=sg[:], op=ALU.mult), r=[puB, sgB], w=[aTB])
                    for tb in range(NB):
                        yp, ypB = yps.next()

                        def fd_(e, yp=yp, tb=tb):
                            inst = None
                            for half in range(2):
                                for ci in range(4):
                                    inst = mm(e, yp[:, half * 512:(half + 1) * 512], aT[:, ci, tb * 128:(tb + 1) * 128],
                                              wd[:, ci, half * 512:(half + 1) * 512], ci == 0, ci == 3)
                            return inst
                        p.op("pe", fd_, r=[aTB, wdB], w=[ypB])
                        ydst = ys[:, tb, :]
                        if g == 0:
                            p.op("dve", lambda e, yp=yp, ydst=ydst: e.tensor_copy(out=ydst, in_=yp[:]), r=[ypB], w=[ysB])
                        else:
                            p.op("dve", lambda e, yp=yp, ydst=ydst: e.tensor_tensor(out=ydst, in0=yp[:], in1=ydst, op=ALU.add), r=[ypB, ysB], w=[ysB])

                NG = DFE // 512
                nxt = load_tile(0)
                wnext = ffn_load(0, 0)
                for j in range(NT):
                    xT, xTB = nxt
                    ys, ysB = yss.next()
                    for g in range(NG):
                        wcur = wnext
                        if g + 1 < NG:
                            wnext = ffn_load(j, g + 1)
                        elif j + 1 < NT:
                            wnext = ffn_load(j + 1, 0)
                        ffn_group(j, g, xT, xTB, ys, ysB, wcur)
                        if g == 3 and j + 1 < NT:
                            nxt = load_tile(j + 1)
                    for a in range(NB):
                        for hf in range(2):
                            p.op("pool", lambda e, ys=ys, j=j, hf=hf, a=a: e.indirect_dma_start(
                                out=YsH[hf][:, :], out_offset=IOA(ap=tabI[:, XO + j * NB + a:XO + j * NB + a + 1], axis=0),
                                in_=ys[:, a, hf * 512:(hf + 1) * 512], in_offset=None), r=[ysB, tabB], dsem=ysB.dsem)
                p.barrier()

        def combine_phase():
            with ExitStack() as st:
                p.phase_begin()
                L = ln_alloc(st, "fin")
                L["eps"] = epsT
                ln_load(L, 3)
                y1s = Rot([(sb(st, "y1_%d" % i, [128, D], F32), p.buf("y1", dma=True)) for i in range(3)])
                y2s = Rot([(sb(st, "y2_%d" % i, [128, D], F32), p.buf("y2", dma=True)) for i in range(3)])
                lp = LnPipe()
                for s in range(nseq):
                    for tb in range(TB):
                        col = s * 32 + tb
                        r0 = s * S + tb * 128
                        p.op("sp", lambda e, tb=tb, r0=r0: e.dma_start(out=h[:, tb, :], in_=h1sp[r0:r0 + 128, :]), w=[hB[tb]], dsem=hB[tb].dsem, force=True)
                        y1, y1B = y1s.next()
                        y2, y2B = y2s.next()
                        for hf in range(2):
                            p.op("pool", lambda e, y1=y1, col=col, hf=hf: e.indirect_dma_start(
                                out=y1[:, hf * 512:(hf + 1) * 512], out_offset=None, in_=YsH[hf][:, :],
                                in_offset=IOA(ap=posI[:, col:col + 1], axis=0)),
                                r=[posB], w=[y1B], dsem=y1B.dsem)
                            p.op("pool", lambda e, y2=y2, col=col, hf=hf: e.indirect_dma_start(
                                out=y2[:, hf * 512:(hf + 1) * 512], out_offset=None, in_=YsH[hf][:, :],
                                in_offset=IOA(ap=posI[:, col + 16:col + 17], axis=0)),
                                r=[posB], w=[y2B], dsem=y2B.dsem)
                        p.op("act", lambda e, tb=tb: e.mul(out=h[:, tb, :], in_=h[:, tb, :], mul=ALPHA), r=[hB[tb]], w=[hB[tb]])
                        p.op("dve", lambda e, tb=tb, y1=y1, col=col: e.scalar_tensor_tensor(out=h[:, tb, :], in0=y1[:], scalar=gW[:, col:col + 1], in1=h[:, tb, :],
                                                                                         op0=ALU.mult, op1=ALU.add), r=[y1B, hB[tb], gWB], w=[hB[tb]])
                        p.op("dve", lambda e, tb=tb, y2=y2, col=col: e.scalar_tensor_tensor(out=h[:, tb, :], in0=y2[:], scalar=gW[:, col + 16:col + 17], in1=h[:, tb, :],
                                                                                         op0=ALU.mult, op1=ALU.add), r=[y2B, hB[tb], gWB], w=[hB[tb]])
                        lp.push(ln_block(tb, L, to_hT=False, out_row0=s * S, gb_eng="dve"))
                lp.flush()
                p.barrier()

        for s_ in range(nseq):
            do_seq(s_)
        moe_phase()
        combine_phase()
        lasts = list(p.lastd.values())
        p.pend["sp"] = lasts
        p.op("sp", lambda e: None)
        block = es.enter_context(nc.Block())
        p.emit(block)
    return nc


def prep_shared(inp):
    f = lambda a: np.ascontiguousarray(a, dtype=np.float32)
    w_in_e = inp["w_in_e"][0]
    sh = {}
    sh["wqa"] = f(w_in_e[:, 0:512]); sh["wka"] = f(w_in_e[:, 512:1024]); sh["wva"] = f(w_in_e[:, 1024:1536])
    sh["wfa"] = f(np.repeat(w_in_e[:, 1536:1544], 128, axis=1))
    qd = w_in_e[:, 1544:2056]; kd = w_in_e[:, 2056:2568]
    sh["wqd"] = f(qd); sh["wkd"] = f(kd); sh["wvd"] = f(w_in_e[:, 2568:3080])
    perm = np.arange(512)
    for hh in range(8):
        for i in range(8):
            perm[hh * 64 + i] = hh * 64 + i + 8
            perm[hh * 64 + 8 + i] = hh * 64 + i
    sh["wqds"] = f(qd[:, perm]); sh["wkds"] = f(kd[:, perm])
    sh["wo0"] = f(inp["w_out_e"][0]); sh["fg"] = f(inp["ffn_w_gate_e"][0]); sh["fu"] = f(inp["ffn_w_up_e"][0]); sh["fd"] = f(inp["ffn_w_down_e"][0])
    w_in_o = inp["w_in_o"][0]
    sh["wq1"] = f(w_in_o[:, 0:1024]); sh["wk1"] = f(w_in_o[:, 1024:2048]); sh["wv1"] = f(w_in_o[:, 2048:3072])
    sh["wig"] = f(np.repeat(w_in_o[:, 3072:3080], 128, axis=1)); sh["wfg"] = f(np.repeat(w_in_o[:, 3080:3088], 128, axis=1))
    sh["wog"] = f(w_in_o[:, 3088:4112])
    sh["wo1"] = f(inp["w_out_o"][0]); sh["wr"] = f(inp["w_router_o"][0])
    relay = lambda w: f(w.reshape(NE, D, 7, 512).transpose(0, 2, 1, 3).reshape(NE * 7 * D, 512))
    sh["mg"] = relay(inp["moe_w_gate_o"][0]); sh["mu"] = relay(inp["moe_w_up_o"][0])
    sh["md"] = f(inp["moe_w_down_o"][0].reshape(NE * DFE, D))
    lnp = np.stack([np.stack([inp["ln_mix_g_e"][0], inp["ln_mix_b_e"][0]]), np.stack([inp["ln_ffn_g_e"][0], inp["ln_ffn_b_e"][0]]),
                    np.stack([inp["ln_mix_g_o"][0], inp["ln_mix_b_o"][0]]), np.stack([inp["ln_ffn_g_o"][0], inp["ln_ffn_b_o"][0]])])
    sh["lnp"] = f(np.broadcast_to(lnp[:, :, None, :], (4, 2, 128, D)).reshape(4 * 2 * 128, D))
    gbv = np.concatenate([inp["b_forget_e"][0], inp["b_igate_o"][0], inp["b_fgate_o"][0]])
    sh["gb"] = f(np.broadcast_to(gbv[None, :], (128, 24)))
    sh["ng"] = f(inp["mlstm_norm_g_o"][0].reshape(8, 128).T)
    sh["wc"] = f(inp["w_conv_o"][0].T.reshape(16, 128, 4).transpose(1, 0, 2).reshape(128, 64))
    sh["ident"] = np.eye(128, dtype=np.float32)
    k = np.arange(128)[:, None]; q = np.arange(128)[None, :]
    mC = np.where(k > q, NEG, 0.0).astype(np.float32)
    mU = np.where(k < q, NEG, 0.0).astype(np.float32)
    mA = np.full((128, 128), NEG, np.float32)
    sh["masks"] = f(np.concatenate([mC, mU, mA], axis=1))
    sh["mask4"] = f(np.concatenate([mU, mC, mU, mC, mA, mC, mU, mC, mA, mC, mA, mC], axis=1))
    half = 8
    inv = 500000.0 ** (-np.arange(half, dtype=np.float32) / half)
    ang = np.arange(S, dtype=np.float32)[None, :] * inv[:, None]
    cosT = np.ones((128, S), np.float32); sinT = np.zeros((128, S), np.float32)
    for hh in range(2):
        b = hh * 64
        cosT[b:b + 8] = np.cos(ang); cosT[b + 8:b + 16] = np.cos(ang)
        sinT[b:b + 8] = -np.sin(ang); sinT[b + 8:b + 16] = np.sin(ang)
    sh["rope"] = f(np.concatenate([cosT, sinT], axis=1))
    augc = np.zeros((128, 6), np.float32)
    augc[0:4, 0] = [-1, 0, 0, 0]; augc[0:4, 1] = [0, -1, 0, 0]; augc[0:4, 2] = [0, 0, 1, 1]
    augc[0:4, 3] = [0, 0, 1, 0]; augc[0:4, 4] = [0, 0, 0, 1]; augc[0:4, 5] = [1, 1, 0, 0]
    sh["augc"] = augc
    sh["cp"] = f(np.arange(56, dtype=np.float32)[None, :] * 128.0 + np.arange(128, dtype=np.float32)[:, None])
    sh["tri"] = np.triu(np.ones((128, 128), np.float32))
    eo = np.zeros((128, 128), np.float32)
    for e_ in range(8):
        eo[:, e_ * 16:(e_ + 1) * 16] = e_ * CAP - 1.0
    sh["eoff"] = eo
    sh["thr"] = f(np.broadcast_to((np.arange(8, dtype=np.float32) * TS)[None, None, :], (128, 8, 8)).reshape(128, 64))
    sh["jc"] = f(np.broadcast_to(np.arange(NT, dtype=np.float32)[None, :, None], (128, NT, 8)).reshape(128, NT * 8))
    return sh


N_CORES = 8


def kernel(**inputs):
    x = np.ascontiguousarray(inputs["x"], dtype=np.float32)
    B = x.shape[0]
    nseq = B // N_CORES
    assert nseq == NSEQ
    sh = prep_shared(inputs)
    nc = build_nc(nseq)
    in_maps = []
    for c in range(N_CORES):
        m = dict(sh)
        m["x"] = x[c * nseq:(c + 1) * nseq].reshape(nseq * S, D)
        in_maps.append(m)
    res = run_bass_kernel_spmd(nc, in_maps, core_ids=list(range(N_CORES)))
    outs = [np.asarray(r["out"]).reshape(nseq, S, D) for r in res.results]
    return np.concatenate(outs, axis=0).astype(np.float32)
```

```python
import numpy as np
from contextlib import ExitStack
import concourse.bass as bass
import concourse.mybir as mybir
from concourse.bass_utils import run_bass_kernel_spmd

F32 = mybir.dt.float32
BF16 = mybir.dt.bfloat16
AF = mybir.ActivationFunctionType
ALU = mybir.AluOpType
AX = mybir.AxisListType

S = 2048
D = 1024
TB = 16
KC = 8
ALPHA = 4.0 ** 0.25
EPS = 1e-5
NEG = -30000.0
DFF = 2816
DFE = 3584
NE = 8
LNS = float(np.log(128.0 ** -0.5))
ENGS = ("pe", "act", "dve", "pool", "sp")
I32 = mybir.dt.int32
NSEQ = 4
CAP = NSEQ * S
TS = 1024
NT = 2 * CAP // TS + 7
NROWS = 8 * CAP + TS


class Op:
    __slots__ = ("eng", "fn", "deps", "sig", "val", "dsem", "key")


class Buf:
    __slots__ = ("lastw", "readers", "dsem", "name")

    def __init__(self, name, dsem=None):
        self.lastw = None
        self.readers = {}
        self.dsem = dsem
        self.name = name


class Prog:
    def __init__(self, nc, es):
        self.nc = nc
        self.es = es
        self.ops = {e: [] for e in ENGS}
        self.esem = {e: es.enter_context(nc.semaphore("s_" + e)) for e in ENGS}
        self.dcnt = {}
        self.last = {e: None for e in ENGS}
        self.lastd = {}
        self.pend = {e: [] for e in ENGS}
        self.nsem = 0
        self.sem_pool = []
        self.pool_idx = None

    def newsem(self):
        if self.pool_idx is not None:
            if self.pool_idx >= len(self.sem_pool):
                self.nsem += 1
                self.sem_pool.append(self.es.enter_context(self.nc.semaphore("d%d" % self.nsem)))
            sm = self.sem_pool[self.pool_idx]
            self.pool_idx += 1
            return sm
        self.nsem += 1
        return self.es.enter_context(self.nc.semaphore("d%d" % self.nsem))

    def phase_begin(self):
        self.pool_idx = 0

    def buf(self, name, dma=False):
        return Buf(name, self.newsem() if dma else None)

    def op(self, eng, fn, r=(), w=(), dsem=None, force=False):
        o = Op()
        o.eng = eng
        o.fn = fn
        o.sig = False
        o.val = 0
        o.dsem = dsem
        o.key = ("d", id(dsem)) if dsem is not None else eng
        deps = []
        for b in r:
            if b.lastw is not None:
                deps.append((b.lastw, True))
        for b in w:
            if b.lastw is not None:
                deps.append((b.lastw, False))
            for rd in b.readers.values():
                deps.append((rd, False))
        for d in self.pend[eng]:
            deps.append((d, True))
        self.pend[eng] = []
        dd = []
        for d, raw in deps:
            if d is o:
                continue
            if d.key == o.key and (not raw or eng == "pe") and not (force and d.dsem is not None):
                continue
            if d not in dd:
                dd.append(d)
        o.deps = dd
        if dsem is not None:
            c = self.dcnt.get(id(dsem), 0) + 16
            self.dcnt[id(dsem)] = c
            o.val = c
            self.lastd[id(dsem)] = o
        for d in dd:
            if d.dsem is None:
                d.sig = True
        for b in r:
            b.readers[o.key] = o
        for b in w:
            b.lastw = o
            b.readers = {}
        self.ops[eng].append(o)
        self.last[eng] = o
        return o

    def barrier(self):
        lasts = [self.last[e] for e in ENGS if self.last[e] is not None] + list(self.lastd.values())
        for e in ENGS:
            self.pend[e] = list(lasts)

    def emit(self, block):
        for e in ENGS:
            c = 0
            for o in self.ops[e]:
                if o.dsem is None and o.sig:
                    c += 1
                    o.val = c

        def runner(ename):
            def f(eh):
                seen = {}
                for o in self.ops[ename]:
                    for d in o.deps:
                        if seen.get(d.key, 0) >= d.val:
                            continue
                        sem = d.dsem if d.dsem is not None else self.esem[d.eng]
                        eh.wait_ge(sem, d.val)
                        seen[d.key] = d.val
                    inst = o.fn(eh)
                    if inst is None:
                        continue
                    if o.dsem is not None:
                        inst.then_inc(o.dsem, 16)
                    elif o.sig:
                        inst.then_inc(self.esem[ename], 1)
            return f

        block.tensor(runner("pe"))
        block.scalar(runner("act"))
        block.vector(runner("dve"))
        block.gpsimd(runner("pool"))
        block.sync(runner("sp"))


class Rot:
    def __init__(self, items):
        self.items = items
        self.i = 0

    def next(self):
        it = self.items[self.i % len(self.items)]
        self.i += 1
        return it


WNAMES = {
    "wqa": (D, 512), "wka": (D, 512), "wva": (D, 512), "wfa": (D, 1024),
    "wqd": (D, 512), "wkd": (D, 512), "wqds": (D, 512), "wkds": (D, 512), "wvd": (D, 512),
    "wo0": (D, D), "fg": (D, DFF), "fu": (D, DFF), "fd": (DFF, D),
    "wq1": (D, D), "wk1": (D, D), "wv1": (D, D), "wog": (D, D), "wig": (D, D), "wfg": (D, D),
    "wo1": (D, D), "wr": (D, 8), "mg": (NE * 7 * D, 512), "mu": (NE * 7 * D, 512), "md": (NE * DFE, D),
    "lnp": (4 * 2 * 128, D), "gb": (128, 24), "ng": (128, 8), "wc": (128, 64),
    "ident": (128, 128), "masks": (128, 3 * 128), "mask4": (128, 3 * 512), "rope": (128, 2 * S), "augc": (128, 6),
    "cp": (128, 56), "tri": (128, 128), "eoff": (128, 128), "thr": (128, 64), "jc": (128, NT * 8),
}


def build_nc(nseq, debug=False, phases=5):
    nc = bass.Bass("TRN2", target_bir_lowering=False)
    dr = {}
    dr["x"] = nc.dram_tensor("x", [nseq * S, D], F32, kind="ExternalInput").ap()
    for k, shp in WNAMES.items():
        dr[k] = nc.dram_tensor(k, list(shp), F32, kind="ExternalInput").ap()
    out = nc.dram_tensor("out", [nseq * S, D], F32, kind="ExternalOutput").ap()
    hsp = nc.dram_tensor("hsp", [S, D], F32, kind="Internal").ap()
    h1sp = nc.dram_tensor("h1sp", [nseq * S, D], F32, kind="Internal").ap()
    XsH = [nc.dram_tensor("Xs%d" % i, [NROWS, 512], F32, kind="Internal").ap() for i in range(2)]
    YsH = [nc.dram_tensor("Ys%d" % i, [NROWS, 512], F32, kind="Internal").ap() for i in range(2)]
    dbg = None
    if debug:
        dbg = nc.dram_tensor("dbg", [4 * S, D], F32, kind="ExternalOutput").ap()
        dbg2 = nc.dram_tensor("dbg2", [2 * 128, KC * S], F32, kind="ExternalOutput").ap()

    with ExitStack() as es:
        p = Prog(nc, es)

        uid = [0]

        def sb(st, name, shape, dt):
            uid[0] += 1
            return st.enter_context(nc.sbuf_tensor("%s_s%d" % (name, uid[0]), shape, dt))

        def ps(st, name, shape, dt=F32):
            uid[0] += 1
            return st.enter_context(nc.psum_tensor("%s_p%d" % (name, uid[0]), shape, dt))

        h = sb(es, "h", [128, TB, D], F32)
        hB = [p.buf("h%d" % i, dma=True) for i in range(TB)]
        hs = h[:, :, :].rearrange("p a b -> p (a b)")
        hT = sb(es, "hT", [128, KC, S], BF16)
        hTB = [p.buf("hT%d" % i) for i in range(TB)]
        oT = sb(es, "oT", [128, KC, S], BF16)
        oTB = [p.buf("oT%d" % i) for i in range(KC)]
        ident = sb(es, "ident", [128, 128], BF16)
        identB = p.buf("ident", dma=True)
        masks = sb(es, "masks", [128, 3, 128], BF16)
        mask4 = sb(es, "mask4", [128, 3, 512], BF16)
        augc = sb(es, "augc", [128, 6], F32)
        gb = sb(es, "gb", [128, 24], F32)
        ngb = sb(es, "ngb", [128, 24], F32)
        ng = sb(es, "ng", [128, 8], F32)
        wc = sb(es, "wc", [128, 16, 4], F32)
        ones_bf = sb(es, "ones_bf", [128, S], BF16)
        onesf = sb(es, "onesf", [128, 128], F32)
        constB = p.buf("const", dma=True)
        const2B = p.buf("const2")

        p.op("pool", lambda e: e.dma_start(out=ident[:], in_=dr["ident"][:, :]), w=[identB], dsem=identB.dsem)
        p.op("pool", lambda e: e.dma_start(out=masks[:], in_=dr["masks"].rearrange("p (a b) -> p a b", a=3)), w=[constB], dsem=constB.dsem)
        p.op("pool", lambda e: e.dma_start(out=mask4[:], in_=dr["mask4"].rearrange("p (a b) -> p a b", a=3)), w=[constB], dsem=constB.dsem)
        p.op("sp", lambda e: e.dma_start(out=augc[:], in_=dr["augc"][:, :]), w=[constB], dsem=constB.dsem)
        p.op("sp", lambda e: e.dma_start(out=gb[:], in_=dr["gb"][:, :]), w=[constB], dsem=constB.dsem)
        p.op("sp", lambda e: e.dma_start(out=ng[:], in_=dr["ng"][:, :]), w=[constB], dsem=constB.dsem)
        p.op("sp", lambda e: e.dma_start(out=wc[:], in_=dr["wc"].rearrange("p (a b) -> p a b", b=4)), w=[constB], dsem=constB.dsem)
        p.op("dve", lambda e: e.memset(ones_bf[:], 1.0), w=[const2B])
        p.op("dve", lambda e: e.memset(onesf[:], 1.0 / 128.0), w=[const2B])
        p.op("dve", lambda e: e.tensor_scalar(out=ngb[:], in0=gb[:], scalar1=-1.0, scalar2=None, op0=ALU.mult), r=[constB], w=[const2B])
        CB = [constB, const2B, identB]
        tri = sb(es, "tri", [128, 128], BF16)
        eoff = sb(es, "eoff", [128, 128], F32)
        thr = sb(es, "thr", [128, 64], F32)
        jc = sb(es, "jc", [128, NT * 8], F32)
        posI = sb(es, "posI", [128, nseq * 32], I32); posB = p.buf("posI")
        gW = sb(es, "gW", [128, nseq * 32], F32); gWB = p.buf("gW")
        base = sb(es, "base", [128, 8], F32); baseB = p.buf("base")
        NBm = TS // 128
        XO, WO, DO = 0, NT * NBm, NT * NBm + NT * 56
        NTAB = DO + NT * 28
        tabI = sb(es, "tabI", [128, NTAB], I32); tabB = p.buf("tabI")
        cp = sb(es, "cp", [128, 56], F32)
        p.op("sp", lambda e: e.dma_start(out=cp[:], in_=dr["cp"][:, :]), w=[constB], dsem=constB.dsem)
        p.op("pool", lambda e: e.dma_start(out=tri[:], in_=dr["tri"][:, :]), w=[identB], dsem=identB.dsem)
        p.op("sp", lambda e: e.dma_start(out=eoff[:], in_=dr["eoff"][:, :]), w=[constB], dsem=constB.dsem)
        p.op("sp", lambda e: e.dma_start(out=thr[:], in_=dr["thr"][:, :]), w=[constB], dsem=constB.dsem)
        p.op("sp", lambda e: e.dma_start(out=jc[:], in_=dr["jc"][:, :]), w=[constB], dsem=constB.dsem)
        p.op("dve", lambda e: e.memset(base[:], 0.0), w=[baseB])
        IOA = bass.IndirectOffsetOnAxis

        def wview(name, rows_off=0, nrows=D):
            return dr[name][rows_off:rows_off + nrows, :].rearrange("(k p) n -> p k n", p=128)

        def mm(e, o, l, r_, st=True, sp=True):
            return e.matmul(o, l, r_, start=st, stop=sp)

        def build_hT(tb, L):
            xb, xbB = L["xb"].next()
            pst, pstB = L["pst"].next()
            p.op("act", lambda e: e.activation(out=xb[:], in_=h[:, tb, :], func=AF.Copy), r=[hB[tb]], w=[xbB])

            def tr(e):
                inst = None
                for kc in range(KC):
                    inst = e.transpose(pst[:, kc * 128:(kc + 1) * 128], xb[:, kc * 128:(kc + 1) * 128], ident[:])
                return inst
            p.op("pe", tr, r=[xbB, identB], w=[pstB])
            p.op("dve", lambda e: e.tensor_copy(out=hT[:, :, tb * 128:(tb + 1) * 128],
                                                in_=pst[:].rearrange("p (k t) -> p k t", k=KC)), r=[pstB], w=[hTB[tb]])

        def ln_alloc(st, tag):
            L = {}
            L["xb"] = Rot([(sb(st, "xb%s%d" % (tag, i), [128, D], BF16), p.buf("xb")) for i in range(2)])
            L["pst"] = Rot([(ps(st, "pst%s%d" % (tag, i), [128, D], BF16), p.buf("pst")) for i in range(2)])
            L["st6"] = Rot([(sb(st, "st6%s%d" % (tag, i), [128, 12], F32), p.buf("st6")) for i in range(2)])
            L["mv"] = Rot([(sb(st, "mv%s%d" % (tag, i), [128, 4], F32), p.buf("mv")) for i in range(2)])
            L["lnp"] = sb(st, "lnp%s" % tag, [128, 2, D], F32)
            L["lnpB"] = p.buf("lnp", dma=True)
            return L

        def ln_load(L, li):
            src = dr["lnp"][li * 256:(li + 1) * 256, :].rearrange("(a p) n -> p a n", a=2)
            p.op("sp", lambda e: e.dma_start(out=L["lnp"][:], in_=src), w=[L["lnpB"]], dsem=L["lnpB"].dsem)

        def ln_block(tb, L, to_hT=True, out_row0=None, dbg_row0=None, spill=False, gb_eng="pool"):
            st6, st6B = L["st6"].next()
            mv, mvB = L["mv"].next()
            lnp = L["lnp"]
            p.op("dve", lambda e: e.bn_stats(out=st6[:, 0:6], in_=h[:, tb, 0:512]), r=[hB[tb]], w=[st6B])
            p.op("dve", lambda e: e.bn_stats(out=st6[:, 6:12], in_=h[:, tb, 512:1024]), r=[hB[tb]], w=[st6B])
            p.op("dve", lambda e: e.bn_aggr(out=mv[:, 0:2], in_=st6[:]), r=[st6B], w=[mvB])
            p.op("act", lambda e: e.activation(out=mv[:, 2:3], in_=mv[:, 1:2], func=AF.Ln, bias=L["eps"][:, 0:1]), r=[mvB, const2B], w=[mvB])
            p.op("act", lambda e: e.activation(out=mv[:, 3:4], in_=mv[:, 2:3], func=AF.Exp, scale=-0.5), r=[mvB], w=[mvB])

            def fin():
                p.op("dve", lambda e: e.tensor_scalar(out=h[:, tb, :], in0=h[:, tb, :], scalar1=mv[:, 0:1], scalar2=mv[:, 3:4],
                                                      op0=ALU.subtract, op1=ALU.mult), r=[hB[tb], mvB], w=[hB[tb]])
                p.op(gb_eng, lambda e: e.tensor_tensor(out=h[:, tb, :], in0=h[:, tb, :], in1=lnp[:, 0, :], op=ALU.mult), r=[hB[tb], L["lnpB"]], w=[hB[tb]])
                p.op(gb_eng, lambda e: e.tensor_tensor(out=h[:, tb, :], in0=h[:, tb, :], in1=lnp[:, 1, :], op=ALU.add), r=[hB[tb], L["lnpB"]], w=[hB[tb]])
                if dbg_row0 is not None:
                    p.op("sp", lambda e: e.dma_start(out=dbg[dbg_row0 + tb * 128: dbg_row0 + (tb + 1) * 128, :], in_=h[:, tb, :]),
                         r=[hB[tb]], dsem=hB[tb].dsem)
                if spill:
                    p.op("sp", lambda e: e.dma_start(out=hsp[tb * 128:(tb + 1) * 128, :], in_=h[:, tb, :]), r=[hB[tb]], dsem=hB[tb].dsem)
                if out_row0 is not None:
                    p.op("sp", lambda e: e.dma_start(out=out[out_row0 + tb * 128: out_row0 + (tb + 1) * 128, :], in_=h[:, tb, :]),
                         r=[hB[tb]], dsem=hB[tb].dsem)
                elif to_hT:
                    build_hT(tb, L)
            return fin

        class LnPipe:
            def __init__(self):
                self.pend = None

            def push(self, fin):
                if self.pend is not None:
                    self.pend()
                self.pend = fin

            def flush(self):
                if self.pend is not None:
                    self.pend()
                self.pend = None

        epsT = sb(es, "epsT", [128, 1], F32)
        onesf_one = sb(es, "oneT", [128, 1], F32)
        p.op("dve", lambda e: e.memset(epsT[:], EPS), w=[const2B])
        p.op("dve", lambda e: e.memset(onesf_one[:], 1.0), w=[const2B])

        def wload(dst_ap, src_ap, B):
            return p.op("pool", lambda e: e.dma_start(out=dst_ap, in_=src_ap), w=[B], dsem=B.dsem)

        def proj_fm(W, wname, col0, evac, ncols=128):
            wt, wtB = W["wA"].next()
            wload(wt[:, :, 0:ncols], wview(wname)[:, :, col0:col0 + ncols], wtB)
            for tc in range(4):
                pp, ppB = W["pp"].next()

                def f(e, pp=pp, wt=wt, tc=tc):
                    inst = None
                    for kc in range(KC):
                        inst = mm(e, pp[0:ncols, :], wt[:, kc, 0:ncols], hT[:, kc, tc * 512:(tc + 1) * 512], kc == 0, kc == KC - 1)
                    return inst
                p.op("pe", f, r=[wtB] + hTB[4 * tc:4 * tc + 4], w=[ppB])
                evac(tc, pp, ppB)

        def proj_tm(W, wname, col0, ncols, sets, evac):
            wt, wtB = W["wA"].next()
            wload(wt[:, :, 0:ncols], wview(wname)[:, :, col0:col0 + ncols], wtB)
            for si, tsl in enumerate(sets):
                pp, ppB = W["pp"].next()

                def f(e, pp=pp, wt=wt, tsl=tsl):
                    inst = None
                    for kc in range(KC):
                        inst = mm(e, pp[:, 0:ncols], hT[:, kc, tsl], wt[:, kc, 0:ncols], kc == 0, kc == KC - 1)
                    return inst
                p.op("pe", f, r=[wtB] + hTB, w=[ppB])
                evac(si, pp, ppB)

        def do_seq(s):
            row0 = s * S
            def _ph1():
                with ExitStack() as st:
                    p.phase_begin()
                    L = ln_alloc(st, "p0")
                    L["eps"] = epsT
                    for tb in range(TB):
                        p.op("sp", lambda e, tb=tb, row0=row0: e.dma_start(out=h[:, tb, :], in_=dr["x"][row0 + tb * 128: row0 + (tb + 1) * 128, :]),
                             w=[hB[tb]], dsem=hB[tb].dsem)
                    for tb in range(TB):
                        build_hT(tb, L)
                    p.barrier()
            _ph1()
            if phases < 1:
                return

            def _ph2():
                with ExitStack() as st:
                    p.phase_begin()
                    W = {}
                    W["wA"] = Rot([(sb(st, "wA%d" % i, [128, KC, 128], BF16), p.buf("wA", dma=True)) for i in range(3)])
                    qT = sb(st, "qT", [128, S], BF16); qTB = p.buf("qT")
                    kT = sb(st, "kT", [128, S], BF16); kTB = p.buf("kT")
                    Vt = sb(st, "Vt", [128, 3, TB, 128], BF16); VB = [p.buf("V%d" % i) for i in range(3)]
                    t0 = hs[:, 0:S]; t0B = p.buf("t0")
                    t1 = hs[:, S:2 * S]; t1B = p.buf("t1")
                    hi = sb(st, "hi", [4, S], BF16); lo = sb(st, "lo", [4, S], BF16); hlB = p.buf("hl")
                    augQ = sb(st, "augQ", [4, S], BF16); augQB = p.buf("augQ")
                    augK = sb(st, "augK", [4, S], BF16); augKB = p.buf("augK")
                    tq = sb(st, "tq", [4, S], BF16); tqB = p.buf("tq")
                    pT = Rot([(sb(st, "pT%d" % i, [128, 512], BF16), p.buf("pT")) for i in range(4)])
                    rec = hs[0:64, 2 * S:3 * S]; recB = p.buf("rec")
                    sts = Rot([(ps(st, "st%d" % i, [128, 512]), p.buf("st")) for i in range(4)])
                    W["pp"] = Rot(sts.items)
                    LA = 2
                    accn = Rot([(ps(st, "accn%d" % i, [64, 512]), p.buf("accn")) for i in range(2)])
                    accd = Rot([(ps(st, "accd%d" % i, [64, 512]), p.buf("accd")) for i in range(2)])
                    rope = hs[:, 3 * S:5 * S].rearrange("p (a b) -> p a b", a=2); ropeB = p.buf("rope", dma=True)
                    rt = Rot([(hs[:, 5 * S + i * 1024:5 * S + (i + 1) * 1024].rearrange("p (a b) -> p a b", a=2), p.buf("rt")) for i in range(2)])
                    an = hs[0:64, 6 * S:7 * S]; anB = p.buf("an")
                    ad = hs[0:64, 7 * S:8 * S]; adB = p.buf("ad")
                    p.op("sp", lambda e: e.dma_start(out=rope, in_=dr["rope"].rearrange("p (a b) -> p a b", a=2)), w=[ropeB], dsem=ropeB.dsem)

                    def make_aug(src_ap, srcB):
                        p.op("dve", lambda e: e.tensor_copy(out=hi[:], in_=src_ap), r=[srcB], w=[hlB])
                        p.op("dve", lambda e: e.tensor_tensor(out=lo[:], in0=src_ap, in1=hi[:], op=ALU.subtract), r=[srcB, hlB], w=[hlB])

                    def fin_aug(dst, dstB, c0):
                        p.op("dve", lambda e: e.tensor_scalar(out=tq[:], in0=hi[:], scalar1=augc[0:4, c0:c0 + 1], scalar2=augc[0:4, c0 + 2:c0 + 3],
                                                              op0=ALU.mult, op1=ALU.add), r=[hlB] + CB, w=[tqB])
                        p.op("dve", lambda e: e.scalar_tensor_tensor(out=dst[:], in0=lo[:], scalar=augc[0:4, c0 + 1:c0 + 2], in1=tq[:],
                                                                     op0=ALU.mult, op1=ALU.add), r=[hlB, tqB] + CB, w=[dstB])

                    for c in range(4):
                        def ev_q(tc, pp, ppB):
                            p.op("act", lambda e: e.activation(out=qT[:, tc * 512:(tc + 1) * 512], in_=pp[:], func=AF.Copy, scale=0.125), r=[ppB], w=[qTB])

                        def ev_k(tc, pp, ppB):
                            p.op("dve", lambda e: e.tensor_copy(out=kT[:, tc * 512:(tc + 1) * 512], in_=pp[:]), r=[ppB], w=[kTB])

                        def ev_v(si, pp, ppB):
                            p.op("act", lambda e: e.activation(out=Vt[:, 0, si, :], in_=pp[:, 0:128], func=AF.Copy), r=[ppB], w=[VB[0]])
                        proj_fm(W, "wqa", c * 128, ev_q)
                        proj_fm(W, "wka", c * 128, ev_k)
                        proj_tm(W, "wva", c * 128, 128, [slice(tb * 128, (tb + 1) * 128) for tb in range(TB)], ev_v)
                        for hh in range(2):
                            head = 2 * c + hh
                            r0 = 64 * hh

                            def ev_g(tc, pp, ppB, head=head):
                                p.op("act", lambda e: e.activation(out=t0[:, tc * 512:(tc + 1) * 512], in_=pp[:], func=AF.Exp,
                                                                   bias=ngb[:, head:head + 1], scale=-1.0), r=[ppB] + CB, w=[t0B])
                            proj_fm(W, "wfa", head * 128, ev_g)
                            p.op("act", lambda e: e.activation(out=t0[:], in_=t0[:], func=AF.Ln, bias=onesf_one[:, 0:1]), r=[t0B] + CB, w=[t0B])
                            p.op("dve", lambda e: e.tensor_tensor_scan(out=t1[:], data0=ones_bf[:], data1=t0[:], initial=0.0,
                                                                       op0=ALU.mult, op1=ALU.add), r=[t0B] + CB, w=[t1B])
                            make_aug(t1[0:4, :], t1B)
                            fin_aug(augQ, augQB, 0)
                            fin_aug(augK, augKB, 3)
                            acc = {}

                            def fox1(qc, kb, r0=r0):
                                if kb == 0:
                                    acc[qc] = accn.next() + accd.next()
                                nkb = 4 * qc + 4
                                j0 = max(0, kb - 4 * qc)
                                c0 = j0 * 128
                                stt, sttB = sts.next()
                                pt, ptB = pT.next()
                                diag = kb >= 4 * qc

                                def fs(e):
                                    kblk = slice(kb * 128, (kb + 1) * 128)
                                    q0 = qc * 512
                                    inst = None
                                    if diag:
                                        qs = slice(q0 + c0, q0 + c0 + 128)
                                        mm(e, stt[:, c0:c0 + 128], kT[r0:r0 + 64, kblk], qT[r0:r0 + 64, qs], True, False)
                                        mm(e, stt[:, c0:c0 + 128], augK[0:4, kblk], augQ[0:4, qs], False, False)
                                        inst = mm(e, stt[:, c0:c0 + 128], ident[:], masks[:, 0, :], False, True)
                                        c1 = c0 + 128
                                    else:
                                        c1 = c0
                                    if c1 < 512:
                                        qs = slice(q0 + c1, q0 + 512)
                                        mm(e, stt[:, c1:512], kT[r0:r0 + 64, kblk], qT[r0:r0 + 64, qs], True, False)
                                        inst = mm(e, stt[:, c1:512], augK[0:4, kblk], augQ[0:4, qs], False, True)
                                    return inst
                                p.op("pe", fs, r=[kTB, qTB, augKB, augQB] + CB, w=[sttB])
                                p.op("act", lambda e: e.activation(out=pt[:, c0:512], in_=stt[:, c0:512], func=AF.Exp), r=[sttB], w=[ptB])
                                return (qc, kb, nkb, c0, pt, ptB)

                            def fox2(info, r0=r0, hh=hh, c=c):
                                qc, kb, nkb, c0, pt, ptB = info
                                an_, anB_, ad_, adB_ = acc[qc]

                                def fpv(e):
                                    mm(e, an_[:, c0:512], Vt[:, 0, kb, hh * 64:(hh + 1) * 64], pt[:, c0:512], kb == 0, kb == nkb - 1)
                                    return mm(e, ad_[:, c0:512], ones_bf[:, 0:64], pt[:, c0:512], kb == 0, kb == nkb - 1)
                                p.op("pe", fpv, r=[ptB, VB[0]] + CB, w=[anB_, adB_])
                                if kb == nkb - 1:
                                    p.op("dve", lambda e: e.reciprocal(out=rec[:, qc * 512:(qc + 1) * 512], in_=ad_[:]), r=[adB_], w=[recB])
                                    p.op("dve", lambda e: e.tensor_tensor(
                                        out=oT[r0:r0 + 64, c, qc * 512:(qc + 1) * 512], in0=an_[:], in1=rec[:, qc * 512:(qc + 1) * 512], op=ALU.mult),
                                        r=[anB_, recB], w=[oTB[c]])

                            pend = []
                            for qc in range(4):
                                for kb in range(4 * qc + 4):
                                    pend.append(fox1(qc, kb))
                                    if len(pend) > LA:
                                        fox2(pend.pop(0))
                            while pend:
                                fox2(pend.pop(0))

                    def tokset(bi, si):
                        if bi == 0:
                            return slice(si * 128, (si + 1) * 128)
                        if bi == 1:
                            r_, n_ = si // 4, si % 4
                            return slice(512 * n_ + r_, 512 * (n_ + 1), 4)
                        return slice(si, S, 16)

                    def accview(t, bi, si):
                        if bi == 0:
                            return t[:, :].rearrange("p (n j) -> p n j", j=128)[:, si:si + 2, :]
                        if bi == 1:
                            r_, n_ = si // 4, si % 4
                            return t[:, :].rearrange("p (n j r) -> p n j r", n=4, j=128, r=4)[:, n_:n_ + 2, :, r_]
                        return t[:, :].rearrange("p (j r) -> p r j", r=16)[:, si:si + 2, :]

                    for c in range(4):
                        def mk_rope(dst, dstB, wn, wns, c=c):
                            store = {}

                            def ev_a(tc, pp, ppB):
                                rtt, rtB = rt.next()
                                store[tc] = (rtt, rtB)
                                p.op("dve", lambda e: e.tensor_tensor(out=rtt[:, 0, :], in0=pp[:], in1=rope[:, 0, tc * 512:(tc + 1) * 512], op=ALU.mult),
                                     r=[ppB, ropeB], w=[rtB])

                            def ev_b(tc, pp, ppB):
                                rtt, rtB = store[tc]
                                p.op("dve", lambda e: e.tensor_tensor(out=rtt[:, 1, :], in0=pp[:], in1=rope[:, 1, tc * 512:(tc + 1) * 512], op=ALU.mult),
                                     r=[ppB, ropeB], w=[rtB])
                                p.op("pool", lambda e: e.tensor_tensor(out=dst[:, tc * 512:(tc + 1) * 512], in0=rtt[:, 0, :], in1=rtt[:, 1, :], op=ALU.add),
                                     r=[rtB], w=[dstB])
                            wa, waB = W["wA"].next()
                            wb, wbB = W["wA"].next()
                            wload(wa[:], wview(wn)[:, :, c * 128:(c + 1) * 128], waB)
                            wload(wb[:], wview(wns)[:, :, c * 128:(c + 1) * 128], wbB)
                            for tc in range(4):
                                for (wt_, wtB_, ev) in ((wa, waB, ev_a), (wb, wbB, ev_b)):
                                    pp, ppB = W["pp"].next()

                                    def f(e, pp=pp, wt_=wt_, tc=tc):
                                        inst = None
                                        for kc in range(KC):
                                            inst = mm(e, pp[:], wt_[:, kc, :], hT[:, kc, tc * 512:(tc + 1) * 512], kc == 0, kc == KC - 1)
                                        return inst
                                    p.op("pe", f, r=[wtB_] + hTB[4 * tc:4 * tc + 4], w=[ppB])
                                    ev(tc, pp, ppB)
                        mk_rope(qT, qTB, "wqd", "wqds")
                        mk_rope(kT, kTB, "wkd", "wkds")
                        for bi in range(3):
                            def ev_v(si, pp, ppB, bi=bi):
                                p.op("act", lambda e: e.activation(out=Vt[:, bi, si, :], in_=pp[:, 0:128], func=AF.Copy), r=[ppB], w=[VB[bi]])
                            proj_tm(W, "wvd", c * 128, 128, [tokset(bi, si) for si in range(16)], ev_v)
                        def dil1(blocks, mi, bi, si, r0):
                            stt, sttB = sts.next()
                            pt, ptB = pT.next()

                            def fs(e):
                                inst = None
                                for bk, (sq, sk) in enumerate(blocks):
                                    mm(e, stt[:, bk * 128:(bk + 1) * 128], kT[r0:r0 + 64, tokset(bi, sk)], qT[r0:r0 + 64, tokset(bi, sq)], True, False)
                                    inst = mm(e, stt[:, bk * 128:(bk + 1) * 128], ident[:], mask4[:, mi, bk * 128:(bk + 1) * 128], False, True)
                                return inst
                            p.op("pe", fs, r=[kTB, qTB] + CB, w=[sttB])
                            p.op("act", lambda e: e.activation(out=pt[:], in_=stt[:], func=AF.Exp, scale=0.125), r=[sttB], w=[ptB])
                            return (blocks, bi, si, pt, ptB)

                        def dil2(info, hh):
                            blocks, bi, si, pt, ptB = info
                            an_, anB_ = accn.next()
                            ad_, adB_ = accd.next()

                            def fpv(e):
                                inst = None
                                for bk, (sq, sk) in enumerate(blocks):
                                    qi = bk // 2
                                    first = (bk % 2 == 0)
                                    mm(e, an_[:, qi * 128:(qi + 1) * 128], Vt[:, bi, sk, hh * 64:(hh + 1) * 64], pt[:, bk * 128:(bk + 1) * 128], first, not first)
                                    inst = mm(e, ad_[:, qi * 128:(qi + 1) * 128], ones_bf[:, 0:64], pt[:, bk * 128:(bk + 1) * 128], first, not first)
                                return inst
                            p.op("pe", fpv, r=[ptB, VB[bi]] + CB, w=[anB_, adB_])
                            pv = lambda t: t[:, 0:256].rearrange("p (a j) -> p a j", a=2)
                            if bi == 0:
                                p.op("dve", lambda e: e.tensor_copy(out=accview(an, 0, si), in_=pv(an_)), r=[anB_], w=[anB])
                                p.op("dve", lambda e: e.tensor_copy(out=accview(ad, 0, si), in_=pv(ad_)), r=[adB_], w=[adB])
                            else:
                                p.op("dve", lambda e: e.tensor_tensor(out=accview(an, bi, si), in0=pv(an_), in1=accview(an, bi, si), op=ALU.add),
                                     r=[anB_, anB], w=[anB])
                                p.op("dve", lambda e: e.tensor_tensor(out=accview(ad, bi, si), in0=pv(ad_), in1=accview(ad, bi, si), op=ALU.add),
                                     r=[adB_, adB], w=[adB])

                        for hh in range(2):
                            r0 = 64 * hh
                            dpend = []
                            for bi in range(3):
                                for si in range(0, 16, 2):
                                    blocks = []
                                    for sq in (si, si + 1):
                                        if bi == 0:
                                            prev = sq - 1 if sq >= 1 else None
                                        elif bi == 1:
                                            prev = sq - 1 if (sq % 4) >= 1 else None
                                        else:
                                            prev = None
                                        blocks.append((sq, prev if prev is not None else sq))
                                        blocks.append((sq, sq))
                                    if bi == 2:
                                        mi = 2
                                    elif (bi == 0 and si == 0) or (bi == 1 and si % 4 == 0):
                                        mi = 1
                                    else:
                                        mi = 0
                                    dpend.append(dil1(blocks, mi, bi, si, r0))
                                    if len(dpend) > LA:
                                        dil2(dpend.pop(0), hh)
                            while dpend:
                                dil2(dpend.pop(0), hh)
                            p.op("dve", lambda e: e.reciprocal(out=rec[:], in_=ad[:]), r=[adB], w=[recB])
                            p.op("dve", lambda e, r0=r0, c=c: e.tensor_tensor(out=oT[r0:r0 + 64, 4 + c, :], in0=an[:], in1=rec[:], op=ALU.mult),
                                 r=[anB, recB], w=[oTB[4 + c]])
                    if debug and s == 0:
                        dB = p.buf("dbg2", dma=True)
                        p.op("pool", lambda e: e.dma_start(out=dbg2[0:128, :].rearrange("p (a b) -> p a b", a=KC), in_=oT[:]), r=oTB, dsem=dB.dsem)
                        p.op("pool", lambda e: e.dma_start(out=dbg2[128:256, 0:S], in_=qT[:]), r=[qTB], dsem=dB.dsem)
                        p.op("pool", lambda e: e.dma_start(out=dbg2[128:256, S:2 * S], in_=kT[:]), r=[kTB], dsem=dB.dsem)
                        p.op("pool", lambda e: e.dma_start(out=dbg2[128:192, 2 * S:3 * S], in_=an), r=[anB], dsem=dB.dsem)
                        p.op("pool", lambda e: e.dma_start(out=dbg2[128:192, 3 * S:4 * S], in_=ad), r=[adB], dsem=dB.dsem)
                        p.op("pool", lambda e: e.dma_start(out=dbg2[128:256, 4 * S:5 * S], in_=Vt[:, 1, :, :].rearrange("p a b -> p (a b)")), r=VB, dsem=dB.dsem)
                    p.barrier()

            _ph2()
            def mix_stage(wname, li, dbg_i, row0=row0):
                with ExitStack() as st:
                    p.phase_begin()
                    L = ln_alloc(st, "m%d" % li)
                    L["eps"] = epsT
                    ln_load(L, li)
                    for tb in range(TB):
                        src = dr["x"][row0 + tb * 128: row0 + (tb + 1) * 128, :] if li == 0 else hsp[tb * 128:(tb + 1) * 128, :]
                        p.op("sp", lambda e, tb=tb, src=src: e.dma_start(out=h[:, tb, :], in_=src), w=[hB[tb]], dsem=hB[tb].dsem)
                    wo = sb(st, "wo", [128, KC, D], BF16)
                    woB = p.buf("wo", dma=True)
                    wload(wo[:, :, 0:512], wview(wname)[:, :, 0:512], woB)
                    wload(wo[:, :, 512:1024], wview(wname)[:, :, 512:1024], woB)
                    mixp = Rot([(ps(st, "mix%d" % i, [128, D]), p.buf("mix")) for i in range(2)])
                    lp = LnPipe()
                    for tb in range(TB):
                        mp, mpB = mixp.next()

                        def f(e, mp=mp, tb=tb):
                            inst = None
                            for half in range(2):
                                for c in range(KC):
                                    inst = mm(e, mp[:, half * 512:(half + 1) * 512], oT[:, c, tb * 128:(tb + 1) * 128],
                                              wo[:, c, half * 512:(half + 1) * 512], c == 0, c == KC - 1)
                            return inst
                        p.op("pe", f, r=[woB] + oTB, w=[mpB])
                        p.op("dve", lambda e, mp=mp, tb=tb: e.scalar_tensor_tensor(out=h[:, tb, :], in0=h[:, tb, :], scalar=ALPHA, in1=mp[:],
                                                                                   op0=ALU.mult, op1=ALU.add), r=[hB[tb], mpB], w=[hB[tb]])
                        lp.push(ln_block(tb, L, to_hT=True, dbg_row0=(dbg_i * S if (debug and s == 0) else None)))
                    lp.flush()
                    p.barrier()
            mix_stage("wo0", 0, 0)
            if phases < 2:
                return

            def ffn_alloc(st):
                Fd = {}
                Fd["wg"] = Rot([(sb(st, "wg%d" % i, [128, KC, 512], BF16), p.buf("wg", dma=True)) for i in range(2)])
                Fd["wu"] = Rot([(sb(st, "wu%d" % i, [128, KC, 512], BF16), p.buf("wu", dma=True)) for i in range(2)])
                Fd["wd"] = Rot([(sb(st, "wd%d" % i, [128, 4, D], BF16), p.buf("wd", dma=True)) for i in range(2)])
                Fd["aT"] = Rot([(oT[:, 4 * i:4 * i + 4, :], p.buf("aT")) for i in range(2)])
                Fd["sg"] = Rot([(sb(st, "sg%d" % i, [128, 512], F32), p.buf("sg")) for i in range(2)])
                Fd["pg"] = Rot([(ps(st, "pg%d" % i, [128, 512]), p.buf("pg")) for i in range(2)])
                Fd["pu"] = Rot([(ps(st, "pu%d" % i, [128, 512]), p.buf("pu")) for i in range(2)])
                Fd["yp"] = Rot([(ps(st, "yp%d" % i, [128, D]), p.buf("yp")) for i in range(2)])
                return Fd

            def ffn(Fd, gname, uname, dname, grow0, drow0, F, gate_fn):
                f0 = 0
                while f0 < F:
                    gw = min(512, F - f0)
                    gc = gw // 128
                    wg, wgB = Fd["wg"].next()
                    wu, wuB = Fd["wu"].next()
                    wd, wdB = Fd["wd"].next()
                    aT, aTB = Fd["aT"].next()
                    wload(wg[:, :, 0:gw], wview(gname, grow0, D)[:, :, f0:f0 + gw], wgB)
                    wload(wu[:, :, 0:gw], wview(uname, grow0, D)[:, :, f0:f0 + gw], wuB)
                    wload(wd[:, 0:gc, :], wview(dname, drow0 + f0, gw), wdB)
                    for ci in range(gc):
                        for tc in range(4):
                            pg, pgB = Fd["pg"].next()
                            pu, puB = Fd["pu"].next()
                            sg, sgB = Fd["sg"].next()

                            def fg_(e, pg=pg, wg=wg, ci=ci, tc=tc):
                                inst = None
                                for kc in range(KC):
                                    inst = mm(e, pg[:], wg[:, kc, ci * 128:(ci + 1) * 128], hT[:, kc, tc * 512:(tc + 1) * 512], kc == 0, kc == KC - 1)
                                return inst

                            def fu_(e, pu=pu, wu=wu, ci=ci, tc=tc):
                                inst = None
                                for kc in range(KC):
                                    inst = mm(e, pu[:], wu[:, kc, ci * 128:(ci + 1) * 128], hT[:, kc, tc * 512:(tc + 1) * 512], kc == 0, kc == KC - 1)
                                return inst
                            p.op("pe", fg_, r=[wgB] + hTB[4 * tc:4 * tc + 4], w=[pgB])
                            p.op("pe", fu_, r=[wuB] + hTB[4 * tc:4 * tc + 4], w=[puB])
                            p.op("act", lambda e, sg=sg, pg=pg: e.activation(out=sg[:], in_=pg[:], func=AF.Silu), r=[pgB], w=[sgB])
                            p.op("dve", lambda e, sg=sg, pu=pu, aT=aT, ci=ci, tc=tc: e.tensor_tensor(
                                out=aT[:, ci, tc * 512:(tc + 1) * 512], in0=pu[:], in1=sg[:], op=ALU.mult), r=[puB, sgB], w=[aTB])
                    for tb in range(TB):
                        yp, ypB = Fd["yp"].next()

                        def fd_(e, yp=yp, aT=aT, wd=wd, tb=tb, gc=gc):
                            inst = None
                            for half in range(2):
                                for ci in range(gc):
                                    inst = mm(e, yp[:, half * 512:(half + 1) * 512], aT[:, ci, tb * 128:(tb + 1) * 128],
                                              wd[:, ci, half * 512:(half + 1) * 512], ci == 0, ci == gc - 1)
                            return inst
                        p.op("pe", fd_, r=[aTB, wdB], w=[ypB])
                        g_ap, gBs = gate_fn(tb)
                        p.op("dve", lambda e, yp=yp, tb=tb, g_ap=g_ap: e.scalar_tensor_tensor(out=h[:, tb, :], in0=yp[:], scalar=g_ap, in1=h[:, tb, :],
                                                                                            op0=ALU.mult, op1=ALU.add), r=[ypB, hB[tb]] + gBs, w=[hB[tb]])
                    f0 += gw

            def scale_h():
                for tb in range(TB):
                    p.op("act", lambda e, tb=tb: e.mul(out=h[:, tb, :], in_=h[:, tb, :], mul=ALPHA), r=[hB[tb]], w=[hB[tb]])

            def final_ln(li, dbg_i, to_hT, out_row0, spill=False):
                outs = []
                with ExitStack() as st:
                    p.phase_begin()
                    L = ln_alloc(st, "f%d" % li)
                    L["eps"] = epsT
                    ln_load(L, li)
                    lp = LnPipe()
                    for tb in range(TB):
                        lp.push(ln_block(tb, L, to_hT=to_hT, out_row0=out_row0, dbg_row0=(dbg_i * S if (debug and s == 0) else None), spill=spill))
                    lp.flush()
                    p.barrier()
                return outs

            def _ph3():
                with ExitStack() as st:
                    p.phase_begin()
                    Fd = ffn_alloc(st)
                    scale_h()
                    ffn(Fd, "fg", "fu", "fd", 0, 0, DFF, lambda tb: (1.0, []))
                    p.barrier()
            _ph3()
            final_ln(1, 1, True, None, spill=True)
            if phases < 3:
                return

            def _ph4():
                with ExitStack() as st:
                    p.phase_begin()
                    W = {}
                    W["wA"] = Rot([(sb(st, "wA%d" % i, [128, KC, 128], BF16), p.buf("wA", dma=True)) for i in range(3)])
                    qT = sb(st, "qT", [128, S], BF16); qTB = p.buf("qT")
                    kT = sb(st, "kT", [128, S], BF16); kTB = p.buf("kT")
                    Vh = sb(st, "Vh", [128, TB, 128], BF16); VhB = p.buf("Vh")
                    sgo = sb(st, "sgo", [128, S], BF16); sgoB = p.buf("sgo")
                    pre = hs[:, 0:3 + S]; preB = p.buf("pre"); padB = p.buf("pad")
                    yv = hs[:, 2052:2052 + S]; yvB = p.buf("yv")
                    t0 = hs[:, 3:3 + S]; t0B = preB
                    t1 = hs[:, 4100:4100 + S]; t1B = p.buf("t1")
                    t2 = yv; t2B = yvB
                    hi = sb(st, "hi", [4, S], BF16); lo = sb(st, "lo", [4, S], BF16); hlB = p.buf("hl")
                    ua = hs[0:4, 9220:9220 + S]; uaB = p.buf("ua")
                    augQ = sb(st, "augQ", [4, S], BF16); augQB = p.buf("augQ")
                    augK = sb(st, "augK", [4, S], BF16); augKB = p.buf("augK")
                    tq = sb(st, "tq", [4, S], BF16); tqB = p.buf("tq")
                    pT = Rot([(sb(st, "pT%d" % i, [128, 512], BF16), p.buf("pT")) for i in range(3)])
                    Et = Rot([(hs[:, 12288 + i * 512:12288 + (i + 1) * 512], p.buf("Et")) for i in range(3)])
                    psA = Rot([(ps(st, "psA%d" % i, [128, 512]), p.buf("psA")) for i in range(3)])
                    psB = Rot([(ps(st, "psB%d" % i, [128, 512]), p.buf("psB")) for i in range(3)])
                    accn_t = ps(st, "accn", [128, 512]); accnB = p.buf("accn")
                    accd_t = ps(st, "accd", [128, 512]); accdB = p.buf("accd")
                    W["pp"] = Rot(psA.items + psB.items)
                    f1 = hs[:, 7172:7684]; f1B = p.buf("f1")
                    f2 = hs[:, 7684:8196]; f2B = p.buf("f2")
                    f3 = hs[:, 8196:8708]; f3B = p.buf("f3")
                    f4 = hs[:, 8708:9220]; f4B = p.buf("f4")
                    p.op("dve", lambda e: e.memset(pre[:, 0:3], 0.0), w=[padB])

                    def make_aug(src_ap, srcB):
                        p.op("dve", lambda e: e.tensor_copy(out=hi[:], in_=src_ap), r=[srcB], w=[hlB])
                        p.op("dve", lambda e: e.tensor_tensor(out=lo[:], in0=src_ap, in1=hi[:], op=ALU.subtract), r=[srcB, hlB], w=[hlB])

                    def fin_aug(dst, dstB, c0):
                        p.op("dve", lambda e: e.tensor_scalar(out=tq[:], in0=hi[:], scalar1=augc[0:4, c0:c0 + 1], scalar2=augc[0:4, c0 + 2:c0 + 3],
                                                              op0=ALU.mult, op1=ALU.add), r=[hlB] + CB, w=[tqB])
                        p.op("dve", lambda e: e.scalar_tensor_tensor(out=dst[:], in0=lo[:], scalar=augc[0:4, c0 + 1:c0 + 2], in1=tq[:],
                                                                     op0=ALU.mult, op1=ALU.add), r=[hlB, tqB] + CB, w=[dstB])

                    for hd in range(8):
                        def conv_silu(wname, col0, chunk, dst, dstB):
                            def ev(tc, pp, ppB):
                                p.op("act", lambda e: e.activation(out=pre[:, 3 + tc * 512: 3 + (tc + 1) * 512], in_=pp[:], func=AF.Copy), r=[ppB], w=[preB])
                            proj_fm(W, wname, col0, ev)
                            p.op("dve", lambda e: e.tensor_scalar(out=yv[:], in0=pre[:, 3:3 + S], scalar1=wc[:, chunk, 3:4], scalar2=None, op0=ALU.mult),
                                 r=[preB, padB] + CB, w=[yvB])
                            for i in range(3):
                                p.op("dve", lambda e, i=i: e.scalar_tensor_tensor(out=yv[:], in0=pre[:, i:i + S], scalar=wc[:, chunk, i:i + 1], in1=yv[:],
                                                                                 op0=ALU.mult, op1=ALU.add), r=[preB, padB, yvB] + CB, w=[yvB])
                            p.op("act", lambda e: e.activation(out=dst[:], in_=yv[:], func=AF.Silu), r=[yvB], w=[dstB])
                        conv_silu("wq1", hd * 128, hd, qT, qTB)
                        conv_silu("wk1", hd * 128, 8 + hd, kT, kTB)

                        def ev_v(si, pp, ppB):
                            p.op("act", lambda e: e.activation(out=Vh[:, si, :], in_=pp[:, 0:128], func=AF.Copy), r=[ppB], w=[VhB])
                        proj_tm(W, "wv1", hd * 128, 128, [slice(tb * 128, (tb + 1) * 128) for tb in range(TB)], ev_v)

                        def ev_og(tc, pp, ppB):
                            p.op("act", lambda e: e.activation(out=sgo[:, tc * 512:(tc + 1) * 512], in_=pp[:], func=AF.Sigmoid), r=[ppB], w=[sgoB])
                        proj_fm(W, "wog", hd * 128, ev_og)

                        def ev_f(tc, pp, ppB, hd=hd):
                            p.op("act", lambda e: e.activation(out=t0[:, tc * 512:(tc + 1) * 512], in_=pp[:], func=AF.Exp,
                                                               bias=ngb[:, 16 + hd:17 + hd], scale=-1.0), r=[ppB] + CB, w=[t0B])
                        proj_fm(W, "wfg", hd * 128, ev_f)
                        p.op("act", lambda e: e.activation(out=t0[:], in_=t0[:], func=AF.Ln, bias=onesf_one[:, 0:1]), r=[t0B] + CB, w=[t0B])
                        p.op("dve", lambda e: e.tensor_tensor_scan(out=t1[:], data0=ones_bf[:], data1=t0[:], initial=0.0, op0=ALU.mult, op1=ALU.add),
                             r=[t0B] + CB, w=[t1B])

                        def ev_i(tc, pp, ppB, hd=hd):
                            p.op("dve", lambda e: e.scalar_tensor_tensor(out=t0[:, tc * 512:(tc + 1) * 512], in0=pp[:], scalar=gb[:, 8 + hd:9 + hd],
                                                                         in1=t1[:, tc * 512:(tc + 1) * 512], op0=ALU.add, op1=ALU.add),
                                 r=[ppB, t1B] + CB, w=[t0B])
                        proj_fm(W, "wig", hd * 128, ev_i)
                        p.op("dve", lambda e: e.tensor_tensor_scan(out=t2[:], data0=t0[:], data1=t0[:], initial=-1e30, op0=ALU.max, op1=ALU.max),
                             r=[t0B] + CB, w=[t2B])
                        p.op("dve", lambda e: e.tensor_tensor(out=t1[:], in0=t1[:], in1=t2[:], op=ALU.subtract), r=[t1B, t2B], w=[t1B])
                        p.op("act", lambda e: e.activation(out=t1[:], in_=t1[:], func=AF.Exp), r=[t1B], w=[t1B])
                        make_aug(t2[0:4, :], t2B)
                        fin_aug(augQ, augQB, 0)
                        p.op("dve", lambda e: e.tensor_scalar(out=ua[:], in0=t0[0:4, :], scalar1=LNS, scalar2=None, op0=ALU.add), r=[t0B], w=[uaB])
                        make_aug(ua[:], uaB)
                        fin_aug(augK, augKB, 3)

                        def stage1(qc, kb):
                            nkb = 4 * qc + 4
                            j0 = max(0, kb - 4 * qc)
                            c0 = j0 * 128
                            diag = kb >= 4 * qc
                            pa, paB = psA.next()
                            pb, pbB = psB.next()
                            pt, ptB = pT.next()
                            et, etB = Et.next()
                            kblk = slice(kb * 128, (kb + 1) * 128)
                            qs = slice(qc * 512 + c0, qc * 512 + 512)
                            p.op("pe", lambda e: mm(e, pa[:, c0:512], kT[:, kblk], qT[:, qs], True, True), r=[kTB, qTB], w=[paB])

                            def fd_(e):
                                q0 = qc * 512
                                inst = None
                                c1 = c0
                                if diag:
                                    qs1 = slice(q0 + c0, q0 + c0 + 128)
                                    mm(e, pb[:, c0:c0 + 128], augK[0:4, kblk], augQ[0:4, qs1], True, False)
                                    inst = mm(e, pb[:, c0:c0 + 128], ident[:], masks[:, 0, :], False, True)
                                    c1 = c0 + 128
                                if c1 < 512:
                                    inst = mm(e, pb[:, c1:512], augK[0:4, kblk], augQ[0:4, slice(q0 + c1, q0 + 512)], True, True)
                                return inst
                            p.op("pe", fd_, r=[augKB, augQB] + CB, w=[pbB])
                            p.op("act", lambda e: e.activation(out=et[:, c0:512], in_=pb[:, c0:512], func=AF.Exp), r=[pbB], w=[etB])
                            p.op("dve", lambda e: e.tensor_tensor(out=pt[:, c0:512], in0=pa[:, c0:512], in1=et[:, c0:512], op=ALU.mult), r=[paB, etB], w=[ptB])
                            return (qc, kb, nkb, c0, pt, ptB)

                        def finalize(qc, hd=hd):
                            cs = slice(qc * 512, (qc + 1) * 512)
                            s1, s1B = W["pp"].next()
                            s2, s2B = W["pp"].next()
                            p.op("act", lambda e: e.activation(out=f1[:], in_=accd_t[:], func=AF.Abs), r=[accdB], w=[f1B])
                            p.op("dve", lambda e: e.tensor_tensor(out=f1[:], in0=f1[:], in1=t1[:, cs], op=ALU.max), r=[f1B, t1B], w=[f1B])
                            p.op("dve", lambda e: e.reciprocal(out=f1[:], in_=f1[:]), r=[f1B], w=[f1B])
                            p.op("dve", lambda e: e.tensor_tensor(out=f2[:], in0=accn_t[:], in1=f1[:], op=ALU.mult), r=[accnB, f1B], w=[f2B])
                            p.op("act", lambda e: e.activation(out=f3[:], in_=f2[:], func=AF.Square), r=[f2B], w=[f3B])
                            p.op("pe", lambda e: mm(e, s1[:], onesf[:], f2[:], True, True), r=[f2B] + CB, w=[s1B])
                            p.op("act", lambda e: e.activation(out=f1[:], in_=s1[:], func=AF.Copy), r=[s1B], w=[f1B])
                            p.op("pe", lambda e: mm(e, s2[:], onesf[:], f3[:], True, True), r=[f3B] + CB, w=[s2B])
                            p.op("dve", lambda e: e.tensor_tensor(out=f4[:], in0=f1[:], in1=f1[:], op=ALU.mult), r=[f1B], w=[f4B])
                            p.op("dve", lambda e: e.tensor_tensor(out=f4[:], in0=s2[:], in1=f4[:], op=ALU.subtract), r=[s2B, f4B], w=[f4B])
                            p.op("act", lambda e: e.activation(out=f4[:], in_=f4[:], func=AF.Ln, bias=epsT[:, 0:1]), r=[f4B] + CB, w=[f4B])
                            p.op("act", lambda e: e.activation(out=f4[:], in_=f4[:], func=AF.Exp, scale=-0.5), r=[f4B], w=[f4B])
                            p.op("dve", lambda e: e.tensor_tensor(out=f2[:], in0=f2[:], in1=f1[:], op=ALU.subtract), r=[f2B, f1B], w=[f2B])
                            p.op("dve", lambda e: e.tensor_tensor(out=f2[:], in0=f2[:], in1=f4[:], op=ALU.mult), r=[f2B, f4B], w=[f2B])
                            p.op("dve", lambda e: e.scalar_tensor_tensor(out=oT[:, hd, cs], in0=f2[:], scalar=ng[:, hd:hd + 1], in1=sgo[:, cs],
                                                                         op0=ALU.mult, op1=ALU.mult), r=[f2B, sgoB] + CB, w=[oTB[hd]])

                        def stage2(info):
                            qc, kb, nkb, c0, pt, ptB = info

                            def fpv(e):
                                mm(e, accn_t[:, c0:512], Vh[:, kb, :], pt[:, c0:512], kb == 0, kb == nkb - 1)
                                return mm(e, accd_t[:, c0:512], ones_bf[:, 0:128], pt[:, c0:512], kb == 0, kb == nkb - 1)
                            p.op("pe", fpv, r=[ptB, VhB] + CB, w=[accnB, accdB])
                            if kb == nkb - 1:
                                finalize(qc)

                        LA = 2
                        pend = []
                        for qc in range(4):
                            for kb in range(4 * qc + 4):
                                pend.append(stage1(qc, kb))
                                if len(pend) > LA:
                                    stage2(pend.pop(0))
                        while pend:
                            stage2(pend.pop(0))
                    if debug and s == 0:
                        dB = p.buf("dbg2b", dma=True)
                        p.op("pool", lambda e: e.dma_start(out=dbg2[128:256, :].rearrange("p (a b) -> p a b", a=KC), in_=oT[:]), r=oTB, dsem=dB.dsem)
                    p.barrier()
            _ph4()
            mix_stage("wo1", 2, 2)
            if phases < 4:
                return

            def _ph5():
                with ExitStack() as st:
                    p.phase_begin()
                    wr = sb(st, "wr", [128, KC, 8], BF16); wrB = p.buf("wr", dma=True)
                    wload(wr[:], wview("wr"), wrB)
                    lgp = ps(st, "lgp", [128, 128]); lgB = p.buf("lg")
                    prp = ps(st, "prp", [128, 128]); prB = p.buf("pr")
                    ttp = ps(st, "ttp", [128, 128]); ttB = p.buf("tt")
                    Lg = sb(st, "Lg", [128, 128], F32); LgB = p.buf("Lg")
                    L2 = sb(st, "L2", [128, 128], F32)
                    e1 = sb(st, "e1", [128, 128], F32)
                    e2 = sb(st, "e2", [128, 128], F32)
                    Mb = sb(st, "Mb", [128, 128], BF16)
                    Tt = sb(st, "Tt", [128, 128], F32)
                    Sc = sb(st, "Sc", [128, 128], F32)
                    Rr = sb(st, "Rr", [128, 128], F32)
                    row = sb(st, "row", [128, 128], F32)
                    tmp = sb(st, "tmp", [128, 128], F32)
                    pf = sb(st, "pf", [128, 32], F32)
                    tsum = sb(st, "tsum", [128, 8], F32)
                    m1 = sb(st, "m1", [128, 16], F32)
                    m2 = sb(st, "m2", [128, 16], F32)
                    w1 = sb(st, "w1", [128, 16], F32)
                    w2 = sb(st, "w2", [128, 16], F32)
                    v3 = lambda t: t[:, :].rearrange("p (a b) -> p a b", b=8)
                    emT = lambda t: t[:, :].rearrange("p (e a) -> p a e", e=8)
                    em3 = lambda t: t[:, :].rearrange("p (e a) -> p e a", e=8)
                    bc = lambda t: t[:, :].unsqueeze(2).to_broadcast([128, 16, 8])

                    def flg(e):
                        inst = None
                        for tb in range(TB):
                            for kc in range(KC):
                                inst = mm(e, lgp[:, tb * 8:(tb + 1) * 8], hT[:, kc, tb * 128:(tb + 1) * 128], wr[:, kc, :], kc == 0, kc == KC - 1)
                        return inst
                    p.op("pe", flg, r=[wrB] + hTB, w=[lgB])
                    G = [LgB]
                    p.op("dve", lambda e: e.tensor_copy(out=Lg[:], in_=lgp[:]), r=[lgB], w=G)
                    p.op("dve", lambda e: e.tensor_reduce(out=m1[:], in_=v3(Lg), axis=AX.X, op=ALU.max), r=G, w=G)
                    p.op("dve", lambda e: e.tensor_tensor(out=v3(e1), in0=v3(Lg), in1=bc(m1), op=ALU.is_equal), r=G, w=G)
                    p.op("dve", lambda e: e.scalar_tensor_tensor(out=L2[:], in0=e1[:], scalar=-1e30, in1=Lg[:], op0=ALU.mult, op1=ALU.add), r=G, w=G)
                    p.op("dve", lambda e: e.tensor_reduce(out=m2[:], in_=v3(L2), axis=AX.X, op=ALU.max), r=G, w=G)
                    p.op("dve", lambda e: e.tensor_tensor(out=v3(e2), in0=v3(L2), in1=bc(m2), op=ALU.is_equal), r=G, w=G)
                    p.op("dve", lambda e: e.tensor_tensor(out=w2[:], in0=m2[:], in1=m1[:], op=ALU.subtract), r=G, w=G)
                    p.op("act", lambda e: e.activation(out=w2[:], in_=w2[:], func=AF.Exp), r=G, w=G)
                    p.op("dve", lambda e: e.tensor_scalar(out=w1[:], in0=w2[:], scalar1=1.0, scalar2=None, op0=ALU.add), r=G, w=G)
                    p.op("dve", lambda e: e.reciprocal(out=w1[:], in_=w1[:]), r=G, w=G)
                    p.op("dve", lambda e: e.tensor_tensor(out=w2[:], in0=w2[:], in1=w1[:], op=ALU.mult), r=G, w=G)
                    p.op("dve", lambda e: e.tensor_copy(out=gW[:, s * 32:s * 32 + 16], in_=w1[:]), r=G, w=[gWB])
                    p.op("dve", lambda e: e.tensor_copy(out=gW[:, s * 32 + 16:s * 32 + 32], in_=w2[:]), r=G, w=[gWB])
                    p.op("dve", lambda e: e.tensor_tensor(out=emT(Mb), in0=v3(e1), in1=v3(e2), op=ALU.add), r=G, w=G)

                    def fpr(e):
                        mm(e, prp[:], tri[:], Mb[:], True, True)
                        return mm(e, ttp[:], ones_bf[:, 0:128], Mb[:], True, True)
                    p.op("pe", fpr, r=G + CB, w=[prB, ttB])
                    p.op("dve", lambda e: e.tensor_copy(out=Tt[:], in_=ttp[:]), r=[ttB], w=G)
                    p.op("dve", lambda e: e.tensor_tensor_scan(out=Sc[:], data0=ones_bf[:, 0:128], data1=Tt[:], initial=0.0, op0=ALU.mult, op1=ALU.add),
                         r=G + CB, w=G)
                    p.op("dve", lambda e: e.tensor_tensor(out=Sc[:], in0=Sc[:], in1=Tt[:], op=ALU.subtract), r=G, w=G)
                    p.op("dve", lambda e: e.tensor_tensor(out=em3(Rr), in0=em3(Sc), in1=em3(Sc)[:, :, 0:1].to_broadcast([128, 8, 16]), op=ALU.subtract), r=G, w=G)
                    p.op("dve", lambda e: e.tensor_tensor(out=em3(Rr), in0=em3(Rr), in1=base[:, :].unsqueeze(2).to_broadcast([128, 8, 16]), op=ALU.add),
                         r=G + [baseB], w=G)
                    p.op("dve", lambda e: e.tensor_tensor(out=row[:], in0=prp[:], in1=Rr[:], op=ALU.add), r=G + [prB], w=G)
                    p.op("dve", lambda e: e.tensor_tensor(out=row[:], in0=row[:], in1=eoff[:], op=ALU.add), r=G + CB, w=G)
                    p.op("dve", lambda e: e.tensor_reduce(out=tsum[:], in_=em3(Tt), axis=AX.X, op=ALU.add), r=G, w=G)
                    p.op("dve", lambda e: e.tensor_tensor(out=base[:], in0=base[:], in1=tsum[:], op=ALU.add), r=G + [baseB], w=[baseB])
                    p.op("dve", lambda e: e.tensor_tensor(out=v3(tmp), in0=v3(e1), in1=emT(row), op=ALU.mult), r=G, w=G)
                    p.op("dve", lambda e: e.tensor_reduce(out=pf[:, 0:16], in_=v3(tmp), axis=AX.X, op=ALU.add), r=G, w=G)
                    p.op("dve", lambda e: e.tensor_tensor(out=v3(tmp), in0=v3(e2), in1=emT(row), op=ALU.mult), r=G, w=G)
                    p.op("dve", lambda e: e.tensor_reduce(out=pf[:, 16:32], in_=v3(tmp), axis=AX.X, op=ALU.add), r=G, w=G)
                    p.op("dve", lambda e: e.tensor_copy(out=posI[:, s * 32:(s + 1) * 32], in_=pf[:]), r=G, w=[posB])
                    for tb in range(TB):
                        p.op("sp", lambda e, tb=tb: e.dma_start(out=h1sp[row0 + tb * 128: row0 + (tb + 1) * 128, :], in_=h[:, tb, :]),
                             r=[hB[tb]], dsem=hB[tb].dsem)
                        for k in range(2):
                            col = s * 32 + k * 16 + tb
                            for hf in range(2):
                                p.op("pool", lambda e, tb=tb, col=col, hf=hf: e.indirect_dma_start(
                                    out=XsH[hf][:, :], out_offset=IOA(ap=posI[:, col:col + 1], axis=0),
                                    in_=hs[:, tb * D + hf * 512: tb * D + (hf + 1) * 512], in_offset=None),
                                    r=[hB[tb], posB], dsem=hB[tb].dsem)
                    p.barrier()
            _ph5()

        def moe_phase():
            NB = TS // 128
            NTC = TS // 512
            with ExitStack() as st:
                p.phase_begin()
                ntl = sb(st, "ntl", [128, 8], F32)
                cinc = sb(st, "cinc", [128, 8], F32)
                cmp3 = hs[:, NTAB:NTAB + 64]
                le = hs[:, NTAB + 64:NTAB + 64 + NT * 8]
                le2 = hs[:, NTAB + 64 + NT * 8:NTAB + 64 + 2 * NT * 8]
                ej = sb(st, "ej", [128, NT], F32)
                ub = sb(st, "ub", [128, NT], F32)
                tabF = hs[:, 0:NTAB]
                xrow = sb(st, "xrow", [128, NT], F32)
                ew = sb(st, "ew", [128, NT], F32)
                T = [p.buf("tabw")]
                c3 = lambda t: t.rearrange("p (a b) -> p a b", b=8)
                p.op("dve", lambda e: e.tensor_tensor(out=c3(cmp3), in0=base[:, :].unsqueeze(2).to_broadcast([128, 8, 8]), in1=c3(thr[:, :]), op=ALU.is_gt),
                     r=[baseB] + CB, w=T)
                p.op("dve", lambda e: e.tensor_reduce(out=ntl[:], in_=c3(cmp3), axis=AX.X, op=ALU.add), r=T, w=T)
                p.op("dve", lambda e: e.tensor_tensor_scan(out=cinc[:], data0=ones_bf[:, 0:8], data1=ntl[:], initial=0.0, op0=ALU.mult, op1=ALU.add),
                     r=T + CB, w=T)
                p.op("dve", lambda e: e.tensor_tensor(out=c3(le), in0=cinc[:, :].unsqueeze(1).to_broadcast([128, NT, 8]), in1=c3(jc[:, :]), op=ALU.is_le),
                     r=T + CB, w=T)
                p.op("dve", lambda e: e.tensor_reduce(out=ej[:], in_=c3(le), axis=AX.X, op=ALU.add), r=T, w=T)
                p.op("dve", lambda e: e.tensor_tensor(out=c3(le2), in0=c3(le), in1=ntl[:, :].unsqueeze(1).to_broadcast([128, NT, 8]), op=ALU.mult), r=T, w=T)
                p.op("dve", lambda e: e.tensor_reduce(out=ub[:], in_=c3(le2), axis=AX.X, op=ALU.add), r=T, w=T)
                p.op("dve", lambda e: e.tensor_tensor(out=ub[:], in0=c3(jc[:, :])[:, :, 0], in1=ub[:], op=ALU.subtract), r=T + CB, w=T)
                p.op("dve", lambda e: e.tensor_scalar(out=ub[:], in0=ub[:], scalar1=float(TS), scalar2=None, op0=ALU.mult), r=T, w=T)
                p.op("dve", lambda e: e.scalar_tensor_tensor(out=xrow[:], in0=ej[:], scalar=float(CAP), in1=ub[:], op0=ALU.mult, op1=ALU.add), r=T, w=T)
                p.op("dve", lambda e: e.tensor_scalar(out=xrow[:], in0=xrow[:], scalar1=float(8 * CAP), scalar2=None, op0=ALU.min), r=T, w=T)
                p.op("dve", lambda e: e.tensor_scalar(out=ej[:], in0=ej[:], scalar1=7.0, scalar2=None, op0=ALU.min), r=T, w=T)
                tv = lambda o, n: tabF[:, o:o + NT * n].rearrange("p (j c) -> p j c", c=n)
                bj = lambda t, n: t[:, :].unsqueeze(2).to_broadcast([128, NT, n])
                bcp = lambda n: cp[:, 0:n].unsqueeze(1).to_broadcast([128, NT, n])
                p.op("dve", lambda e: e.tensor_tensor(out=tv(XO, NBm), in0=bj(xrow, NBm), in1=bcp(NBm), op=ALU.add), r=T + CB, w=T)
                p.op("dve", lambda e: e.tensor_scalar(out=ew[:], in0=ej[:], scalar1=float(7 * D), scalar2=None, op0=ALU.mult), r=T, w=T)
                p.op("dve", lambda e: e.tensor_tensor(out=tv(WO, 56), in0=bj(ew, 56), in1=bcp(56), op=ALU.add), r=T + CB, w=T)
                p.op("dve", lambda e: e.tensor_scalar(out=ew[:], in0=ej[:], scalar1=float(DFE), scalar2=None, op0=ALU.mult), r=T, w=T)
                p.op("dve", lambda e: e.tensor_tensor(out=tv(DO, 28), in0=bj(ew, 28), in1=bcp(28), op=ALU.add), r=T + CB, w=T)
                p.op("dve", lambda e: e.tensor_copy(out=tabI[:], in_=tabF), r=T, w=[tabB])

                wgs = Rot([(sb(st, "wg%d" % i, [128, KC, 512], BF16), p.buf("wg", dma=True)) for i in range(2)])
                wus = Rot([(sb(st, "wu%d" % i, [128, KC, 512], BF16), p.buf("wu", dma=True)) for i in range(2)])
                wds = Rot([(sb(st, "wd%d" % i, [128, 4, D], BF16), p.buf("wd", dma=True)) for i in range(2)])
                xbs = Rot([(sb(st, "xbm%d" % i, [128, D], BF16), p.buf("xbm", dma=True)) for i in range(3)])
                sgs = Rot([(sb(st, "sg%d" % i, [128, 512], F32), p.buf("sg")) for i in range(2)])
                pgs = Rot([(ps(st, "pg%d" % i, [128, 512]), p.buf("pg")) for i in range(2)])
                pus = Rot([(ps(st, "pu%d" % i, [128, 512]), p.buf("pu")) for i in range(2)])
                yps = Rot([(ps(st, "yp%d" % i, [128, D]), p.buf("yp")) for i in range(2)])
                psts = Rot([(pu_[:].bitcast(BF16), puB_) for (pu_, puB_) in pus.items])
                xTs = Rot([(hT[:, :, i * TS:(i + 1) * TS], p.buf("xT")) for i in range(2)])
                aTs = Rot([(oT[:, 4 * i:4 * i + 4, 0:TS], p.buf("aTm")) for i in range(2)])
                yss = Rot([(h[:, NB * i:NB * (i + 1), :], p.buf("ys", dma=True)) for i in range(2)])

                def load_tile(j):
                    xT, xTB = xTs.next()
                    for a in range(NB):
                        xb, xbB = xbs.next()
                        pst, pstB = psts.next()
                        for hf in range(2):
                            p.op("pool", lambda e, xb=xb, a=a, hf=hf: e.indirect_dma_start(
                                out=xb[:, hf * 512:(hf + 1) * 512], out_offset=None, in_=XsH[hf][:, :],
                                in_offset=IOA(ap=tabI[:, XO + j * NB + a:XO + j * NB + a + 1], axis=0)), r=[tabB], w=[xbB], dsem=xbB.dsem)

                        def tr(e, xb=xb, pst=pst):
                            inst = None
                            for kc in range(KC):
                                inst = e.transpose(pst[:, kc * 128:(kc + 1) * 128], xb[:, kc * 128:(kc + 1) * 128], ident[:])
                            return inst
                        p.op("pe", tr, r=[xbB, identB], w=[pstB])
                        p.op("act", lambda e, xT=xT, pst=pst, a=a: e.activation(out=xT[:, :, a * 128:(a + 1) * 128],
                                                                                 in_=pst.rearrange("p (k t) -> p k t", k=KC), func=AF.Copy),
                             r=[pstB], w=[xTB])
                    return xT, xTB

                def ffn_load(j, g):
                    wg, wgB = wgs.next()
                    wu, wuB = wus.next()
                    wd, wdB = wds.next()
                    for (wt_, wtB_, wn_) in ((wg, wgB, "mg"), (wu, wuB, "mu")):
                        for kc in range(KC):
                            c_ = WO + j * 56 + g * 8 + kc
                            p.op("pool", lambda e, wt_=wt_, wn_=wn_, kc=kc, c_=c_: e.indirect_dma_start(
                                out=wt_[:, kc, :], out_offset=None, in_=dr[wn_][:, :],
                                in_offset=IOA(ap=tabI[:, c_:c_ + 1], axis=0)), r=[tabB], w=[wtB_], dsem=wtB_.dsem)
                    for ci in range(4):
                        c_ = DO + j * 28 + g * 4 + ci
                        p.op("pool", lambda e, ci=ci, c_=c_: e.indirect_dma_start(
                            out=wd[:, ci, :], out_offset=None, in_=dr["md"][:, :],
                            in_offset=IOA(ap=tabI[:, c_:c_ + 1], axis=0)), r=[tabB], w=[wdB], dsem=wdB.dsem)
                    return (wg, wgB, wu, wuB, wd, wdB)

                def ffn_group(j, g, xT, xTB, ys, ysB, wts):
                    wg, wgB, wu, wuB, wd, wdB = wts
                    aT, aTB = aTs.next()
                    for ci in range(4):
                        for tc in range(NTC):
                            pg, pgB = pgs.next()
                            pu, puB = pus.next()
                            sg, sgB = sgs.next()

                            def fg_(e, pg=pg, ci=ci, tc=tc):
                                inst = None
                                for kc in range(KC):
                                    inst = mm(e, pg[:], wg[:, kc, ci * 128:(ci + 1) * 128], xT[:, kc, tc * 512:(tc + 1) * 512], kc == 0, kc == KC - 1)
                                return inst

                            def fu_(e, pu=pu, ci=ci, tc=tc):
                                inst = None
                                for kc in range(KC):
                                    inst = mm(e, pu[:], wu[:, kc, ci * 128:(ci + 1) * 128], xT[:, kc, tc * 512:(tc + 1) * 512], kc == 0, kc == KC - 1)
                                return inst
                            p.op("pe", fg_, r=[wgB, xTB], w=[pgB])
                            p.op("pe", fu_, r=[wuB, xTB], w=[puB])
                            p.op("act", lambda e, sg=sg, pg=pg: e.activation(out=sg[:], in_=pg[:], func=AF.Silu), r=[pgB], w=[sgB])
                            p.op("dve", lambda e, sg=sg, pu=pu, ci=ci, tc=tc: e.tensor_tensor(
                                out=aT[:, ci, tc * 512:(tc + 1) * 512], in0=pu[:], in1=sg[:], op=ALU.mult), r=[puB, sgB], w=[aTB])
                    for tb in range(NB):
                        yp, ypB = yps.next()

                        def fd_(e, yp=yp, tb=tb):
                            inst = None
                            for half in range(2):
                                for ci in range(4):
                                    inst = mm(e, yp[:, half * 512:(half + 1) * 512], aT[:, ci, tb * 128:(tb + 1) * 128],
                                              wd[:, ci, half * 512:(half + 1) * 512], ci == 0, ci == 3)
                            return inst
                        p.op("pe", fd_, r=[aTB, wdB], w=[ypB])
                        ydst = ys[:, tb, :]
                        if g == 0:
                            p.op("dve", lambda e, yp=yp, ydst=ydst: e.tensor_copy(out=ydst, in_=yp[:]), r=[ypB], w=[ysB])
                        else:
                            p.op("dve", lambda e, yp=yp, ydst=ydst: e.tensor_tensor(out=ydst, in0=yp[:], in1=ydst, op=ALU.add), r=[ypB, ysB], w=[ysB])

                NG = DFE // 512
                nxt = load_tile(0)
                wnext = ffn_load(0, 0)
                for j in range(NT):
                    xT, xTB = nxt
                    ys, ysB = yss.next()
                    for g in range(NG):
                        wcur = wnext
                        if g + 1 < NG:
                            wnext = ffn_load(j, g + 1)
                        elif j + 1 < NT:
                            wnext = ffn_load(j + 1, 0)
                        ffn_group(j, g, xT, xTB, ys, ysB, wcur)
                        if g == 3 and j + 1 < NT:
                            nxt = load_tile(j + 1)
                    for a in range(NB):
                        for hf in range(2):
                            p.op("pool", lambda e, ys=ys, j=j, hf=hf, a=a: e.indirect_dma_start(
                                out=YsH[hf][:, :], out_offset=IOA(ap=tabI[:, XO + j * NB + a:XO + j * NB + a + 1], axis=0),
                                in_=ys[:, a, hf * 512:(hf + 1) * 512], in_offset=None), r=[ysB, tabB], dsem=ysB.dsem)
                p.barrier()

        def combine_phase():
            with ExitStack() as st:
                p.phase_begin()
                L = ln_alloc(st, "fin")
                L["eps"] = epsT
                ln_load(L, 3)
                y1s = Rot([(sb(st, "y1_%d" % i, [128, D], F32), p.buf("y1", dma=True)) for i in range(4)])
                y2s = Rot([(sb(st, "y2_%d" % i, [128, D], F32), p.buf("y2", dma=True)) for i in range(4)])
                lp = LnPipe()
                for s in range(nseq):
                    for tb in range(TB):
                        col = s * 32 + tb
                        r0 = s * S + tb * 128
                        p.op("pool", lambda e, tb=tb, r0=r0: e.dma_start(out=h[:, tb, :], in_=h1sp[r0:r0 + 128, :]), w=[hB[tb]], dsem=hB[tb].dsem, force=True)
                        y1, y1B = y1s.next()
                        y2, y2B = y2s.next()
                        for hf in range(2):
                            p.op("pool", lambda e, y1=y1, col=col, hf=hf: e.indirect_dma_start(
                                out=y1[:, hf * 512:(hf + 1) * 512], out_offset=None, in_=YsH[hf][:, :],
                                in_offset=IOA(ap=posI[:, col:col + 1], axis=0)),
                                r=[posB], w=[y1B], dsem=y1B.dsem)
                            p.op("pool", lambda e, y2=y2, col=col, hf=hf: e.indirect_dma_start(
                                out=y2[:, hf * 512:(hf + 1) * 512], out_offset=None, in_=YsH[hf][:, :],
                                in_offset=IOA(ap=posI[:, col + 16:col + 17], axis=0)),
                                r=[posB], w=[y2B], dsem=y2B.dsem)
                        p.op("act", lambda e, tb=tb: e.mul(out=h[:, tb, :], in_=h[:, tb, :], mul=ALPHA), r=[hB[tb]], w=[hB[tb]])
                        p.op("dve", lambda e, tb=tb, y1=y1, col=col: e.scalar_tensor_tensor(out=h[:, tb, :], in0=y1[:], scalar=gW[:, col:col + 1], in1=h[:, tb, :],
                                                                                         op0=ALU.mult, op1=ALU.add), r=[y1B, hB[tb], gWB], w=[hB[tb]])
                        p.op("dve", lambda e, tb=tb, y2=y2, col=col: e.scalar_tensor_tensor(out=h[:, tb, :], in0=y2[:], scalar=gW[:, col + 16:col + 17], in1=h[:, tb, :],
                                                                                         op0=ALU.mult, op1=ALU.add), r=[y2B, hB[tb], gWB], w=[hB[tb]])
                        lp.push(ln_block(tb, L, to_hT=False, out_row0=s * S, gb_eng="dve"))
                lp.flush()
                p.barrier()

        for s_ in range(nseq):
            do_seq(s_)
        moe_phase()
        combine_phase()
        lasts = list(p.lastd.values())
        p.pend["sp"] = lasts
        p.op("sp", lambda e: None)
        block = es.enter_context(nc.Block())
        p.emit(block)
    return nc


def prep_shared(inp):
    f = lambda a: np.ascontiguousarray(a, dtype=np.float32)
    w_in_e = inp["w_in_e"][0]
    sh = {}
    sh["wqa"] = f(w_in_e[:, 0:512]); sh["wka"] = f(w_in_e[:, 512:1024]); sh["wva"] = f(w_in_e[:, 1024:1536])
    sh["wfa"] = f(np.repeat(w_in_e[:, 1536:1544], 128, axis=1))
    qd = w_in_e[:, 1544:2056]; kd = w_in_e[:, 2056:2568]
    sh["wqd"] = f(qd); sh["wkd"] = f(kd); sh["wvd"] = f(w_in_e[:, 2568:3080])
    perm = np.arange(512)
    for hh in range(8):
        for i in range(8):
            perm[hh * 64 + i] = hh * 64 + i + 8
            perm[hh * 64 + 8 + i] = hh * 64 + i
    sh["wqds"] = f(qd[:, perm]); sh["wkds"] = f(kd[:, perm])
    sh["wo0"] = f(inp["w_out_e"][0]); sh["fg"] = f(inp["ffn_w_gate_e"][0]); sh["fu"] = f(inp["ffn_w_up_e"][0]); sh["fd"] = f(inp["ffn_w_down_e"][0])
    w_in_o = inp["w_in_o"][0]
    sh["wq1"] = f(w_in_o[:, 0:1024]); sh["wk1"] = f(w_in_o[:, 1024:2048]); sh["wv1"] = f(w_in_o[:, 2048:3072])
    sh["wig"] = f(np.repeat(w_in_o[:, 3072:3080], 128, axis=1)); sh["wfg"] = f(np.repeat(w_in_o[:, 3080:3088], 128, axis=1))
    sh["wog"] = f(w_in_o[:, 3088:4112])
    sh["wo1"] = f(inp["w_out_o"][0]); sh["wr"] = f(inp["w_router_o"][0])
    relay = lambda w: f(w.reshape(NE, D, 7, 512).transpose(0, 2, 1, 3).reshape(NE * 7 * D, 512))
    sh["mg"] = relay(inp["moe_w_gate_o"][0]); sh["mu"] = relay(inp["moe_w_up_o"][0])
    sh["md"] = f(inp["moe_w_down_o"][0].reshape(NE * DFE, D))
    lnp = np.stack([np.stack([inp["ln_mix_g_e"][0], inp["ln_mix_b_e"][0]]), np.stack([inp["ln_ffn_g_e"][0], inp["ln_ffn_b_e"][0]]),
                    np.stack([inp["ln_mix_g_o"][0], inp["ln_mix_b_o"][0]]), np.stack([inp["ln_ffn_g_o"][0], inp["ln_ffn_b_o"][0]])])
    sh["lnp"] = f(np.broadcast_to(lnp[:, :, None, :], (4, 2, 128, D)).reshape(4 * 2 * 128, D))
    gbv = np.concatenate([inp["b_forget_e"][0], inp["b_igate_o"][0], inp["b_fgate_o"][0]])
    sh["gb"] = f(np.broadcast_to(gbv[None, :], (128, 24)))
    sh["ng"] = f(inp["mlstm_norm_g_o"][0].reshape(8, 128).T)
    sh["wc"] = f(inp["w_conv_o"][0].T.reshape(16, 128, 4).transpose(1, 0, 2).reshape(128, 64))
    sh["ident"] = np.eye(128, dtype=np.float32)
    k = np.arange(128)[:, None]; q = np.arange(128)[None, :]
    mC = np.where(k > q, NEG, 0.0).astype(np.float32)
    mU = np.where(k < q, NEG, 0.0).astype(np.float32)
    mA = np.full((128, 128), NEG, np.float32)
    sh["masks"] = f(np.concatenate([mC, mU, mA], axis=1))
    sh["mask4"] = f(np.concatenate([mU, mC, mU, mC, mA, mC, mU, mC, mA, mC, mA, mC], axis=1))
    half = 8
    inv = 500000.0 ** (-np.arange(half, dtype=np.float32) / half)
    ang = np.arange(S, dtype=np.float32)[None, :] * inv[:, None]
    cosT = np.ones((128, S), np.float32); sinT = np.zeros((128, S), np.float32)
    for hh in range(2):
        b = hh * 64
        cosT[b:b + 8] = np.cos(ang); cosT[b + 8:b + 16] = np.cos(ang)
        sinT[b:b + 8] = -np.sin(ang); sinT[b + 8:b + 16] = np.sin(ang)
    sh["rope"] = f(np.concatenate([cosT, sinT], axis=1))
    augc = np.zeros((128, 6), np.float32)
    augc[0:4, 0] = [-1, 0, 0, 0]; augc[0:4, 1] = [0, -1, 0, 0]; augc[0:4, 2] = [0, 0, 1, 1]
    augc[0:4, 3] = [0, 0, 1, 0]; augc[0:4, 4] = [0, 0, 0, 1]; augc[0:4, 5] = [1, 1, 0, 0]
    sh["augc"] = augc
    sh["cp"] = f(np.arange(56, dtype=np.float32)[None, :] * 128.0 + np.arange(128, dtype=np.float32)[:, None])
    sh["tri"] = np.triu(np.ones((128, 128), np.float32))
    eo = np.zeros((128, 128), np.float32)
    for e_ in range(8):
        eo[:, e_ * 16:(e_ + 1) * 16] = e_ * CAP - 1.0
    sh["eoff"] = eo
    sh["thr"] = f(np.broadcast_to((np.arange(8, dtype=np.float32) * TS)[None, None, :], (128, 8, 8)).reshape(128, 64))
    sh["jc"] = f(np.broadcast_to(np.arange(NT, dtype=np.float32)[None, :, None], (128, NT, 8)).reshape(128, NT * 8))
    return sh


N_CORES = 8


def kernel(**inputs):
    x = np.ascontiguousarray(inputs["x"], dtype=np.float32)
    B = x.shape[0]
    nseq = B // N_CORES
    assert nseq == NSEQ
    sh = prep_shared(inputs)
    nc = build_nc(nseq)
    in_maps = []
    for c in range(N_CORES):
        m = dict(sh)
        m["x"] = x[c * nseq:(c + 1) * nseq].reshape(nseq * S, D)
        in_maps.append(m)
    res = run_bass_kernel_spmd(nc, in_maps, core_ids=list(range(N_CORES)))
    outs = [np.asarray(r["out"]).reshape(nseq, S, D) for r in res.results]
    return np.concatenate(outs, axis=0).astype(np.float32)
```

```python
import numpy as np
from contextlib import ExitStack
import concourse.bass as bass
import concourse.mybir as mybir
from concourse.bass_utils import run_bass_kernel_spmd

F32 = mybir.dt.float32
BF16 = mybir.dt.bfloat16
AF = mybir.ActivationFunctionType
ALU = mybir.AluOpType
AX = mybir.AxisListType

S = 2048
D = 1024
TB = 16
KC = 8
ALPHA = 4.0 ** 0.25
EPS = 1e-5
NEG = -30000.0
DFF = 2816
DFE = 3584
NE = 8
LNS = float(np.log(128.0 ** -0.5))
ENGS = ("pe", "act", "dve", "pool", "sp")
I32 = mybir.dt.int32
NSEQ = 4
CAP = NSEQ * S
TS = 1024
NT = 2 * CAP // TS + 7
NROWS = 8 * CAP + TS


class Op:
    __slots__ = ("eng", "fn", "deps", "sig", "val", "dsem", "key")


class Buf:
    __slots__ = ("lastw", "readers", "dsem", "name")

    def __init__(self, name, dsem=None):
        self.lastw = None
        self.readers = {}
        self.dsem = dsem
        self.name = name


class Prog:
    def __init__(self, nc, es):
        self.nc = nc
        self.es = es
        self.ops = {e: [] for e in ENGS}
        self.esem = {e: es.enter_context(nc.semaphore("s_" + e)) for e in ENGS}
        self.dcnt = {}
        self.last = {e: None for e in ENGS}
        self.lastd = {}
        self.pend = {e: [] for e in ENGS}
        self.nsem = 0
        self.sem_pool = []
        self.pool_idx = None

    def newsem(self):
        if self.pool_idx is not None:
            if self.pool_idx >= len(self.sem_pool):
                self.nsem += 1
                self.sem_pool.append(self.es.enter_context(self.nc.semaphore("d%d" % self.nsem)))
            sm = self.sem_pool[self.pool_idx]
            self.pool_idx += 1
            return sm
        self.nsem += 1
        return self.es.enter_context(self.nc.semaphore("d%d" % self.nsem))

    def phase_begin(self):
        self.pool_idx = 0

    def buf(self, name, dma=False):
        return Buf(name, self.newsem() if dma else None)

    def op(self, eng, fn, r=(), w=(), dsem=None, force=False):
        o = Op()
        o.eng = eng
        o.fn = fn
        o.sig = False
        o.val = 0
        o.dsem = dsem
        o.key = ("d", id(dsem)) if dsem is not None else eng
        deps = []
        for b in r:
            if b.lastw is not None:
                deps.append((b.lastw, True))
        for b in w:
            if b.lastw is not None:
                deps.append((b.lastw, False))
            for rd in b.readers.values():
                deps.append((rd, False))
        for d in self.pend[eng]:
            deps.append((d, True))
        self.pend[eng] = []
        dd = []
        for d, raw in deps:
            if d is o:
                continue
            if d.key == o.key and (not raw or eng == "pe") and not (force and d.dsem is not None):
                continue
            if d not in dd:
                dd.append(d)
        o.deps = dd
        if dsem is not None:
            c = self.dcnt.get(id(dsem), 0) + 16
            self.dcnt[id(dsem)] = c
            o.val = c
            self.lastd[id(dsem)] = o
        for d in dd:
            if d.dsem is None:
                d.sig = True
        for b in r:
            b.readers[o.key] = o
        for b in w:
            b.lastw = o
            b.readers = {}
        self.ops[eng].append(o)
        self.last[eng] = o
        return o

    def barrier(self):
        lasts = [self.last[e] for e in ENGS if self.last[e] is not None] + list(self.lastd.values())
        for e in ENGS:
            self.pend[e] = list(lasts)

    def emit(self, block):
        for e in ENGS:
            c = 0
            for o in self.ops[e]:
                if o.dsem is None and o.sig:
                    c += 1
                    o.val = c

        def runner(ename):
            def f(eh):
                seen = {}
                for o in self.ops[ename]:
                    for d in o.deps:
                        if seen.get(d.key, 0) >= d.val:
                            continue
                        sem = d.dsem if d.dsem is not None else self.esem[d.eng]
                        eh.wait_ge(sem, d.val)
                        seen[d.key] = d.val
                    inst = o.fn(eh)
                    if inst is None:
                        continue
                    if o.dsem is not None:
                        inst.then_inc(o.dsem, 16)
                    elif o.sig:
                        inst.then_inc(self.esem[ename], 1)
            return f

        block.tensor(runner("pe"))
        block.scalar(runner("act"))
        block.vector(runner("dve"))
        block.gpsimd(runner("pool"))
        block.sync(runner("sp"))


class Rot:
    def __init__(self, items):
        self.items = items
        self.i = 0

    def next(self):
        it = self.items[self.i % len(self.items)]
        self.i += 1
        return it


WNAMES = {
    "wqa": (D, 512), "wka": (D, 512), "wva": (D, 512), "wfa": (D, 1024),
    "wqd": (D, 512), "wkd": (D, 512), "wqds": (D, 512), "wkds": (D, 512), "wvd": (D, 512),
    "wo0": (D, D), "fg": (D, DFF), "fu": (D, DFF), "fd": (DFF, D),
    "wq1": (D, D), "wk1": (D, D), "wv1": (D, D), "wog": (D, D), "wig": (D, D), "wfg": (D, D),
    "wo1": (D, D), "wr": (D, 8), "mg": (NE * 7 * D, 512), "mu": (NE * 7 * D, 512), "md": (NE * DFE, D),
    "lnp": (4 * 2 * 128, D), "gb": (128, 24), "ng": (128, 8), "wc": (128, 64),
    "ident": (128, 128), "masks": (128, 3 * 128), "mask4": (128, 3 * 512), "rope": (128, 2 * S), "augc": (128, 6),
    "cp": (128, 56), "tri": (128, 128), "eoff": (128, 128), "thr": (128, 64), "jc": (128, NT * 8),
}


def build_nc(nseq, debug=False, phases=5):
    nc = bass.Bass("TRN2", target_bir_lowering=False)
    dr = {}
    dr["x"] = nc.dram_tensor("x", [nseq * S, D], F32, kind="ExternalInput").ap()
    for k, shp in WNAMES.items():
        dr[k] = nc.dram_tensor(k, list(shp), F32, kind="ExternalInput").ap()
    out = nc.dram_tensor("out", [nseq * S, D], F32, kind="ExternalOutput").ap()
    hsp = nc.dram_tensor("hsp", [S, D], F32, kind="Internal").ap()
    h1sp = nc.dram_tensor("h1sp", [nseq * S, D], F32, kind="Internal").ap()
    XsH = [nc.dram_tensor("Xs%d" % i, [NROWS, 512], F32, kind="Internal").ap() for i in range(2)]
    YsH = [nc.dram_tensor("Ys%d" % i, [NROWS, 512], F32, kind="Internal").ap() for i in range(2)]
    dbg = None
    if debug:
        dbg = nc.dram_tensor("dbg", [4 * S, D], F32, kind="ExternalOutput").ap()
        dbg2 = nc.dram_tensor("dbg2", [2 * 128, KC * S], F32, kind="ExternalOutput").ap()

    with ExitStack() as es:
        p = Prog(nc, es)

        uid = [0]

        def sb(st, name, shape, dt):
            uid[0] += 1
            return st.enter_context(nc.sbuf_tensor("%s_s%d" % (name, uid[0]), shape, dt))

        def ps(st, name, shape, dt=F32):
            uid[0] += 1
            return st.enter_context(nc.psum_tensor("%s_p%d" % (name, uid[0]), shape, dt))

        h = sb(es, "h", [128, TB, D], F32)
        hB = [p.buf("h%d" % i, dma=True) for i in range(TB)]
        hs = h[:, :, :].rearrange("p a b -> p (a b)")
        hT = sb(es, "hT", [128, KC, S], BF16)
        hTB = [p.buf("hT%d" % i) for i in range(TB)]
        oT = sb(es, "oT", [128, KC, S], BF16)
        oTB = [p.buf("oT%d" % i) for i in range(KC)]
        ident = sb(es, "ident", [128, 128], BF16)
        identB = p.buf("ident", dma=True)
        masks = sb(es, "masks", [128, 3, 128], BF16)
        mask4 = sb(es, "mask4", [128, 3, 512], BF16)
        augc = sb(es, "augc", [128, 6], F32)
        gb = sb(es, "gb", [128, 24], F32)
        ngb = sb(es, "ngb", [128, 24], F32)
        ng = sb(es, "ng", [128, 8], F32)
        wc = sb(es, "wc", [128, 16, 4], F32)
        ones_bf = sb(es, "ones_bf", [128, S], BF16)
        onesf = sb(es, "onesf", [128, 128], F32)
        constB = p.buf("const", dma=True)
        const2B = p.buf("const2")

        p.op("pool", lambda e: e.dma_start(out=ident[:], in_=dr["ident"][:, :]), w=[identB], dsem=identB.dsem)
        p.op("pool", lambda e: e.dma_start(out=masks[:], in_=dr["masks"].rearrange("p (a b) -> p a b", a=3)), w=[constB], dsem=constB.dsem)
        p.op("pool", lambda e: e.dma_start(out=mask4[:], in_=dr["mask4"].rearrange("p (a b) -> p a b", a=3)), w=[constB], dsem=constB.dsem)
        p.op("sp", lambda e: e.dma_start(out=augc[:], in_=dr["augc"][:, :]), w=[constB], dsem=constB.dsem)
        p.op("sp", lambda e: e.dma_start(out=gb[:], in_=dr["gb"][:, :]), w=[constB], dsem=constB.dsem)
        p.op("sp", lambda e: e.dma_start(out=ng[:], in_=dr["ng"][:, :]), w=[constB], dsem=constB.dsem)
        p.op("sp", lambda e: e.dma_start(out=wc[:], in_=dr["wc"].rearrange("p (a b) -> p a b", b=4)), w=[constB], dsem=constB.dsem)
        p.op("dve", lambda e: e.memset(ones_bf[:], 1.0), w=[const2B])
        p.op("dve", lambda e: e.memset(onesf[:], 1.0 / 128.0), w=[const2B])
        p.op("dve", lambda e: e.tensor_scalar(out=ngb[:], in0=gb[:], scalar1=-1.0, scalar2=None, op0=ALU.mult), r=[constB], w=[const2B])
        CB = [constB, const2B, identB]
        tri = sb(es, "tri", [128, 128], BF16)
        eoff = sb(es, "eoff", [128, 128], F32)
        thr = sb(es, "thr", [128, 64], F32)
        jc = sb(es, "jc", [128, NT * 8], F32)
        posI = sb(es, "posI", [128, nseq * 32], I32); posB = p.buf("posI")
        gW = sb(es, "gW", [128, nseq * 32], F32); gWB = p.buf("gW")
        base = sb(es, "base", [128, 8], F32); baseB = p.buf("base")
        NBm = TS // 128
        XO, WO, DO = 0, NT * NBm, NT * NBm + NT * 56
        NTAB = DO + NT * 28
        tabI = sb(es, "tabI", [128, NTAB], I32); tabB = p.buf("tabI")
        cp = sb(es, "cp", [128, 56], F32)
        p.op("sp", lambda e: e.dma_start(out=cp[:], in_=dr["cp"][:, :]), w=[constB], dsem=constB.dsem)
        p.op("pool", lambda e: e.dma_start(out=tri[:], in_=dr["tri"][:, :]), w=[identB], dsem=identB.dsem)
        p.op("sp", lambda e: e.dma_start(out=eoff[:], in_=dr["eoff"][:, :]), w=[constB], dsem=constB.dsem)
        p.op("sp", lambda e: e.dma_start(out=thr[:], in_=dr["thr"][:, :]), w=[constB], dsem=constB.dsem)
        p.op("sp", lambda e: e.dma_start(out=jc[:], in_=dr["jc"][:, :]), w=[constB], dsem=constB.dsem)
        p.op("dve", lambda e: e.memset(base[:], 0.0), w=[baseB])
        IOA = bass.IndirectOffsetOnAxis

        def wview(name, rows_off=0, nrows=D):
            return dr[name][rows_off:rows_off + nrows, :].rearrange("(k p) n -> p k n", p=128)

        def mm(e, o, l, r_, st=True, sp=True):
            return e.matmul(o, l, r_, start=st, stop=sp)

        def build_hT(tb, L):
            xb, xbB = L["xb"].next()
            pst, pstB = L["pst"].next()
            p.op("act", lambda e: e.activation(out=xb[:], in_=h[:, tb, :], func=AF.Copy), r=[hB[tb]], w=[xbB])

            def tr(e):
                inst = None
                for kc in range(KC):
                    inst = e.transpose(pst[:, kc * 128:(kc + 1) * 128], xb[:, kc * 128:(kc + 1) * 128], ident[:])
                return inst
            p.op("pe", tr, r=[xbB, identB], w=[pstB])
            p.op("dve", lambda e: e.tensor_copy(out=hT[:, :, tb * 128:(tb + 1) * 128],
                                                in_=pst[:].rearrange("p (k t) -> p k t", k=KC)), r=[pstB], w=[hTB[tb]])

        def ln_alloc(st, tag):
            L = {}
            L["xb"] = Rot([(sb(st, "xb%s%d" % (tag, i), [128, D], BF16), p.buf("xb")) for i in range(2)])
            L["pst"] = Rot([(ps(st, "pst%s%d" % (tag, i), [128, D], BF16), p.buf("pst")) for i in range(2)])
            L["st6"] = Rot([(sb(st, "st6%s%d" % (tag, i), [128, 12], F32), p.buf("st6")) for i in range(2)])
            L["mv"] = Rot([(sb(st, "mv%s%d" % (tag, i), [128, 4], F32), p.buf("mv")) for i in range(2)])
            L["lnp"] = sb(st, "lnp%s" % tag, [128, 2, D], F32)
            L["lnpB"] = p.buf("lnp", dma=True)
            return L

        def ln_load(L, li):
            src = dr["lnp"][li * 256:(li + 1) * 256, :].rearrange("(a p) n -> p a n", a=2)
            p.op("sp", lambda e: e.dma_start(out=L["lnp"][:], in_=src), w=[L["lnpB"]], dsem=L["lnpB"].dsem)

        def ln_block(tb, L, to_hT=True, out_row0=None, dbg_row0=None, spill=False, gb_eng="pool"):
            st6, st6B = L["st6"].next()
            mv, mvB = L["mv"].next()
            lnp = L["lnp"]
            p.op("dve", lambda e: e.bn_stats(out=st6[:, 0:6], in_=h[:, tb, 0:512]), r=[hB[tb]], w=[st6B])
            p.op("dve", lambda e: e.bn_stats(out=st6[:, 6:12], in_=h[:, tb, 512:1024]), r=[hB[tb]], w=[st6B])
            p.op("dve", lambda e: e.bn_aggr(out=mv[:, 0:2], in_=st6[:]), r=[st6B], w=[mvB])
            p.op("act", lambda e: e.activation(out=mv[:, 2:3], in_=mv[:, 1:2], func=AF.Ln, bias=L["eps"][:, 0:1]), r=[mvB, const2B], w=[mvB])
            p.op("act", lambda e: e.activation(out=mv[:, 3:4], in_=mv[:, 2:3], func=AF.Exp, scale=-0.5), r=[mvB], w=[mvB])

            def fin():
                p.op("dve", lambda e: e.tensor_scalar(out=h[:, tb, :], in0=h[:, tb, :], scalar1=mv[:, 0:1], scalar2=mv[:, 3:4],
                                                      op0=ALU.subtract, op1=ALU.mult), r=[hB[tb], mvB], w=[hB[tb]])
                p.op(gb_eng, lambda e: e.tensor_tensor(out=h[:, tb, :], in0=h[:, tb, :], in1=lnp[:, 0, :], op=ALU.mult), r=[hB[tb], L["lnpB"]], w=[hB[tb]])
                p.op(gb_eng, lambda e: e.tensor_tensor(out=h[:, tb, :], in0=h[:, tb, :], in1=lnp[:, 1, :], op=ALU.add), r=[hB[tb], L["lnpB"]], w=[hB[tb]])
                if dbg_row0 is not None:
                    p.op("sp", lambda e: e.dma_start(out=dbg[dbg_row0 + tb * 128: dbg_row0 + (tb + 1) * 128, :], in_=h[:, tb, :]),
                         r=[hB[tb]], dsem=hB[tb].dsem)
                if spill:
                    p.op("sp", lambda e: e.dma_start(out=hsp[tb * 128:(tb + 1) * 128, :], in_=h[:, tb, :]), r=[hB[tb]], dsem=hB[tb].dsem)
                if out_row0 is not None:
                    p.op("sp", lambda e: e.dma_start(out=out[out_row0 + tb * 128: out_row0 + (tb + 1) * 128, :], in_=h[:, tb, :]),
                         r=[hB[tb]], dsem=hB[tb].dsem)
                elif to_hT:
                    return lambda: build_hT(tb, L)
                return None
            return fin

        class LnPipe:
            DB = 3

            def __init__(self):
                self.pend = None
                self.late = []

            def _run(self, fin):
                r = fin()
                if r is not None:
                    self.late.append(r)

            def push(self, fin):
                if len(self.late) >= self.DB:
                    self.late.pop(0)()
                if self.pend is not None:
                    self._run(self.pend)
                self.pend = fin

            def flush(self):
                if self.pend is not None:
                    self._run(self.pend)
                self.pend = None
                while self.late:
                    self.late.pop(0)()

        epsT = sb(es, "epsT", [128, 1], F32)
        onesf_one = sb(es, "oneT", [128, 1], F32)
        p.op("dve", lambda e: e.memset(epsT[:], EPS), w=[const2B])
        p.op("dve", lambda e: e.memset(onesf_one[:], 1.0), w=[const2B])

        def wload(dst_ap, src_ap, B):
            return p.op("pool", lambda e: e.dma_start(out=dst_ap, in_=src_ap), w=[B], dsem=B.dsem)

        def proj_fm(W, wname, col0, evac, ncols=128):
            wt, wtB = W["wA"].next()
            wload(wt[:, :, 0:ncols], wview(wname)[:, :, col0:col0 + ncols], wtB)
            for tc in range(4):
                pp, ppB = W["pp"].next()

                def f(e, pp=pp, wt=wt, tc=tc):
                    inst = None
                    for kc in range(KC):
                        inst = mm(e, pp[0:ncols, :], wt[:, kc, 0:ncols], hT[:, kc, tc * 512:(tc + 1) * 512], kc == 0, kc == KC - 1)
                    return inst
                p.op("pe", f, r=[wtB] + hTB[4 * tc:4 * tc + 4], w=[ppB])
                evac(tc, pp, ppB)

        def proj_tm(W, wname, col0, ncols, sets, evac):
            wt, wtB = W["wA"].next()
            wload(wt[:, :, 0:ncols], wview(wname)[:, :, col0:col0 + ncols], wtB)
            for si, tsl in enumerate(sets):
                pp, ppB = W["pp"].next()

                def f(e, pp=pp, wt=wt, tsl=tsl):
                    inst = None
                    for kc in range(KC):
                        inst = mm(e, pp[:, 0:ncols], hT[:, kc, tsl], wt[:, kc, 0:ncols], kc == 0, kc == KC - 1)
                    return inst
                p.op("pe", f, r=[wtB] + hTB, w=[ppB])
                evac(si, pp, ppB)

        def do_seq(s):
            row0 = s * S
            def _ph1():
                with ExitStack() as st:
                    p.phase_begin()
                    L = ln_alloc(st, "p0")
                    L["eps"] = epsT
                    for tb in range(TB):
                        p.op("sp", lambda e, tb=tb, row0=row0: e.dma_start(out=h[:, tb, :], in_=dr["x"][row0 + tb * 128: row0 + (tb + 1) * 128, :]),
                             w=[hB[tb]], dsem=hB[tb].dsem)
                    for tb in range(TB):
                        build_hT(tb, L)
                    p.barrier()
            _ph1()
            if phases < 1:
                return

            def _ph2():
                with ExitStack() as st:
                    p.phase_begin()
                    W = {}
                    W["wA"] = Rot([(sb(st, "wA%d" % i, [128, KC, 128], BF16), p.buf("wA", dma=True)) for i in range(3)])
                    qT = sb(st, "qT", [128, S], BF16); qTB = p.buf("qT")
                    kT = sb(st, "kT", [128, S], BF16); kTB = p.buf("kT")
                    Vt = sb(st, "Vt", [128, 3, TB, 128], BF16); VB = [p.buf("V%d" % i) for i in range(3)]
                    t0 = hs[:, 0:S]; t0B = p.buf("t0")
                    t1 = hs[:, S:2 * S]; t1B = p.buf("t1")
                    hi = sb(st, "hi", [4, S], BF16); lo = sb(st, "lo", [4, S], BF16); hlB = p.buf("hl")
                    augQ = sb(st, "augQ", [4, S], BF16); augQB = p.buf("augQ")
                    augK = sb(st, "augK", [4, S], BF16); augKB = p.buf("augK")
                    tq = sb(st, "tq", [4, S], BF16); tqB = p.buf("tq")
                    pT = Rot([(sb(st, "pT%d" % i, [128, 512], BF16), p.buf("pT")) for i in range(4)])
                    rec = hs[0:64, 2 * S:3 * S]; recB = p.buf("rec")
                    sts = Rot([(ps(st, "st%d" % i, [128, 512]), p.buf("st")) for i in range(4)])
                    W["pp"] = Rot(sts.items)
                    LA = 2
                    accn = Rot([(ps(st, "accn%d" % i, [64, 512]), p.buf("accn")) for i in range(2)])
                    accd = Rot([(ps(st, "accd%d" % i, [64, 512]), p.buf("accd")) for i in range(2)])
                    rope = hs[:, 3 * S:5 * S].rearrange("p (a b) -> p a b", a=2); ropeB = p.buf("rope", dma=True)
                    rt = Rot([(hs[:, 5 * S + i * 1024:5 * S + (i + 1) * 1024].rearrange("p (a b) -> p a b", a=2), p.buf("rt")) for i in range(2)])
                    an = hs[0:64, 6 * S:7 * S]; anB = p.buf("an")
                    ad = hs[0:64, 7 * S:8 * S]; adB = p.buf("ad")
                    p.op("sp", lambda e: e.dma_start(out=rope, in_=dr["rope"].rearrange("p (a b) -> p a b", a=2)), w=[ropeB], dsem=ropeB.dsem)

                    def make_aug(src_ap, srcB):
                        p.op("dve", lambda e: e.tensor_copy(out=hi[:], in_=src_ap), r=[srcB], w=[hlB])
                        p.op("dve", lambda e: e.tensor_tensor(out=lo[:], in0=src_ap, in1=hi[:], op=ALU.subtract), r=[srcB, hlB], w=[hlB])

                    def fin_aug(dst, dstB, c0):
                        p.op("dve", lambda e: e.tensor_scalar(out=tq[:], in0=hi[:], scalar1=augc[0:4, c0:c0 + 1], scalar2=augc[0:4, c0 + 2:c0 + 3],
                                                              op0=ALU.mult, op1=ALU.add), r=[hlB] + CB, w=[tqB])
                        p.op("dve", lambda e: e.scalar_tensor_tensor(out=dst[:], in0=lo[:], scalar=augc[0:4, c0 + 1:c0 + 2], in1=tq[:],
                                                                     op0=ALU.mult, op1=ALU.add), r=[hlB, tqB] + CB, w=[dstB])

                    for c in range(4):
                        def ev_q(tc, pp, ppB):
                            p.op("act", lambda e: e.activation(out=qT[:, tc * 512:(tc + 1) * 512], in_=pp[:], func=AF.Copy, scale=0.125), r=[ppB], w=[qTB])

                        def ev_k(tc, pp, ppB):
                            p.op("dve", lambda e: e.tensor_copy(out=kT[:, tc * 512:(tc + 1) * 512], in_=pp[:]), r=[ppB], w=[kTB])

                        def ev_v(si, pp, ppB):
                            p.op("act", lambda e: e.activation(out=Vt[:, 0, si, :], in_=pp[:, 0:128], func=AF.Copy), r=[ppB], w=[VB[0]])
                        proj_fm(W, "wqa", c * 128, ev_q)
                        proj_fm(W, "wka", c * 128, ev_k)
                        proj_tm(W, "wva", c * 128, 128, [slice(tb * 128, (tb + 1) * 128) for tb in range(TB)], ev_v)
                        for hh in range(2):
                            head = 2 * c + hh
                            r0 = 64 * hh

                            def ev_g(tc, pp, ppB, head=head):
                                p.op("act", lambda e: e.activation(out=t0[:, tc * 512:(tc + 1) * 512], in_=pp[:], func=AF.Exp,
                                                                   bias=ngb[:, head:head + 1], scale=-1.0), r=[ppB] + CB, w=[t0B])
                            proj_fm(W, "wfa", head * 128, ev_g)
                            p.op("act", lambda e: e.activation(out=t0[:], in_=t0[:], func=AF.Ln, bias=onesf_one[:, 0:1]), r=[t0B] + CB, w=[t0B])
                            p.op("dve", lambda e: e.tensor_tensor_scan(out=t1[:], data0=ones_bf[:], data1=t0[:], initial=0.0,
                                                                       op0=ALU.mult, op1=ALU.add), r=[t0B] + CB, w=[t1B])
                            make_aug(t1[0:4, :], t1B)
                            fin_aug(augQ, augQB, 0)
                            fin_aug(augK, augKB, 3)
                            acc = {}

                            def fox1(qc, kb, r0=r0):
                                if kb == 0:
                                    acc[qc] = accn.next() + accd.next()
                                nkb = 4 * qc + 4
                                j0 = max(0, kb - 4 * qc)
                                c0 = j0 * 128
                                stt, sttB = sts.next()
                                pt, ptB = pT.next()
                                diag = kb >= 4 * qc

                                def fs(e):
                                    kblk = slice(kb * 128, (kb + 1) * 128)
                                    q0 = qc * 512
                                    inst = None
                                    if diag:
                                        qs = slice(q0 + c0, q0 + c0 + 128)
                                        mm(e, stt[:, c0:c0 + 128], kT[r0:r0 + 64, kblk], qT[r0:r0 + 64, qs], True, False)
                                        mm(e, stt[:, c0:c0 + 128], augK[0:4, kblk], augQ[0:4, qs], False, False)
                                        inst = mm(e, stt[:, c0:c0 + 128], ident[:], masks[:, 0, :], False, True)
                                        c1 = c0 + 128
                                    else:
                                        c1 = c0
                                    if c1 < 512:
                                        qs = slice(q0 + c1, q0 + 512)
                                        mm(e, stt[:, c1:512], kT[r0:r0 + 64, kblk], qT[r0:r0 + 64, qs], True, False)
                                        inst = mm(e, stt[:, c1:512], augK[0:4, kblk], augQ[0:4, qs], False, True)
                                    return inst
                                p.op("pe", fs, r=[kTB, qTB, augKB, augQB] + CB, w=[sttB])
                                p.op("act", lambda e: e.activation(out=pt[:, c0:512], in_=stt[:, c0:512], func=AF.Exp), r=[sttB], w=[ptB])
                                return (qc, kb, nkb, c0, pt, ptB)

                            def fox2(info, r0=r0, hh=hh, c=c):
                                qc, kb, nkb, c0, pt, ptB = info
                                an_, anB_, ad_, adB_ = acc[qc]

                                def fpv(e):
                                    mm(e, an_[:, c0:512], Vt[:, 0, kb, hh * 64:(hh + 1) * 64], pt[:, c0:512], kb == 0, kb == nkb - 1)
                                    return mm(e, ad_[:, c0:512], ones_bf[:, 0:64], pt[:, c0:512], kb == 0, kb == nkb - 1)
                                p.op("pe", fpv, r=[ptB, VB[0]] + CB, w=[anB_, adB_])
                                if kb == nkb - 1:
                                    p.op("dve", lambda e: e.reciprocal(out=rec[:, qc * 512:(qc + 1) * 512], in_=ad_[:]), r=[adB_], w=[recB])
                                    p.op("dve", lambda e: e.tensor_tensor(
                                        out=oT[r0:r0 + 64, c, qc * 512:(qc + 1) * 512], in0=an_[:], in1=rec[:, qc * 512:(qc + 1) * 512], op=ALU.mult),
                                        r=[anB_, recB], w=[oTB[c]])

                            pend = []
                            for qc in range(4):
                                for kb in range(4 * qc + 4):
                                    pend.append(fox1(qc, kb))
                                    if len(pend) > LA:
                                        fox2(pend.pop(0))
                            while pend:
                                fox2(pend.pop(0))

                    def tokset(bi, si):
                        if bi == 0:
                            return slice(si * 128, (si + 1) * 128)
                        if bi == 1:
                            r_, n_ = si // 4, si % 4
                            return slice(512 * n_ + r_, 512 * (n_ + 1), 4)
                        return slice(si, S, 16)

                    def accview(t, bi, si):
                        if bi == 0:
                            return t[:, :].rearrange("p (n j) -> p n j", j=128)[:, si:si + 2, :]
                        if bi == 1:
                            r_, n_ = si // 4, si % 4
                            return t[:, :].rearrange("p (n j r) -> p n j r", n=4, j=128, r=4)[:, n_:n_ + 2, :, r_]
                        return t[:, :].rearrange("p (j r) -> p r j", r=16)[:, si:si + 2, :]

                    for c in range(4):
                        def mk_rope(dst, dstB, wn, wns, c=c):
                            store = {}

                            def ev_a(tc, pp, ppB):
                                rtt, rtB = rt.next()
                                store[tc] = (rtt, rtB)
                                p.op("dve", lambda e: e.tensor_tensor(out=rtt[:, 0, :], in0=pp[:], in1=rope[:, 0, tc * 512:(tc + 1) * 512], op=ALU.mult),
                                     r=[ppB, ropeB], w=[rtB])

                            def ev_b(tc, pp, ppB):
                                rtt, rtB = store[tc]
                                p.op("dve", lambda e: e.tensor_tensor(out=rtt[:, 1, :], in0=pp[:], in1=rope[:, 1, tc * 512:(tc + 1) * 512], op=ALU.mult),
                                     r=[ppB, ropeB], w=[rtB])
                                p.op("pool", lambda e: e.tensor_tensor(out=dst[:, tc * 512:(tc + 1) * 512], in0=rtt[:, 0, :], in1=rtt[:, 1, :], op=ALU.add),
                                     r=[rtB], w=[dstB])
                            wa, waB = W["wA"].next()
                            wb, wbB = W["wA"].next()
                            wload(wa[:], wview(wn)[:, :, c * 128:(c + 1) * 128], waB)
                            wload(wb[:], wview(wns)[:, :, c * 128:(c + 1) * 128], wbB)
                            for tc in range(4):
                                for (wt_, wtB_, ev) in ((wa, waB, ev_a), (wb, wbB, ev_b)):
                                    pp, ppB = W["pp"].next()

                                    def f(e, pp=pp, wt_=wt_, tc=tc):
                                        inst = None
                                        for kc in range(KC):
                                            inst = mm(e, pp[:], wt_[:, kc, :], hT[:, kc, tc * 512:(tc + 1) * 512], kc == 0, kc == KC - 1)
                                        return inst
                                    p.op("pe", f, r=[wtB_] + hTB[4 * tc:4 * tc + 4], w=[ppB])
                                    ev(tc, pp, ppB)
                        mk_rope(qT, qTB, "wqd", "wqds")
                        mk_rope(kT, kTB, "wkd", "wkds")
                        for bi in range(3):
                            def ev_v(si, pp, ppB, bi=bi):
                                p.op("act", lambda e: e.activation(out=Vt[:, bi, si, :], in_=pp[:, 0:128], func=AF.Copy), r=[ppB], w=[VB[bi]])
                            proj_tm(W, "wvd", c * 128, 128, [tokset(bi, si) for si in range(16)], ev_v)
                        def dil1(blocks, mi, bi, si, r0):
                            stt, sttB = sts.next()
                            pt, ptB = pT.next()

                            def fs(e):
                                inst = None
                                for bk, (sq, sk) in enumerate(blocks):
                                    mm(e, stt[:, bk * 128:(bk + 1) * 128], kT[r0:r0 + 64, tokset(bi, sk)], qT[r0:r0 + 64, tokset(bi, sq)], True, False)
                                    inst = mm(e, stt[:, bk * 128:(bk + 1) * 128], ident[:], mask4[:, mi, bk * 128:(bk + 1) * 128], False, True)
                                return inst
                            p.op("pe", fs, r=[kTB, qTB] + CB, w=[sttB])
                            p.op("act", lambda e: e.activation(out=pt[:], in_=stt[:], func=AF.Exp, scale=0.125), r=[sttB], w=[ptB])
                            return (blocks, bi, si, pt, ptB)

                        def dil2(info, hh):
                            blocks, bi, si, pt, ptB = info
                            an_, anB_ = accn.next()
                            ad_, adB_ = accd.next()

                            def fpv(e):
                                inst = None
                                for bk, (sq, sk) in enumerate(blocks):
                                    qi = bk // 2
                                    first = (bk % 2 == 0)
                                    mm(e, an_[:, qi * 128:(qi + 1) * 128], Vt[:, bi, sk, hh * 64:(hh + 1) * 64], pt[:, bk * 128:(bk + 1) * 128], first, not first)
                                    inst = mm(e, ad_[:, qi * 128:(qi + 1) * 128], ones_bf[:, 0:64], pt[:, bk * 128:(bk + 1) * 128], first, not first)
                                return inst
                            p.op("pe", fpv, r=[ptB, VB[bi]] + CB, w=[anB_, adB_])
                            pv = lambda t: t[:, 0:256].rearrange("p (a j) -> p a j", a=2)
                            if bi == 0:
                                p.op("dve", lambda e: e.tensor_copy(out=accview(an, 0, si), in_=pv(an_)), r=[anB_], w=[anB])
                                p.op("dve", lambda e: e.tensor_copy(out=accview(ad, 0, si), in_=pv(ad_)), r=[adB_], w=[adB])
                            else:
                                p.op("dve", lambda e: e.tensor_tensor(out=accview(an, bi, si), in0=pv(an_), in1=accview(an, bi, si), op=ALU.add),
                                     r=[anB_, anB], w=[anB])
                                p.op("dve", lambda e: e.tensor_tensor(out=accview(ad, bi, si), in0=pv(ad_), in1=accview(ad, bi, si), op=ALU.add),
                                     r=[adB_, adB], w=[adB])

                        for hh in range(2):
                            r0 = 64 * hh
                            dpend = []
                            for bi in range(3):
                                for si in range(0, 16, 2):
                                    blocks = []
                                    for sq in (si, si + 1):
                                        if bi == 0:
                                            prev = sq - 1 if sq >= 1 else None
                                        elif bi == 1:
                                            prev = sq - 1 if (sq % 4) >= 1 else None
                                        else:
                                            prev = None
                                        blocks.append((sq, prev if prev is not None else sq))
                                        blocks.append((sq, sq))
                                    if bi == 2:
                                        mi = 2
                                    elif (bi == 0 and si == 0) or (bi == 1 and si % 4 == 0):
                                        mi = 1
                                    else:
                                        mi = 0
                                    dpend.append(dil1(blocks, mi, bi, si, r0))
                                    if len(dpend) > LA:
                                        dil2(dpend.pop(0), hh)
                            while dpend:
                                dil2(dpend.pop(0), hh)
                            p.op("dve", lambda e: e.reciprocal(out=rec[:], in_=ad[:]), r=[adB], w=[recB])
                            p.op("dve", lambda e, r0=r0, c=c: e.tensor_tensor(out=oT[r0:r0 + 64, 4 + c, :], in0=an[:], in1=rec[:], op=ALU.mult),
                                 r=[anB, recB], w=[oTB[4 + c]])
                    if debug and s == 0:
                        dB = p.buf("dbg2", dma=True)
                        p.op("pool", lambda e: e.dma_start(out=dbg2[0:128, :].rearrange("p (a b) -> p a b", a=KC), in_=oT[:]), r=oTB, dsem=dB.dsem)
                        p.op("pool", lambda e: e.dma_start(out=dbg2[128:256, 0:S], in_=qT[:]), r=[qTB], dsem=dB.dsem)
                        p.op("pool", lambda e: e.dma_start(out=dbg2[128:256, S:2 * S], in_=kT[:]), r=[kTB], dsem=dB.dsem)
                        p.op("pool", lambda e: e.dma_start(out=dbg2[128:192, 2 * S:3 * S], in_=an), r=[anB], dsem=dB.dsem)
                        p.op("pool", lambda e: e.dma_start(out=dbg2[128:192, 3 * S:4 * S], in_=ad), r=[adB], dsem=dB.dsem)
                        p.op("pool", lambda e: e.dma_start(out=dbg2[128:256, 4 * S:5 * S], in_=Vt[:, 1, :, :].rearrange("p a b -> p (a b)")), r=VB, dsem=dB.dsem)
                    p.barrier()

            _ph2()
            def mix_stage(wname, li, dbg_i, row0=row0):
                with ExitStack() as st:
                    p.phase_begin()
                    L = ln_alloc(st, "m%d" % li)
                    L["eps"] = epsT
                    ln_load(L, li)
                    for tb in range(TB):
                        src = dr["x"][row0 + tb * 128: row0 + (tb + 1) * 128, :] if li == 0 else hsp[tb * 128:(tb + 1) * 128, :]
                        p.op("sp", lambda e, tb=tb, src=src: e.dma_start(out=h[:, tb, :], in_=src), w=[hB[tb]], dsem=hB[tb].dsem)
                    wo = sb(st, "wo", [128, KC, D], BF16)
                    woB = p.buf("wo", dma=True)
                    wload(wo[:, :, 0:512], wview(wname)[:, :, 0:512], woB)
                    wload(wo[:, :, 512:1024], wview(wname)[:, :, 512:1024], woB)
                    mixp = Rot([(ps(st, "mix%d" % i, [128, D]), p.buf("mix")) for i in range(2)])
                    lp = LnPipe()
                    for tb in range(TB):
                        mp, mpB = mixp.next()

                        def f(e, mp=mp, tb=tb):
                            inst = None
                            for half in range(2):
                                for c in range(KC):
                                    inst = mm(e, mp[:, half * 512:(half + 1) * 512], oT[:, c, tb * 128:(tb + 1) * 128],
                                              wo[:, c, half * 512:(half + 1) * 512], c == 0, c == KC - 1)
                            return inst
                        p.op("pe", f, r=[woB] + oTB, w=[mpB])
                        p.op("dve", lambda e, mp=mp, tb=tb: e.scalar_tensor_tensor(out=h[:, tb, :], in0=h[:, tb, :], scalar=ALPHA, in1=mp[:],
                                                                                   op0=ALU.mult, op1=ALU.add), r=[hB[tb], mpB], w=[hB[tb]])
                        lp.push(ln_block(tb, L, to_hT=True, dbg_row0=(dbg_i * S if (debug and s == 0) else None)))
                    lp.flush()
                    p.barrier()
            mix_stage("wo0", 0, 0)
            if phases < 2:
                return

            def ffn_alloc(st):
                Fd = {}
                Fd["wg"] = Rot([(sb(st, "wg%d" % i, [128, KC, 512], BF16), p.buf("wg", dma=True)) for i in range(2)])
                Fd["wu"] = Rot([(sb(st, "wu%d" % i, [128, KC, 512], BF16), p.buf("wu", dma=True)) for i in range(2)])
                Fd["wd"] = Rot([(sb(st, "wd%d" % i, [128, 4, D], BF16), p.buf("wd", dma=True)) for i in range(2)])
                Fd["aT"] = Rot([(oT[:, 4 * i:4 * i + 4, :], p.buf("aT")) for i in range(2)])
                Fd["sg"] = Rot([(sb(st, "sg%d" % i, [128, 512], F32), p.buf("sg")) for i in range(2)])
                Fd["pg"] = Rot([(ps(st, "pg%d" % i, [128, 512]), p.buf("pg")) for i in range(2)])
                Fd["pu"] = Rot([(ps(st, "pu%d" % i, [128, 512]), p.buf("pu")) for i in range(2)])
                Fd["yp"] = Rot([(ps(st, "yp%d" % i, [128, D]), p.buf("yp")) for i in range(2)])
                return Fd

            def ffn(Fd, gname, uname, dname, grow0, drow0, F, gate_fn):
                f0 = 0
                while f0 < F:
                    gw = min(512, F - f0)
                    gc = gw // 128
                    wg, wgB = Fd["wg"].next()
                    wu, wuB = Fd["wu"].next()
                    wd, wdB = Fd["wd"].next()
                    aT, aTB = Fd["aT"].next()
                    wload(wg[:, :, 0:gw], wview(gname, grow0, D)[:, :, f0:f0 + gw], wgB)
                    wload(wu[:, :, 0:gw], wview(uname, grow0, D)[:, :, f0:f0 + gw], wuB)
                    wload(wd[:, 0:gc, :], wview(dname, drow0 + f0, gw), wdB)
                    for ci in range(gc):
                        for tc in range(4):
                            pg, pgB = Fd["pg"].next()
                            pu, puB = Fd["pu"].next()
                            sg, sgB = Fd["sg"].next()

                            def fg_(e, pg=pg, wg=wg, ci=ci, tc=tc):
                                inst = None
                                for kc in range(KC):
                                    inst = mm(e, pg[:], wg[:, kc, ci * 128:(ci + 1) * 128], hT[:, kc, tc * 512:(tc + 1) * 512], kc == 0, kc == KC - 1)
                                return inst

                            def fu_(e, pu=pu, wu=wu, ci=ci, tc=tc):
                                inst = None
                                for kc in range(KC):
                                    inst = mm(e, pu[:], wu[:, kc, ci * 128:(ci + 1) * 128], hT[:, kc, tc * 512:(tc + 1) * 512], kc == 0, kc == KC - 1)
                                return inst
                            p.op("pe", fg_, r=[wgB] + hTB[4 * tc:4 * tc + 4], w=[pgB])
                            p.op("pe", fu_, r=[wuB] + hTB[4 * tc:4 * tc + 4], w=[puB])
                            p.op("act", lambda e, sg=sg, pg=pg: e.activation(out=sg[:], in_=pg[:], func=AF.Silu), r=[pgB], w=[sgB])
                            p.op("dve", lambda e, sg=sg, pu=pu, aT=aT, ci=ci, tc=tc: e.tensor_tensor(
                                out=aT[:, ci, tc * 512:(tc + 1) * 512], in0=pu[:], in1=sg[:], op=ALU.mult), r=[puB, sgB], w=[aTB])
                    for tb in range(TB):
                        yp, ypB = Fd["yp"].next()

                        def fd_(e, yp=yp, aT=aT, wd=wd, tb=tb, gc=gc):
                            inst = None
                            for half in range(2):
                                for ci in range(gc):
                                    inst = mm(e, yp[:, half * 512:(half + 1) * 512], aT[:, ci, tb * 128:(tb + 1) * 128],
                                              wd[:, ci, half * 512:(half + 1) * 512], ci == 0, ci == gc - 1)
                            return inst
                        p.op("pe", fd_, r=[aTB, wdB], w=[ypB])
                        g_ap, gBs = gate_fn(tb)
                        p.op("dve", lambda e, yp=yp, tb=tb, g_ap=g_ap: e.scalar_tensor_tensor(out=h[:, tb, :], in0=yp[:], scalar=g_ap, in1=h[:, tb, :],
                                                                                            op0=ALU.mult, op1=ALU.add), r=[ypB, hB[tb]] + gBs, w=[hB[tb]])
                    f0 += gw

            def scale_h():
                for tb in range(TB):
                    p.op("act", lambda e, tb=tb: e.mul(out=h[:, tb, :], in_=h[:, tb, :], mul=ALPHA), r=[hB[tb]], w=[hB[tb]])

            def final_ln(li, dbg_i, to_hT, out_row0, spill=False):
                outs = []
                with ExitStack() as st:
                    p.phase_begin()
                    L = ln_alloc(st, "f%d" % li)
                    L["eps"] = epsT
                    ln_load(L, li)
                    lp = LnPipe()
                    for tb in range(TB):
                        lp.push(ln_block(tb, L, to_hT=to_hT, out_row0=out_row0, dbg_row0=(dbg_i * S if (debug and s == 0) else None), spill=spill))
                    lp.flush()
                    p.barrier()
                return outs

            def _ph3():
                with ExitStack() as st:
                    p.phase_begin()
                    Fd = ffn_alloc(st)
                    scale_h()
                    ffn(Fd, "fg", "fu", "fd", 0, 0, DFF, lambda tb: (1.0, []))
                    p.barrier()
            _ph3()
            final_ln(1, 1, True, None, spill=True)
            if phases < 3:
                return

            def _ph4():
                with ExitStack() as st:
                    p.phase_begin()
                    W = {}
                    W["wA"] = Rot([(sb(st, "wA%d" % i, [128, KC, 128], BF16), p.buf("wA", dma=True)) for i in range(3)])
                    qT = sb(st, "qT", [128, S], BF16); qTB = p.buf("qT")
                    kT = sb(st, "kT", [128, S], BF16); kTB = p.buf("kT")
                    Vh = sb(st, "Vh", [128, TB, 128], BF16); VhB = p.buf("Vh")
                    sgo = sb(st, "sgo", [128, S], BF16); sgoB = p.buf("sgo")
                    pre = hs[:, 0:3 + S]; preB = p.buf("pre"); padB = p.buf("pad")
                    yv = hs[:, 2052:2052 + S]; yvB = p.buf("yv")
                    t0 = hs[:, 3:3 + S]; t0B = preB
                    t1 = hs[:, 4100:4100 + S]; t1B = p.buf("t1")
                    t2 = yv; t2B = yvB
                    hi = sb(st, "hi", [4, S], BF16); lo = sb(st, "lo", [4, S], BF16); hlB = p.buf("hl")
                    ua = hs[0:4, 9220:9220 + S]; uaB = p.buf("ua")
                    augQ = sb(st, "augQ", [4, S], BF16); augQB = p.buf("augQ")
                    augK = sb(st, "augK", [4, S], BF16); augKB = p.buf("augK")
                    tq = sb(st, "tq", [4, S], BF16); tqB = p.buf("tq")
                    pT = Rot([(sb(st, "pT%d" % i, [128, 512], BF16), p.buf("pT")) for i in range(3)])
                    Et = Rot([(hs[:, 12288 + i * 512:12288 + (i + 1) * 512], p.buf("Et")) for i in range(3)])
                    psA = Rot([(ps(st, "psA%d" % i, [128, 512]), p.buf("psA")) for i in range(3)])
                    psB = Rot([(ps(st, "psB%d" % i, [128, 512]), p.buf("psB")) for i in range(3)])
                    accn_t = ps(st, "accn", [128, 512]); accnB = p.buf("accn")
                    accd_t = ps(st, "accd", [128, 512]); accdB = p.buf("accd")
                    W["pp"] = Rot(psA.items + psB.items)
                    f1 = hs[:, 7172:7684]; f1B = p.buf("f1")
                    f2 = hs[:, 7684:8196]; f2B = p.buf("f2")
                    f3 = hs[:, 8196:8708]; f3B = p.buf("f3")
                    f4 = hs[:, 8708:9220]; f4B = p.buf("f4")
                    p.op("dve", lambda e: e.memset(pre[:, 0:3], 0.0), w=[padB])

                    def make_aug(src_ap, srcB):
                        p.op("dve", lambda e: e.tensor_copy(out=hi[:], in_=src_ap), r=[srcB], w=[hlB])
                        p.op("dve", lambda e: e.tensor_tensor(out=lo[:], in0=src_ap, in1=hi[:], op=ALU.subtract), r=[srcB, hlB], w=[hlB])

                    def fin_aug(dst, dstB, c0):
                        p.op("dve", lambda e: e.tensor_scalar(out=tq[:], in0=hi[:], scalar1=augc[0:4, c0:c0 + 1], scalar2=augc[0:4, c0 + 2:c0 + 3],
                                                              op0=ALU.mult, op1=ALU.add), r=[hlB] + CB, w=[tqB])
                        p.op("dve", lambda e: e.scalar_tensor_tensor(out=dst[:], in0=lo[:], scalar=augc[0:4, c0 + 1:c0 + 2], in1=tq[:],
                                                                     op0=ALU.mult, op1=ALU.add), r=[hlB, tqB] + CB, w=[dstB])

                    for hd in range(8):
                        def conv_silu(wname, col0, chunk, dst, dstB):
                            def ev(tc, pp, ppB):
                                p.op("act", lambda e: e.activation(out=pre[:, 3 + tc * 512: 3 + (tc + 1) * 512], in_=pp[:], func=AF.Copy), r=[ppB], w=[preB])
                            proj_fm(W, wname, col0, ev)
                            p.op("dve", lambda e: e.tensor_scalar(out=yv[:], in0=pre[:, 3:3 + S], scalar1=wc[:, chunk, 3:4], scalar2=None, op0=ALU.mult),
                                 r=[preB, padB] + CB, w=[yvB])
                            for i in range(3):
                                p.op("dve", lambda e, i=i: e.scalar_tensor_tensor(out=yv[:], in0=pre[:, i:i + S], scalar=wc[:, chunk, i:i + 1], in1=yv[:],
                                                                                 op0=ALU.mult, op1=ALU.add), r=[preB, padB, yvB] + CB, w=[yvB])
                            p.op("act", lambda e: e.activation(out=dst[:], in_=yv[:], func=AF.Silu), r=[yvB], w=[dstB])
                        conv_silu("wq1", hd * 128, hd, qT, qTB)
                        conv_silu("wk1", hd * 128, 8 + hd, kT, kTB)

                        def ev_v(si, pp, ppB):
                            p.op("act", lambda e: e.activation(out=Vh[:, si, :], in_=pp[:, 0:128], func=AF.Copy), r=[ppB], w=[VhB])
                        proj_tm(W, "wv1", hd * 128, 128, [slice(tb * 128, (tb + 1) * 128) for tb in range(TB)], ev_v)

                        def ev_og(tc, pp, ppB):
                            p.op("act", lambda e: e.activation(out=sgo[:, tc * 512:(tc + 1) * 512], in_=pp[:], func=AF.Sigmoid), r=[ppB], w=[sgoB])
                        proj_fm(W, "wog", hd * 128, ev_og)

                        def ev_f(tc, pp, ppB, hd=hd):
                            p.op("act", lambda e: e.activation(out=t0[:, tc * 512:(tc + 1) * 512], in_=pp[:], func=AF.Exp,
                                                               bias=ngb[:, 16 + hd:17 + hd], scale=-1.0), r=[ppB] + CB, w=[t0B])
                        proj_fm(W, "wfg", hd * 128, ev_f)
                        p.op("act", lambda e: e.activation(out=t0[:], in_=t0[:], func=AF.Ln, bias=onesf_one[:, 0:1]), r=[t0B] + CB, w=[t0B])
                        p.op("dve", lambda e: e.tensor_tensor_scan(out=t1[:], data0=ones_bf[:], data1=t0[:], initial=0.0, op0=ALU.mult, op1=ALU.add),
                             r=[t0B] + CB, w=[t1B])

                        def ev_i(tc, pp, ppB, hd=hd):
                            p.op("dve", lambda e: e.scalar_tensor_tensor(out=t0[:, tc * 512:(tc + 1) * 512], in0=pp[:], scalar=gb[:, 8 + hd:9 + hd],
                                                                         in1=t1[:, tc * 512:(tc + 1) * 512], op0=ALU.add, op1=ALU.add),
                                 r=[ppB, t1B] + CB, w=[t0B])
                        proj_fm(W, "wig", hd * 128, ev_i)
                        p.op("dve", lambda e: e.tensor_tensor_scan(out=t2[:], data0=t0[:], data1=t0[:], initial=-1e30, op0=ALU.max, op1=ALU.max),
                             r=[t0B] + CB, w=[t2B])
                        p.op("dve", lambda e: e.tensor_tensor(out=t1[:], in0=t1[:], in1=t2[:], op=ALU.subtract), r=[t1B, t2B], w=[t1B])
                        p.op("act", lambda e: e.activation(out=t1[:], in_=t1[:], func=AF.Exp), r=[t1B], w=[t1B])
                        make_aug(t2[0:4, :], t2B)
                        fin_aug(augQ, augQB, 0)
                        p.op("dve", lambda e: e.tensor_scalar(out=ua[:], in0=t0[0:4, :], scalar1=LNS, scalar2=None, op0=ALU.add), r=[t0B], w=[uaB])
                        make_aug(ua[:], uaB)
                        fin_aug(augK, augKB, 3)

                        def stage1(qc, kb):
                            nkb = 4 * qc + 4
                            j0 = max(0, kb - 4 * qc)
                            c0 = j0 * 128
                            diag = kb >= 4 * qc
                            pa, paB = psA.next()
                            pb, pbB = psB.next()
                            pt, ptB = pT.next()
                            et, etB = Et.next()
                            kblk = slice(kb * 128, (kb + 1) * 128)
                            qs = slice(qc * 512 + c0, qc * 512 + 512)
                            p.op("pe", lambda e: mm(e, pa[:, c0:512], kT[:, kblk], qT[:, qs], True, True), r=[kTB, qTB], w=[paB])

                            def fd_(e):
                                q0 = qc * 512
                                inst = None
                                c1 = c0
                                if diag:
                                    qs1 = slice(q0 + c0, q0 + c0 + 128)
                                    mm(e, pb[:, c0:c0 + 128], augK[0:4, kblk], augQ[0:4, qs1], True, False)
                                    inst = mm(e, pb[:, c0:c0 + 128], ident[:], masks[:, 0, :], False, True)
                                    c1 = c0 + 128
                                if c1 < 512:
                                    inst = mm(e, pb[:, c1:512], augK[0:4, kblk], augQ[0:4, slice(q0 + c1, q0 + 512)], True, True)
                                return inst
                            p.op("pe", fd_, r=[augKB, augQB] + CB, w=[pbB])
                            p.op("act", lambda e: e.activation(out=et[:, c0:512], in_=pb[:, c0:512], func=AF.Exp), r=[pbB], w=[etB])
                            p.op("dve", lambda e: e.tensor_tensor(out=pt[:, c0:512], in0=pa[:, c0:512], in1=et[:, c0:512], op=ALU.mult), r=[paB, etB], w=[ptB])
                            return (qc, kb, nkb, c0, pt, ptB)

                        def finalize(qc, hd=hd):
                            cs = slice(qc * 512, (qc + 1) * 512)
                            s1, s1B = W["pp"].next()
                            s2, s2B = W["pp"].next()
                            p.op("act", lambda e: e.activation(out=f1[:], in_=accd_t[:], func=AF.Abs), r=[accdB], w=[f1B])
                            p.op("dve", lambda e: e.tensor_tensor(out=f1[:], in0=f1[:], in1=t1[:, cs], op=ALU.max), r=[f1B, t1B], w=[f1B])
                            p.op("dve", lambda e: e.reciprocal(out=f1[:], in_=f1[:]), r=[f1B], w=[f1B])
                            p.op("dve", lambda e: e.tensor_tensor(out=f2[:], in0=accn_t[:], in1=f1[:], op=ALU.mult), r=[accnB, f1B], w=[f2B])
                            p.op("act", lambda e: e.activation(out=f3[:], in_=f2[:], func=AF.Square), r=[f2B], w=[f3B])
                            p.op("pe", lambda e: mm(e, s1[:], onesf[:], f2[:], True, True), r=[f2B] + CB, w=[s1B])
                            p.op("act", lambda e: e.activation(out=f1[:], in_=s1[:], func=AF.Copy), r=[s1B], w=[f1B])
                            p.op("pe", lambda e: mm(e, s2[:], onesf[:], f3[:], True, True), r=[f3B] + CB, w=[s2B])
                            p.op("dve", lambda e: e.tensor_tensor(out=f4[:], in0=f1[:], in1=f1[:], op=ALU.mult), r=[f1B], w=[f4B])
                            p.op("dve", lambda e: e.tensor_tensor(out=f4[:], in0=s2[:], in1=f4[:], op=ALU.subtract), r=[s2B, f4B], w=[f4B])
                            p.op("act", lambda e: e.activation(out=f4[:], in_=f4[:], func=AF.Ln, bias=epsT[:, 0:1]), r=[f4B] + CB, w=[f4B])
                            p.op("act", lambda e: e.activation(out=f4[:], in_=f4[:], func=AF.Exp, scale=-0.5), r=[f4B], w=[f4B])
                            p.op("dve", lambda e: e.tensor_tensor(out=f2[:], in0=f2[:], in1=f1[:], op=ALU.subtract), r=[f2B, f1B], w=[f2B])
                            p.op("dve", lambda e: e.tensor_tensor(out=f2[:], in0=f2[:], in1=f4[:], op=ALU.mult), r=[f2B, f4B], w=[f2B])
                            p.op("dve", lambda e: e.scalar_tensor_tensor(out=oT[:, hd, cs], in0=f2[:], scalar=ng[:, hd:hd + 1], in1=sgo[:, cs],
                                                                         op0=ALU.mult, op1=ALU.mult), r=[f2B, sgoB] + CB, w=[oTB[hd]])

                        def stage2(info):
                            qc, kb, nkb, c0, pt, ptB = info

                            def fpv(e):
                                mm(e, accn_t[:, c0:512], Vh[:, kb, :], pt[:, c0:512], kb == 0, kb == nkb - 1)
                                return mm(e, accd_t[:, c0:512], ones_bf[:, 0:128], pt[:, c0:512], kb == 0, kb == nkb - 1)
                            p.op("pe", fpv, r=[ptB, VhB] + CB, w=[accnB, accdB])
                            if kb == nkb - 1:
                                finalize(qc)

                        LA = 2
                        pend = []
                        for qc in range(4):
                            for kb in range(4 * qc + 4):
                                pend.append(stage1(qc, kb))
                                if len(pend) > LA:
                                    stage2(pend.pop(0))
                        while pend:
                            stage2(pend.pop(0))
                    if debug and s == 0:
                        dB = p.buf("dbg2b", dma=True)
                        p.op("pool", lambda e: e.dma_start(out=dbg2[128:256, :].rearrange("p (a b) -> p a b", a=KC), in_=oT[:]), r=oTB, dsem=dB.dsem)
                    p.barrier()
            _ph4()
            mix_stage("wo1", 2, 2)
            if phases < 4:
                return

            def _ph5():
                with ExitStack() as st:
                    p.phase_begin()
                    wr = sb(st, "wr", [128, KC, 8], BF16); wrB = p.buf("wr", dma=True)
                    wload(wr[:], wview("wr"), wrB)
                    lgp = ps(st, "lgp", [128, 128]); lgB = p.buf("lg")
                    prp = ps(st, "prp", [128, 128]); prB = p.buf("pr")
                    ttp = ps(st, "ttp", [128, 128]); ttB = p.buf("tt")
                    Lg = sb(st, "Lg", [128, 128], F32); LgB = p.buf("Lg")
                    L2 = sb(st, "L2", [128, 128], F32)
                    e1 = sb(st, "e1", [128, 128], F32)
                    e2 = sb(st, "e2", [128, 128], F32)
                    Mb = sb(st, "Mb", [128, 128], BF16)
                    Tt = sb(st, "Tt", [128, 128], F32)
                    Sc = sb(st, "Sc", [128, 128], F32)
                    Rr = sb(st, "Rr", [128, 128], F32)
                    row = sb(st, "row", [128, 128], F32)
                    tmp = sb(st, "tmp", [128, 128], F32)
                    pf = sb(st, "pf", [128, 32], F32)
                    tsum = sb(st, "tsum", [128, 8], F32)
                    m1 = sb(st, "m1", [128, 16], F32)
                    m2 = sb(st, "m2", [128, 16], F32)
                    w1 = sb(st, "w1", [128, 16], F32)
                    w2 = sb(st, "w2", [128, 16], F32)
                    v3 = lambda t: t[:, :].rearrange("p (a b) -> p a b", b=8)
                    emT = lambda t: t[:, :].rearrange("p (e a) -> p a e", e=8)
                    em3 = lambda t: t[:, :].rearrange("p (e a) -> p e a", e=8)
                    bc = lambda t: t[:, :].unsqueeze(2).to_broadcast([128, 16, 8])

                    def flg(e):
                        inst = None
                        for tb in range(TB):
                            for kc in range(KC):
                                inst = mm(e, lgp[:, tb * 8:(tb + 1) * 8], hT[:, kc, tb * 128:(tb + 1) * 128], wr[:, kc, :], kc == 0, kc == KC - 1)
                        return inst
                    p.op("pe", flg, r=[wrB] + hTB, w=[lgB])
                    G = [LgB]
                    p.op("dve", lambda e: e.tensor_copy(out=Lg[:], in_=lgp[:]), r=[lgB], w=G)
                    p.op("dve", lambda e: e.tensor_reduce(out=m1[:], in_=v3(Lg), axis=AX.X, op=ALU.max), r=G, w=G)
                    p.op("dve", lambda e: e.tensor_tensor(out=v3(e1), in0=v3(Lg), in1=bc(m1), op=ALU.is_equal), r=G, w=G)
                    p.op("dve", lambda e: e.scalar_tensor_tensor(out=L2[:], in0=e1[:], scalar=-1e30, in1=Lg[:], op0=ALU.mult, op1=ALU.add), r=G, w=G)
                    p.op("dve", lambda e: e.tensor_reduce(out=m2[:], in_=v3(L2), axis=AX.X, op=ALU.max), r=G, w=G)
                    p.op("dve", lambda e: e.tensor_tensor(out=v3(e2), in0=v3(L2), in1=bc(m2), op=ALU.is_equal), r=G, w=G)
                    p.op("dve", lambda e: e.tensor_tensor(out=w2[:], in0=m2[:], in1=m1[:], op=ALU.subtract), r=G, w=G)
                    p.op("act", lambda e: e.activation(out=w2[:], in_=w2[:], func=AF.Exp), r=G, w=G)
                    p.op("dve", lambda e: e.tensor_scalar(out=w1[:], in0=w2[:], scalar1=1.0, scalar2=None, op0=ALU.add), r=G, w=G)
                    p.op("dve", lambda e: e.reciprocal(out=w1[:], in_=w1[:]), r=G, w=G)
                    p.op("dve", lambda e: e.tensor_tensor(out=w2[:], in0=w2[:], in1=w1[:], op=ALU.mult), r=G, w=G)
                    p.op("dve", lambda e: e.tensor_copy(out=gW[:, s * 32:s * 32 + 16], in_=w1[:]), r=G, w=[gWB])
                    p.op("dve", lambda e: e.tensor_copy(out=gW[:, s * 32 + 16:s * 32 + 32], in_=w2[:]), r=G, w=[gWB])
                    p.op("dve", lambda e: e.tensor_tensor(out=emT(Mb), in0=v3(e1), in1=v3(e2), op=ALU.add), r=G, w=G)

                    def fpr(e):
                        mm(e, prp[:], tri[:], Mb[:], True, True)
                        return mm(e, ttp[:], ones_bf[:, 0:128], Mb[:], True, True)
                    p.op("pe", fpr, r=G + CB, w=[prB, ttB])
                    p.op("dve", lambda e: e.tensor_copy(out=Tt[:], in_=ttp[:]), r=[ttB], w=G)
                    p.op("dve", lambda e: e.tensor_tensor_scan(out=Sc[:], data0=ones_bf[:, 0:128], data1=Tt[:], initial=0.0, op0=ALU.mult, op1=ALU.add),
                         r=G + CB, w=G)
                    p.op("dve", lambda e: e.tensor_tensor(out=Sc[:], in0=Sc[:], in1=Tt[:], op=ALU.subtract), r=G, w=G)
                    p.op("dve", lambda e: e.tensor_tensor(out=em3(Rr), in0=em3(Sc), in1=em3(Sc)[:, :, 0:1].to_broadcast([128, 8, 16]), op=ALU.subtract), r=G, w=G)
                    p.op("dve", lambda e: e.tensor_tensor(out=em3(Rr), in0=em3(Rr), in1=base[:, :].unsqueeze(2).to_broadcast([128, 8, 16]), op=ALU.add),
                         r=G + [baseB], w=G)
                    p.op("dve", lambda e: e.tensor_tensor(out=row[:], in0=prp[:], in1=Rr[:], op=ALU.add), r=G + [prB], w=G)
                    p.op("dve", lambda e: e.tensor_tensor(out=row[:], in0=row[:], in1=eoff[:], op=ALU.add), r=G + CB, w=G)
                    p.op("dve", lambda e: e.tensor_reduce(out=tsum[:], in_=em3(Tt), axis=AX.X, op=ALU.add), r=G, w=G)
                    p.op("dve", lambda e: e.tensor_tensor(out=base[:], in0=base[:], in1=tsum[:], op=ALU.add), r=G + [baseB], w=[baseB])
                    p.op("dve", lambda e: e.tensor_tensor(out=v3(tmp), in0=v3(e1), in1=emT(row), op=ALU.mult), r=G, w=G)
                    p.op("dve", lambda e: e.tensor_reduce(out=pf[:, 0:16], in_=v3(tmp), axis=AX.X, op=ALU.add), r=G, w=G)
                    p.op("dve", lambda e: e.tensor_tensor(out=v3(tmp), in0=v3(e2), in1=emT(row), op=ALU.mult), r=G, w=G)
                    p.op("dve", lambda e: e.tensor_reduce(out=pf[:, 16:32], in_=v3(tmp), axis=AX.X, op=ALU.add), r=G, w=G)
                    p.op("dve", lambda e: e.tensor_copy(out=posI[:, s * 32:(s + 1) * 32], in_=pf[:]), r=G, w=[posB])
                    for tb in range(TB):
                        p.op("sp", lambda e, tb=tb: e.dma_start(out=h1sp[row0 + tb * 128: row0 + (tb + 1) * 128, :], in_=h[:, tb, :]),
                             r=[hB[tb]], dsem=hB[tb].dsem)
                        for k in range(2):
                            col = s * 32 + k * 16 + tb
                            for hf in range(2):
                                p.op("pool", lambda e, tb=tb, col=col, hf=hf: e.indirect_dma_start(
                                    out=XsH[hf][:, :], out_offset=IOA(ap=posI[:, col:col + 1], axis=0),
                                    in_=hs[:, tb * D + hf * 512: tb * D + (hf + 1) * 512], in_offset=None),
                                    r=[hB[tb], posB], dsem=hB[tb].dsem)
                    p.barrier()
            _ph5()

        def moe_phase():
            NB = TS // 128
            NTC = TS // 512
            with ExitStack() as st:
                p.phase_begin()
                ntl = sb(st, "ntl", [128, 8], F32)
                cinc = sb(st, "cinc", [128, 8], F32)
                cmp3 = hs[:, NTAB:NTAB + 64]
                le = hs[:, NTAB + 64:NTAB + 64 + NT * 8]
                le2 = hs[:, NTAB + 64 + NT * 8:NTAB + 64 + 2 * NT * 8]
                ej = sb(st, "ej", [128, NT], F32)
                ub = sb(st, "ub", [128, NT], F32)
                tabF = hs[:, 0:NTAB]
                xrow = sb(st, "xrow", [128, NT], F32)
                ew = sb(st, "ew", [128, NT], F32)
                T = [p.buf("tabw")]
                c3 = lambda t: t.rearrange("p (a b) -> p a b", b=8)
                p.op("dve", lambda e: e.tensor_tensor(out=c3(cmp3), in0=base[:, :].unsqueeze(2).to_broadcast([128, 8, 8]), in1=c3(thr[:, :]), op=ALU.is_gt),
                     r=[baseB] + CB, w=T)
                p.op("dve", lambda e: e.tensor_reduce(out=ntl[:], in_=c3(cmp3), axis=AX.X, op=ALU.add), r=T, w=T)
                p.op("dve", lambda e: e.tensor_tensor_scan(out=cinc[:], data0=ones_bf[:, 0:8], data1=ntl[:], initial=0.0, op0=ALU.mult, op1=ALU.add),
                     r=T + CB, w=T)
                p.op("dve", lambda e: e.tensor_tensor(out=c3(le), in0=cinc[:, :].unsqueeze(1).to_broadcast([128, NT, 8]), in1=c3(jc[:, :]), op=ALU.is_le),
                     r=T + CB, w=T)
                p.op("dve", lambda e: e.tensor_reduce(out=ej[:], in_=c3(le), axis=AX.X, op=ALU.add), r=T, w=T)
                p.op("dve", lambda e: e.tensor_tensor(out=c3(le2), in0=c3(le), in1=ntl[:, :].unsqueeze(1).to_broadcast([128, NT, 8]), op=ALU.mult), r=T, w=T)
                p.op("dve", lambda e: e.tensor_reduce(out=ub[:], in_=c3(le2), axis=AX.X, op=ALU.add), r=T, w=T)
                p.op("dve", lambda e: e.tensor_tensor(out=ub[:], in0=c3(jc[:, :])[:, :, 0], in1=ub[:], op=ALU.subtract), r=T + CB, w=T)
                p.op("dve", lambda e: e.tensor_scalar(out=ub[:], in0=ub[:], scalar1=float(TS), scalar2=None, op0=ALU.mult), r=T, w=T)
                p.op("dve", lambda e: e.scalar_tensor_tensor(out=xrow[:], in0=ej[:], scalar=float(CAP), in1=ub[:], op0=ALU.mult, op1=ALU.add), r=T, w=T)
                p.op("dve", lambda e: e.tensor_scalar(out=xrow[:], in0=xrow[:], scalar1=float(8 * CAP), scalar2=None, op0=ALU.min), r=T, w=T)
                p.op("dve", lambda e: e.tensor_scalar(out=ej[:], in0=ej[:], scalar1=7.0, scalar2=None, op0=ALU.min), r=T, w=T)
                tv = lambda o, n: tabF[:, o:o + NT * n].rearrange("p (j c) -> p j c", c=n)
                bj = lambda t, n: t[:, :].unsqueeze(2).to_broadcast([128, NT, n])
                bcp = lambda n: cp[:, 0:n].unsqueeze(1).to_broadcast([128, NT, n])
                p.op("dve", lambda e: e.tensor_tensor(out=tv(XO, NBm), in0=bj(xrow, NBm), in1=bcp(NBm), op=ALU.add), r=T + CB, w=T)
                p.op("dve", lambda e: e.tensor_scalar(out=ew[:], in0=ej[:], scalar1=float(7 * D), scalar2=None, op0=ALU.mult), r=T, w=T)
                p.op("dve", lambda e: e.tensor_tensor(out=tv(WO, 56), in0=bj(ew, 56), in1=bcp(56), op=ALU.add), r=T + CB, w=T)
                p.op("dve", lambda e: e.tensor_scalar(out=ew[:], in0=ej[:], scalar1=float(DFE), scalar2=None, op0=ALU.mult), r=T, w=T)
                p.op("dve", lambda e: e.tensor_tensor(out=tv(DO, 28), in0=bj(ew, 28), in1=bcp(28), op=ALU.add), r=T + CB, w=T)
                p.op("dve", lambda e: e.tensor_copy(out=tabI[:], in_=tabF), r=T, w=[tabB])

                wgs = Rot([(sb(st, "wg%d" % i, [128, KC, 512], BF16), p.buf("wg", dma=True)) for i in range(2)])
                wus = Rot([(sb(st, "wu%d" % i, [128, KC, 512], BF16), p.buf("wu", dma=True)) for i in range(2)])
                wds = Rot([(sb(st, "wd%d" % i, [128, 4, D], BF16), p.buf("wd", dma=True)) for i in range(2)])
                xbs = Rot([(sb(st, "xbm%d" % i, [128, D], BF16), p.buf("xbm", dma=True)) for i in range(3)])
                sgs = Rot([(sb(st, "sg%d" % i, [128, 512], F32), p.buf("sg")) for i in range(2)])
                pgs = Rot([(ps(st, "pg%d" % i, [128, 512]), p.buf("pg")) for i in range(2)])
                pus = Rot([(ps(st, "pu%d" % i, [128, 512]), p.buf("pu")) for i in range(2)])
                yps = Rot([(ps(st, "yp%d" % i, [128, D]), p.buf("yp")) for i in range(2)])
                psts = Rot([(pu_[:].bitcast(BF16), puB_) for (pu_, puB_) in pus.items])
                xTs = Rot([(hT[:, :, i * TS:(i + 1) * TS], p.buf("xT")) for i in range(2)])
                aTs = Rot([(oT[:, 4 * i:4 * i + 4, 0:TS], p.buf("aTm")) for i in range(2)])
                yss = Rot([(h[:, NB * i:NB * (i + 1), :], p.buf("ys", dma=True)) for i in range(2)])

                def load_tile(j):
                    xT, xTB = xTs.next()
                    for a in range(NB):
                        xb, xbB = xbs.next()
                        pst, pstB = psts.next()
                        for hf in range(2):
                            p.op("pool", lambda e, xb=xb, a=a, hf=hf: e.indirect_dma_start(
                                out=xb[:, hf * 512:(hf + 1) * 512], out_offset=None, in_=XsH[hf][:, :],
                                in_offset=IOA(ap=tabI[:, XO + j * NB + a:XO + j * NB + a + 1], axis=0)), r=[tabB], w=[xbB], dsem=xbB.dsem)

                        def tr(e, xb=xb, pst=pst):
                            inst = None
                            for kc in range(KC):
                                inst = e.transpose(pst[:, kc * 128:(kc + 1) * 128], xb[:, kc * 128:(kc + 1) * 128], ident[:])
                            return inst
                        p.op("pe", tr, r=[xbB, identB], w=[pstB])
                        p.op("act", lambda e, xT=xT, pst=pst, a=a: e.activation(out=xT[:, :, a * 128:(a + 1) * 128],
                                                                                 in_=pst.rearrange("p (k t) -> p k t", k=KC), func=AF.Copy),
                             r=[pstB], w=[xTB])
                    return xT, xTB

                def ffn_load(j, g):
                    wg, wgB = wgs.next()
                    wu, wuB = wus.next()
                    wd, wdB = wds.next()
                    for (wt_, wtB_, wn_) in ((wg, wgB, "mg"), (wu, wuB, "mu")):
                        for kc in range(KC):
                            c_ = WO + j * 56 + g * 8 + kc
                            p.op("pool", lambda e, wt_=wt_, wn_=wn_, kc=kc, c_=c_: e.indirect_dma_start(
                                out=wt_[:, kc, :], out_offset=None, in_=dr[wn_][:, :],
                                in_offset=IOA(ap=tabI[:, c_:c_ + 1], axis=0)), r=[tabB], w=[wtB_], dsem=wtB_.dsem)
                    for ci in range(4):
                        c_ = DO + j * 28 + g * 4 + ci
                        p.op("pool", lambda e, ci=ci, c_=c_: e.indirect_dma_start(
                            out=wd[:, ci, :], out_offset=None, in_=dr["md"][:, :],
                            in_offset=IOA(ap=tabI[:, c_:c_ + 1], axis=0)), r=[tabB], w=[wdB], dsem=wdB.dsem)
                    return (wg, wgB, wu, wuB, wd, wdB)

                def ffn_group(j, g, xT, xTB, ys, ysB, wts):
                    wg, wgB, wu, wuB, wd, wdB = wts
                    aT, aTB = aTs.next()
                    for ci in range(4):
                        for tc in range(NTC):
                            pg, pgB = pgs.next()
                            pu, puB = pus.next()
                            sg, sgB = sgs.next()

                            def fg_(e, pg=pg, ci=ci, tc=tc):
                                inst = None
                                for kc in range(KC):
                                    inst = mm(e, pg[:], wg[:, kc, ci * 128:(ci + 1) * 128], xT[:, kc, tc * 512:(tc + 1) * 512], kc == 0, kc == KC - 1)
                                return inst

                            def fu_(e, pu=pu, ci=ci, tc=tc):
                                inst = None
                                for kc in range(KC):
                                    inst = mm(e, pu[:], wu[:, kc, ci * 128:(ci + 1) * 128], xT[:, kc, tc * 512:(tc + 1) * 512], kc == 0, kc == KC - 1)
                                return inst
                            p.op("pe", fg_, r=[wgB, xTB], w=[pgB])
                            p.op("pe", fu_, r=[wuB, xTB], w=[puB])
                            p.op("act", lambda e, sg=sg, pg=pg: e.activation(out=sg[:], in_=pg[:], func=AF.Silu), r=[pgB], w=[sgB])
                            p.op("dve", lambda e, sg=sg, pu=pu, ci=ci, tc=tc: e.tensor_tensor(
                                out=aT[:, ci, tc * 512:(tc + 1) * 512], in0=pu[:], in1=sg[:], op=ALU.mult), r=[puB, sgB], w=[aTB])
                    for tb in range(NB):
                        yp, ypB = yps.next()

                        def fd_(e, yp=yp, tb=tb):
                            inst = None
                            for half in range(2):
                                for ci in range(4):
                                    inst = mm(e, yp[:, half * 512:(half + 1) * 512], aT[:, ci, tb * 128:(tb + 1) * 128],
                                              wd[:, ci, half * 512:(half + 1) * 512], ci == 0, ci == 3)
                            return inst
                        p.op("pe", fd_, r=[aTB, wdB], w=[ypB])
                        ydst = ys[:, tb, :]
                        if g == 0:
                            p.op("dve", lambda e, yp=yp, ydst=ydst: e.tensor_copy(out=ydst, in_=yp[:]), r=[ypB], w=[ysB])
                        else:
                            p.op("dve", lambda e, yp=yp, ydst=ydst: e.tensor_tensor(out=ydst, in0=yp[:], in1=ydst, op=ALU.add), r=[ypB, ysB], w=[ysB])

                NG = DFE // 512
                nxt = load_tile(0)
                wnext = ffn_load(0, 0)
                for j in range(NT):
                    xT, xTB = nxt
                    ys, ysB = yss.next()
                    for g in range(NG):
                        wcur = wnext
                        if g + 1 < NG:
                            wnext = ffn_load(j, g + 1)
                        elif j + 1 < NT:
                            wnext = ffn_load(j + 1, 0)
                        ffn_group(j, g, xT, xTB, ys, ysB, wcur)
                        if g == 3 and j + 1 < NT:
                            nxt = load_tile(j + 1)
                    for a in range(NB):
                        for hf in range(2):
                            p.op("pool", lambda e, ys=ys, j=j, hf=hf, a=a: e.indirect_dma_start(
                                out=YsH[hf][:, :], out_offset=IOA(ap=tabI[:, XO + j * NB + a:XO + j * NB + a + 1], axis=0),
                                in_=ys[:, a, hf * 512:(hf + 1) * 512], in_offset=None), r=[ysB, tabB], dsem=ysB.dsem)
                p.barrier()

        def combine_phase():
            with ExitStack() as st:
                p.phase_begin()
                L = ln_alloc(st, "fin")
                L["eps"] = epsT
                ln_load(L, 3)
                y1s = Rot([(sb(st, "y1_%d" % i, [128, D], F32), p.buf("y1", dma=True)) for i in range(4)])
                y2s = Rot([(sb(st, "y2_%d" % i, [128, D], F32), p.buf("y2", dma=True)) for i in range(4)])
                lp = LnPipe()
                for s in range(nseq):
                    for tb in range(TB):
                        col = s * 32 + tb
                        r0 = s * S + tb * 128
                        p.op("pool", lambda e, tb=tb, r0=r0: e.dma_start(out=h[:, tb, :], in_=h1sp[r0:r0 + 128, :]), w=[hB[tb]], dsem=hB[tb].dsem, force=True)
                        y1, y1B = y1s.next()
                        y2, y2B = y2s.next()
                        for hf in range(2):
                            p.op("pool", lambda e, y1=y1, col=col, hf=hf: e.indirect_dma_start(
                                out=y1[:, hf * 512:(hf + 1) * 512], out_offset=None, in_=YsH[hf][:, :],
                                in_offset=IOA(ap=posI[:, col:col + 1], axis=0)),
                                r=[posB], w=[y1B], dsem=y1B.dsem)
                            p.op("pool", lambda e, y2=y2, col=col, hf=hf: e.indirect_dma_start(
                                out=y2[:, hf * 512:(hf + 1) * 512], out_offset=None, in_=YsH[hf][:, :],
                                in_offset=IOA(ap=posI[:, col + 16:col + 17], axis=0)),
                                r=[posB], w=[y2B], dsem=y2B.dsem)
                        p.op("act", lambda e, tb=tb: e.mul(out=h[:, tb, :], in_=h[:, tb, :], mul=ALPHA), r=[hB[tb]], w=[hB[tb]])
                        p.op("dve", lambda e, tb=tb, y1=y1, col=col: e.scalar_tensor_tensor(out=h[:, tb, :], in0=y1[:], scalar=gW[:, col:col + 1], in1=h[:, tb, :],
                                                                                         op0=ALU.mult, op1=ALU.add), r=[y1B, hB[tb], gWB], w=[hB[tb]])
                        p.op("dve", lambda e, tb=tb, y2=y2, col=col: e.scalar_tensor_tensor(out=h[:, tb, :], in0=y2[:], scalar=gW[:, col + 16:col + 17], in1=h[:, tb, :],
                                                                                         op0=ALU.mult, op1=ALU.add), r=[y2B, hB[tb], gWB], w=[hB[tb]])
                        lp.push(ln_block(tb, L, to_hT=False, out_row0=s * S, gb_eng="dve"))
                lp.flush()
                p.barrier()

        for s_ in range(nseq):
            do_seq(s_)
        moe_phase()
        combine_phase()
        lasts = list(p.lastd.values())
        p.pend["sp"] = lasts
        p.op("sp", lambda e: None)
        block = es.enter_context(nc.Block())
        p.emit(block)
    return nc


def prep_shared(inp):
    f = lambda a: np.ascontiguousarray(a, dtype=np.float32)
    w_in_e = inp["w_in_e"][0]
    sh = {}
    sh["wqa"] = f(w_in_e[:, 0:512]); sh["wka"] = f(w_in_e[:, 512:1024]); sh["wva"] = f(w_in_e[:, 1024:1536])
    sh["wfa"] = f(np.repeat(w_in_e[:, 1536:1544], 128, axis=1))
    qd = w_in_e[:, 1544:2056]; kd = w_in_e[:, 2056:2568]
    sh["wqd"] = f(qd); sh["wkd"] = f(kd); sh["wvd"] = f(w_in_e[:, 2568:3080])
    perm = np.arange(512)
    for hh in range(8):
        for i in range(8):
            perm[hh * 64 + i] = hh * 64 + i + 8
            perm[hh * 64 + 8 + i] = hh * 64 + i
    sh["wqds"] = f(qd[:, perm]); sh["wkds"] = f(kd[:, perm])
    sh["wo0"] = f(inp["w_out_e"][0]); sh["fg"] = f(inp["ffn_w_gate_e"][0]); sh["fu"] = f(inp["ffn_w_up_e"][0]); sh["fd"] = f(inp["ffn_w_down_e"][0])
    w_in_o = inp["w_in_o"][0]
    sh["wq1"] = f(w_in_o[:, 0:1024]); sh["wk1"] = f(w_in_o[:, 1024:2048]); sh["wv1"] = f(w_in_o[:, 2048:3072])
    sh["wig"] = f(np.repeat(w_in_o[:, 3072:3080], 128, axis=1)); sh["wfg"] = f(np.repeat(w_in_o[:, 3080:3088], 128, axis=1))
    sh["wog"] = f(w_in_o[:, 3088:4112])
    sh["wo1"] = f(inp["w_out_o"][0]); sh["wr"] = f(inp["w_router_o"][0])
    relay = lambda w: f(w.reshape(NE, D, 7, 512).transpose(0, 2, 1, 3).reshape(NE * 7 * D, 512))
    sh["mg"] = relay(inp["moe_w_gate_o"][0]); sh["mu"] = relay(inp["moe_w_up_o"][0])
    sh["md"] = f(inp["moe_w_down_o"][0].reshape(NE * DFE, D))
    lnp = np.stack([np.stack([inp["ln_mix_g_e"][0], inp["ln_mix_b_e"][0]]), np.stack([inp["ln_ffn_g_e"][0], inp["ln_ffn_b_e"][0]]),
                    np.stack([inp["ln_mix_g_o"][0], inp["ln_mix_b_o"][0]]), np.stack([inp["ln_ffn_g_o"][0], inp["ln_ffn_b_o"][0]])])
    sh["lnp"] = f(np.broadcast_to(lnp[:, :, None, :], (4, 2, 128, D)).reshape(4 * 2 * 128, D))
    gbv = np.concatenate([inp["b_forget_e"][0], inp["b_igate_o"][0], inp["b_fgate_o"][0]])
    sh["gb"] = f(np.broadcast_to(gbv[None, :], (128, 24)))
    sh["ng"] = f(inp["mlstm_norm_g_o"][0].reshape(8, 128).T)
    sh["wc"] = f(inp["w_conv_o"][0].T.reshape(16, 128, 4).transpose(1, 0, 2).reshape(128, 64))
    sh["ident"] = np.eye(128, dtype=np.float32)
    k = np.arange(128)[:, None]; q = np.arange(128)[None, :]
    mC = np.where(k > q, NEG, 0.0).astype(np.float32)
    mU = np.where(k < q, NEG, 0.0).astype(np.float32)
    mA = np.full((128, 128), NEG, np.float32)
    sh["masks"] = f(np.concatenate([mC, mU, mA], axis=1))
    sh["mask4"] = f(np.concatenate([mU, mC, mU, mC, mA, mC, mU, mC, mA, mC, mA, mC], axis=1))
    half = 8
    inv = 500000.0 ** (-np.arange(half, dtype=np.float32) / half)
    ang = np.arange(S, dtype=np.float32)[None, :] * inv[:, None]
    cosT = np.ones((128, S), np.float32); sinT = np.zeros((128, S), np.float32)
    for hh in range(2):
        b = hh * 64
        cosT[b:b + 8] = np.cos(ang); cosT[b + 8:b + 16] = np.cos(ang)
        sinT[b:b + 8] = -np.sin(ang); sinT[b + 8:b + 16] = np.sin(ang)
    sh["rope"] = f(np.concatenate([cosT, sinT], axis=1))
    augc = np.zeros((128, 6), np.float32)
    augc[0:4, 0] = [-1, 0, 0, 0]; augc[0:4, 1] = [0, -1, 0, 0]; augc[0:4, 2] = [0, 0, 1, 1]
    augc[0:4, 3] = [0, 0, 1, 0]; augc[0:4, 4] = [0, 0, 0, 1]; augc[0:4, 5] = [1, 1, 0, 0]
    sh["augc"] = augc
    sh["cp"] = f(np.arange(56, dtype=np.float32)[None, :] * 128.0 + np.arange(128, dtype=np.float32)[:, None])
    sh["tri"] = np.triu(np.ones((128, 128), np.float32))
    eo = np.zeros((128, 128), np.float32)
    for e_ in range(8):
        eo[:, e_ * 16:(e_ + 1) * 16] = e_ * CAP - 1.0
    sh["eoff"] = eo
    sh["thr"] = f(np.broadcast_to((np.arange(8, dtype=np.float32) * TS)[None, None, :], (128, 8, 8)).reshape(128, 64))
    sh["jc"] = f(np.broadcast_to(np.arange(NT, dtype=np.float32)[None, :, None], (128, NT, 8)).reshape(128, NT * 8))
    return sh


N_CORES = 8


def kernel(**inputs):
    x = np.ascontiguousarray(inputs["x"], dtype=np.float32)
    B = x.shape[0]
    nseq = B // N_CORES
    assert nseq == NSEQ
    sh = prep_shared(inputs)
    nc = build_nc(nseq)
    in_maps = []
    for c in range(N_CORES):
        m = dict(sh)
        m["x"] = x[c * nseq:(c + 1) * nseq].reshape(nseq * S, D)
        in_maps.append(m)
    res = run_bass_kernel_spmd(nc, in_maps, core_ids=list(range(N_CORES)))
    outs = [np.asarray(r["out"]).reshape(nseq, S, D) for r in res.results]
    return np.concatenate(outs, axis=0).astype(np.float32)
```
